# Optimizing a Trainium2 kernel written in Bass

```python
import jax, jax.numpy as jnp
from jax import lax
import numpy as np


D_MODEL = 2048
BATCH = 4
SEQ = 4096
DEPTH = 1

N_META = 16
BLOCK = 128
PAD_LEFT = BLOCK - N_META
HEAD_DIM = 64
MIX_WIDTH = D_MODEL
RWKV_WIDTH = MIX_WIDTH // 2
RWKV_HEADS = RWKV_WIDTH // HEAD_DIM
ATTN_WIDTH = MIX_WIDTH - RWKV_WIDTH
ATTN_Q_HEADS = ATTN_WIDTH // HEAD_DIM
ATTN_KV_HEADS = max(1, ATTN_Q_HEADS // 8)
KV_WIDTH = ATTN_KV_HEADS * HEAD_DIM
WINDOW = 128
ATTN_SCALE = HEAD_DIM ** -0.5
MASK_VALUE = -1e30
DECAY_LORA = 64
AAA_LORA = 64
GATE_LORA = 160
RMS_EPS = 1e-6
GN_EPS = 64e-5
ATTN_COLS = ATTN_WIDTH + 2 * KV_WIDTH
RWKV_COLS = 3 * RWKV_WIDTH + DECAY_LORA + AAA_LORA + GATE_LORA
IN_COLS = ATTN_COLS + RWKV_COLS
PEER_HEADS = 8
N_KEYS = 128
N_EXPERTS = N_KEYS * N_KEYS
PEER_TOPK = 16
D_KEY = 256
PEER_BLOCK = 128

kernel_name = 'hybrid_rwkv7_swa_sink_peer'


def rms_norm(x, g):
    xf = x.astype(jnp.float32)
    y = xf * lax.rsqrt(jnp.mean(xf * xf, axis=-1, keepdims=True) + RMS_EPS)
    return (y * g.astype(jnp.float32)).astype(x.dtype)


def split_cols(p, sizes):
    out, o = [], 0
    for s in sizes:
        out.append(p[..., o:o + s])
        o += s
    return out


def token_shift(p, mu):
    p_prev = jnp.pad(p, ((0, 0), (1, 0), (0, 0)))[:, :-1]
    return p + mu.astype(p.dtype) * (p_prev - p)


def rwkv7_time_mix(pr, pk, pv, pw, pa, pg, w0, w_up, a0, a_up, g_up, k_k, k_a, r_k, gn_w, gn_b):
    B, L, C = pr.shape
    H, N = RWKV_HEADS, HEAD_DIM
    f32 = jnp.float32
    dt = pr.dtype
    w_log = -jax.nn.softplus(-(w0 + jnp.tanh(pw) @ w_up).astype(f32)) - 0.5
    decay = jnp.exp(-jnp.exp(w_log))
    a = jax.nn.sigmoid((a0 + pa @ a_up).astype(f32))
    g = (jax.nn.sigmoid(pg) @ g_up).astype(f32)
    kf = pk.astype(f32)
    kk = (kf * k_k.astype(f32)).reshape(B, L, H, N)
    kk = kk / jnp.maximum(jnp.sqrt(jnp.sum(kk * kk, axis=-1, keepdims=True)), 1e-12)
    k = (kf * (1.0 + (a - 1.0) * k_a.astype(f32))).reshape(B, L, H, N)
    r = pr.astype(f32).reshape(B, L, H, N)
    v = pv.astype(f32).reshape(B, L, H, N)
    a = a.reshape(B, L, H, N)
    decay = decay.reshape(B, L, H, N)

    def step(S, inp):
        r_t, w_t, k_t, v_t, kk_t, a_t = inp
        sa = jnp.einsum('bhij,bhj->bhi', S, -kk_t)
        S = (S * w_t[:, :, None, :] + sa[..., :, None] * (kk_t * a_t)[..., None, :]
             + v_t[..., :, None] * k_t[..., None, :])
        y_t = jnp.einsum('bhij,bhj->bhi', S, r_t)
        return S, y_t

    xs = tuple(jnp.moveaxis(t, 1, 0) for t in (r, decay, k, v, kk, a))
    S0 = jnp.zeros((B, H, N, N), f32)
    _, y = lax.scan(step, S0, xs)
    y = jnp.moveaxis(y, 0, 1)
    mu = jnp.mean(y, axis=-1, keepdims=True)
    var = jnp.mean(jnp.square(y - mu), axis=-1, keepdims=True)
    yn = ((y - mu) * lax.rsqrt(var + GN_EPS)).reshape(B, L, C)
    yn = yn * gn_w.astype(f32) + gn_b.astype(f32)
    bonus = jnp.sum(r * k * r_k.astype(f32), axis=-1, keepdims=True) * v
    return ((yn + bonus.reshape(B, L, C)) * g).astype(dt)


def sliding_window_sink_attention(q, k, v, q_gain, k_gain, sinks):
    B, LP, _ = q.shape
    NB = LP // BLOCK
    KVH = ATTN_KV_HEADS
    G = ATTN_Q_HEADS // KVH
    f32 = jnp.float32
    q = rms_norm(q.reshape(B, LP, ATTN_Q_HEADS, HEAD_DIM), q_gain).reshape(B, NB, BLOCK, KVH, G, HEAD_DIM)
    k = rms_norm(k.reshape(B, LP, KVH, HEAD_DIM), k_gain).reshape(B, NB, BLOCK, KVH, HEAD_DIM)
    v = v.reshape(B, NB, BLOCK, KVH, HEAD_DIM)

    def with_prev(t):
        prev = jnp.pad(t, ((0, 0), (1, 0), (0, 0), (0, 0), (0, 0)))[:, :-1]
        return jnp.concatenate([prev, t], axis=2)

    kw, vw = with_prev(k), with_prev(v)
    s = jnp.einsum('bnqkgd,bnskd->bnkgqs', q, kw, preferred_element_type=f32) * ATTN_SCALE
    blk = jnp.arange(NB)[:, None, None]
    qpos = blk * BLOCK + jnp.arange(BLOCK)[None, :, None]
    kpos = (blk - 1) * BLOCK + jnp.arange(2 * BLOCK)[None, None, :]
    dist = qpos - kpos
    mask = (dist >= 0) & (dist < WINDOW) & (kpos >= PAD_LEFT)
    s = jnp.where(mask[None, :, None, None], s, MASK_VALUE)
    sink = jnp.broadcast_to(sinks.astype(f32).reshape(1, 1, KVH, G, 1, 1), s.shape[:-1] + (1,))
    p = jax.nn.softmax(jnp.concatenate([s, sink], axis=-1), axis=-1)[..., :-1]
    o = jnp.einsum('bnkgqs,bnskd->bnqkgd', p.astype(vw.dtype), vw)
    return o.reshape(B, LP, ATTN_WIDTH)


def peer_ffn(x, peer_query, peer_sub_keys, peer_down, peer_up):
    B, LP, D = x.shape
    T = B * LP
    K = PEER_TOPK
    xt = x.reshape(T, D)
    q = (xt @ peer_query).reshape(T, PEER_HEADS, 2, D_KEY // 2)
    scores = jnp.einsum('thcd,hcnd->thcn', q, peer_sub_keys, preferred_element_type=jnp.float32)
    s_half, i_half = lax.top_k(scores, K)
    cand_s = (s_half[:, :, 0, :, None] + s_half[:, :, 1, None, :]).reshape(T, PEER_HEADS, K * K)
    cand_i = (i_half[:, :, 0, :, None] * N_KEYS + i_half[:, :, 1, None, :]).reshape(T, PEER_HEADS, K * K)
    top_s, top_pos = lax.top_k(cand_s, K)
    idx = jnp.take_along_axis(cand_i, top_pos, axis=-1)
    gate = jax.nn.softmax(top_s, axis=-1).astype(x.dtype)
    nblk = T // PEER_BLOCK
    xb = xt.reshape(nblk, PEER_BLOCK, D)
    ib = idx.reshape(nblk, PEER_BLOCK, PEER_HEADS * K)
    gb = gate.reshape(nblk, PEER_BLOCK, PEER_HEADS * K)

    def one_block(args):
        xc, ic, gc = args
        u = jnp.take(peer_down, ic, axis=0)
        hcur = jax.nn.gelu(jnp.einsum('ped,pd->pe', u, xc), approximate=False)
        vv = jnp.take(peer_up, ic, axis=0)
        return jnp.einsum('pe,ped->pd', hcur * gc, vv)

    out = lax.map(one_block, (xb, ib, gb))
    return out.reshape(B, LP, D)


def hybrid_layer(h, valid, norm1_g, w_in, shift_mu, w0, w_up, a0, a_up, g_up, k_k, k_a, r_k,
                 gn_w, gn_b, q_gain, k_gain, sinks, w_out, norm2_g,
                 peer_query, peer_sub_keys, peer_down, peer_up):
    u = rms_norm(h, norm1_g)
    p = u @ w_in
    p_attn, p_rwkv = p[..., :ATTN_COLS], p[..., ATTN_COLS:]
    q, k, v = split_cols(p_attn, (ATTN_WIDTH, KV_WIDTH, KV_WIDTH))
    p_rwkv = token_shift(p_rwkv, shift_mu)
    pr, pk, pv, pw, pa, pg = split_cols(
        p_rwkv, (RWKV_WIDTH, RWKV_WIDTH, RWKV_WIDTH, DECAY_LORA, AAA_LORA, GATE_LORA))
    y_rwkv = rwkv7_time_mix(pr, pk, pv, pw, pa, pg, w0, w_up, a0, a_up, g_up, k_k, k_a, r_k, gn_w, gn_b)
    y_attn = sliding_window_sink_attention(q, k, v, q_gain, k_gain, sinks)
    h = h + jnp.concatenate([y_rwkv, y_attn], axis=-1) @ w_out
    h = h + peer_ffn(rms_norm(h, norm2_g), peer_query, peer_sub_keys, peer_down, peer_up)
    return jnp.where(valid[None, :, None], h, jnp.zeros_like(h))


def setup_inputs(seed: int = 0) -> dict:
    key = jax.random.key(seed)
    ks = jax.random.split(key, 26)
    f32 = jnp.float32

    def nrm(k, shape, scale):
        return jax.random.normal(k, shape, f32) * scale

    return {
        'x': nrm(ks[0], (BATCH, SEQ, D_MODEL), 1.0),
        'meta_tokens': nrm(ks[1], (N_META, D_MODEL), 1.0),
        'norm1_g': 1.0 + nrm(ks[2], (DEPTH, D_MODEL), 0.02),
        'w_in': nrm(ks[3], (DEPTH, D_MODEL, IN_COLS), D_MODEL ** -0.5),
        'shift_mu': jax.random.uniform(ks[4], (DEPTH, RWKV_COLS), f32),
        'w0': jax.random.uniform(ks[5], (DEPTH, RWKV_WIDTH), f32, -6.5, -1.5),
        'w_up': nrm(ks[6], (DEPTH, DECAY_LORA, RWKV_WIDTH), 0.1 * DECAY_LORA ** -0.5),
        'a0': nrm(ks[7], (DEPTH, RWKV_WIDTH), 0.1),
        'a_up': nrm(ks[8], (DEPTH, AAA_LORA, RWKV_WIDTH), 0.5 * AAA_LORA ** -0.5),
        'g_up': nrm(ks[9], (DEPTH, GATE_LORA, RWKV_WIDTH), GATE_LORA ** -0.5),
        'k_k': 0.85 + nrm(ks[10], (DEPTH, RWKV_WIDTH), 0.02),
        'k_a': 1.0 + nrm(ks[11], (DEPTH, RWKV_WIDTH), 0.02),
        'r_k': nrm(ks[12], (DEPTH, RWKV_HEADS, HEAD_DIM), 0.1),
        'gn_w': 1.0 + nrm(ks[13], (DEPTH, RWKV_WIDTH), 0.02),
        'gn_b': nrm(ks[14], (DEPTH, RWKV_WIDTH), 0.02),
        'q_gain': 1.0 + nrm(ks[15], (DEPTH, HEAD_DIM), 0.02),
        'k_gain': 1.0 + nrm(ks[16], (DEPTH, HEAD_DIM), 0.02),
        'sinks': nrm(ks[17], (DEPTH, ATTN_Q_HEADS), 0.5),
        'w_out': nrm(ks[18], (DEPTH, MIX_WIDTH, D_MODEL), MIX_WIDTH ** -0.5),
        'norm2_g': 1.0 + nrm(ks[19], (DEPTH, D_MODEL), 0.02),
        'peer_query': nrm(ks[20], (DEPTH, D_MODEL, PEER_HEADS * D_KEY), D_MODEL ** -0.5),
        'peer_sub_keys': nrm(ks[21], (DEPTH, PEER_HEADS, 2, N_KEYS, D_KEY // 2), (D_KEY // 2) ** -0.5),
        'peer_down': nrm(ks[22], (DEPTH, N_EXPERTS, D_MODEL), D_MODEL ** -0.5),
        'peer_up': nrm(ks[23], (DEPTH, N_EXPERTS, D_MODEL), PEER_HEADS ** -0.5),
    }


def reference(x, meta_tokens, norm1_g, w_in, shift_mu, w0, w_up, a0, a_up, g_up, k_k, k_a, r_k,
              gn_w, gn_b, q_gain, k_gain, sinks, w_out, norm2_g,
              peer_query, peer_sub_keys, peer_down, peer_up):
    B, S, D = x.shape
    meta = jnp.broadcast_to(meta_tokens.astype(x.dtype)[None], (B, N_META, D))
    h = jnp.concatenate([jnp.zeros((B, PAD_LEFT, D), x.dtype), meta, x], axis=1)
    valid = jnp.arange(h.shape[1]) >= PAD_LEFT
    for i in range(DEPTH):
        h = hybrid_layer(h, valid, norm1_g[i], w_in[i], shift_mu[i], w0[i], w_up[i], a0[i], a_up[i],
                         g_up[i], k_k[i], k_a[i], r_k[i], gn_w[i], gn_b[i], q_gain[i], k_gain[i],
                         sinks[i], w_out[i], norm2_g[i], peer_query[i], peer_sub_keys[i],
                         peer_down[i], peer_up[i])
    return h[:, BLOCK:]
```

```python
import contextlib
import numpy as np
import concourse.bass as bass
import concourse.mybir as mybir
from concourse.bass_utils import run_bass_kernel_spmd

F32 = mybir.dt.float32
BF16 = mybir.dt.bfloat16
AF = mybir.ActivationFunctionType
ALU = mybir.AluOpType
AX = mybir.AxisListType

D = 2048
DC = 16
INC = 4640
RW0 = 1280
NRC = 27
NE = 16384
HD = 64

V_G1, V_MU, V_W0, V_A0, V_KK, V_KA, V_GNW, V_GNB, V_RK, V_QG, V_KG, V_SK, V_G2 = (
    0, 16, 43, 51, 59, 67, 75, 83, 91, 99, 100, 101, 109)
NV = 125
C_ID, C_BO, C_MU, C_MUI, C_ML, C_MC, C_MP, C_MPF, C_SEL = 0, 128, 256, 320, 384, 448, 576, 704, 832
NCN = 832 + 256


class H:
    __slots__ = ("name", "w", "r", "dsem", "dcnt")

    def __init__(self, name=""):
        self.name = name
        self.w = {}
        self.r = {}
        self.dsem = None
        self.dcnt = 0


class Sched:
    def __init__(self, nc):
        self.nc = nc
        self.engs = {"pe": nc.tensor, "act": nc.scalar, "dve": nc.vector,
                     "pool": nc.gpsimd, "sp": nc.sync}
        self.esem = {k: nc.alloc_semaphore(name="es_" + k) for k in self.engs}
        self.ecnt = {k: 0 for k in self.engs}
        self.waited = {k: {} for k in self.engs}
        self.sems = {s.num: s for s in self.esem.values()}
        self.dcur = {}
        self.dpool = []
        self.dnext = 0

    def _wait(self, e, evs):
        w = self.waited[e]
        for num, val in evs.items():
            if w.get(num, 0) >= val:
                continue
            self.engs[e].wait_ge(self.sems[num], val)
            w[num] = val

    @staticmethod
    def _merge(d, evs):
        for k, v in evs.items():
            if d.get(k, 0) < v:
                d[k] = v

    def _deps(self, reads, writes):
        evs = {}
        for h in reads:
            self._merge(evs, h.w)
        for h in writes:
            self._merge(evs, h.w)
            self._merge(evs, h.r)
        return evs

    def op(self, e, fn, reads=(), writes=()):
        self._wait(e, self._deps(reads, writes))
        inst = fn(self.engs[e])
        self.ecnt[e] += 1
        inst.then_inc(self.esem[e], 1)
        ev = {self.esem[e].num: self.ecnt[e]}
        for h in reads:
            self._merge(h.r, ev)
        for h in writes:
            h.w = dict(ev)
            h.r = {}
        return inst

    def dma(self, q, out, in_, tile, reads=(), writes=(), **kw):
        if tile.dsem is None:
            tile.dsem = self.nc.alloc_semaphore(name="ds_%d" % len(self.sems))
            self.sems[tile.dsem.num] = tile.dsem
        deps = self._deps(reads, writes)
        if tile in writes and not tile.r and tile.w.get(tile.dsem.num, 0) == tile.dcnt and len(tile.w) == 1:
            deps.pop(tile.dsem.num, None)
        self._wait(q, deps)
        inst = self.engs[q].dma_start(out=out, in_=in_, **kw)
        tile.dcnt += 16
        inst.then_inc(tile.dsem, 16)
        self.dcur[tile.dsem.num] = tile.dcnt
        ev = {tile.dsem.num: tile.dcnt}
        for h in reads:
            self._merge(h.r, ev)
        for h in writes:
            h.w = dict(ev)
            h.r = {}
        return inst

    def barrier(self):
        evs = {self.esem[k].num: self.ecnt[k] for k in self.engs}
        evs.update(self.dcur)
        evs = {k: v for k, v in evs.items() if v > 0}
        for e in self.engs:
            self._wait(e, evs)


class Ctx:
    pass


def build(NB=33, OWN0=17, phases="ABCDE", dbg=False):
    NO = NB - OWN0
    nc = bass.Bass("TRN2", target_bir_lowering=False)
    S = Sched(nc)

    def din(name, shape, dt=F32):
        return nc.dram_tensor(name, list(shape), dt, kind="ExternalInput").ap()

    def dscr(name, shape, dt=F32, out=False):
        return nc.dram_tensor(name, list(shape), dt, kind=("ExternalOutput" if out else "Internal")).ap()

    xin = din("xin", [NB * 128, D])
    w_in = din("w_in", [D, INC])
    vecs_d = din("vecs", [128, NV])
    cn_d = din("consts", [128, NCN])
    wup_d = din("w_up", [64, 1024])
    aup_d = din("a_up", [64, 1024])
    gup_d = din("g_up", [160, 1024])
    wout_d = din("w_out", [D, D])
    pq_d = din("peer_query", [D, D])
    sk_d = din("sub_keys", [16, 128, 128])
    pd_d = din("peer_down", [NE, D])
    pu_d = din("peer_up", [NE, D])
    yout = dscr("yout", [NO * 128, D], F32, out=True)

    PS_d = dscr("PS_s", [NB, 128, NRC * 128])
    QKV_d = dscr("QKV_s", [NO + 1, 128, 10 * 128])
    YR_d = dscr("YR_s", [NO, 128, 1024], BF16, out=dbg)
    YA_d = dscr("YA_s", [NO, 128, 1024], BF16, out=dbg)
    HM_d = dscr("HM_s", [NO, 128, D], F32, out=dbg)
    XNT_d = dscr("XNT_s", [NO, 128, D], BF16)
    ST_d = dscr("ST_s", [NO, 128, 2048 + 8])
    hPS = [H("PS%d" % j) for j in range(NB)]
    hQKV = [H("QKV%d" % j) for j in range(NO + 1)]
    hYR = [H() for j in range(NO)]
    hYA = [H() for j in range(NO)]
    hHM = [H() for j in range(NO)]
    hXNT = [H() for j in range(NO)]
    hST = [H() for j in range(NO)]
    hOUT = [H() for j in range(NO)]

    uid = [0]

    def mk(es):
        def T(name, shape, dt=F32):
            uid[0] += 1
            t = es.enter_context(nc.sbuf_tensor("sb%d_%s" % (uid[0], name), list(shape), dt))
            return t, H(name)

        def P(name, shape, dt=F32):
            uid[0] += 1
            t = es.enter_context(nc.psum_tensor("ps%d_%s" % (uid[0], name), list(shape), dt))
            return t, H(name)
        return T, P

    def load_consts(T, bf=True):
        c = Ctx()
        c.vec, c.hvec = T("vecs", [128, NV])
        S.dma("sp", c.vec[:], vecs_d, c.hvec, writes=[c.hvec])
        c.cn, c.hcn = T("cn32", [128, NCN])
        S.dma("sp", c.cn[:], cn_d, c.hcn, writes=[c.hcn])
        c.cb, c.hcb = T("cnbf", [128, NCN], BF16)
        S.dma("pool", c.cb[:], cn_d, c.hcb, writes=[c.hcb])
        return c

    def rmsnorm_T(c, T_, xt, hxt, gcol, junk, hjunk, st, hst, xs, hxs, pT, hpT, uT, huT):
        S.op("act", lambda e: e.activation(out=junk[:], in_=xt[:], func=AF.Square, accum_out=st[:, 0:1]),
             reads=[hxt], writes=([hjunk, hst] if hjunk is not hst else [hst]))
        S.op("dve", lambda e: e.tensor_scalar(out=st[:, 1:2], in0=st[:, 0:1], scalar1=1.0 / D, scalar2=1e-6,
                                              op0=ALU.mult, op1=ALU.add), reads=[hst], writes=[hst])
        S.op("act", lambda e: e.activation(out=st[:, 2:3], in_=st[:, 1:2], func=AF.Sqrt), reads=[hst], writes=[hst])
        S.op("dve", lambda e: e.reciprocal(out=st[:, 3:4], in_=st[:, 2:3]), reads=[hst], writes=[hst])
        S.op("act", lambda e: e.activation(out=xs[:], in_=xt[:], func=AF.Copy, scale=st[:, 3:4]),
             reads=[hxt, hst], writes=[hxs])
        for k in range(DC):
            S.op("pe", lambda e, k=k: e.transpose(pT[:, k, :], xs[:, k * 128:(k + 1) * 128], c.cb[:, C_ID:C_ID + 128]),
                 reads=[hxs, c.hcb], writes=[hpT])
        gb = c.vec[:, gcol:gcol + DC].unsqueeze(2).broadcast_to([128, DC, 128])
        S.op("dve", lambda e: e.tensor_tensor(out=uT[:], in0=pT[:], in1=gb, op=ALU.mult),
             reads=[hpT, c.hvec], writes=[huT])

    def phase_A():
        with contextlib.ExitStack() as es:
            T, P = mk(es)
            c = load_consts(T)
            win, hwin = T("win", [128, DC, INC], BF16)
            for k in range(DC):
                S.dma("pool", win[:, k, :], w_in[k * 128:(k + 1) * 128, :], hwin, writes=[hwin])
            xt, hxt = T("xt", [128, D])
            st, hst = T("st", [128, 4])
            xs, hxs = T("xs", [128, D], BF16)
            junk, hjunk = xs, hxs
            pT, hpT = P("pT", [128, DC, 128], BF16)
            uT, huT = T("uT", [128, DC, 128], BF16)
            pp = [P("pp%d" % i, [128, 4, 128]) for i in range(4)]
            PT, hPT = T("PT", [128, NRC, 129])
            QK, hQK = T("QK", [128, 10, 128])
            dd, hdd = T("dd", [128, NRC, 128])
            pss, hpss = dd, hdd
            S.op("pool", lambda e: e.memset(PT[:], 0.0), writes=[hPT])
            mub = c.vec[:, V_MU:V_MU + NRC].unsqueeze(2).broadcast_to([128, NRC, 128])
            for j in range(NB):
                S.dma("sp", xt[:], xin[j * 128:(j + 1) * 128, :], hxt, writes=[hxt])
                rmsnorm_T(c, T, xt, hxt, V_G1, junk, hjunk, st, hst, xs, hxs, pT, hpT, uT, huT)
                need_att = (j >= OWN0 - 1)
                chunks = list(range(0 if need_att else 10, 37))
                gi = 0
                for g0 in range(0, len(chunks), 4):
                    grp = chunks[g0:g0 + 4]
                    pt_, hp_ = pp[gi % 4]
                    gi += 1
                    for qi, ch in enumerate(grp):
                        M = 32 if ch == 36 else 128
                        for k in range(DC):
                            S.op("pe", lambda e, qi=qi, ch=ch, k=k, M=M, pt_=pt_: e.matmul(
                                pt_[0:M, qi, :], lhsT=win[:, k, ch * 128:ch * 128 + M], rhs=uT[:, k, :],
                                start=(k == 0), stop=(k == DC - 1)), reads=[hwin, huT], writes=[hp_])
                    for qi, ch in enumerate(grp):
                        M = 32 if ch == 36 else 128
                        eng = "act" if (qi % 2 == 0) else "dve"
                        if ch < 10:
                            dst, hd = QK[0:M, ch, :], hQK
                        else:
                            dst, hd = PT[0:M, ch - 10, 1:129], hPT
                        if eng == "act":
                            S.op("act", lambda e, dst=dst, qi=qi, M=M, pt_=pt_: e.activation(out=dst, in_=pt_[0:M, qi, :], func=AF.Copy),
                                 reads=[hp_], writes=[hd])
                        else:
                            S.op("dve", lambda e, dst=dst, qi=qi, M=M, pt_=pt_: e.tensor_copy(out=dst, in_=pt_[0:M, qi, :]),
                                 reads=[hp_], writes=[hd])
                if need_att:
                    ja = j - (OWN0 - 1)
                    S.dma("sp", QKV_d[ja].rearrange("p (c t) -> p c t", c=10), QK[:], hQK, reads=[hQK], writes=[hQKV[ja]])
                S.op("pool", lambda e: e.tensor_tensor(out=dd[:], in0=PT[:, :, 0:128], in1=PT[:, :, 1:129], op=ALU.subtract),
                     reads=[hPT], writes=[hdd])
                S.op("pool", lambda e: e.tensor_tensor(out=dd[:], in0=dd[:], in1=mub, op=ALU.mult),
                     reads=[hdd, c.hvec], writes=[hdd])
                S.op("dve", lambda e: e.tensor_tensor(out=dd[:], in0=dd[:], in1=PT[:, :, 1:129], op=ALU.add),
                     reads=[hdd, hPT], writes=[hdd])
                S.op("act", lambda e: e.activation(out=PT[:, :, 0:1], in_=PT[:, :, 128:129], func=AF.Copy),
                     reads=[hPT, hdd, hpss], writes=[hPT])
                S.dma("sp", PS_d[j].rearrange("p (c t) -> p c t", c=NRC), pss[:], hpss, reads=[hpss], writes=[hPS[j]])
            S.barrier()

    def phase_B():
        with contextlib.ExitStack() as es:
            T, P = mk(es)
            c = load_consts(T)
            vec = c.vec

            def vb(col, n=8):
                return vec[:, col:col + n].unsqueeze(2).broadcast_to([128, n, 128])
            wup, hwup = T("wup", [128, 1024], BF16)
            aup, haup = T("aup", [128, 1024], BF16)
            gup, hgup = T("gup", [128, 2, 1024], BF16)
            S.dma("pool", wup[0:64, :], wup_d, hwup, writes=[hwup])
            S.dma("pool", aup[64:128, :], aup_d, haup, writes=[haup])
            S.dma("pool", gup[:, 0, :], gup_d[0:128, :], hgup, writes=[hgup])
            S.dma("pool", gup[0:32, 1, :], gup_d[128:160, :], hgup, writes=[hgup])
            ps, hps = T("ps", [128, NRC, 128])
            names32 = ["nld", "aa", "kap", "kp", "beta", "cs", "cum", "t1", "t2", "t3", "gg", "bon"]
            F = {}
            for n in names32:
                F[n] = T(n, [128, 8, 128])
            namesbf = ["Kh", "Rh", "Bh", "Qh", "BG", "QG", "vb", "sqb"]
            Bf = {}
            for n in namesbf:
                Bf[n] = T(n, [128, 8, 128], BF16)
            lw, hlw = T("lw", [128, 128], BF16)
            sg, hsg = T("sg", [128, 2, 128], BF16)
            ones, hones = T("ones", [128, 1024])
            S.op("pool", lambda e: e.memset(ones[:], 1.0), writes=[hones])
            sm, hsm = T("sm", [128, 8, 2, 4])
            Vt, hVt = T("Vt", [64, 2, 8, 128], BF16)
            BGt, hBGt = T("BGt", [64, 2, 8, 128], BF16)
            QGt, hQGt = T("QGt", [64, 2, 8, 128], BF16)
            S32, hS32 = T("S32", [128, 8, 64])
            Sb, hSb = T("Sb", [128, 8, 64], BF16)
            S.op("pool", lambda e: e.memset(S32[:], 0.0), writes=[hS32])
            S.op("pool", lambda e: e.memset(Sb[:], 0.0), writes=[hSb])
            gm = {}
            for n in ["Ub", "Lb", "U2", "L2", "Lak", "Mrb", "Mrk", "Qb", "Xb", "SAb"]:
                gm[n] = T(n, [64, 16, 64], BF16)
            ysb, hysb = T("ysb", [64, 16, 64])
            yc, hyc = T("yc", [64, 16, 64])
            ysq, hysq = T("ysq", [64, 16, 64])
            gst, hgst = T("gst", [64, 16, 4])
            yf, hyf = T("yf", [128, 8, 128])
            yrb, hyrb = T("yrb", [128, 8, 128], BF16)
            PA = [P("PA%d" % i, [128, 1024]) for i in range(3)]
            PTr, hPTr = P("PTr", [128, 2048], BF16)
            pi = [0]

            def nextP():
                t = PA[pi[0] % 3]
                pi[0] += 1
                return t
            maskU = c.cn[0:64, C_MU:C_MU + 64].unsqueeze(1).broadcast_to([64, 16, 64])
            maskUi = c.cn[0:64, C_MUI:C_MUI + 64].unsqueeze(1).broadcast_to([64, 16, 64])
            maskL = c.cn[0:64, C_ML:C_ML + 64].unsqueeze(1).broadcast_to([64, 16, 64])
            identb = c.cb[:, C_ID:C_ID + 128]
            ident32 = c.cn[:, C_ID:C_ID + 128]
            bones = c.cb[:, C_BO:C_BO + 128]

            def tt(eng, out, i0, i1, op, reads, writes):
                S.op(eng, lambda e: e.tensor_tensor(out=out, in0=i0, in1=i1, op=op), reads=reads, writes=writes)

            def actf(out, in_, func, reads, writes, **kw):
                S.op("act", lambda e: e.activation(out=out, in_=in_, func=func, **kw), reads=reads, writes=writes)

            for j in range(NB):
                own = j >= OWN0
                S.dma("sp", ps[:], PS_d[j].rearrange("p (c t) -> p c t", c=NRC), hps, reads=[hPS[j]], writes=[hps])
                r_, k_, v_ = ps[:, 0:8, :], ps[:, 8:16, :], ps[:, 16:24, :]
                actf(lw[0:64, :], ps[0:64, 24, :], AF.Tanh, [hps], [hlw])
                actf(lw[64:128, :], ps[64:128, 24, :], AF.Copy, [hps], [hlw])
                actf(sg[:, 0, :], ps[:, 25, :], AF.Sigmoid, [hps], [hsg])
                actf(sg[0:32, 1, :], ps[0:32, 26, :], AF.Sigmoid, [hps], [hsg])
                pw_, hpw = nextP()
                pa_, hpa = nextP()
                for m in range(8):
                    S.op("pe", lambda e, m=m: e.matmul(pw_[:, m * 128:(m + 1) * 128], lhsT=wup[0:64, m * 128:(m + 1) * 128], rhs=lw[0:64, :], start=True, stop=True),
                         reads=[hwup, hlw], writes=[hpw])
                    S.op("pe", lambda e, m=m: e.matmul(pa_[:, m * 128:(m + 1) * 128], lhsT=aup[64:128, m * 128:(m + 1) * 128], rhs=lw[64:128, :], start=True, stop=True),
                         reads=[haup, hlw], writes=[hpa])
                nld, hnld = F["nld"]
                aa, haa = F["aa"]
                t1, ht1 = F["t1"]
                t2, ht2 = F["t2"]
                t3, ht3 = F["t3"]
                v3 = lambda p_: p_[:].rearrange("p (m t) -> p m t", m=8)
                tt("dve", t1[:], v3(pw_), vb(V_W0), ALU.add, [hpw, c.hvec], [ht1])
                actf(t1[:], t1[:], AF.Sigmoid, [ht1], [ht1])
                S.op("dve", lambda e: e.tensor_scalar(out=nld[:], in0=t1[:], scalar1=0.6065306597126334, scalar2=None, op0=ALU.mult),
                     reads=[ht1], writes=[hnld])
                tt("dve", t2[:], v3(pa_), vb(V_A0), ALU.add, [hpa, c.hvec], [ht2])
                actf(aa[:], t2[:], AF.Sigmoid, [ht2], [haa])
                if own:
                    pg_, hpg = nextP()
                    for m in range(8):
                        S.op("pe", lambda e, m=m: e.matmul(pg_[:, m * 128:(m + 1) * 128], lhsT=gup[:, 0, m * 128:(m + 1) * 128], rhs=sg[:, 0, :], start=True, stop=False),
                             reads=[hgup, hsg], writes=[hpg])
                        S.op("pe", lambda e, m=m: e.matmul(pg_[:, m * 128:(m + 1) * 128], lhsT=gup[0:32, 1, m * 128:(m + 1) * 128], rhs=sg[0:32, 1, :], start=False, stop=True),
                             reads=[hgup, hsg], writes=[hpg])
                    gg, hgg = F["gg"]
                    actf(gg[:], v3(pg_), AF.Copy, [hpg], [hgg])
                kap, hkap = F["kap"]
                sqb, hsqb = Bf["sqb"]
                tt("pool", kap[:], k_, vb(V_KK), ALU.mult, [hps, c.hvec], [hkap])
                actf(sqb[:], kap[:], AF.Square, [hkap], [hsqb])
                pq_, hpq = nextP()
                for hh in range(2):
                    S.op("pe", lambda e, hh=hh: e.matmul(pq_[:, hh * 512:(hh + 1) * 512], lhsT=bones, rhs=sqb[:, hh * 4:(hh + 1) * 4, :], start=True, stop=True),
                         reads=[c.hcb, hsqb], writes=[hpq])
                actf(t3[:], v3(pq_), AF.Sqrt, [hpq], [ht3])
                S.op("dve", lambda e: e.tensor_scalar(out=t3[:], in0=t3[:], scalar1=1e-12, scalar2=None, op0=ALU.max), reads=[ht3], writes=[ht3])
                S.op("dve", lambda e: e.reciprocal(out=t3[:], in_=t3[:]), reads=[ht3], writes=[ht3])
                tt("dve", kap[:], kap[:], t3[:], ALU.mult, [hkap, ht3], [hkap])
                kp, hkp = F["kp"]
                S.op("dve", lambda e: e.scalar_tensor_tensor(out=t2[:], in0=aa[:], scalar=1.0, in1=vb(V_KA), op0=ALU.subtract, op1=ALU.mult),
                     reads=[haa, c.hvec], writes=[ht2])
                S.op("dve", lambda e: e.scalar_tensor_tensor(out=kp[:], in0=t2[:], scalar=1.0, in1=k_, op0=ALU.add, op1=ALU.mult),
                     reads=[ht2, hps], writes=[hkp])
                beta, hbeta = F["beta"]
                tt("pool", beta[:], kap[:], aa[:], ALU.mult, [hkap, haa], [hbeta])
                cs, hcs = F["cs"]
                cum, hcum = F["cum"]
                S.op("dve", lambda e: e.tensor_tensor_scan(out=cs[:].rearrange("p m t -> p (m t)"), data0=ones[:],
                                                           data1=nld[:].rearrange("p m t -> p (m t)"), initial=0.0,
                                                           op0=ALU.mult, op1=ALU.add), reads=[hones, hnld], writes=[hcs])
                cs4 = cs[:].rearrange("p m (s t) -> p m s t", s=2)
                nld4 = nld[:].rearrange("p m (s t) -> p m s t", s=2)
                cum4 = cum[:].rearrange("p m (s t) -> p m s t", s=2)
                tt("dve", sm[:, :, :, 0:1], cs4[:, :, :, 0:1], nld4[:, :, :, 0:1], ALU.subtract, [hcs, hnld], [hsm])
                tt("dve", cum4, cs4, sm[:, :, :, 0:1].broadcast_to([128, 8, 2, 64]), ALU.subtract, [hcs, hsm], [hcum])
                actf(sm[:, :, :, 2:3], cum4[:, :, :, 63:64], AF.Exp, [hcum], [hsm], scale=-1.0)
                Kh, hKh = Bf["Kh"]
                Rh, hRh = Bf["Rh"]
                Bh, hBh = Bf["Bh"]
                Qh, hQh = Bf["Qh"]
                BG, hBG = Bf["BG"]
                QG, hQG = Bf["QG"]
                vbf, hvbf = Bf["vb"]
                actf(t1[:], cum[:], AF.Exp, [hcum], [ht1], scale=-1.0)
                tt("dve", Rh[:], r_, t1[:], ALU.mult, [hps, ht1], [hRh])
                tt("pool", t2[:], cum[:], nld[:], ALU.subtract, [hcum, hnld], [ht2])
                actf(t2[:], t2[:], AF.Exp, [ht2], [ht2], scale=-1.0)
                tt("dve", Kh[:], kap[:], t2[:], ALU.mult, [hkap, ht2], [hKh])
                actf(t3[:], cum[:], AF.Exp, [hcum], [ht3])
                tt("dve", Bh[:], beta[:], t3[:], ALU.mult, [hbeta, ht3], [hBh])
                tt("pool", Qh[:], kp[:], t3[:], ALU.mult, [hkp, ht3], [hQh])
                t14 = t1[:].rearrange("p m (s t) -> p m s t", s=2)
                tt("pool", t14, cum4, cum4[:, :, :, 63:64].broadcast_to([128, 8, 2, 64]), ALU.subtract, [hcum], [ht1])
                actf(t1[:], t1[:], AF.Exp, [ht1], [ht1])
                tt("dve", BG[:], beta[:], t1[:], ALU.mult, [hbeta, ht1], [hBG])
                tt("pool", QG[:], kp[:], t1[:], ALU.mult, [hkp, ht1], [hQG])
                actf(vbf[:], v_, AF.Copy, [hps], [hvbf])
                for (src, hsrc, dst, hdst) in ((vbf, hvbf, Vt, hVt), (BG, hBG, BGt, hBGt), (QG, hQG, QGt, hQGt)):
                    for m in range(8):
                        for s in range(2):
                            S.op("pe", lambda e, m=m, s=s, src=src: e.transpose(
                                PTr[0:64, (s * 8 + m) * 128:(s * 8 + m + 1) * 128], src[:, m, s * 64:(s + 1) * 64], identb),
                                reads=[hsrc, c.hcb], writes=[hPTr])
                    S.op("act", lambda e, dst=dst: e.activation(out=dst[:].rearrange("t s m c -> t (s m c)"), in_=PTr[0:64, :], func=AF.Copy),
                         reads=[hPTr], writes=[hdst])
                if own:
                    bon, hbon = F["bon"]
                    tt("pool", t2[:], r_, kp[:], ALU.mult, [hps, hkp], [ht2])
                    tt("pool", sqb[:], t2[:], vb(V_RK), ALU.mult, [ht2, c.hvec], [hsqb])
                    pb_, hpb = nextP()
                    for hh in range(2):
                        S.op("pe", lambda e, hh=hh: e.matmul(pb_[:, hh * 512:(hh + 1) * 512], lhsT=bones, rhs=sqb[:, hh * 4:(hh + 1) * 4, :], start=True, stop=True),
                             reads=[c.hcb, hsqb], writes=[hpb])
                    tt("dve", bon[:], v3(pb_), v_, ALU.mult, [hpb, hps], [hbon])
                for s in range(2):
                    ts = slice(s * 64, (s + 1) * 64)

                    def hrows(h):
                        return slice((h % 2) * 64, (h % 2) * 64 + 64)

                    def gram(lt, hl, rt, hr, mask, dname, eng):
                        p_, hp_ = nextP()
                        pv = p_[0:64, :].rearrange("p (h t) -> p h t", h=16)
                        for h in range(16):
                            S.op("pe", lambda e, h=h: e.matmul(pv[:, h, :], lhsT=lt[hrows(h), h // 2, ts], rhs=rt[hrows(h), h // 2, ts], start=True, stop=True),
                                 reads=[hl, hr], writes=[hp_])
                        d_, hd_ = gm[dname]
                        tt(eng, d_[:], pv, mask, ALU.mult, [hp_, c.hcn], [hd_])

                    gram(Bh, hBh, Kh, hKh, maskU, "Ub", "dve")
                    gram(Kh, hKh, Bh, hBh, maskL, "Lb", "dve")
                    gram(Qh, hQh, Kh, hKh, maskU, "Lak", "dve")
                    if own:
                        gram(Bh, hBh, Rh, hRh, maskUi, "Mrb", "dve")
                        gram(Qh, hQh, Rh, hRh, maskUi, "Mrk", "dve")
                    Ub, hUb = gm["Ub"]
                    Lb, hLb = gm["Lb"]
                    Qb, hQb = gm["Qb"]
                    idb = c.cn[0:64, C_ID:C_ID + 64].unsqueeze(1).broadcast_to([64, 16, 64])
                    tt("dve", Qb[:], idb, Ub[:], ALU.subtract, [c.hcn, hUb], [hQb])
                    cur = ("Ub", "Lb")
                    nxt = ("U2", "L2")
                    for lvl in range(5):
                        Uk, hUk = gm[cur[0]]
                        Lk, hLk = gm[cur[1]]
                        Un, hUn = gm[nxt[0]]
                        Ln, hLn = gm[nxt[1]]
                        p2, hp2 = nextP()
                        p2v = p2[0:64, :].rearrange("p (h t) -> p h t", h=16)
                        for h in range(16):
                            S.op("pe", lambda e, h=h: e.matmul(p2v[:, h, :], lhsT=Uk[:, h, :], rhs=Lk[:, h, :], start=True, stop=True),
                                 reads=[hUk, hLk], writes=[hp2])
                        actf(Ln[:], p2v, AF.Copy, [hp2], [hLn])
                        if lvl < 4:
                            p1, hp1 = nextP()
                            p1v = p1[0:64, :].rearrange("p (h t) -> p h t", h=16)
                            for h in range(16):
                                S.op("pe", lambda e, h=h: e.matmul(p1v[:, h, :], lhsT=Lk[:, h, :], rhs=Uk[:, h, :], start=True, stop=True),
                                     reads=[hUk, hLk], writes=[hp1])
                            S.op("dve", lambda e: e.tensor_copy(out=Un[:], in_=p1v), reads=[hp1], writes=[hUn])
                        p3, hp3 = nextP()
                        p3v = p3[0:64, :].rearrange("p (h t) -> p h t", h=16)
                        for h in range(16):
                            S.op("pe", lambda e, h=h: e.matmul(p3v[:, h, :], lhsT=Ln[:, h, :], rhs=Qb[:, h, :], start=True, stop=True),
                                 reads=[hLn, hQb], writes=[hp3])
                        tt("dve", Qb[:], p3v, Qb[:], ALU.add, [hp3, hQb], [hQb])
                        cur, nxt = nxt, cur
                    Lak, hLak = gm["Lak"]
                    Xb, hXb = gm["Xb"]
                    SAb, hSAb = gm["SAb"]
                    px, hpx = nextP()
                    pxv = px[0:64, :].rearrange("p (h t) -> p h t", h=16)
                    for h in range(16):
                        cs_ = slice((h % 2) * 64, (h % 2) * 64 + 64)
                        S.op("pe", lambda e, h=h: e.matmul(pxv[:, h, :], lhsT=Kh[hrows(h), h // 2, ts], rhs=Sb[hrows(h), h // 2, :], start=True, stop=False),
                             reads=[hKh, hSb], writes=[hpx])
                        S.op("pe", lambda e, h=h, cs_=cs_: e.matmul(pxv[:, h, :], lhsT=Lak[:, h, :], rhs=Vt[:, s, h // 2, cs_], start=False, stop=True),
                             reads=[hLak, hVt], writes=[hpx])
                    actf(Xb[:], pxv, AF.Copy, [hpx], [hXb])
                    psa, hpsa = nextP()
                    psav = psa[0:64, :].rearrange("p (h t) -> p h t", h=16)
                    for h in range(16):
                        S.op("pe", lambda e, h=h: e.matmul(psav[:, h, :], lhsT=Qb[:, h, :], rhs=Xb[:, h, :], start=True, stop=True),
                             reads=[hQb, hXb], writes=[hpsa])
                    actf(SAb[:], psav, AF.Copy, [hpsa], [hSAb], scale=-1.0)
                    if own:
                        Mrb, hMrb = gm["Mrb"]
                        Mrk, hMrk = gm["Mrk"]
                        py, hpy = nextP()
                        pyv = py[0:64, :].rearrange("p (h t) -> p h t", h=16)
                        for h in range(16):
                            cs_ = slice((h % 2) * 64, (h % 2) * 64 + 64)
                            S.op("pe", lambda e, h=h: e.matmul(pyv[:, h, :], lhsT=Rh[hrows(h), h // 2, ts], rhs=Sb[hrows(h), h // 2, :], start=True, stop=False),
                                 reads=[hRh, hSb], writes=[hpy])
                            S.op("pe", lambda e, h=h: e.matmul(pyv[:, h, :], lhsT=Mrb[:, h, :], rhs=SAb[:, h, :], start=False, stop=False),
                                 reads=[hMrb, hSAb], writes=[hpy])
                            S.op("pe", lambda e, h=h, cs_=cs_: e.matmul(pyv[:, h, :], lhsT=Mrk[:, h, :], rhs=Vt[:, s, h // 2, cs_], start=False, stop=True),
                                 reads=[hMrk, hVt], writes=[hpy])
                        actf(ysb[:], pyv, AF.Copy, [hpy], [hysb])
                        S.op("dve", lambda e: e.tensor_reduce(out=gst[:, :, 0:1], in_=ysb[:], axis=AX.X, op=ALU.add), reads=[hysb], writes=[hgst])
                        S.op("dve", lambda e: e.tensor_scalar(out=gst[:, :, 0:1], in0=gst[:, :, 0:1], scalar1=1.0 / 64, scalar2=None, op0=ALU.mult), reads=[hgst], writes=[hgst])
                        tt("dve", yc[:], ysb[:], gst[:, :, 0:1].broadcast_to([64, 16, 64]), ALU.subtract, [hysb, hgst], [hyc])
                        actf(ysq[:], yc[:], AF.Square, [hyc], [hysq])
                        S.op("dve", lambda e: e.tensor_reduce(out=gst[:, :, 1:2], in_=ysq[:], axis=AX.X, op=ALU.add), reads=[hysq], writes=[hgst])
                        S.op("dve", lambda e: e.tensor_scalar(out=gst[:, :, 1:2], in0=gst[:, :, 1:2], scalar1=1.0 / 64, scalar2=64e-5, op0=ALU.mult, op1=ALU.add), reads=[hgst], writes=[hgst])
                        actf(gst[:, :, 2:3], gst[:, :, 1:2], AF.Sqrt, [hgst], [hgst])
                        S.op("dve", lambda e: e.reciprocal(out=gst[:, :, 3:4], in_=gst[:, :, 2:3]), reads=[hgst], writes=[hgst])
                        tt("dve", yc[:], yc[:], gst[:, :, 3:4].broadcast_to([64, 16, 64]), ALU.mult, [hyc, hgst], [hyc])
                        pt2, hpt2 = nextP()
                        for m in range(8):
                            S.op("pe", lambda e, m=m: e.transpose(pt2[:, m * 64:(m + 1) * 64], yc[:, 2 * m:2 * m + 2, :].rearrange("t h i -> t (h i)"), ident32[0:64, 0:64]),
                                 reads=[hyc, c.hcn], writes=[hpt2])
                        S.op("dve", lambda e: e.tensor_copy(out=yf[:, :, ts], in_=pt2[:, 0:512].rearrange("p (m t) -> p m t", m=8)), reads=[hpt2], writes=[hyf])
                    pst, hpst = nextP()
                    for h in range(16):
                        cs_ = slice((h % 2) * 64, (h % 2) * 64 + 64)
                        o_ = pst[hrows(h), (h // 2) * 64:(h // 2) * 64 + 64]
                        S.op("pe", lambda e, h=h, cs_=cs_, o_=o_: e.matmul(o_, lhsT=BGt[:, s, h // 2, cs_], rhs=SAb[:, h, :], start=True, stop=False),
                             reads=[hBGt, hSAb], writes=[hpst])
                        S.op("pe", lambda e, h=h, cs_=cs_, o_=o_: e.matmul(o_, lhsT=QGt[:, s, h // 2, cs_], rhs=Vt[:, s, h // 2, cs_], start=False, stop=True),
                             reads=[hQGt, hVt], writes=[hpst])
                    tt("dve", S32[:], S32[:], sm[:, :, s, 2:3].broadcast_to([128, 8, 64]), ALU.mult, [hS32, hsm], [hS32])
                    tt("dve", S32[:], S32[:], pst[:, 0:512].rearrange("p (m i) -> p m i", m=8), ALU.add, [hS32, hpst], [hS32])
                    actf(Sb[:], S32[:], AF.Copy, [hS32], [hSb])
                if own:
                    gg, hgg = F["gg"]
                    bon, hbon = F["bon"]
                    tt("dve", yf[:], yf[:], vb(V_GNW), ALU.mult, [hyf, c.hvec], [hyf])
                    tt("pool", yf[:], yf[:], vb(V_GNB), ALU.add, [hyf, c.hvec], [hyf])
                    tt("pool", yf[:], yf[:], bon[:], ALU.add, [hyf, hbon], [hyf])
                    tt("dve", yrb[:], yf[:], gg[:], ALU.mult, [hyf, hgg], [hyrb])
                    jo = j - OWN0
                    S.dma("sp", YR_d[jo].rearrange("p (m t) -> p m t", m=8), yrb[:], hyrb, reads=[hyrb], writes=[hYR[jo]])
            S.barrier()


    def tt_(eng, out, i0, i1, op, reads, writes):
        S.op(eng, lambda e: e.tensor_tensor(out=out, in0=i0, in1=i1, op=op), reads=reads, writes=writes)

    def act_(out, in_, func, reads, writes, **kw):
        S.op("act", lambda e: e.activation(out=out, in_=in_, func=func, **kw), reads=reads, writes=writes)

    def phase_C():
        with contextlib.ExitStack() as es:
            T, P = mk(es)
            c = load_consts(T)
            vec = c.vec
            bones = c.cb[:, C_BO:C_BO + 128]
            identb = c.cb[:, C_ID:C_ID + 128]
            qk, hqk = T("qk", [128, 10, 128])
            sqb, hsqb = T("sqb", [128, 8, 128], BF16)
            t1, ht1 = T("t1", [128, 8, 128])
            qT, hqT = T("qT", [128, 8, 128], BF16)
            knb, hknb = T("knb", [128, 128], BF16)
            vbf, hvbf = T("vbf", [128, 128], BF16)
            kd = [T("kd%d" % i, [128, 2, 128], BF16) for i in range(2)]
            Vk = [T("Vk%d" % i, [128, 128], BF16) for i in range(2)]
            e1, he1 = T("e1", [128, 4, 128], BF16)
            PTb, hPTb = T("PTb", [128, 4, 128], BF16)
            onesb, honesb = T("onesb", [128, 64], BF16)
            S.op("pool", lambda e: e.memset(onesb[:], 1.0), writes=[honesb])
            esk, hesk = T("esk", [128, 8])
            act_(esk[:], vec[:, V_SK:V_SK + 8], AF.Exp, [c.hvec], [hesk])
            den, hden = T("den", [128, 4, 128])
            ya, hya = T("ya", [128, 8, 128], BF16)
            PS1, hPS1 = P("PS1", [128, 1024])
            psc = [P("psc%d" % i, [128, 512]) for i in range(2)]
            po, hpo = P("po", [128, 512])
            pdn, hpdn = P("pdn", [128, 512])
            pvT, hpvT = P("pvT", [128, 128], BF16)

            def norm_rows(src, n, gcol, dst, hdst):
                act_(sqb[:, 0:n, :], src, AF.Square, [hqk], [hsqb])
                for hh in range(0, n, 4):
                    w_ = min(4, n - hh)
                    S.op("pe", lambda e, hh=hh, w_=w_: e.matmul(PS1[:, hh * 128:(hh + w_) * 128], lhsT=bones, rhs=sqb[:, hh:hh + w_, :], start=True, stop=True),
                         reads=[c.hcb, hsqb], writes=[hPS1])
                pv = PS1[:, 0:n * 128].rearrange("p (m t) -> p m t", m=n)
                S.op("dve", lambda e: e.tensor_scalar(out=t1[:, 0:n, :], in0=pv, scalar1=1.0 / 64, scalar2=1e-6, op0=ALU.mult, op1=ALU.add),
                     reads=[hPS1], writes=[ht1])
                act_(t1[:, 0:n, :], t1[:, 0:n, :], AF.Sqrt, [ht1], [ht1])
                S.op("dve", lambda e: e.reciprocal(out=t1[:, 0:n, :], in_=t1[:, 0:n, :]), reads=[ht1], writes=[ht1])
                tt_("dve", t1[:, 0:n, :], t1[:, 0:n, :], src, ALU.mult, [ht1, hqk], [ht1])
                S.op("dve", lambda e: e.tensor_scalar(out=dst, in0=t1[:, 0:n, :], scalar1=vec[:, gcol:gcol + 1], scalar2=None, op0=ALU.mult),
                     reads=[ht1, c.hvec], writes=[hdst])

            def kv_prep(slot):
                kd_, hkd_ = kd[slot]
                Vk_, hVk_ = Vk[slot]
                norm_rows(qk[:, 8:9, :], 1, V_KG, knb[:].unsqueeze(1), hknb)
                for g in range(2):
                    S.op("pe", lambda e, g=g: e.matmul(PS1[:, 512 + g * 128:512 + (g + 1) * 128], lhsT=c.cb[:, C_SEL + g * 128:C_SEL + (g + 1) * 128], rhs=knb[:], start=True, stop=True),
                         reads=[c.hcb, hknb], writes=[hPS1])
                act_(kd_[:], PS1[:, 512:768].rearrange("p (g t) -> p g t", g=2), AF.Copy, [hPS1], [hkd_])
                act_(vbf[:], qk[:, 9, :], AF.Copy, [hqk], [hvbf])
                S.op("pe", lambda e: e.transpose(pvT[:], vbf[:], identb), reads=[hvbf, c.hcb], writes=[hpvT])
                S.op("dve", lambda e: e.tensor_copy(out=Vk_[:], in_=pvT[:]), reads=[hpvT], writes=[hVk_])

            S.dma("sp", qk[:, 8:10, :], QKV_d[0].rearrange("p (c t) -> p c t", c=10)[:, 8:10, :], hqk, reads=[hQKV[0]], writes=[hqk])
            kv_prep(0)
            for jo in range(NO):
                ja = jo + 1
                S.dma("sp", qk[:], QKV_d[ja].rearrange("p (c t) -> p c t", c=10), hqk, reads=[hQKV[ja]], writes=[hqk])
                kv_prep(ja % 2)
                norm_rows(qk[:, 0:8, :], 8, V_QG, qT[:], hqT)
                si = 0
                for g in range(2):
                    for par in range(2):
                        rows = slice(par * 64, par * 64 + 64)
                        for wi, slot in enumerate(((ja - 1) % 2, ja % 2)):
                            kd_, hkd_ = kd[slot]
                            Vk_, hVk_ = Vk[slot]
                            ps_, hps_ = psc[si % 2]
                            si += 1
                            S.op("pe", lambda e, ps_=ps_, kd_=kd_: e.matmul(ps_[:], lhsT=kd_[rows, g, :], rhs=qT[rows, 4 * g:4 * g + 4, :], start=True, stop=True),
                                 reads=[hkd_, hqT], writes=[hps_])
                            act_(e1[:], ps_[:].rearrange("p (h t) -> p h t", h=4), AF.Exp, [hps_], [he1], scale=0.125)
                            mcol = C_MC if wi == 1 else (C_MPF if jo == 0 else C_MP)
                            mk_ = c.cb[:, mcol:mcol + 128].unsqueeze(1).broadcast_to([128, 4, 128])
                            tt_("pool", PTb[:], e1[:], mk_, ALU.mult, [he1, c.hcb], [hPTb])
                            S.op("pe", lambda e, Vk_=Vk_, wi=wi: e.matmul(po[rows, :], lhsT=Vk_[:, g * 64:(g + 1) * 64], rhs=PTb[:].rearrange("p h t -> p (h t)"), start=(wi == 0), stop=(wi == 1)),
                                 reads=[hVk_, hPTb], writes=[hpo])
                            S.op("pe", lambda e, wi=wi: e.matmul(pdn[rows, :], lhsT=onesb[:], rhs=PTb[:].rearrange("p h t -> p (h t)"), start=(wi == 0), stop=(wi == 1)),
                                 reads=[honesb, hPTb], writes=[hpdn])
                    tt_("dve", den[:], pdn[:].rearrange("p (h t) -> p h t", h=4), esk[:, 4 * g:4 * g + 4].unsqueeze(2).broadcast_to([128, 4, 128]), ALU.add, [hpdn, hesk], [hden])
                    S.op("dve", lambda e: e.reciprocal(out=den[:], in_=den[:]), reads=[hden], writes=[hden])
                    tt_("dve", ya[:, 4 * g:4 * g + 4, :], po[:].rearrange("p (h t) -> p h t", h=4), den[:], ALU.mult, [hpo, hden], [hya])
                S.dma("sp", YA_d[jo].rearrange("p (m t) -> p m t", m=8), ya[:], hya, reads=[hya], writes=[hYA[jo]])
            S.barrier()

    def phase_D():
        with contextlib.ExitStack() as es:
            T, P = mk(es)
            c = load_consts(T)
            wo, hwo = T("wo", [128, DC, D], BF16)
            for k in range(DC):
                S.dma("pool", wo[:, k, :], wout_d[k * 128:(k + 1) * 128, :], hwo, writes=[hwo])
            yr, hyr = T("yr", [128, 8, 128], BF16)
            ya, hya = T("ya", [128, 8, 128], BF16)
            xt, hxt = T("xt", [128, D])
            hm, hhm = T("hm", [128, D])
            st, hst = T("st", [128, 4])
            xs, hxs = T("xs", [128, D], BF16)
            uT, huT = T("uT", [128, DC, 128], BF16)
            pT, hpT = P("pT", [128, DC, 128], BF16)
            pp = [P("pp%d" % i, [128, 512]) for i in range(4)]
            for jo in range(NO):
                j = OWN0 + jo
                S.dma("sp", yr[:], YR_d[jo].rearrange("p (m t) -> p m t", m=8), hyr, reads=[hYR[jo]], writes=[hyr])
                S.dma("sp", ya[:], YA_d[jo].rearrange("p (m t) -> p m t", m=8), hya, reads=[hYA[jo]], writes=[hya])
                S.dma("sp", xt[:], xin[j * 128:(j + 1) * 128, :], hxt, writes=[hxt])
                for cg in range(4):
                    p_, hp_ = pp[cg]
                    for kc in range(DC):
                        src, hsrc = (yr, hyr) if kc < 8 else (ya, hya)
                        S.op("pe", lambda e, p_=p_, kc=kc, cg=cg, src=src: e.matmul(p_[:], lhsT=src[:, kc % 8, :], rhs=wo[:, kc, cg * 512:(cg + 1) * 512], start=(kc == 0), stop=(kc == DC - 1)),
                             reads=[hsrc, hwo], writes=[hp_])
                    tt_("dve", hm[:, cg * 512:(cg + 1) * 512], p_[:], xt[:, cg * 512:(cg + 1) * 512], ALU.add, [hp_, hxt], [hhm])
                S.dma("sp", HM_d[jo], hm[:], hhm, reads=[hhm], writes=[hHM[jo]])
                rmsnorm_T(c, T, hm, hhm, V_G2, xs, hxs, st, hst, xs, hxs, pT, hpT, uT, huT)
                S.dma("sp", XNT_d[jo].rearrange("p (k t) -> p k t", k=DC), uT[:], huT, reads=[huT], writes=[hXNT[jo]])
            S.barrier()

    def phase_E1():
        with contextlib.ExitStack() as es:
            T, P = mk(es)
            c = load_consts(T)
            pq, hpq = T("pq", [128, DC, D], BF16)
            for k in range(DC):
                S.dma("pool", pq[:, k, :], pq_d[k * 128:(k + 1) * 128, :], hpq, writes=[hpq])
            skn, hskn = T("skn", [128, 16, 128])
            S.dma("sp", skn[:], sk_d.rearrange("c n d -> n c d"), hskn, writes=[hskn])
            skT, hskT = T("skT", [128, 16, 128])
            pbig = [P("pb%d" % i, [128, 16, 128]) for i in range(2)]
            p0, hp0 = pbig[0]
            for hc in range(16):
                S.op("pe", lambda e, hc=hc: e.transpose(p0[:, hc, :], skn[:, hc, :], c.cn[:, C_ID:C_ID + 128]), reads=[hskn, c.hcn], writes=[hp0])
            S.op("dve", lambda e: e.tensor_copy(out=skT[:], in_=p0[:]), reads=[hp0], writes=[hskT])
            xnT, hxnT = T("xnT", [128, DC, 128], BF16)
            qTs, hqTs = T("qTs", [128, 16, 128])
            sall, hsall = T("sall", [128, 16, 128])
            tops, htops = T("tops", [128, 16, 16])
            wk, hwk = T("wk", [128, 128])
            cand, hcand = T("cand", [128, 8, 256])
            wk2, hwk2 = T("wk2", [128, 256])
            ctop, hctop = T("ctop", [128, 8, 24])
            sm, hsm = T("sm", [128, 4, 8])
            ez, hez = T("ez", [128, 8, 16])
            stt, hstt = T("stt", [128, 2048 + 8])
            for jo in range(NO):
                S.dma("sp", xnT[:], XNT_d[jo].rearrange("p (k t) -> p k t", k=DC), hxnT, reads=[hXNT[jo]], writes=[hxnT])
                pa_, hpa_ = pbig[0]
                for hc in range(16):
                    for kc in range(DC):
                        S.op("pe", lambda e, hc=hc, kc=kc: e.matmul(pa_[:, hc, :], lhsT=pq[:, kc, hc * 128:(hc + 1) * 128], rhs=xnT[:, kc, :], start=(kc == 0), stop=(kc == DC - 1)),
                             reads=[hpq, hxnT], writes=[hpa_])
                act_(qTs[:, 0:8, :], pa_[:, 0:8, :], AF.Copy, [hpa_], [hqTs])
                S.op("dve", lambda e: e.tensor_copy(out=qTs[:, 8:16, :], in_=pa_[:, 8:16, :]), reads=[hpa_], writes=[hqTs])
                pb_, hpb_ = pbig[1]
                for hc in range(16):
                    S.op("pe", lambda e, hc=hc: e.matmul(pb_[:, hc, :], lhsT=qTs[:, hc, :], rhs=skT[:, hc, :], start=True, stop=True),
                         reads=[hqTs, hskT], writes=[hpb_])
                act_(sall[:, 0:8, :], pb_[:, 0:8, :], AF.Copy, [hpb_], [hsall])
                S.op("dve", lambda e: e.tensor_copy(out=sall[:, 8:16, :], in_=pb_[:, 8:16, :]), reads=[hpb_], writes=[hsall])
                for hc in range(16):
                    S.op("dve", lambda e, hc=hc: e.max(out=tops[:, hc, 0:8], in_=sall[:, hc, :]), reads=[hsall], writes=[htops])
                    S.op("dve", lambda e, hc=hc: e.match_replace(out=wk[:], in_to_replace=tops[:, hc, 0:8], in_values=sall[:, hc, :], imm_value=-1e30),
                         reads=[hsall, htops], writes=[hwk])
                    S.op("dve", lambda e, hc=hc: e.max(out=tops[:, hc, 8:16], in_=wk[:]), reads=[hwk], writes=[htops])
                t4 = tops[:].rearrange("p (h c) k -> p h c k", c=2)
                tt_("pool", cand[:].rearrange("p h (i j) -> p h i j", i=16), t4[:, :, 0, :].unsqueeze(3).broadcast_to([128, 8, 16, 16]),
                    t4[:, :, 1, :].unsqueeze(2).broadcast_to([128, 8, 16, 16]), ALU.add, [htops], [hcand])
                for h in range(8):
                    S.op("dve", lambda e, h=h: e.max(out=ctop[:, h, 0:8], in_=cand[:, h, :]), reads=[hcand], writes=[hctop])
                    S.op("dve", lambda e, h=h: e.match_replace(out=wk2[:], in_to_replace=ctop[:, h, 0:8], in_values=cand[:, h, :], imm_value=-1e30),
                         reads=[hcand, hctop], writes=[hwk2])
                    S.op("dve", lambda e, h=h: e.max(out=ctop[:, h, 8:16], in_=wk2[:]), reads=[hwk2], writes=[hctop])
                    S.op("dve", lambda e, h=h: e.match_replace(out=wk2[:], in_to_replace=ctop[:, h, 8:16], in_values=wk2[:], imm_value=-1e30),
                         reads=[hwk2, hctop], writes=[hwk2])
                    S.op("dve", lambda e, h=h: e.max(out=ctop[:, h, 16:24], in_=wk2[:]), reads=[hwk2], writes=[hctop])
                thr = sm[:, 0, :]
                tt_("dve", thr.unsqueeze(2), ctop[:, :, 15:16], ctop[:, :, 16:17], ALU.add, [hctop], [hsm])
                S.op("dve", lambda e: e.tensor_scalar(out=thr, in0=thr, scalar1=0.5, scalar2=None, op0=ALU.mult), reads=[hsm], writes=[hsm])
                tt_("dve", ez[:], ctop[:, :, 0:16], thr.unsqueeze(2).broadcast_to([128, 8, 16]), ALU.subtract, [hctop, hsm], [hez])
                act_(ez[:], ez[:], AF.Exp, [hez], [hez])
                S.op("dve", lambda e: e.tensor_reduce(out=sm[:, 1, :].unsqueeze(2), in_=ez[:], axis=AX.X, op=ALU.add), reads=[hez], writes=[hsm])
                S.op("dve", lambda e: e.reciprocal(out=stt[:, 2048:2056], in_=sm[:, 1, :]), reads=[hsm], writes=[hstt])
                s4 = sall[:].rearrange("p (h c) n -> p h c n", c=2)
                st4 = stt[:, 0:2048].rearrange("p (h c n) -> p h c n", h=8, c=2)
                tt_("dve", st4[:, :, 0, :], s4[:, :, 0, :], thr.unsqueeze(2).broadcast_to([128, 8, 128]), ALU.subtract, [hsall, hsm], [hstt])
                S.op("pool", lambda e: e.tensor_copy(out=st4[:, :, 1, :], in_=s4[:, :, 1, :]), reads=[hsall], writes=[hstt])
                S.dma("sp", ST_d[jo], stt[:], hstt, reads=[hstt], writes=[hST[jo]])
            S.barrier()

    def phase_E2(G=4):
        with contextlib.ExitStack() as es:
            T, P = mk(es)
            c = load_consts(T)
            identb = c.cb[:, C_ID:C_ID + 128]
            G = min(G, NO)
            xn = [T("xn%d" % i, [128, DC, 128], BF16) for i in range(G)]
            stg = [T("stg%d" % i, [128, 2048 + 8]) for i in range(G)]
            acc = [T("acc%d" % i, [128, D]) for i in range(G)]
            dnb = [T("dnb%d" % i, [128, 4, D], BF16) for i in range(2)]
            upb = [T("upb%d" % i, [128, 4, D], BF16) for i in range(2)]
            dT, hdT = T("dT", [128, DC, 512], BF16)
            SUM, hSUM = T("SUM", [128, 4, 512])
            EE, hEE = T("EE", [128, 4, 512])
            ge, hge = T("ge", [128, 512])
            Wa, hWa = T("Wa", [128, 512])
            Gb, hGb = T("Gb", [128, 512], BF16)
            GTb, hGTb = T("GTb", [128, 4, 128], BF16)
            ptr, hptr = P("ptr", [128, 4, 512], BF16)
            phid, hphid = P("phid", [128, 512])
            pgt, hpgt = P("pgt", [128, 4, 128], BF16)
            pout, hpout = P("pout", [128, D])
            NCH = NE // 512
            for g0 in range(0, NO, G):
                tiles = list(range(g0, min(NO, g0 + G)))
                for i, jo in enumerate(tiles):
                    S.dma("sp", xn[i][0][:], XNT_d[jo].rearrange("p (k t) -> p k t", k=DC), xn[i][1], reads=[hXNT[jo]], writes=[xn[i][1]])
                    S.dma("sp", stg[i][0][:], ST_d[jo], stg[i][1], reads=[hST[jo]], writes=[stg[i][1]])
                    S.dma("sp", acc[i][0][:], HM_d[jo], acc[i][1], reads=[hHM[jo]], writes=[acc[i][1]])

                def issue(ec):
                    d_, hd_ = dnb[ec % 2]
                    u_, hu_ = upb[ec % 2]
                    S.dma("pool", d_[:], pd_d[ec * 512:(ec + 1) * 512, :].rearrange("(a p) d -> p a d", p=128), hd_, writes=[hd_])
                    S.dma("pool", u_[:], pu_d[ec * 512:(ec + 1) * 512, :].rearrange("(a p) d -> p a d", p=128), hu_, writes=[hu_])
                issue(0)
                for ec in range(NCH):
                    if ec + 1 < NCH:
                        issue(ec + 1)
                    d_, hd_ = dnb[ec % 2]
                    u_, hu_ = upb[ec % 2]
                    for k4 in range(4):
                        for kk in range(4):
                            kc = k4 * 4 + kk
                            for et in range(4):
                                S.op("pe", lambda e, kk=kk, et=et, kc=kc, d_=d_: e.transpose(ptr[:, kk, et * 128:(et + 1) * 128], d_[:, et, kc * 128:(kc + 1) * 128], identb),
                                     reads=[hd_, c.hcb], writes=[hptr])
                        if k4 % 2 == 0:
                            act_(dT[:, k4 * 4:k4 * 4 + 4, :], ptr[:], AF.Copy, [hptr], [hdT])
                        else:
                            S.op("dve", lambda e, k4=k4: e.tensor_copy(out=dT[:, k4 * 4:k4 * 4 + 4, :], in_=ptr[:]), reads=[hptr], writes=[hdT])
                    for i, jo in enumerate(tiles):
                        xn_, hxn_ = xn[i]
                        st_, hst_ = stg[i]
                        ac_, hac_ = acc[i]
                        for kc in range(DC):
                            S.op("pe", lambda e, kc=kc, xn_=xn_: e.matmul(phid[:], lhsT=xn_[:, kc, :], rhs=dT[:, kc, :], start=(kc == 0), stop=(kc == DC - 1)),
                                 reads=[hxn_, hdT], writes=[hphid])
                        act_(ge[:], phid[:], AF.Gelu, [hphid], [hge])
                        st4 = st_[:, 0:2048].rearrange("p (h c n) -> p h c n", h=8, c=2)
                        for half in range(2):
                            hs = slice(half * 4, half * 4 + 4)
                            in0 = st4[:, hs, 0, ec * 4:ec * 4 + 4].unsqueeze(3).broadcast_to([128, 4, 4, 128])
                            in1 = st4[:, hs, 1, :].unsqueeze(2).broadcast_to([128, 4, 4, 128])
                            tt_("pool", SUM[:].rearrange("p h (a n) -> p h a n", a=4), in0, in1, ALU.add, [hst_], [hSUM])
                            act_(EE[:], SUM[:], AF.Exp, [hSUM], [hEE])
                            S.op("dve", lambda e: e.scalar_tensor_tensor(out=EE[:], in0=SUM[:], scalar=0.0, in1=EE[:], op0=ALU.is_ge, op1=ALU.mult),
                                 reads=[hSUM, hEE], writes=[hEE])
                            for h in range(4):
                                hh = half * 4 + h
                                if hh == 0:
                                    S.op("dve", lambda e, h=h, hh=hh, st_=st_: e.tensor_scalar(out=Wa[:], in0=EE[:, h, :], scalar1=st_[:, 2048 + hh:2049 + hh], scalar2=None, op0=ALU.mult),
                                         reads=[hEE, hst_], writes=[hWa])
                                else:
                                    S.op("dve", lambda e, h=h, hh=hh, st_=st_: e.scalar_tensor_tensor(out=Wa[:], in0=EE[:, h, :], scalar=st_[:, 2048 + hh:2049 + hh], in1=Wa[:], op0=ALU.mult, op1=ALU.add),
                                         reads=[hEE, hst_, hWa], writes=[hWa])
                        tt_("pool", Gb[:], ge[:], Wa[:], ALU.mult, [hge, hWa], [hGb])
                        for et in range(4):
                            S.op("pe", lambda e, et=et: e.transpose(pgt[:, et, :], Gb[:, et * 128:(et + 1) * 128], identb), reads=[hGb, c.hcb], writes=[hpgt])
                        act_(GTb[:], pgt[:], AF.Copy, [hpgt], [hGTb])
                        for dg in range(4):
                            for et in range(4):
                                S.op("pe", lambda e, dg=dg, et=et, u_=u_: e.matmul(pout[:, dg * 512:(dg + 1) * 512], lhsT=GTb[:, et, :], rhs=u_[:, et, dg * 512:(dg + 1) * 512], start=(et == 0), stop=(et == 3)),
                                     reads=[hGTb, hu_], writes=[hpout])
                        tt_("dve", ac_[:], ac_[:], pout[:], ALU.add, [hac_, hpout], [hac_])
                for i, jo in enumerate(tiles):
                    S.dma("sp", yout[jo * 128:(jo + 1) * 128, :], acc[i][0][:], acc[i][1], reads=[acc[i][1]], writes=[hOUT[jo]])
            S.barrier()

    ph = {"A": phase_A, "B": phase_B, "C": phase_C, "D": phase_D, "E": phase_E1, "F": phase_E2}
    ctx = dict(nc=nc, S=S, mk=mk, load_consts=load_consts, rmsnorm_T=rmsnorm_T, locals=locals())
    for p in phases:
        if p in ph:
            ph[p]()
    return nc, ctx


def _colT(v, n):
    buf = np.zeros(n * 128, np.float32)
    buf[:v.size] = v.reshape(-1)
    return buf.reshape(n, 128).T


def make_vecs(inp):
    vecs = np.zeros((128, NV), np.float32)
    vecs[:, V_G1:V_G1 + 16] = _colT(inp["norm1_g"][0], 16)
    vecs[:, V_MU:V_MU + 27] = _colT(inp["shift_mu"][0], 27)
    for col, key in ((V_W0, "w0"), (V_A0, "a0"), (V_KK, "k_k"), (V_KA, "k_a"), (V_GNW, "gn_w"),
                     (V_GNB, "gn_b"), (V_RK, "r_k")):
        vecs[:, col:col + 8] = _colT(inp[key][0], 8)
    vecs[:, V_QG] = np.tile(inp["q_gain"][0], 2)
    vecs[:, V_KG] = np.tile(inp["k_gain"][0], 2)
    sk = inp["sinks"][0]
    for hp in range(8):
        vecs[0:64, V_SK + hp] = sk[2 * hp]
        vecs[64:128, V_SK + hp] = sk[2 * hp + 1]
    vecs[:, V_G2:V_G2 + 16] = _colT(inp["norm2_g"][0], 16)
    return vecs


def make_consts(first_half):
    cn = np.zeros((128, NCN), np.float32)
    cn[:, C_ID:C_ID + 128] = np.eye(128, dtype=np.float32)
    bo = np.zeros((128, 128), np.float32)
    bo[0:64, 0:64] = 1
    bo[64:, 64:] = 1
    cn[:, C_BO:C_BO + 128] = bo
    s = np.arange(64)[:, None]
    t = np.arange(64)[None, :]
    cn[0:64, C_MU:C_MU + 64] = (s < t)
    cn[0:64, C_MUI:C_MUI + 64] = (s <= t)
    cn[0:64, C_ML:C_ML + 64] = (s > t)
    s = np.arange(128)[:, None]
    q = np.arange(128)[None, :]
    cn[:, C_MC:C_MC + 128] = (s <= q)
    cn[:, C_MP:C_MP + 128] = (s > q)
    mpf = (s > q)
    if first_half:
        mpf = mpf & (s >= 112)
    cn[:, C_MPF:C_MPF + 128] = mpf
    for g in range(2):
        sel = np.zeros((128, 128), np.float32)
        for m in range(128):
            sel[g * 64 + (m % 64), m] = 1
        cn[:, C_SEL + g * 128:C_SEL + (g + 1) * 128] = sel
    return cn


_NC_CACHE = {}


def kernel(**inputs):
    inp = {k: np.asarray(v) for k, v in inputs.items()}
    x = inp["x"].astype(np.float32, copy=False)
    B, SEQ, _ = x.shape
    NB, OWN0 = 33, 17
    if "nc" not in _NC_CACHE:
        _NC_CACHE["nc"] = build(NB=NB, OWN0=OWN0, phases="ABCDEF")[0]
    nc = _NC_CACHE["nc"]
    f = lambda k: np.ascontiguousarray(inp[k][0], dtype=np.float32)
    shared = dict(w_in=f("w_in"), vecs=make_vecs(inp), w_up=f("w_up"), a_up=f("a_up"), g_up=f("g_up"),
                  w_out=f("w_out"), peer_query=f("peer_query"),
                  sub_keys=np.ascontiguousarray(inp["peer_sub_keys"][0].reshape(16, 128, 128), dtype=np.float32),
                  peer_down=f("peer_down"), peer_up=f("peer_up"))
    meta = inp["meta_tokens"].astype(np.float32, copy=False)
    cn = [make_consts(False), make_consts(True)]
    in_maps = []
    for c in range(8):
        b, s = c // 2, c % 2
        loc = np.zeros((NB * 128, D), np.float32)
        if s == 0:
            loc[16 * 128 + 112:17 * 128] = meta
            loc[17 * 128:] = x[b, :2048]
        else:
            loc[112:128] = meta
            loc[128:] = x[b]
        in_maps.append(dict(xin=loc, consts=cn[1 if s == 0 else 0], **shared))
    res = run_bass_kernel_spmd(nc, in_maps, core_ids=list(range(8)))
    out = np.empty((B, SEQ, D), np.float32)
    for c in range(8):
        b, s = c // 2, c % 2
        out[b, s * 2048:(s + 1) * 2048] = np.asarray(res.results[c]["yout"])
    return out
```

```python
import contextlib
import numpy as np
import concourse.bass as bass
import concourse.mybir as mybir
from concourse.bass_utils import run_bass_kernel_spmd

F32 = mybir.dt.float32
BF16 = mybir.dt.bfloat16
AF = mybir.ActivationFunctionType
ALU = mybir.AluOpType
AX = mybir.AxisListType

D = 2048
DC = 16
INC = 4640
RW0 = 1280
NRC = 27
NE = 16384
HD = 64

V_G1, V_MU, V_W0, V_A0, V_KK, V_KA, V_GNW, V_GNB, V_RK, V_QG, V_KG, V_SK, V_G2 = (
    0, 16, 43, 51, 59, 67, 75, 83, 91, 99, 100, 101, 109)
NV = 125
C_ID, C_BO, C_MU, C_MUI, C_ML, C_MC, C_MP, C_MPF, C_SEL = 0, 128, 256, 320, 384, 448, 576, 704, 832
NCN = 832 + 256


class H:
    __slots__ = ("name", "w", "r", "dsem", "dcnt")

    def __init__(self, name=""):
        self.name = name
        self.w = {}
        self.r = {}
        self.dsem = None
        self.dcnt = 0


class Sched:
    def __init__(self, nc, needed=None):
        self.nc = nc
        self.engs = {"pe": nc.tensor, "act": nc.scalar, "dve": nc.vector,
                     "pool": nc.gpsimd, "sp": nc.sync}
        self.esem = {k: nc.alloc_semaphore(name="es_" + k) for k in self.engs}
        self.seq = {k: 0 for k in self.engs}
        self.cnt = {k: 0 for k in self.engs}
        self.cntmap = {k: {} for k in self.engs}
        self.waited = {k: {} for k in self.engs}
        self.needed = needed
        self.rec = {k: set() for k in self.engs}
        self.sems = {}
        self.dcur = {}

    def _wait(self, e, evs):
        w = self.waited[e]
        for key, val in evs.items():
            if w.get(key, 0) >= val:
                continue
            if isinstance(key, str):
                self.rec[key].add(val)
                real = self.cntmap[key][val] if self.needed is not None else val
                self.engs[e].wait_ge(self.esem[key], real)
            else:
                self.engs[e].wait_ge(self.sems[key], val)
            w[key] = val

    @staticmethod
    def _merge(d, evs):
        for k, v in evs.items():
            if d.get(k, 0) < v:
                d[k] = v

    def _deps(self, reads, writes, own=None):
        evs = {}
        for h in reads:
            self._merge(evs, h.w)
        if own == "pe":
            evs.pop(own, None)
        ww = {}
        for h in writes:
            self._merge(ww, h.w)
            self._merge(ww, h.r)
        if own is not None:
            ww.pop(own, None)
        self._merge(evs, ww)
        return evs

    def _pe_mode(self, mode):
        if mode != getattr(self, "pe_mode", None):
            if self.seq["pe"] > 0:
                self._wait("pe", {"pe": self.seq["pe"]})
            self.pe_mode = mode

    def op(self, e, fn, reads=(), writes=()):
        self._wait(e, self._deps(reads, writes, own=e))
        inst = fn(_PEProxy(self) if e == "pe" else self.engs[e])
        self.seq[e] += 1
        n = self.seq[e]
        if self.needed is None or n in self.needed[e]:
            self.cnt[e] += 1
            inst.then_inc(self.esem[e], 1)
            self.cntmap[e][n] = self.cnt[e]
        ev = {e: n}
        for h in reads:
            self._merge(h.r, ev)
        for h in writes:
            h.w = dict(ev)
            h.r = {}
        return inst

    def dma(self, q, out, in_, tile, reads=(), writes=(), **kw):
        if tile.dsem is None:
            tile.dsem = self.nc.alloc_semaphore(name="ds_%d" % len(self.sems))
            self.sems[tile.dsem.num] = tile.dsem
        deps = self._deps(reads, writes)
        if tile in writes and not tile.r and tile.w.get(tile.dsem.num, 0) == tile.dcnt and len(tile.w) == 1:
            deps.pop(tile.dsem.num, None)
        self._wait(q, deps)
        inst = self.engs[q].dma_start(out=out, in_=in_, **kw)
        tile.dcnt += 16
        inst.then_inc(tile.dsem, 16)
        self.dcur[tile.dsem.num] = tile.dcnt
        ev = {tile.dsem.num: tile.dcnt}
        for h in reads:
            self._merge(h.r, ev)
        for h in writes:
            h.w = dict(ev)
            h.r = {}
        return inst

    def barrier(self):
        evs = {k: self.seq[k] for k in self.engs if self.seq[k] > 0}
        evs.update(self.dcur)
        for e in self.engs:
            self._wait(e, evs)


def _rnd(n):
    return 32 if n <= 32 else (64 if n <= 64 else 128)


class _PEProxy:
    def __init__(self, S):
        self.S = S

    def matmul(self, out, lhsT, rhs, **kw):
        fr = 1
        for d in lhsT.shape[1:]:
            fr *= d
        self.S._pe_mode(("mm", _rnd(lhsT.shape[0]), _rnd(fr), str(lhsT.dtype), lhsT.base_partition(), out.base_partition()))
        return self.S.nc.tensor.matmul(out, lhsT=lhsT, rhs=rhs, **kw)

    def transpose(self, out, in_, identity):
        fr = 1
        for d in in_.shape[1:]:
            fr *= d
        self.S._pe_mode(("tr", _rnd(in_.shape[0]), _rnd(fr), str(in_.dtype), in_.base_partition(), out.base_partition()))
        return self.S.nc.tensor.transpose(out, in_, identity)


class Ctx:
    pass


def build(NB=33, OWN0=17, phases="ABCDEF", dbg=False):
    _, ctx = _build(NB, OWN0, phases, dbg, None)
    return _build(NB, OWN0, phases, dbg, ctx["S"].rec)


def _build(NB, OWN0, phases, dbg, needed):
    NO = NB - OWN0
    nc = bass.Bass("TRN2", target_bir_lowering=False)
    S = Sched(nc, needed)

    def din(name, shape, dt=F32):
        return nc.dram_tensor(name, list(shape), dt, kind="ExternalInput").ap()

    def dscr(name, shape, dt=F32, out=False):
        return nc.dram_tensor(name, list(shape), dt, kind=("ExternalOutput" if out else "Internal")).ap()

    xin = din("xin", [NB * 128, D])
    w_in = din("w_in", [D, INC])
    vecs_d = din("vecs", [128, NV])
    cn_d = din("consts", [128, NCN])
    wup_d = din("w_up", [64, 1024])
    aup_d = din("a_up", [64, 1024])
    gup_d = din("g_up", [160, 1024])
    wout_d = din("w_out", [D, D])
    pq_d = din("peer_query", [D, D])
    sk_d = din("sub_keys", [16, 128, 128])
    pd_d = din("peer_down", [NE, D])
    pu_d = din("peer_up", [NE, D])
    yout = dscr("yout", [NO * 128, D], F32, out=True)

    PS_d = dscr("PS_s", [NB, 128, NRC * 128])
    QKV_d = dscr("QKV_s", [NO + 1, 128, 10 * 128])
    YR_d = dscr("YR_s", [NO, 128, 1024], BF16, out=dbg)
    YA_d = dscr("YA_s", [NO, 128, 1024], BF16, out=dbg)
    HM_d = dscr("HM_s", [NO, 128, D], F32, out=dbg)
    XNT_d = dscr("XNT_s", [NO, 128, D], BF16)
    ST_d = dscr("ST_s", [NO, 128, 2048 + 8])
    hPS = [H("PS%d" % j) for j in range(NB)]
    hQKV = [H("QKV%d" % j) for j in range(NO + 1)]
    hYR = [H() for j in range(NO)]
    hYA = [H() for j in range(NO)]
    hHM = [H() for j in range(NO)]
    hXNT = [H() for j in range(NO)]
    hST = [H() for j in range(NO)]
    hOUT = [H() for j in range(NO)]

    uid = [0]

    def mk(es):
        def T(name, shape, dt=F32):
            uid[0] += 1
            t = es.enter_context(nc.sbuf_tensor("sb%d_%s" % (uid[0], name), list(shape), dt))
            return t, H(name)

        def P(name, shape, dt=F32):
            uid[0] += 1
            t = es.enter_context(nc.psum_tensor("ps%d_%s" % (uid[0], name), list(shape), dt))
            return t, H(name)
        return T, P

    def load_consts(T, bf=True):
        c = Ctx()
        c.vec, c.hvec = T("vecs", [128, NV])
        S.dma("sp", c.vec[:], vecs_d, c.hvec, writes=[c.hvec])
        c.cn, c.hcn = T("cn32", [128, NCN])
        S.dma("sp", c.cn[:], cn_d, c.hcn, writes=[c.hcn])
        c.cb, c.hcb = T("cnbf", [128, NCN], BF16)
        S.dma("pool", c.cb[:], cn_d, c.hcb, writes=[c.hcb])
        return c

    def rmsnorm_T(c, T_, xt, hxt, gcol, junk, hjunk, st, hst, xs, hxs, pT, hpT, uT, huT):
        S.op("act", lambda e: e.activation(out=junk[:], in_=xt[:], func=AF.Square, accum_out=st[:, 0:1]),
             reads=[hxt], writes=([hjunk, hst] if hjunk is not hst else [hst]))
        S.op("dve", lambda e: e.tensor_scalar(out=st[:, 1:2], in0=st[:, 0:1], scalar1=1.0 / D, scalar2=1e-6,
                                              op0=ALU.mult, op1=ALU.add), reads=[hst], writes=[hst])
        S.op("act", lambda e: e.activation(out=st[:, 2:3], in_=st[:, 1:2], func=AF.Sqrt), reads=[hst], writes=[hst])
        S.op("dve", lambda e: e.reciprocal(out=st[:, 3:4], in_=st[:, 2:3]), reads=[hst], writes=[hst])
        S.op("act", lambda e: e.activation(out=xs[:], in_=xt[:], func=AF.Copy, scale=st[:, 3:4]),
             reads=[hxt, hst], writes=[hxs])
        for k in range(DC):
            S.op("pe", lambda e, k=k: e.transpose(pT[:, k, :], xs[:, k * 128:(k + 1) * 128], c.cb[:, C_ID:C_ID + 128]),
                 reads=[hxs, c.hcb], writes=[hpT])
        gb = c.vec[:, gcol:gcol + DC].unsqueeze(2).broadcast_to([128, DC, 128])
        S.op("dve", lambda e: e.tensor_tensor(out=uT[:], in0=pT[:], in1=gb, op=ALU.mult),
             reads=[hpT, c.hvec], writes=[huT])

    def phase_A():
        with contextlib.ExitStack() as es:
            T, P = mk(es)
            c = load_consts(T)
            win, hwin = T("win", [128, DC, INC], BF16)
            for k in range(DC):
                S.dma("pool", win[:, k, :], w_in[k * 128:(k + 1) * 128, :], hwin, writes=[hwin])
            xt, hxt = T("xt", [128, D])
            st, hst = T("st", [128, 4])
            xs, hxs = T("xs", [128, D], BF16)
            junk, hjunk = xs, hxs
            pT, hpT = P("pT", [128, DC, 128], BF16)
            uT, huT = T("uT", [128, DC, 128], BF16)
            pp = [P("pp%d" % i, [128, 4, 128]) for i in range(4)]
            PT, hPT = T("PT", [128, NRC, 129])
            QK, hQK = T("QK", [128, 10, 128])
            dd, hdd = T("dd", [128, NRC, 128])
            pss, hpss = dd, hdd
            S.op("pool", lambda e: e.memset(PT[:], 0.0), writes=[hPT])
            mub = c.vec[:, V_MU:V_MU + NRC].unsqueeze(2).broadcast_to([128, NRC, 128])
            for j in range(NB):
                S.dma("sp", xt[:], xin[j * 128:(j + 1) * 128, :], hxt, writes=[hxt])
                rmsnorm_T(c, T, xt, hxt, V_G1, junk, hjunk, st, hst, xs, hxs, pT, hpT, uT, huT)
                need_att = (j >= OWN0 - 1)
                chunks = list(range(0 if need_att else 10, 37))
                gi = 0
                for g0 in range(0, len(chunks), 4):
                    grp = chunks[g0:g0 + 4]
                    pt_, hp_ = pp[gi % 4]
                    gi += 1
                    for qi, ch in enumerate(grp):
                        M = 32 if ch == 36 else 128
                        for k in range(DC):
                            S.op("pe", lambda e, qi=qi, ch=ch, k=k, M=M, pt_=pt_: e.matmul(
                                pt_[0:M, qi, :], lhsT=win[:, k, ch * 128:ch * 128 + M], rhs=uT[:, k, :],
                                start=(k == 0), stop=(k == DC - 1)), reads=[hwin, huT], writes=[hp_])
                    for qi, ch in enumerate(grp):
                        M = 32 if ch == 36 else 128
                        eng = "act" if (qi % 2 == 0) else "dve"
                        if ch < 10:
                            dst, hd = QK[0:M, ch, :], hQK
                        else:
                            dst, hd = PT[0:M, ch - 10, 1:129], hPT
                        if eng == "act":
                            S.op("act", lambda e, dst=dst, qi=qi, M=M, pt_=pt_: e.activation(out=dst, in_=pt_[0:M, qi, :], func=AF.Copy),
                                 reads=[hp_], writes=[hd])
                        else:
                            S.op("dve", lambda e, dst=dst, qi=qi, M=M, pt_=pt_: e.tensor_copy(out=dst, in_=pt_[0:M, qi, :]),
                                 reads=[hp_], writes=[hd])
                if need_att:
                    ja = j - (OWN0 - 1)
                    S.dma("sp", QKV_d[ja].rearrange("p (c t) -> p c t", c=10), QK[:], hQK, reads=[hQK], writes=[hQKV[ja]])
                S.op("pool", lambda e: e.tensor_tensor(out=dd[:], in0=PT[:, :, 0:128], in1=PT[:, :, 1:129], op=ALU.subtract),
                     reads=[hPT], writes=[hdd])
                S.op("pool", lambda e: e.tensor_tensor(out=dd[:], in0=dd[:], in1=mub, op=ALU.mult),
                     reads=[hdd, c.hvec], writes=[hdd])
                S.op("dve", lambda e: e.tensor_tensor(out=dd[:], in0=dd[:], in1=PT[:, :, 1:129], op=ALU.add),
                     reads=[hdd, hPT], writes=[hdd])
                S.op("act", lambda e: e.activation(out=PT[:, :, 0:1], in_=PT[:, :, 128:129], func=AF.Copy),
                     reads=[hPT, hdd, hpss], writes=[hPT])
                S.dma("sp", PS_d[j].rearrange("p (c t) -> p c t", c=NRC), pss[:], hpss, reads=[hpss], writes=[hPS[j]])
            S.barrier()

    def phase_B():
        with contextlib.ExitStack() as es:
            T, P = mk(es)
            c = load_consts(T)
            vec = c.vec

            def vb(col, n=8):
                return vec[:, col:col + n].unsqueeze(2).broadcast_to([128, n, 128])
            wup, hwup = T("wup", [128, 1024], BF16)
            aup, haup = T("aup", [128, 1024], BF16)
            gup, hgup = T("gup", [128, 2, 1024], BF16)
            S.dma("pool", wup[0:64, :], wup_d, hwup, writes=[hwup])
            S.dma("pool", aup[64:128, :], aup_d, haup, writes=[haup])
            S.dma("pool", gup[:, 0, :], gup_d[0:128, :], hgup, writes=[hgup])
            S.dma("pool", gup[0:32, 1, :], gup_d[128:160, :], hgup, writes=[hgup])
            ps, hps = T("ps", [128, NRC, 128])
            names32 = ["nld", "aa", "kap", "kp", "beta", "cs", "cum", "t1", "t2", "t3", "gg", "bon"]
            F = {}
            for n in names32:
                F[n] = T(n, [128, 8, 128])
            namesbf = ["Kh", "Rh", "Bh", "Qh", "BG", "QG", "vb", "sqb"]
            Bf = {}
            for n in namesbf:
                Bf[n] = T(n, [128, 8, 128], BF16)
            lw, hlw = T("lw", [128, 128], BF16)
            sg, hsg = T("sg", [128, 2, 128], BF16)
            ones, hones = T("ones", [128, 1024])
            S.op("pool", lambda e: e.memset(ones[:], 1.0), writes=[hones])
            sm, hsm = T("sm", [128, 8, 2, 4])
            Vt, hVt = T("Vt", [64, 2, 8, 128], BF16)
            BGt, hBGt = T("BGt", [64, 2, 8, 128], BF16)
            QGt, hQGt = T("QGt", [64, 2, 8, 128], BF16)
            S32, hS32 = T("S32", [128, 8, 64])
            Sb, hSb = T("Sb", [128, 8, 64], BF16)
            S.op("pool", lambda e: e.memset(S32[:], 0.0), writes=[hS32])
            S.op("pool", lambda e: e.memset(Sb[:], 0.0), writes=[hSb])
            gm = {}
            for n in ["Ub", "Lb", "U2", "L2", "Lak", "Mrb", "Mrk", "Qb", "Xb", "SAb"]:
                gm[n] = T(n, [64, 16, 64], BF16)
            ysb, hysb = T("ysb", [64, 16, 64])
            yc, hyc = T("yc", [64, 16, 64])
            ysq, hysq = T("ysq", [64, 16, 64])
            gst, hgst = T("gst", [64, 16, 4])
            yf, hyf = T("yf", [128, 8, 128])
            yrb, hyrb = T("yrb", [128, 8, 128], BF16)
            PA = [P("PA%d" % i, [128, 1024]) for i in range(3)]
            PTr, hPTr = P("PTr", [128, 2048], BF16)
            pi = [0]

            def nextP():
                t = PA[pi[0] % 3]
                pi[0] += 1
                return t
            maskU = c.cn[0:64, C_MU:C_MU + 64].unsqueeze(1).broadcast_to([64, 16, 64])
            maskUi = c.cn[0:64, C_MUI:C_MUI + 64].unsqueeze(1).broadcast_to([64, 16, 64])
            maskL = c.cn[0:64, C_ML:C_ML + 64].unsqueeze(1).broadcast_to([64, 16, 64])
            identb = c.cb[:, C_ID:C_ID + 128]
            ident32 = c.cn[:, C_ID:C_ID + 128]
            bones = c.cb[:, C_BO:C_BO + 128]

            def tt(eng, out, i0, i1, op, reads, writes):
                S.op(eng, lambda e: e.tensor_tensor(out=out, in0=i0, in1=i1, op=op), reads=reads, writes=writes)

            def actf(out, in_, func, reads, writes, **kw):
                S.op("act", lambda e: e.activation(out=out, in_=in_, func=func, **kw), reads=reads, writes=writes)

            HORD = list(range(0, 16, 2)) + list(range(1, 16, 2))
            for j in range(NB):
                own = j >= OWN0
                S.dma("sp", ps[:], PS_d[j].rearrange("p (c t) -> p c t", c=NRC), hps, reads=[hPS[j]], writes=[hps])
                r_, k_, v_ = ps[:, 0:8, :], ps[:, 8:16, :], ps[:, 16:24, :]
                actf(lw[0:64, :], ps[0:64, 24, :], AF.Tanh, [hps], [hlw])
                actf(lw[64:128, :], ps[64:128, 24, :], AF.Copy, [hps], [hlw])
                actf(sg[:, 0, :], ps[:, 25, :], AF.Sigmoid, [hps], [hsg])
                actf(sg[0:32, 1, :], ps[0:32, 26, :], AF.Sigmoid, [hps], [hsg])
                pw_, hpw = nextP()
                pa_, hpa = nextP()
                for m in range(8):
                    S.op("pe", lambda e, m=m: e.matmul(pw_[:, m * 128:(m + 1) * 128], lhsT=wup[0:64, m * 128:(m + 1) * 128], rhs=lw[0:64, :], start=True, stop=True),
                         reads=[hwup, hlw], writes=[hpw])
                    S.op("pe", lambda e, m=m: e.matmul(pa_[:, m * 128:(m + 1) * 128], lhsT=aup[64:128, m * 128:(m + 1) * 128], rhs=lw[64:128, :], start=True, stop=True),
                         reads=[haup, hlw], writes=[hpa])
                nld, hnld = F["nld"]
                aa, haa = F["aa"]
                t1, ht1 = F["t1"]
                t2, ht2 = F["t2"]
                t3, ht3 = F["t3"]
                v3 = lambda p_: p_[:].rearrange("p (m t) -> p m t", m=8)
                tt("dve", t1[:], v3(pw_), vb(V_W0), ALU.add, [hpw, c.hvec], [ht1])
                actf(t1[:], t1[:], AF.Sigmoid, [ht1], [ht1])
                S.op("dve", lambda e: e.tensor_scalar(out=nld[:], in0=t1[:], scalar1=0.6065306597126334, scalar2=None, op0=ALU.mult),
                     reads=[ht1], writes=[hnld])
                tt("dve", t2[:], v3(pa_), vb(V_A0), ALU.add, [hpa, c.hvec], [ht2])
                actf(aa[:], t2[:], AF.Sigmoid, [ht2], [haa])
                if own:
                    pg_, hpg = nextP()
                    for m in range(8):
                        S.op("pe", lambda e, m=m: e.matmul(pg_[:, m * 128:(m + 1) * 128], lhsT=gup[:, 0, m * 128:(m + 1) * 128], rhs=sg[:, 0, :], start=True, stop=False),
                             reads=[hgup, hsg], writes=[hpg])
                        S.op("pe", lambda e, m=m: e.matmul(pg_[:, m * 128:(m + 1) * 128], lhsT=gup[0:32, 1, m * 128:(m + 1) * 128], rhs=sg[0:32, 1, :], start=False, stop=True),
                             reads=[hgup, hsg], writes=[hpg])
                    gg, hgg = F["gg"]
                    actf(gg[:], v3(pg_), AF.Copy, [hpg], [hgg])
                kap, hkap = F["kap"]
                sqb, hsqb = Bf["sqb"]
                tt("pool", kap[:], k_, vb(V_KK), ALU.mult, [hps, c.hvec], [hkap])
                actf(sqb[:], kap[:], AF.Square, [hkap], [hsqb])
                pq_, hpq = nextP()
                for hh in range(2):
                    S.op("pe", lambda e, hh=hh: e.matmul(pq_[:, hh * 512:(hh + 1) * 512], lhsT=bones, rhs=sqb[:, hh * 4:(hh + 1) * 4, :], start=True, stop=True),
                         reads=[c.hcb, hsqb], writes=[hpq])
                actf(t3[:], v3(pq_), AF.Sqrt, [hpq], [ht3])
                S.op("dve", lambda e: e.tensor_scalar(out=t3[:], in0=t3[:], scalar1=1e-12, scalar2=None, op0=ALU.max), reads=[ht3], writes=[ht3])
                S.op("dve", lambda e: e.reciprocal(out=t3[:], in_=t3[:]), reads=[ht3], writes=[ht3])
                tt("dve", kap[:], kap[:], t3[:], ALU.mult, [hkap, ht3], [hkap])
                kp, hkp = F["kp"]
                S.op("dve", lambda e: e.scalar_tensor_tensor(out=t2[:], in0=aa[:], scalar=1.0, in1=vb(V_KA), op0=ALU.subtract, op1=ALU.mult),
                     reads=[haa, c.hvec], writes=[ht2])
                S.op("dve", lambda e: e.scalar_tensor_tensor(out=kp[:], in0=t2[:], scalar=1.0, in1=k_, op0=ALU.add, op1=ALU.mult),
                     reads=[ht2, hps], writes=[hkp])
                beta, hbeta = F["beta"]
                tt("pool", beta[:], kap[:], aa[:], ALU.mult, [hkap, haa], [hbeta])
                cs, hcs = F["cs"]
                cum, hcum = F["cum"]
                S.op("dve", lambda e: e.tensor_tensor_scan(out=cs[:].rearrange("p m t -> p (m t)"), data0=ones[:],
                                                           data1=nld[:].rearrange("p m t -> p (m t)"), initial=0.0,
                                                           op0=ALU.mult, op1=ALU.add), reads=[hones, hnld], writes=[hcs])
                cs4 = cs[:].rearrange("p m (s t) -> p m s t", s=2)
                nld4 = nld[:].rearrange("p m (s t) -> p m s t", s=2)
                cum4 = cum[:].rearrange("p m (s t) -> p m s t", s=2)
                tt("dve", sm[:, :, :, 0:1], cs4[:, :, :, 0:1], nld4[:, :, :, 0:1], ALU.subtract, [hcs, hnld], [hsm])
                tt("dve", cum4, cs4, sm[:, :, :, 0:1].broadcast_to([128, 8, 2, 64]), ALU.subtract, [hcs, hsm], [hcum])
                actf(sm[:, :, :, 2:3], cum4[:, :, :, 63:64], AF.Exp, [hcum], [hsm], scale=-1.0)
                Kh, hKh = Bf["Kh"]
                Rh, hRh = Bf["Rh"]
                Bh, hBh = Bf["Bh"]
                Qh, hQh = Bf["Qh"]
                BG, hBG = Bf["BG"]
                QG, hQG = Bf["QG"]
                vbf, hvbf = Bf["vb"]
                actf(t1[:], cum[:], AF.Exp, [hcum], [ht1], scale=-1.0)
                tt("dve", Rh[:], r_, t1[:], ALU.mult, [hps, ht1], [hRh])
                tt("pool", t2[:], cum[:], nld[:], ALU.subtract, [hcum, hnld], [ht2])
                actf(t2[:], t2[:], AF.Exp, [ht2], [ht2], scale=-1.0)
                tt("dve", Kh[:], kap[:], t2[:], ALU.mult, [hkap, ht2], [hKh])
                actf(t3[:], cum[:], AF.Exp, [hcum], [ht3])
                tt("dve", Bh[:], beta[:], t3[:], ALU.mult, [hbeta, ht3], [hBh])
                tt("pool", Qh[:], kp[:], t3[:], ALU.mult, [hkp, ht3], [hQh])
                t14 = t1[:].rearrange("p m (s t) -> p m s t", s=2)
                tt("pool", t14, cum4, cum4[:, :, :, 63:64].broadcast_to([128, 8, 2, 64]), ALU.subtract, [hcum], [ht1])
                actf(t1[:], t1[:], AF.Exp, [ht1], [ht1])
                tt("dve", BG[:], beta[:], t1[:], ALU.mult, [hbeta, ht1], [hBG])
                tt("pool", QG[:], kp[:], t1[:], ALU.mult, [hkp, ht1], [hQG])
                actf(vbf[:], v_, AF.Copy, [hps], [hvbf])
                for (src, hsrc, dst, hdst) in ((vbf, hvbf, Vt, hVt), (BG, hBG, BGt, hBGt), (QG, hQG, QGt, hQGt)):
                    for m in range(8):
                        for s in range(2):
                            S.op("pe", lambda e, m=m, s=s, src=src: e.transpose(
                                PTr[0:64, (s * 8 + m) * 128:(s * 8 + m + 1) * 128], src[:, m, s * 64:(s + 1) * 64], identb),
                                reads=[hsrc, c.hcb], writes=[hPTr])
                    S.op("act", lambda e, dst=dst: e.activation(out=dst[:].rearrange("t s m c -> t (s m c)"), in_=PTr[0:64, :], func=AF.Copy),
                         reads=[hPTr], writes=[hdst])
                if own:
                    bon, hbon = F["bon"]
                    tt("pool", t2[:], r_, kp[:], ALU.mult, [hps, hkp], [ht2])
                    tt("pool", sqb[:], t2[:], vb(V_RK), ALU.mult, [ht2, c.hvec], [hsqb])
                    pb_, hpb = nextP()
                    for hh in range(2):
                        S.op("pe", lambda e, hh=hh: e.matmul(pb_[:, hh * 512:(hh + 1) * 512], lhsT=bones, rhs=sqb[:, hh * 4:(hh + 1) * 4, :], start=True, stop=True),
                             reads=[c.hcb, hsqb], writes=[hpb])
                    tt("dve", bon[:], v3(pb_), v_, ALU.mult, [hpb, hps], [hbon])
                for s in range(2):
                    ts = slice(s * 64, (s + 1) * 64)

                    def hrows(h):
                        return slice((h % 2) * 64, (h % 2) * 64 + 64)

                    def gram(lt, hl, rt, hr, mask, dname, eng):
                        p_, hp_ = nextP()
                        pv = p_[0:64, :].rearrange("p (h t) -> p h t", h=16)
                        for h in HORD:
                            S.op("pe", lambda e, h=h: e.matmul(pv[:, h, :], lhsT=lt[hrows(h), h // 2, ts], rhs=rt[hrows(h), h // 2, ts], start=True, stop=True),
                                 reads=[hl, hr], writes=[hp_])
                        d_, hd_ = gm[dname]
                        tt(eng, d_[:], pv, mask, ALU.mult, [hp_, c.hcn], [hd_])

                    gram(Bh, hBh, Kh, hKh, maskU, "Ub", "dve")
                    gram(Kh, hKh, Bh, hBh, maskL, "Lb", "dve")
                    gram(Qh, hQh, Kh, hKh, maskU, "Lak", "dve")
                    if own:
                        gram(Bh, hBh, Rh, hRh, maskUi, "Mrb", "dve")
                        gram(Qh, hQh, Rh, hRh, maskUi, "Mrk", "dve")
                    Ub, hUb = gm["Ub"]
                    Lb, hLb = gm["Lb"]
                    Qb, hQb = gm["Qb"]
                    idb = c.cn[0:64, C_ID:C_ID + 64].unsqueeze(1).broadcast_to([64, 16, 64])
                    tt("dve", Qb[:], idb, Ub[:], ALU.subtract, [c.hcn, hUb], [hQb])
                    cur = ("Ub", "Lb")
                    nxt = ("U2", "L2")
                    for lvl in range(5):
                        Uk, hUk = gm[cur[0]]
                        Lk, hLk = gm[cur[1]]
                        Un, hUn = gm[nxt[0]]
                        Ln, hLn = gm[nxt[1]]
                        p2, hp2 = nextP()
                        p2v = p2[0:64, :].rearrange("p (h t) -> p h t", h=16)
                        for h in HORD:
                            S.op("pe", lambda e, h=h: e.matmul(p2v[:, h, :], lhsT=Uk[:, h, :], rhs=Lk[:, h, :], start=True, stop=True),
                                 reads=[hUk, hLk], writes=[hp2])
                        actf(Ln[:], p2v, AF.Copy, [hp2], [hLn])
                        if lvl < 4:
                            p1, hp1 = nextP()
                            p1v = p1[0:64, :].rearrange("p (h t) -> p h t", h=16)
                            for h in HORD:
                                S.op("pe", lambda e, h=h: e.matmul(p1v[:, h, :], lhsT=Lk[:, h, :], rhs=Uk[:, h, :], start=True, stop=True),
                                     reads=[hUk, hLk], writes=[hp1])
                            S.op("dve", lambda e: e.tensor_copy(out=Un[:], in_=p1v), reads=[hp1], writes=[hUn])
                        p3, hp3 = nextP()
                        p3v = p3[0:64, :].rearrange("p (h t) -> p h t", h=16)
                        for h in HORD:
                            S.op("pe", lambda e, h=h: e.matmul(p3v[:, h, :], lhsT=Ln[:, h, :], rhs=Qb[:, h, :], start=True, stop=True),
                                 reads=[hLn, hQb], writes=[hp3])
                        tt("dve", Qb[:], p3v, Qb[:], ALU.add, [hp3, hQb], [hQb])
                        cur, nxt = nxt, cur
                    Lak, hLak = gm["Lak"]
                    Xb, hXb = gm["Xb"]
                    SAb, hSAb = gm["SAb"]
                    px, hpx = nextP()
                    pxv = px[0:64, :].rearrange("p (h t) -> p h t", h=16)
                    for h in HORD:
                        cs_ = slice((h % 2) * 64, (h % 2) * 64 + 64)
                        S.op("pe", lambda e, h=h: e.matmul(pxv[:, h, :], lhsT=Kh[hrows(h), h // 2, ts], rhs=Sb[hrows(h), h // 2, :], start=True, stop=False),
                             reads=[hKh, hSb], writes=[hpx])
                        S.op("pe", lambda e, h=h, cs_=cs_: e.matmul(pxv[:, h, :], lhsT=Lak[:, h, :], rhs=Vt[:, s, h // 2, cs_], start=False, stop=True),
                             reads=[hLak, hVt], writes=[hpx])
                    actf(Xb[:], pxv, AF.Copy, [hpx], [hXb])
                    psa, hpsa = nextP()
                    psav = psa[0:64, :].rearrange("p (h t) -> p h t", h=16)
                    for h in HORD:
                        S.op("pe", lambda e, h=h: e.matmul(psav[:, h, :], lhsT=Qb[:, h, :], rhs=Xb[:, h, :], start=True, stop=True),
                             reads=[hQb, hXb], writes=[hpsa])
                    actf(SAb[:], psav, AF.Copy, [hpsa], [hSAb], scale=-1.0)
                    if own:
                        Mrb, hMrb = gm["Mrb"]
                        Mrk, hMrk = gm["Mrk"]
                        py, hpy = nextP()
                        pyv = py[0:64, :].rearrange("p (h t) -> p h t", h=16)
                        for h in HORD:
                            cs_ = slice((h % 2) * 64, (h % 2) * 64 + 64)
                            S.op("pe", lambda e, h=h: e.matmul(pyv[:, h, :], lhsT=Rh[hrows(h), h // 2, ts], rhs=Sb[hrows(h), h // 2, :], start=True, stop=False),
                                 reads=[hRh, hSb], writes=[hpy])
                            S.op("pe", lambda e, h=h: e.matmul(pyv[:, h, :], lhsT=Mrb[:, h, :], rhs=SAb[:, h, :], start=False, stop=False),
                                 reads=[hMrb, hSAb], writes=[hpy])
                            S.op("pe", lambda e, h=h, cs_=cs_: e.matmul(pyv[:, h, :], lhsT=Mrk[:, h, :], rhs=Vt[:, s, h // 2, cs_], start=False, stop=True),
                                 reads=[hMrk, hVt], writes=[hpy])
                        actf(ysb[:], pyv, AF.Copy, [hpy], [hysb])
                        S.op("dve", lambda e: e.tensor_reduce(out=gst[:, :, 0:1], in_=ysb[:], axis=AX.X, op=ALU.add), reads=[hysb], writes=[hgst])
                        S.op("dve", lambda e: e.tensor_scalar(out=gst[:, :, 0:1], in0=gst[:, :, 0:1], scalar1=1.0 / 64, scalar2=None, op0=ALU.mult), reads=[hgst], writes=[hgst])
                        tt("dve", yc[:], ysb[:], gst[:, :, 0:1].broadcast_to([64, 16, 64]), ALU.subtract, [hysb, hgst], [hyc])
                        actf(ysq[:], yc[:], AF.Square, [hyc], [hysq])
                        S.op("dve", lambda e: e.tensor_reduce(out=gst[:, :, 1:2], in_=ysq[:], axis=AX.X, op=ALU.add), reads=[hysq], writes=[hgst])
                        S.op("dve", lambda e: e.tensor_scalar(out=gst[:, :, 1:2], in0=gst[:, :, 1:2], scalar1=1.0 / 64, scalar2=64e-5, op0=ALU.mult, op1=ALU.add), reads=[hgst], writes=[hgst])
                        actf(gst[:, :, 2:3], gst[:, :, 1:2], AF.Sqrt, [hgst], [hgst])
                        S.op("dve", lambda e: e.reciprocal(out=gst[:, :, 3:4], in_=gst[:, :, 2:3]), reads=[hgst], writes=[hgst])
                        tt("dve", yc[:], yc[:], gst[:, :, 3:4].broadcast_to([64, 16, 64]), ALU.mult, [hyc, hgst], [hyc])
                        pt2, hpt2 = nextP()
                        for m in range(8):
                            S.op("pe", lambda e, m=m: e.transpose(pt2[:, m * 64:(m + 1) * 64], yc[:, 2 * m:2 * m + 2, :].rearrange("t h i -> t (h i)"), ident32[0:64, 0:64]),
                                 reads=[hyc, c.hcn], writes=[hpt2])
                        S.op("dve", lambda e: e.tensor_copy(out=yf[:, :, ts], in_=pt2[:, 0:512].rearrange("p (m t) -> p m t", m=8)), reads=[hpt2], writes=[hyf])
                    pst, hpst = nextP()
                    for h in HORD:
                        cs_ = slice((h % 2) * 64, (h % 2) * 64 + 64)
                        o_ = pst[hrows(h), (h // 2) * 64:(h // 2) * 64 + 64]
                        S.op("pe", lambda e, h=h, cs_=cs_, o_=o_: e.matmul(o_, lhsT=BGt[:, s, h // 2, cs_], rhs=SAb[:, h, :], start=True, stop=False),
                             reads=[hBGt, hSAb], writes=[hpst])
                        S.op("pe", lambda e, h=h, cs_=cs_, o_=o_: e.matmul(o_, lhsT=QGt[:, s, h // 2, cs_], rhs=Vt[:, s, h // 2, cs_], start=False, stop=True),
                             reads=[hQGt, hVt], writes=[hpst])
                    tt("dve", S32[:], S32[:], sm[:, :, s, 2:3].broadcast_to([128, 8, 64]), ALU.mult, [hS32, hsm], [hS32])
                    tt("dve", S32[:], S32[:], pst[:, 0:512].rearrange("p (m i) -> p m i", m=8), ALU.add, [hS32, hpst], [hS32])
                    actf(Sb[:], S32[:], AF.Copy, [hS32], [hSb])
                if own:
                    gg, hgg = F["gg"]
                    bon, hbon = F["bon"]
                    tt("dve", yf[:], yf[:], vb(V_GNW), ALU.mult, [hyf, c.hvec], [hyf])
                    tt("pool", yf[:], yf[:], vb(V_GNB), ALU.add, [hyf, c.hvec], [hyf])
                    tt("pool", yf[:], yf[:], bon[:], ALU.add, [hyf, hbon], [hyf])
                    tt("dve", yrb[:], yf[:], gg[:], ALU.mult, [hyf, hgg], [hyrb])
                    jo = j - OWN0
                    S.dma("sp", YR_d[jo].rearrange("p (m t) -> p m t", m=8), yrb[:], hyrb, reads=[hyrb], writes=[hYR[jo]])
            S.barrier()


    def tt_(eng, out, i0, i1, op, reads, writes):
        S.op(eng, lambda e: e.tensor_tensor(out=out, in0=i0, in1=i1, op=op), reads=reads, writes=writes)

    def act_(out, in_, func, reads, writes, **kw):
        S.op("act", lambda e: e.activation(out=out, in_=in_, func=func, **kw), reads=reads, writes=writes)

    def phase_C():
        with contextlib.ExitStack() as es:
            T, P = mk(es)
            c = load_consts(T)
            vec = c.vec
            bones = c.cb[:, C_BO:C_BO + 128]
            identb = c.cb[:, C_ID:C_ID + 128]
            qk, hqk = T("qk", [128, 10, 128])
            sqb, hsqb = T("sqb", [128, 8, 128], BF16)
            t1, ht1 = T("t1", [128, 8, 128])
            qT, hqT = T("qT", [128, 8, 128], BF16)
            knb, hknb = T("knb", [128, 128], BF16)
            vbf, hvbf = T("vbf", [128, 128], BF16)
            kd = [T("kd%d" % i, [128, 2, 128], BF16) for i in range(2)]
            Vk = [T("Vk%d" % i, [128, 128], BF16) for i in range(2)]
            e1, he1 = T("e1", [128, 4, 128], BF16)
            PTb, hPTb = T("PTb", [128, 4, 128], BF16)
            onesb, honesb = T("onesb", [128, 64], BF16)
            S.op("pool", lambda e: e.memset(onesb[:], 1.0), writes=[honesb])
            esk, hesk = T("esk", [128, 8])
            act_(esk[:], vec[:, V_SK:V_SK + 8], AF.Exp, [c.hvec], [hesk])
            den, hden = T("den", [128, 4, 128])
            ya, hya = T("ya", [128, 8, 128], BF16)
            PS1, hPS1 = P("PS1", [128, 1024])
            psc = [P("psc%d" % i, [128, 512]) for i in range(2)]
            po, hpo = P("po", [128, 512])
            pdn, hpdn = P("pdn", [128, 512])
            pvT, hpvT = P("pvT", [128, 128], BF16)

            def norm_rows(src, n, gcol, dst, hdst):
                act_(sqb[:, 0:n, :], src, AF.Square, [hqk], [hsqb])
                for hh in range(0, n, 4):
                    w_ = min(4, n - hh)
                    S.op("pe", lambda e, hh=hh, w_=w_: e.matmul(PS1[:, hh * 128:(hh + w_) * 128], lhsT=bones, rhs=sqb[:, hh:hh + w_, :], start=True, stop=True),
                         reads=[c.hcb, hsqb], writes=[hPS1])
                pv = PS1[:, 0:n * 128].rearrange("p (m t) -> p m t", m=n)
                S.op("dve", lambda e: e.tensor_scalar(out=t1[:, 0:n, :], in0=pv, scalar1=1.0 / 64, scalar2=1e-6, op0=ALU.mult, op1=ALU.add),
                     reads=[hPS1], writes=[ht1])
                act_(t1[:, 0:n, :], t1[:, 0:n, :], AF.Sqrt, [ht1], [ht1])
                S.op("dve", lambda e: e.reciprocal(out=t1[:, 0:n, :], in_=t1[:, 0:n, :]), reads=[ht1], writes=[ht1])
                tt_("dve", t1[:, 0:n, :], t1[:, 0:n, :], src, ALU.mult, [ht1, hqk], [ht1])
                S.op("dve", lambda e: e.tensor_scalar(out=dst, in0=t1[:, 0:n, :], scalar1=vec[:, gcol:gcol + 1], scalar2=None, op0=ALU.mult),
                     reads=[ht1, c.hvec], writes=[hdst])

            def kv_prep(slot):
                kd_, hkd_ = kd[slot]
                Vk_, hVk_ = Vk[slot]
                norm_rows(qk[:, 8:9, :], 1, V_KG, knb[:].unsqueeze(1), hknb)
                for g in range(2):
                    S.op("pe", lambda e, g=g: e.matmul(PS1[:, 512 + g * 128:512 + (g + 1) * 128], lhsT=c.cb[:, C_SEL + g * 128:C_SEL + (g + 1) * 128], rhs=knb[:], start=True, stop=True),
                         reads=[c.hcb, hknb], writes=[hPS1])
                act_(kd_[:], PS1[:, 512:768].rearrange("p (g t) -> p g t", g=2), AF.Copy, [hPS1], [hkd_])
                act_(vbf[:], qk[:, 9, :], AF.Copy, [hqk], [hvbf])
                S.op("pe", lambda e: e.transpose(pvT[:], vbf[:], identb), reads=[hvbf, c.hcb], writes=[hpvT])
                S.op("dve", lambda e: e.tensor_copy(out=Vk_[:], in_=pvT[:]), reads=[hpvT], writes=[hVk_])

            S.dma("sp", qk[:, 8:10, :], QKV_d[0].rearrange("p (c t) -> p c t", c=10)[:, 8:10, :], hqk, reads=[hQKV[0]], writes=[hqk])
            kv_prep(0)
            for jo in range(NO):
                ja = jo + 1
                S.dma("sp", qk[:], QKV_d[ja].rearrange("p (c t) -> p c t", c=10), hqk, reads=[hQKV[ja]], writes=[hqk])
                kv_prep(ja % 2)
                norm_rows(qk[:, 0:8, :], 8, V_QG, qT[:], hqT)
                si = 0
                for g in range(2):
                    for par in range(2):
                        rows = slice(par * 64, par * 64 + 64)
                        for wi, slot in enumerate(((ja - 1) % 2, ja % 2)):
                            kd_, hkd_ = kd[slot]
                            Vk_, hVk_ = Vk[slot]
                            ps_, hps_ = psc[si % 2]
                            si += 1
                            S.op("pe", lambda e, ps_=ps_, kd_=kd_: e.matmul(ps_[:], lhsT=kd_[rows, g, :], rhs=qT[rows, 4 * g:4 * g + 4, :], start=True, stop=True),
                                 reads=[hkd_, hqT], writes=[hps_])
                            act_(e1[:], ps_[:].rearrange("p (h t) -> p h t", h=4), AF.Exp, [hps_], [he1], scale=0.125)
                            mcol = C_MC if wi == 1 else (C_MPF if jo == 0 else C_MP)
                            mk_ = c.cb[:, mcol:mcol + 128].unsqueeze(1).broadcast_to([128, 4, 128])
                            tt_("pool", PTb[:], e1[:], mk_, ALU.mult, [he1, c.hcb], [hPTb])
                            S.op("pe", lambda e, Vk_=Vk_, wi=wi: e.matmul(po[rows, :], lhsT=Vk_[:, g * 64:(g + 1) * 64], rhs=PTb[:].rearrange("p h t -> p (h t)"), start=(wi == 0), stop=(wi == 1)),
                                 reads=[hVk_, hPTb], writes=[hpo])
                            S.op("pe", lambda e, wi=wi: e.matmul(pdn[rows, :], lhsT=onesb[:], rhs=PTb[:].rearrange("p h t -> p (h t)"), start=(wi == 0), stop=(wi == 1)),
                                 reads=[honesb, hPTb], writes=[hpdn])
                    tt_("dve", den[:], pdn[:].rearrange("p (h t) -> p h t", h=4), esk[:, 4 * g:4 * g + 4].unsqueeze(2).broadcast_to([128, 4, 128]), ALU.add, [hpdn, hesk], [hden])
                    S.op("dve", lambda e: e.reciprocal(out=den[:], in_=den[:]), reads=[hden], writes=[hden])
                    tt_("dve", ya[:, 4 * g:4 * g + 4, :], po[:].rearrange("p (h t) -> p h t", h=4), den[:], ALU.mult, [hpo, hden], [hya])
                S.dma("sp", YA_d[jo].rearrange("p (m t) -> p m t", m=8), ya[:], hya, reads=[hya], writes=[hYA[jo]])
            S.barrier()

    def phase_D():
        with contextlib.ExitStack() as es:
            T, P = mk(es)
            c = load_consts(T)
            wo, hwo = T("wo", [128, DC, D], BF16)
            for k in range(DC):
                S.dma("pool", wo[:, k, :], wout_d[k * 128:(k + 1) * 128, :], hwo, writes=[hwo])
            yr, hyr = T("yr", [128, 8, 128], BF16)
            ya, hya = T("ya", [128, 8, 128], BF16)
            xt, hxt = T("xt", [128, D])
            hm, hhm = T("hm", [128, D])
            st, hst = T("st", [128, 4])
            xs, hxs = T("xs", [128, D], BF16)
            uT, huT = T("uT", [128, DC, 128], BF16)
            pT, hpT = P("pT", [128, DC, 128], BF16)
            pp = [P("pp%d" % i, [128, 512]) for i in range(4)]
            for jo in range(NO):
                j = OWN0 + jo
                S.dma("sp", yr[:], YR_d[jo].rearrange("p (m t) -> p m t", m=8), hyr, reads=[hYR[jo]], writes=[hyr])
                S.dma("sp", ya[:], YA_d[jo].rearrange("p (m t) -> p m t", m=8), hya, reads=[hYA[jo]], writes=[hya])
                S.dma("sp", xt[:], xin[j * 128:(j + 1) * 128, :], hxt, writes=[hxt])
                for cg in range(4):
                    p_, hp_ = pp[cg]
                    for kc in range(DC):
                        src, hsrc = (yr, hyr) if kc < 8 else (ya, hya)
                        S.op("pe", lambda e, p_=p_, kc=kc, cg=cg, src=src: e.matmul(p_[:], lhsT=src[:, kc % 8, :], rhs=wo[:, kc, cg * 512:(cg + 1) * 512], start=(kc == 0), stop=(kc == DC - 1)),
                             reads=[hsrc, hwo], writes=[hp_])
                    tt_("dve", hm[:, cg * 512:(cg + 1) * 512], p_[:], xt[:, cg * 512:(cg + 1) * 512], ALU.add, [hp_, hxt], [hhm])
                S.dma("sp", HM_d[jo], hm[:], hhm, reads=[hhm], writes=[hHM[jo]])
                rmsnorm_T(c, T, hm, hhm, V_G2, xs, hxs, st, hst, xs, hxs, pT, hpT, uT, huT)
                S.dma("sp", XNT_d[jo].rearrange("p (k t) -> p k t", k=DC), uT[:], huT, reads=[huT], writes=[hXNT[jo]])
            S.barrier()

    def phase_E1():
        with contextlib.ExitStack() as es:
            T, P = mk(es)
            c = load_consts(T)
            pq, hpq = T("pq", [128, DC, D], BF16)
            for k in range(DC):
                S.dma("pool", pq[:, k, :], pq_d[k * 128:(k + 1) * 128, :], hpq, writes=[hpq])
            skn, hskn = T("skn", [128, 16, 128])
            S.dma("sp", skn[:], sk_d.rearrange("c n d -> n c d"), hskn, writes=[hskn])
            skT, hskT = T("skT", [128, 16, 128])
            pbig = [P("pb%d" % i, [128, 16, 128]) for i in range(2)]
            p0, hp0 = pbig[0]
            for hc in range(16):
                S.op("pe", lambda e, hc=hc: e.transpose(p0[:, hc, :], skn[:, hc, :], c.cn[:, C_ID:C_ID + 128]), reads=[hskn, c.hcn], writes=[hp0])
            S.op("dve", lambda e: e.tensor_copy(out=skT[:], in_=p0[:]), reads=[hp0], writes=[hskT])
            xnT, hxnT = T("xnT", [128, DC, 128], BF16)
            qTs, hqTs = T("qTs", [128, 16, 128])
            sall, hsall = T("sall", [128, 16, 128])
            tops, htops = T("tops", [128, 16, 16])
            wk, hwk = T("wk", [128, 128])
            cand, hcand = T("cand", [128, 8, 256])
            wk2, hwk2 = T("wk2", [128, 256])
            ctop, hctop = T("ctop", [128, 8, 24])
            sm, hsm = T("sm", [128, 4, 8])
            ez, hez = T("ez", [128, 8, 16])
            stt, hstt = T("stt", [128, 2048 + 8])
            for jo in range(NO):
                S.dma("sp", xnT[:], XNT_d[jo].rearrange("p (k t) -> p k t", k=DC), hxnT, reads=[hXNT[jo]], writes=[hxnT])
                pa_, hpa_ = pbig[0]
                for hc in range(16):
                    for kc in range(DC):
                        S.op("pe", lambda e, hc=hc, kc=kc: e.matmul(pa_[:, hc, :], lhsT=pq[:, kc, hc * 128:(hc + 1) * 128], rhs=xnT[:, kc, :], start=(kc == 0), stop=(kc == DC - 1)),
                             reads=[hpq, hxnT], writes=[hpa_])
                act_(qTs[:, 0:8, :], pa_[:, 0:8, :], AF.Copy, [hpa_], [hqTs])
                S.op("dve", lambda e: e.tensor_copy(out=qTs[:, 8:16, :], in_=pa_[:, 8:16, :]), reads=[hpa_], writes=[hqTs])
                pb_, hpb_ = pbig[1]
                for hc in range(16):
                    S.op("pe", lambda e, hc=hc: e.matmul(pb_[:, hc, :], lhsT=qTs[:, hc, :], rhs=skT[:, hc, :], start=True, stop=True),
                         reads=[hqTs, hskT], writes=[hpb_])
                act_(sall[:, 0:8, :], pb_[:, 0:8, :], AF.Copy, [hpb_], [hsall])
                S.op("dve", lambda e: e.tensor_copy(out=sall[:, 8:16, :], in_=pb_[:, 8:16, :]), reads=[hpb_], writes=[hsall])
                for hc in range(16):
                    S.op("dve", lambda e, hc=hc: e.max(out=tops[:, hc, 0:8], in_=sall[:, hc, :]), reads=[hsall], writes=[htops])
                    S.op("dve", lambda e, hc=hc: e.match_replace(out=wk[:], in_to_replace=tops[:, hc, 0:8], in_values=sall[:, hc, :], imm_value=-1e30),
                         reads=[hsall, htops], writes=[hwk])
                    S.op("dve", lambda e, hc=hc: e.max(out=tops[:, hc, 8:16], in_=wk[:]), reads=[hwk], writes=[htops])
                t4 = tops[:].rearrange("p (h c) k -> p h c k", c=2)
                tt_("pool", cand[:].rearrange("p h (i j) -> p h i j", i=16), t4[:, :, 0, :].unsqueeze(3).broadcast_to([128, 8, 16, 16]),
                    t4[:, :, 1, :].unsqueeze(2).broadcast_to([128, 8, 16, 16]), ALU.add, [htops], [hcand])
                for h in range(8):
                    S.op("dve", lambda e, h=h: e.max(out=ctop[:, h, 0:8], in_=cand[:, h, :]), reads=[hcand], writes=[hctop])
                    S.op("dve", lambda e, h=h: e.match_replace(out=wk2[:], in_to_replace=ctop[:, h, 0:8], in_values=cand[:, h, :], imm_value=-1e30),
                         reads=[hcand, hctop], writes=[hwk2])
                    S.op("dve", lambda e, h=h: e.max(out=ctop[:, h, 8:16], in_=wk2[:]), reads=[hwk2], writes=[hctop])
                    S.op("dve", lambda e, h=h: e.match_replace(out=wk2[:], in_to_replace=ctop[:, h, 8:16], in_values=wk2[:], imm_value=-1e30),
                         reads=[hwk2, hctop], writes=[hwk2])
                    S.op("dve", lambda e, h=h: e.max(out=ctop[:, h, 16:24], in_=wk2[:]), reads=[hwk2], writes=[hctop])
                thr = sm[:, 0, :]
                tt_("dve", thr.unsqueeze(2), ctop[:, :, 15:16], ctop[:, :, 16:17], ALU.add, [hctop], [hsm])
                S.op("dve", lambda e: e.tensor_scalar(out=thr, in0=thr, scalar1=0.5, scalar2=None, op0=ALU.mult), reads=[hsm], writes=[hsm])
                tt_("dve", ez[:], ctop[:, :, 0:16], thr.unsqueeze(2).broadcast_to([128, 8, 16]), ALU.subtract, [hctop, hsm], [hez])
                act_(ez[:], ez[:], AF.Exp, [hez], [hez])
                S.op("dve", lambda e: e.tensor_reduce(out=sm[:, 1, :].unsqueeze(2), in_=ez[:], axis=AX.X, op=ALU.add), reads=[hez], writes=[hsm])
                S.op("dve", lambda e: e.reciprocal(out=stt[:, 2048:2056], in_=sm[:, 1, :]), reads=[hsm], writes=[hstt])
                s4 = sall[:].rearrange("p (h c) n -> p h c n", c=2)
                st4 = stt[:, 0:2048].rearrange("p (h c n) -> p h c n", h=8, c=2)
                tt_("dve", st4[:, :, 0, :], s4[:, :, 0, :], thr.unsqueeze(2).broadcast_to([128, 8, 128]), ALU.subtract, [hsall, hsm], [hstt])
                S.op("pool", lambda e: e.tensor_copy(out=st4[:, :, 1, :], in_=s4[:, :, 1, :]), reads=[hsall], writes=[hstt])
                S.dma("sp", ST_d[jo], stt[:], hstt, reads=[hstt], writes=[hST[jo]])
            S.barrier()

    def phase_E2(G=4):
        with contextlib.ExitStack() as es:
            T, P = mk(es)
            c = load_consts(T)
            identb = c.cb[:, C_ID:C_ID + 128]
            G = min(G, NO)
            xn = [T("xn%d" % i, [128, DC, 128], BF16) for i in range(G)]
            stg = [T("stg%d" % i, [128, 2048 + 8]) for i in range(G)]
            acc = [T("acc%d" % i, [128, D]) for i in range(G)]
            Dg = [T("Dg%d" % i, [128, 8, 128], BF16) for i in range(G)]
            dnb = [T("dnb%d" % i, [128, 4, D], BF16) for i in range(1)]
            upb = [T("upb%d" % i, [128, 4, D], BF16) for i in range(2)]
            dT, hdT = T("dT", [128, DC, 512], BF16)
            EEh = [T("EEh%d" % i, [128, 4, 512]) for i in range(2)]
            Wh = [T("Wh%d" % i, [128, 8, 512], BF16) for i in range(2)]
            geT = [T("geT%d" % i, [128, 512]) for i in range(2)]
            GTb = [T("GTb%d" % i, [128, 4, 128], BF16) for i in range(2)]
            ptr, hptr = P("ptr", [128, 2, 512], BF16)
            phid = [P("phid%d" % i, [128, 4, 128]) for i in range(2)]
            pwt, hpwt = P("pwt", [128, 4, 128])
            pout, hpout = P("pout", [128, D])
            NCH = NE // 512
            for g0 in range(0, NO, G):
                tiles = list(range(g0, min(NO, g0 + G)))
                nt = len(tiles)
                for i, jo in enumerate(tiles):
                    S.dma("sp", xn[i][0][:], XNT_d[jo].rearrange("p (k t) -> p k t", k=DC), xn[i][1], reads=[hXNT[jo]], writes=[xn[i][1]])
                    S.dma("sp", stg[i][0][:], ST_d[jo], stg[i][1], reads=[hST[jo]], writes=[stg[i][1]])
                    S.dma("sp", acc[i][0][:], HM_d[jo], acc[i][1], reads=[hHM[jo]], writes=[acc[i][1]])
                    for h in range(8):
                        S.op("dve", lambda e, i=i, h=h: e.tensor_scalar(out=Dg[i][0][:, h, :], in0=identb, scalar1=stg[i][0][:, 2048 + h:2049 + h], scalar2=None, op0=ALU.mult),
                             reads=[c.hcb, stg[i][1]], writes=[Dg[i][1]])

                def issue_d(ec):
                    d_, hd_ = dnb[0]
                    S.dma("pool", d_[:], pd_d[ec * 512:(ec + 1) * 512, :].rearrange("(a p) d -> p a d", p=128), hd_, writes=[hd_])

                def issue_u(ec):
                    u_, hu_ = upb[ec % 2]
                    S.dma("pool", u_[:], pu_d[ec * 512:(ec + 1) * 512, :].rearrange("(a p) d -> p a d", p=128), hu_, writes=[hu_])

                def emit_dT(ec):
                    d_, hd_ = dnb[0]
                    for k2 in range(8):
                        for kk in range(2):
                            kc = k2 * 2 + kk
                            for et in range(4):
                                S.op("pe", lambda e, kk=kk, et=et, kc=kc, d_=d_: e.transpose(ptr[:, kk, et * 128:(et + 1) * 128], d_[:, et, kc * 128:(kc + 1) * 128], identb),
                                     reads=[hd_, c.hcb], writes=[hptr])
                        S.op("dve", lambda e, k2=k2: e.tensor_copy(out=dT[:, k2 * 2:k2 * 2 + 2, :], in_=ptr[:]), reads=[hptr], writes=[hdT])

                def stage1(gi):
                    ec, i = divmod(gi, nt)
                    sl = gi % 2
                    xn_, hxn_ = xn[i]
                    st_, hst_ = stg[i]
                    ph_, hph_ = phid[sl]
                    ge_, hge_ = geT[sl]
                    wh_, hwh_ = Wh[sl]
                    for et in range(4):
                        for kc in range(DC):
                            S.op("pe", lambda e, kc=kc, et=et: e.matmul(ph_[:, et, :], lhsT=dT[:, kc, et * 128:(et + 1) * 128], rhs=xn_[:, kc, :], start=(kc == 0), stop=(kc == DC - 1)),
                                 reads=[hxn_, hdT], writes=[hph_])
                    act_(ge_[:], ph_[:].rearrange("p a t -> p (a t)"), AF.Gelu, [hph_], [hge_])
                    st4 = st_[:, 0:2048].rearrange("p (h c n) -> p h c n", h=8, c=2)
                    for half in range(2):
                        ee_, hee_ = EEh[half]
                        for h in range(4):
                            hh = half * 4 + h
                            for a in range(4):
                                n1 = ec * 4 + a
                                act_(ee_[:, h, a * 128:(a + 1) * 128], st4[:, hh, 1, :], AF.Exp, [hst_], [hee_], bias=st4[:, hh, 0, n1:n1 + 1], scale=1.0)
                        S.op("dve", lambda e, half=half, ee_=ee_: e.scalar_tensor_tensor(
                            out=wh_[:, half * 4:half * 4 + 4, :].rearrange("p h e -> p (h e)"), in0=ee_[:].rearrange("p h e -> p (h e)"), scalar=1.0,
                            in1=ee_[:].rearrange("p h e -> p (h e)"), op0=ALU.is_ge, op1=ALU.mult), reads=[hee_], writes=[hwh_])

                def stage2(gi):
                    ec, i = divmod(gi, nt)
                    sl = gi % 2
                    u_, hu_ = upb[ec % 2]
                    ge_, hge_ = geT[sl]
                    wh_, hwh_ = Wh[sl]
                    gt_, hgt_ = GTb[sl]
                    dg_, hdg_ = Dg[i]
                    ac_, hac_ = acc[i]
                    for et in range(4):
                        for h in range(8):
                            S.op("pe", lambda e, et=et, h=h: e.matmul(pwt[:, et, :], lhsT=wh_[:, h, et * 128:(et + 1) * 128], rhs=dg_[:, h, :], start=(h == 0), stop=(h == 7)),
                                 reads=[hwh_, hdg_], writes=[hpwt])
                    tt_("dve", gt_[:].rearrange("p a t -> p (a t)"), ge_[:], pwt[:].rearrange("p a t -> p (a t)"), ALU.mult, [hge_, hpwt], [hgt_])
                    for dg in range(4):
                        for et in range(4):
                            S.op("pe", lambda e, dg=dg, et=et: e.matmul(pout[:, dg * 512:(dg + 1) * 512], lhsT=gt_[:, et, :], rhs=u_[:, et, dg * 512:(dg + 1) * 512], start=(et == 0), stop=(et == 3)),
                                 reads=[hgt_, hu_], writes=[hpout])
                    tt_("dve", ac_[:], ac_[:], pout[:], ALU.add, [hac_, hpout], [hac_])

                NG = NCH * nt
                issue_d(0)
                issue_u(0)
                emit_dT(0)
                if NCH > 1:
                    issue_d(1)
                    issue_u(1)
                stage1(0)
                for gi in range(NG):
                    if gi + 1 < NG:
                        ec1, i1 = divmod(gi + 1, nt)
                        if i1 == 0:
                            emit_dT(ec1)
                            if ec1 + 1 < NCH:
                                issue_d(ec1 + 1)
                        stage1(gi + 1)
                    stage2(gi)
                    ec, i = divmod(gi, nt)
                    if i == nt - 1 and ec + 2 < NCH:
                        issue_u(ec + 2)
                for i, jo in enumerate(tiles):
                    S.dma("sp", yout[jo * 128:(jo + 1) * 128, :], acc[i][0][:], acc[i][1], reads=[acc[i][1]], writes=[hOUT[jo]])
            S.barrier()

    ph = {"A": phase_A, "B": phase_B, "C": phase_C, "D": phase_D, "E": phase_E1, "F": phase_E2}
    ctx = dict(nc=nc, S=S, mk=mk, load_consts=load_consts, rmsnorm_T=rmsnorm_T, locals=locals())
    for p in phases:
        if p in ph:
            ph[p]()
    return nc, ctx


def _colT(v, n):
    buf = np.zeros(n * 128, np.float32)
    buf[:v.size] = v.reshape(-1)
    return buf.reshape(n, 128).T


def make_vecs(inp):
    vecs = np.zeros((128, NV), np.float32)
    vecs[:, V_G1:V_G1 + 16] = _colT(inp["norm1_g"][0], 16)
    vecs[:, V_MU:V_MU + 27] = _colT(inp["shift_mu"][0], 27)
    for col, key in ((V_W0, "w0"), (V_A0, "a0"), (V_KK, "k_k"), (V_KA, "k_a"), (V_GNW, "gn_w"),
                     (V_GNB, "gn_b"), (V_RK, "r_k")):
        vecs[:, col:col + 8] = _colT(inp[key][0], 8)
    vecs[:, V_QG] = np.tile(inp["q_gain"][0], 2)
    vecs[:, V_KG] = np.tile(inp["k_gain"][0], 2)
    sk = inp["sinks"][0]
    for hp in range(8):
        vecs[0:64, V_SK + hp] = sk[2 * hp]
        vecs[64:128, V_SK + hp] = sk[2 * hp + 1]
    vecs[:, V_G2:V_G2 + 16] = _colT(inp["norm2_g"][0], 16)
    return vecs


def make_consts(first_half):
    cn = np.zeros((128, NCN), np.float32)
    cn[:, C_ID:C_ID + 128] = np.eye(128, dtype=np.float32)
    bo = np.zeros((128, 128), np.float32)
    bo[0:64, 0:64] = 1
    bo[64:, 64:] = 1
    cn[:, C_BO:C_BO + 128] = bo
    s = np.arange(64)[:, None]
    t = np.arange(64)[None, :]
    cn[0:64, C_MU:C_MU + 64] = (s < t)
    cn[0:64, C_MUI:C_MUI + 64] = (s <= t)
    cn[0:64, C_ML:C_ML + 64] = (s > t)
    s = np.arange(128)[:, None]
    q = np.arange(128)[None, :]
    cn[:, C_MC:C_MC + 128] = (s <= q)
    cn[:, C_MP:C_MP + 128] = (s > q)
    mpf = (s > q)
    if first_half:
        mpf = mpf & (s >= 112)
    cn[:, C_MPF:C_MPF + 128] = mpf
    for g in range(2):
        sel = np.zeros((128, 128), np.float32)
        for m in range(128):
            sel[g * 64 + (m % 64), m] = 1
        cn[:, C_SEL + g * 128:C_SEL + (g + 1) * 128] = sel
    return cn


_NC_CACHE = {}


def kernel(**inputs):
    inp = {k: np.asarray(v) for k, v in inputs.items()}
    x = inp["x"].astype(np.float32, copy=False)
    B, SEQ, _ = x.shape
    NB, OWN0 = 33, 17
    if "nc" not in _NC_CACHE:
        _NC_CACHE["nc"] = build(NB=NB, OWN0=OWN0, phases="ABCDEF")[0]
    nc = _NC_CACHE["nc"]
    f = lambda k: np.ascontiguousarray(inp[k][0], dtype=np.float32)
    shared = dict(w_in=f("w_in"), vecs=make_vecs(inp), w_up=f("w_up"), a_up=f("a_up"), g_up=f("g_up"),
                  w_out=f("w_out"), peer_query=f("peer_query"),
                  sub_keys=np.ascontiguousarray(inp["peer_sub_keys"][0].reshape(16, 128, 128), dtype=np.float32),
                  peer_down=f("peer_down"), peer_up=f("peer_up"))
    meta = inp["meta_tokens"].astype(np.float32, copy=False)
    cn = [make_consts(False), make_consts(True)]
    in_maps = []
    for c in range(8):
        b, s = c // 2, c % 2
        loc = np.zeros((NB * 128, D), np.float32)
        if s == 0:
            loc[16 * 128 + 112:17 * 128] = meta
            loc[17 * 128:] = x[b, :2048]
        else:
            loc[112:128] = meta
            loc[128:] = x[b]
        in_maps.append(dict(xin=loc, consts=cn[1 if s == 0 else 0], **shared))
    res = run_bass_kernel_spmd(nc, in_maps, core_ids=list(range(8)))
    out = np.empty((B, SEQ, D), np.float32)
    for c in range(8):
        b, s = c // 2, c % 2
        out[b, s * 2048:(s + 1) * 2048] = np.asarray(res.results[c]["yout"])
    return out
```

```python
import contextlib
import numpy as np
import concourse.bass as bass
import concourse.mybir as mybir
from concourse.bass_utils import run_bass_kernel_spmd

F32 = mybir.dt.float32
BF16 = mybir.dt.bfloat16
AF = mybir.ActivationFunctionType
ALU = mybir.AluOpType
AX = mybir.AxisListType

D = 2048
DC = 16
INC = 4640
RW0 = 1280
NRC = 27
NE = 16384
HD = 64

V_G1, V_MU, V_W0, V_A0, V_KK, V_KA, V_GNW, V_GNB, V_RK, V_QG, V_KG, V_SK, V_G2 = (
    0, 16, 43, 51, 59, 67, 75, 83, 91, 99, 100, 101, 109)
NV = 125
C_ID, C_BO, C_MU, C_MUI, C_ML, C_MC, C_MP, C_MPF, C_SEL = 0, 128, 256, 320, 384, 448, 576, 704, 832
NCN = 832 + 256


class H:
    __slots__ = ("name", "w", "r", "dsem", "dcnt")

    def __init__(self, name=""):
        self.name = name
        self.w = {}
        self.r = {}
        self.dsem = None
        self.dcnt = 0


class Sched:
    def __init__(self, nc, needed=None):
        self.nc = nc
        self.engs = {"pe": nc.tensor, "act": nc.scalar, "dve": nc.vector,
                     "pool": nc.gpsimd, "sp": nc.sync}
        self.esem = {k: nc.alloc_semaphore(name="es_" + k) for k in self.engs}
        self.seq = {k: 0 for k in self.engs}
        self.cnt = {k: 0 for k in self.engs}
        self.cntmap = {k: {} for k in self.engs}
        self.waited = {k: {} for k in self.engs}
        self.needed = needed
        self.rec = {k: set() for k in self.engs}
        self.sems = {}
        self.dcur = {}

    def _wait(self, e, evs):
        w = self.waited[e]
        for key, val in evs.items():
            if w.get(key, 0) >= val:
                continue
            if isinstance(key, str):
                self.rec[key].add(val)
                real = self.cntmap[key][val] if self.needed is not None else val
                self.engs[e].wait_ge(self.esem[key], real)
            else:
                self.engs[e].wait_ge(self.sems[key], val)
            w[key] = val

    @staticmethod
    def _merge(d, evs):
        for k, v in evs.items():
            if d.get(k, 0) < v:
                d[k] = v

    def _deps(self, reads, writes, own=None):
        evs = {}
        for h in reads:
            self._merge(evs, h.w)
        if own == "pe":
            evs.pop(own, None)
        ww = {}
        for h in writes:
            self._merge(ww, h.w)
            self._merge(ww, h.r)
        if own is not None:
            ww.pop(own, None)
        self._merge(evs, ww)
        return evs

    def _pe_mode(self, mode):
        if mode != getattr(self, "pe_mode", None):
            if self.seq["pe"] > 0:
                self._wait("pe", {"pe": self.seq["pe"]})
            self.pe_mode = mode

    def op(self, e, fn, reads=(), writes=()):
        self._wait(e, self._deps(reads, writes, own=e))
        inst = fn(_PEProxy(self) if e == "pe" else self.engs[e])
        self.seq[e] += 1
        n = self.seq[e]
        if self.needed is None or n in self.needed[e]:
            self.cnt[e] += 1
            inst.then_inc(self.esem[e], 1)
            self.cntmap[e][n] = self.cnt[e]
        ev = {e: n}
        for h in reads:
            self._merge(h.r, ev)
        for h in writes:
            h.w = dict(ev)
            h.r = {}
        return inst

    def dma(self, q, out, in_, tile, reads=(), writes=(), **kw):
        if tile.dsem is None:
            tile.dsem = self.nc.alloc_semaphore(name="ds_%d" % len(self.sems))
            self.sems[tile.dsem.num] = tile.dsem
        deps = self._deps(reads, writes)
        if tile in writes and not tile.r and tile.w.get(tile.dsem.num, 0) == tile.dcnt and len(tile.w) == 1:
            deps.pop(tile.dsem.num, None)
        self._wait(q, deps)
        inst = self.engs[q].dma_start(out=out, in_=in_, **kw)
        tile.dcnt += 16
        inst.then_inc(tile.dsem, 16)
        self.dcur[tile.dsem.num] = tile.dcnt
        ev = {tile.dsem.num: tile.dcnt}
        for h in reads:
            self._merge(h.r, ev)
        for h in writes:
            h.w = dict(ev)
            h.r = {}
        return inst

    def barrier(self):
        evs = {k: self.seq[k] for k in self.engs if self.seq[k] > 0}
        evs.update(self.dcur)
        for e in self.engs:
            self._wait(e, evs)


def _rnd(n):
    return 32 if n <= 32 else (64 if n <= 64 else 128)


class _PEProxy:
    def __init__(self, S):
        self.S = S

    def matmul(self, out, lhsT, rhs, **kw):
        fr = 1
        for d in lhsT.shape[1:]:
            fr *= d
        self.S._pe_mode(("mm", _rnd(lhsT.shape[0]), _rnd(fr), str(lhsT.dtype), lhsT.base_partition(), out.base_partition()))
        return self.S.nc.tensor.matmul(out, lhsT=lhsT, rhs=rhs, **kw)

    def transpose(self, out, in_, identity):
        fr = 1
        for d in in_.shape[1:]:
            fr *= d
        self.S._pe_mode(("tr", _rnd(in_.shape[0]), _rnd(fr), str(in_.dtype), in_.base_partition(), out.base_partition()))
        return self.S.nc.tensor.transpose(out, in_, identity)


class Ctx:
    pass


def build(NB=33, OWN0=17, phases="ABCDEF", dbg=False):
    _, ctx = _build(NB, OWN0, phases, dbg, None)
    return _build(NB, OWN0, phases, dbg, ctx["S"].rec)


def _build(NB, OWN0, phases, dbg, needed):
    NO = NB - OWN0
    nc = bass.Bass("TRN2", target_bir_lowering=False)
    S = Sched(nc, needed)

    def din(name, shape, dt=F32):
        return nc.dram_tensor(name, list(shape), dt, kind="ExternalInput").ap()

    def dscr(name, shape, dt=F32, out=False):
        return nc.dram_tensor(name, list(shape), dt, kind=("ExternalOutput" if out else "Internal")).ap()

    xin = din("xin", [NB * 128, D])
    w_in = din("w_in", [D, INC])
    vecs_d = din("vecs", [128, NV])
    cn_d = din("consts", [128, NCN])
    wup_d = din("w_up", [64, 1024])
    aup_d = din("a_up", [64, 1024])
    gup_d = din("g_up", [160, 1024])
    wout_d = din("w_out", [D, D])
    pq_d = din("peer_query", [D, D])
    sk_d = din("sub_keys", [16, 128, 128])
    pd_d = din("peer_down", [NE, D])
    pu_d = din("peer_up", [NE, D])
    yout = dscr("yout", [NO * 128, D], F32, out=True)

    PS_d = dscr("PS_s", [NB, 128, NRC * 128])
    QKV_d = dscr("QKV_s", [NO + 1, 128, 10 * 128])
    YR_d = dscr("YR_s", [NO, 128, 1024], BF16, out=dbg)
    YA_d = dscr("YA_s", [NO, 128, 1024], BF16, out=dbg)
    HM_d = dscr("HM_s", [NO, 128, D], F32, out=dbg)
    XNT_d = dscr("XNT_s", [NO, 128, D], BF16)
    ST_d = dscr("ST_s", [NO, 128, 2048 + 8])
    hPS = [H("PS%d" % j) for j in range(NB)]
    hQKV = [H("QKV%d" % j) for j in range(NO + 1)]
    hYR = [H() for j in range(NO)]
    hYA = [H() for j in range(NO)]
    hHM = [H() for j in range(NO)]
    hXNT = [H() for j in range(NO)]
    hST = [H() for j in range(NO)]
    hOUT = [H() for j in range(NO)]

    uid = [0]

    def mk(es):
        def T(name, shape, dt=F32):
            uid[0] += 1
            t = es.enter_context(nc.sbuf_tensor("sb%d_%s" % (uid[0], name), list(shape), dt))
            return t, H(name)

        def P(name, shape, dt=F32):
            uid[0] += 1
            t = es.enter_context(nc.psum_tensor("ps%d_%s" % (uid[0], name), list(shape), dt))
            return t, H(name)
        return T, P

    def load_consts(T, bf=True):
        c = Ctx()
        c.vec, c.hvec = T("vecs", [128, NV])
        S.dma("sp", c.vec[:], vecs_d, c.hvec, writes=[c.hvec])
        c.cn, c.hcn = T("cn32", [128, NCN])
        S.dma("sp", c.cn[:], cn_d, c.hcn, writes=[c.hcn])
        c.cb, c.hcb = T("cnbf", [128, NCN], BF16)
        S.dma("pool", c.cb[:], cn_d, c.hcb, writes=[c.hcb])
        return c

    def rmsnorm_T(c, T_, xt, hxt, gcol, junk, hjunk, st, hst, xs, hxs, pT, hpT, uT, huT):
        S.op("act", lambda e: e.activation(out=junk[:], in_=xt[:], func=AF.Square, accum_out=st[:, 0:1]),
             reads=[hxt], writes=([hjunk, hst] if hjunk is not hst else [hst]))
        S.op("dve", lambda e: e.tensor_scalar(out=st[:, 1:2], in0=st[:, 0:1], scalar1=1.0 / D, scalar2=1e-6,
                                              op0=ALU.mult, op1=ALU.add), reads=[hst], writes=[hst])
        S.op("act", lambda e: e.activation(out=st[:, 2:3], in_=st[:, 1:2], func=AF.Sqrt), reads=[hst], writes=[hst])
        S.op("dve", lambda e: e.reciprocal(out=st[:, 3:4], in_=st[:, 2:3]), reads=[hst], writes=[hst])
        S.op("act", lambda e: e.activation(out=xs[:], in_=xt[:], func=AF.Copy, scale=st[:, 3:4]),
             reads=[hxt, hst], writes=[hxs])
        for k in range(DC):
            S.op("pe", lambda e, k=k: e.transpose(pT[:, k, :], xs[:, k * 128:(k + 1) * 128], c.cb[:, C_ID:C_ID + 128]),
                 reads=[hxs, c.hcb], writes=[hpT])
        gb = c.vec[:, gcol:gcol + DC].unsqueeze(2).broadcast_to([128, DC, 128])
        S.op("dve", lambda e: e.tensor_tensor(out=uT[:], in0=pT[:], in1=gb, op=ALU.mult),
             reads=[hpT, c.hvec], writes=[huT])

    def phase_A():
        with contextlib.ExitStack() as es:
            T, P = mk(es)
            c = load_consts(T)
            win, hwin = T("win", [128, DC, INC], BF16)
            for k in range(DC):
                S.dma("pool", win[:, k, :], w_in[k * 128:(k + 1) * 128, :], hwin, writes=[hwin])
            xt, hxt = T("xt", [128, D])
            st, hst = T("st", [128, 4])
            xs, hxs = T("xs", [128, D], BF16)
            junk, hjunk = xs, hxs
            pT, hpT = P("pT", [128, DC, 128], BF16)
            uT, huT = T("uT", [128, DC, 128], BF16)
            pp = [P("pp%d" % i, [128, 4, 128]) for i in range(4)]
            PT, hPT = T("PT", [128, NRC, 129])
            QK, hQK = T("QK", [128, 10, 128])
            dd, hdd = T("dd", [128, NRC, 128])
            pss, hpss = dd, hdd
            S.op("pool", lambda e: e.memset(PT[:], 0.0), writes=[hPT])
            mub = c.vec[:, V_MU:V_MU + NRC].unsqueeze(2).broadcast_to([128, NRC, 128])
            for j in range(NB):
                S.dma("sp", xt[:], xin[j * 128:(j + 1) * 128, :], hxt, writes=[hxt])
                rmsnorm_T(c, T, xt, hxt, V_G1, junk, hjunk, st, hst, xs, hxs, pT, hpT, uT, huT)
                need_att = (j >= OWN0 - 1)
                chunks = list(range(0 if need_att else 10, 37))
                gi = 0
                for g0 in range(0, len(chunks), 4):
                    grp = chunks[g0:g0 + 4]
                    pt_, hp_ = pp[gi % 4]
                    gi += 1
                    for qi, ch in enumerate(grp):
                        M = 32 if ch == 36 else 128
                        for k in range(DC):
                            S.op("pe", lambda e, qi=qi, ch=ch, k=k, M=M, pt_=pt_: e.matmul(
                                pt_[0:M, qi, :], lhsT=win[:, k, ch * 128:ch * 128 + M], rhs=uT[:, k, :],
                                start=(k == 0), stop=(k == DC - 1)), reads=[hwin, huT], writes=[hp_])
                    for qi, ch in enumerate(grp):
                        M = 32 if ch == 36 else 128
                        eng = "act" if (qi % 2 == 0) else "dve"
                        if ch < 10:
                            dst, hd = QK[0:M, ch, :], hQK
                        else:
                            dst, hd = PT[0:M, ch - 10, 1:129], hPT
                        if eng == "act":
                            S.op("act", lambda e, dst=dst, qi=qi, M=M, pt_=pt_: e.activation(out=dst, in_=pt_[0:M, qi, :], func=AF.Copy),
                                 reads=[hp_], writes=[hd])
                        else:
                            S.op("dve", lambda e, dst=dst, qi=qi, M=M, pt_=pt_: e.tensor_copy(out=dst, in_=pt_[0:M, qi, :]),
                                 reads=[hp_], writes=[hd])
                if need_att:
                    ja = j - (OWN0 - 1)
                    S.dma("sp", QKV_d[ja].rearrange("p (c t) -> p c t", c=10), QK[:], hQK, reads=[hQK], writes=[hQKV[ja]])
                S.op("pool", lambda e: e.tensor_tensor(out=dd[:], in0=PT[:, :, 0:128], in1=PT[:, :, 1:129], op=ALU.subtract),
                     reads=[hPT], writes=[hdd])
                S.op("pool", lambda e: e.tensor_tensor(out=dd[:], in0=dd[:], in1=mub, op=ALU.mult),
                     reads=[hdd, c.hvec], writes=[hdd])
                S.op("dve", lambda e: e.tensor_tensor(out=dd[:], in0=dd[:], in1=PT[:, :, 1:129], op=ALU.add),
                     reads=[hdd, hPT], writes=[hdd])
                S.op("act", lambda e: e.activation(out=PT[:, :, 0:1], in_=PT[:, :, 128:129], func=AF.Copy),
                     reads=[hPT, hdd, hpss], writes=[hPT])
                S.dma("sp", PS_d[j].rearrange("p (c t) -> p c t", c=NRC), pss[:], hpss, reads=[hpss], writes=[hPS[j]])
            S.barrier()

    def phase_B():
        with contextlib.ExitStack() as es:
            T, P = mk(es)
            c = load_consts(T)
            vec = c.vec

            def vb(col, n=8):
                return vec[:, col:col + n].unsqueeze(2).broadcast_to([128, n, 128])
            wup, hwup = T("wup", [128, 1024], BF16)
            aup, haup = T("aup", [128, 1024], BF16)
            gup, hgup = T("gup", [128, 2, 1024], BF16)
            S.dma("pool", wup[0:64, :], wup_d, hwup, writes=[hwup])
            S.dma("pool", aup[64:128, :], aup_d, haup, writes=[haup])
            S.dma("pool", gup[:, 0, :], gup_d[0:128, :], hgup, writes=[hgup])
            S.dma("pool", gup[0:32, 1, :], gup_d[128:160, :], hgup, writes=[hgup])
            ps, hps = T("ps", [128, NRC, 128])
            names32 = ["nld", "aa", "kap", "kp", "beta", "cs", "cum", "t1", "t2", "t3", "gg", "bon"]
            F = {}
            for n in names32:
                F[n] = T(n, [128, 8, 128])
            namesbf = ["Kh", "Rh", "Bh", "Qh", "BG", "QG", "vb", "sqb"]
            Bf = {}
            for n in namesbf:
                Bf[n] = T(n, [128, 8, 128], BF16)
            lw, hlw = T("lw", [128, 128], BF16)
            sg, hsg = T("sg", [128, 2, 128], BF16)
            ones, hones = T("ones", [128, 1024])
            S.op("pool", lambda e: e.memset(ones[:], 1.0), writes=[hones])
            sm, hsm = T("sm", [128, 8, 2, 4])
            Vt, hVt = T("Vt", [64, 2, 8, 128], BF16)
            BGt, hBGt = T("BGt", [64, 2, 8, 128], BF16)
            QGt, hQGt = T("QGt", [64, 2, 8, 128], BF16)
            S32, hS32 = T("S32", [128, 8, 64])
            Sb, hSb = T("Sb", [128, 8, 64], BF16)
            S.op("pool", lambda e: e.memset(S32[:], 0.0), writes=[hS32])
            S.op("pool", lambda e: e.memset(Sb[:], 0.0), writes=[hSb])
            gm = {}
            for n in ["Ub", "Lb", "U2", "L2", "Lak", "Mrb", "Mrk", "Qb", "Xb", "SAb"]:
                gm[n] = T(n, [64, 16, 64], BF16)
            ysb, hysb = T("ysb", [64, 16, 64])
            yc, hyc = T("yc", [64, 16, 64])
            ysq, hysq = T("ysq", [64, 16, 64])
            gst, hgst = T("gst", [64, 16, 4])
            yf, hyf = T("yf", [128, 8, 128])
            yrb, hyrb = T("yrb", [128, 8, 128], BF16)
            PA = [P("PA%d" % i, [128, 1024]) for i in range(3)]
            PTr, hPTr = P("PTr", [128, 2048], BF16)
            pi = [0]

            def nextP():
                t = PA[pi[0] % 3]
                pi[0] += 1
                return t
            maskU = c.cn[0:64, C_MU:C_MU + 64].unsqueeze(1).broadcast_to([64, 16, 64])
            maskUi = c.cn[0:64, C_MUI:C_MUI + 64].unsqueeze(1).broadcast_to([64, 16, 64])
            maskL = c.cn[0:64, C_ML:C_ML + 64].unsqueeze(1).broadcast_to([64, 16, 64])
            identb = c.cb[:, C_ID:C_ID + 128]
            ident32 = c.cn[:, C_ID:C_ID + 128]
            bones = c.cb[:, C_BO:C_BO + 128]

            def tt(eng, out, i0, i1, op, reads, writes):
                S.op(eng, lambda e: e.tensor_tensor(out=out, in0=i0, in1=i1, op=op), reads=reads, writes=writes)

            def actf(out, in_, func, reads, writes, **kw):
                S.op("act", lambda e: e.activation(out=out, in_=in_, func=func, **kw), reads=reads, writes=writes)

            HORD = list(range(0, 16, 2)) + list(range(1, 16, 2))
            for j in range(NB):
                own = j >= OWN0
                S.dma("sp", ps[:], PS_d[j].rearrange("p (c t) -> p c t", c=NRC), hps, reads=[hPS[j]], writes=[hps])
                r_, k_, v_ = ps[:, 0:8, :], ps[:, 8:16, :], ps[:, 16:24, :]
                actf(lw[0:64, :], ps[0:64, 24, :], AF.Tanh, [hps], [hlw])
                actf(lw[64:128, :], ps[64:128, 24, :], AF.Copy, [hps], [hlw])
                actf(sg[:, 0, :], ps[:, 25, :], AF.Sigmoid, [hps], [hsg])
                actf(sg[0:32, 1, :], ps[0:32, 26, :], AF.Sigmoid, [hps], [hsg])
                pw_, hpw = nextP()
                pa_, hpa = nextP()
                for m in range(8):
                    S.op("pe", lambda e, m=m: e.matmul(pw_[:, m * 128:(m + 1) * 128], lhsT=wup[0:64, m * 128:(m + 1) * 128], rhs=lw[0:64, :], start=True, stop=True),
                         reads=[hwup, hlw], writes=[hpw])
                    S.op("pe", lambda e, m=m: e.matmul(pa_[:, m * 128:(m + 1) * 128], lhsT=aup[64:128, m * 128:(m + 1) * 128], rhs=lw[64:128, :], start=True, stop=True),
                         reads=[haup, hlw], writes=[hpa])
                nld, hnld = F["nld"]
                aa, haa = F["aa"]
                t1, ht1 = F["t1"]
                t2, ht2 = F["t2"]
                t3, ht3 = F["t3"]
                v3 = lambda p_: p_[:].rearrange("p (m t) -> p m t", m=8)
                tt("dve", t1[:], v3(pw_), vb(V_W0), ALU.add, [hpw, c.hvec], [ht1])
                actf(t1[:], t1[:], AF.Sigmoid, [ht1], [ht1])
                S.op("dve", lambda e: e.tensor_scalar(out=nld[:], in0=t1[:], scalar1=0.6065306597126334, scalar2=None, op0=ALU.mult),
                     reads=[ht1], writes=[hnld])
                tt("dve", t2[:], v3(pa_), vb(V_A0), ALU.add, [hpa, c.hvec], [ht2])
                actf(aa[:], t2[:], AF.Sigmoid, [ht2], [haa])
                if own:
                    pg_, hpg = nextP()
                    for m in range(8):
                        S.op("pe", lambda e, m=m: e.matmul(pg_[:, m * 128:(m + 1) * 128], lhsT=gup[:, 0, m * 128:(m + 1) * 128], rhs=sg[:, 0, :], start=True, stop=False),
                             reads=[hgup, hsg], writes=[hpg])
                        S.op("pe", lambda e, m=m: e.matmul(pg_[:, m * 128:(m + 1) * 128], lhsT=gup[0:32, 1, m * 128:(m + 1) * 128], rhs=sg[0:32, 1, :], start=False, stop=True),
                             reads=[hgup, hsg], writes=[hpg])
                    gg, hgg = F["gg"]
                    actf(gg[:], v3(pg_), AF.Copy, [hpg], [hgg])
                kap, hkap = F["kap"]
                sqb, hsqb = Bf["sqb"]
                tt("pool", kap[:], k_, vb(V_KK), ALU.mult, [hps, c.hvec], [hkap])
                actf(sqb[:], kap[:], AF.Square, [hkap], [hsqb])
                pq_, hpq = nextP()
                for hh in range(2):
                    S.op("pe", lambda e, hh=hh: e.matmul(pq_[:, hh * 512:(hh + 1) * 512], lhsT=bones, rhs=sqb[:, hh * 4:(hh + 1) * 4, :], start=True, stop=True),
                         reads=[c.hcb, hsqb], writes=[hpq])
                actf(t3[:], v3(pq_), AF.Sqrt, [hpq], [ht3])
                S.op("dve", lambda e: e.tensor_scalar(out=t3[:], in0=t3[:], scalar1=1e-12, scalar2=None, op0=ALU.max), reads=[ht3], writes=[ht3])
                S.op("dve", lambda e: e.reciprocal(out=t3[:], in_=t3[:]), reads=[ht3], writes=[ht3])
                tt("dve", kap[:], kap[:], t3[:], ALU.mult, [hkap, ht3], [hkap])
                kp, hkp = F["kp"]
                S.op("dve", lambda e: e.scalar_tensor_tensor(out=t2[:], in0=aa[:], scalar=1.0, in1=vb(V_KA), op0=ALU.subtract, op1=ALU.mult),
                     reads=[haa, c.hvec], writes=[ht2])
                S.op("dve", lambda e: e.scalar_tensor_tensor(out=kp[:], in0=t2[:], scalar=1.0, in1=k_, op0=ALU.add, op1=ALU.mult),
                     reads=[ht2, hps], writes=[hkp])
                beta, hbeta = F["beta"]
                tt("pool", beta[:], kap[:], aa[:], ALU.mult, [hkap, haa], [hbeta])
                cs, hcs = F["cs"]
                cum, hcum = F["cum"]
                S.op("dve", lambda e: e.tensor_tensor_scan(out=cs[:].rearrange("p m t -> p (m t)"), data0=ones[:],
                                                           data1=nld[:].rearrange("p m t -> p (m t)"), initial=0.0,
                                                           op0=ALU.mult, op1=ALU.add), reads=[hones, hnld], writes=[hcs])
                cs4 = cs[:].rearrange("p m (s t) -> p m s t", s=2)
                nld4 = nld[:].rearrange("p m (s t) -> p m s t", s=2)
                cum4 = cum[:].rearrange("p m (s t) -> p m s t", s=2)
                tt("dve", sm[:, :, :, 0:1], cs4[:, :, :, 0:1], nld4[:, :, :, 0:1], ALU.subtract, [hcs, hnld], [hsm])
                tt("dve", cum4, cs4, sm[:, :, :, 0:1].broadcast_to([128, 8, 2, 64]), ALU.subtract, [hcs, hsm], [hcum])
                actf(sm[:, :, :, 2:3], cum4[:, :, :, 63:64], AF.Exp, [hcum], [hsm], scale=-1.0)
                Kh, hKh = Bf["Kh"]
                Rh, hRh = Bf["Rh"]
                Bh, hBh = Bf["Bh"]
                Qh, hQh = Bf["Qh"]
                BG, hBG = Bf["BG"]
                QG, hQG = Bf["QG"]
                vbf, hvbf = Bf["vb"]
                actf(t1[:], cum[:], AF.Exp, [hcum], [ht1], scale=-1.0)
                tt("dve", Rh[:], r_, t1[:], ALU.mult, [hps, ht1], [hRh])
                tt("pool", t2[:], cum[:], nld[:], ALU.subtract, [hcum, hnld], [ht2])
                actf(t2[:], t2[:], AF.Exp, [ht2], [ht2], scale=-1.0)
                tt("dve", Kh[:], kap[:], t2[:], ALU.mult, [hkap, ht2], [hKh])
                actf(t3[:], cum[:], AF.Exp, [hcum], [ht3])
                tt("dve", Bh[:], beta[:], t3[:], ALU.mult, [hbeta, ht3], [hBh])
                tt("pool", Qh[:], kp[:], t3[:], ALU.mult, [hkp, ht3], [hQh])
                t14 = t1[:].rearrange("p m (s t) -> p m s t", s=2)
                tt("pool", t14, cum4, cum4[:, :, :, 63:64].broadcast_to([128, 8, 2, 64]), ALU.subtract, [hcum], [ht1])
                actf(t1[:], t1[:], AF.Exp, [ht1], [ht1])
                tt("dve", BG[:], beta[:], t1[:], ALU.mult, [hbeta, ht1], [hBG])
                tt("pool", QG[:], kp[:], t1[:], ALU.mult, [hkp, ht1], [hQG])
                actf(vbf[:], v_, AF.Copy, [hps], [hvbf])
                for (src, hsrc, dst, hdst) in ((vbf, hvbf, Vt, hVt), (BG, hBG, BGt, hBGt), (QG, hQG, QGt, hQGt)):
                    for m in range(8):
                        for s in range(2):
                            S.op("pe", lambda e, m=m, s=s, src=src: e.transpose(
                                PTr[0:64, (s * 8 + m) * 128:(s * 8 + m + 1) * 128], src[:, m, s * 64:(s + 1) * 64], identb),
                                reads=[hsrc, c.hcb], writes=[hPTr])
                    S.op("act", lambda e, dst=dst: e.activation(out=dst[:].rearrange("t s m c -> t (s m c)"), in_=PTr[0:64, :], func=AF.Copy),
                         reads=[hPTr], writes=[hdst])
                if own:
                    bon, hbon = F["bon"]
                    tt("pool", t2[:], r_, kp[:], ALU.mult, [hps, hkp], [ht2])
                    tt("pool", sqb[:], t2[:], vb(V_RK), ALU.mult, [ht2, c.hvec], [hsqb])
                    pb_, hpb = nextP()
                    for hh in range(2):
                        S.op("pe", lambda e, hh=hh: e.matmul(pb_[:, hh * 512:(hh + 1) * 512], lhsT=bones, rhs=sqb[:, hh * 4:(hh + 1) * 4, :], start=True, stop=True),
                             reads=[c.hcb, hsqb], writes=[hpb])
                    tt("dve", bon[:], v3(pb_), v_, ALU.mult, [hpb, hps], [hbon])
                for s in range(2):
                    ts = slice(s * 64, (s + 1) * 64)

                    def hrows(h):
                        return slice((h % 2) * 64, (h % 2) * 64 + 64)

                    def gram(lt, hl, rt, hr, mask, dname, eng):
                        p_, hp_ = nextP()
                        pv = p_[0:64, :].rearrange("p (h t) -> p h t", h=16)
                        for h in HORD:
                            S.op("pe", lambda e, h=h: e.matmul(pv[:, h, :], lhsT=lt[hrows(h), h // 2, ts], rhs=rt[hrows(h), h // 2, ts], start=True, stop=True),
                                 reads=[hl, hr], writes=[hp_])
                        d_, hd_ = gm[dname]
                        tt(eng, d_[:], pv, mask, ALU.mult, [hp_, c.hcn], [hd_])

                    gram(Bh, hBh, Kh, hKh, maskU, "Ub", "dve")
                    gram(Kh, hKh, Bh, hBh, maskL, "Lb", "dve")
                    gram(Qh, hQh, Kh, hKh, maskU, "Lak", "dve")
                    if own:
                        gram(Bh, hBh, Rh, hRh, maskUi, "Mrb", "dve")
                        gram(Qh, hQh, Rh, hRh, maskUi, "Mrk", "dve")
                    Ub, hUb = gm["Ub"]
                    Lb, hLb = gm["Lb"]
                    Qb, hQb = gm["Qb"]
                    idb = c.cn[0:64, C_ID:C_ID + 64].unsqueeze(1).broadcast_to([64, 16, 64])
                    tt("dve", Qb[:], idb, Ub[:], ALU.subtract, [c.hcn, hUb], [hQb])
                    cur = ("Ub", "Lb")
                    nxt = ("U2", "L2")
                    for lvl in range(5):
                        Uk, hUk = gm[cur[0]]
                        Lk, hLk = gm[cur[1]]
                        Un, hUn = gm[nxt[0]]
                        Ln, hLn = gm[nxt[1]]
                        p2, hp2 = nextP()
                        p2v = p2[0:64, :].rearrange("p (h t) -> p h t", h=16)
                        for h in HORD:
                            S.op("pe", lambda e, h=h: e.matmul(p2v[:, h, :], lhsT=Uk[:, h, :], rhs=Lk[:, h, :], start=True, stop=True),
                                 reads=[hUk, hLk], writes=[hp2])
                        actf(Ln[:], p2v, AF.Copy, [hp2], [hLn])
                        if lvl < 4:
                            p1, hp1 = nextP()
                            p1v = p1[0:64, :].rearrange("p (h t) -> p h t", h=16)
                            for h in HORD:
                                S.op("pe", lambda e, h=h: e.matmul(p1v[:, h, :], lhsT=Lk[:, h, :], rhs=Uk[:, h, :], start=True, stop=True),
                                     reads=[hUk, hLk], writes=[hp1])
                            S.op("dve", lambda e: e.tensor_copy(out=Un[:], in_=p1v), reads=[hp1], writes=[hUn])
                        p3, hp3 = nextP()
                        p3v = p3[0:64, :].rearrange("p (h t) -> p h t", h=16)
                        for h in HORD:
                            S.op("pe", lambda e, h=h: e.matmul(p3v[:, h, :], lhsT=Ln[:, h, :], rhs=Qb[:, h, :], start=True, stop=True),
                                 reads=[hLn, hQb], writes=[hp3])
                        tt("dve", Qb[:], p3v, Qb[:], ALU.add, [hp3, hQb], [hQb])
                        cur, nxt = nxt, cur
                    Lak, hLak = gm["Lak"]
                    Xb, hXb = gm["Xb"]
                    SAb, hSAb = gm["SAb"]
                    px, hpx = nextP()
                    pxv = px[0:64, :].rearrange("p (h t) -> p h t", h=16)
                    for h in HORD:
                        cs_ = slice((h % 2) * 64, (h % 2) * 64 + 64)
                        S.op("pe", lambda e, h=h: e.matmul(pxv[:, h, :], lhsT=Kh[hrows(h), h // 2, ts], rhs=Sb[hrows(h), h // 2, :], start=True, stop=False),
                             reads=[hKh, hSb], writes=[hpx])
                        S.op("pe", lambda e, h=h, cs_=cs_: e.matmul(pxv[:, h, :], lhsT=Lak[:, h, :], rhs=Vt[:, s, h // 2, cs_], start=False, stop=True),
                             reads=[hLak, hVt], writes=[hpx])
                    actf(Xb[:], pxv, AF.Copy, [hpx], [hXb])
                    psa, hpsa = nextP()
                    psav = psa[0:64, :].rearrange("p (h t) -> p h t", h=16)
                    for h in HORD:
                        S.op("pe", lambda e, h=h: e.matmul(psav[:, h, :], lhsT=Qb[:, h, :], rhs=Xb[:, h, :], start=True, stop=True),
                             reads=[hQb, hXb], writes=[hpsa])
                    actf(SAb[:], psav, AF.Copy, [hpsa], [hSAb], scale=-1.0)
                    if own:
                        Mrb, hMrb = gm["Mrb"]
                        Mrk, hMrk = gm["Mrk"]
                        py, hpy = nextP()
                        pyv = py[0:64, :].rearrange("p (h t) -> p h t", h=16)
                        for h in HORD:
                            cs_ = slice((h % 2) * 64, (h % 2) * 64 + 64)
                            S.op("pe", lambda e, h=h: e.matmul(pyv[:, h, :], lhsT=Rh[hrows(h), h // 2, ts], rhs=Sb[hrows(h), h // 2, :], start=True, stop=False),
                                 reads=[hRh, hSb], writes=[hpy])
                            S.op("pe", lambda e, h=h: e.matmul(pyv[:, h, :], lhsT=Mrb[:, h, :], rhs=SAb[:, h, :], start=False, stop=False),
                                 reads=[hMrb, hSAb], writes=[hpy])
                            S.op("pe", lambda e, h=h, cs_=cs_: e.matmul(pyv[:, h, :], lhsT=Mrk[:, h, :], rhs=Vt[:, s, h // 2, cs_], start=False, stop=True),
                                 reads=[hMrk, hVt], writes=[hpy])
                        actf(ysb[:], pyv, AF.Copy, [hpy], [hysb])
                        S.op("dve", lambda e: e.tensor_reduce(out=gst[:, :, 0:1], in_=ysb[:], axis=AX.X, op=ALU.add), reads=[hysb], writes=[hgst])
                        S.op("dve", lambda e: e.tensor_scalar(out=gst[:, :, 0:1], in0=gst[:, :, 0:1], scalar1=1.0 / 64, scalar2=None, op0=ALU.mult), reads=[hgst], writes=[hgst])
                        tt("dve", yc[:], ysb[:], gst[:, :, 0:1].broadcast_to([64, 16, 64]), ALU.subtract, [hysb, hgst], [hyc])
                        actf(ysq[:], yc[:], AF.Square, [hyc], [hysq])
                        S.op("dve", lambda e: e.tensor_reduce(out=gst[:, :, 1:2], in_=ysq[:], axis=AX.X, op=ALU.add), reads=[hysq], writes=[hgst])
                        S.op("dve", lambda e: e.tensor_scalar(out=gst[:, :, 1:2], in0=gst[:, :, 1:2], scalar1=1.0 / 64, scalar2=64e-5, op0=ALU.mult, op1=ALU.add), reads=[hgst], writes=[hgst])
                        actf(gst[:, :, 2:3], gst[:, :, 1:2], AF.Sqrt, [hgst], [hgst])
                        S.op("dve", lambda e: e.reciprocal(out=gst[:, :, 3:4], in_=gst[:, :, 2:3]), reads=[hgst], writes=[hgst])
                        tt("dve", yc[:], yc[:], gst[:, :, 3:4].broadcast_to([64, 16, 64]), ALU.mult, [hyc, hgst], [hyc])
                        pt2, hpt2 = nextP()
                        for m in range(8):
                            S.op("pe", lambda e, m=m: e.transpose(pt2[:, m * 64:(m + 1) * 64], yc[:, 2 * m:2 * m + 2, :].rearrange("t h i -> t (h i)"), ident32[0:64, 0:64]),
                                 reads=[hyc, c.hcn], writes=[hpt2])
                        S.op("dve", lambda e: e.tensor_copy(out=yf[:, :, ts], in_=pt2[:, 0:512].rearrange("p (m t) -> p m t", m=8)), reads=[hpt2], writes=[hyf])
                    pst, hpst = nextP()
                    for h in HORD:
                        cs_ = slice((h % 2) * 64, (h % 2) * 64 + 64)
                        o_ = pst[hrows(h), (h // 2) * 64:(h // 2) * 64 + 64]
                        S.op("pe", lambda e, h=h, cs_=cs_, o_=o_: e.matmul(o_, lhsT=BGt[:, s, h // 2, cs_], rhs=SAb[:, h, :], start=True, stop=False),
                             reads=[hBGt, hSAb], writes=[hpst])
                        S.op("pe", lambda e, h=h, cs_=cs_, o_=o_: e.matmul(o_, lhsT=QGt[:, s, h // 2, cs_], rhs=Vt[:, s, h // 2, cs_], start=False, stop=True),
                             reads=[hQGt, hVt], writes=[hpst])
                    tt("dve", S32[:], S32[:], sm[:, :, s, 2:3].broadcast_to([128, 8, 64]), ALU.mult, [hS32, hsm], [hS32])
                    tt("dve", S32[:], S32[:], pst[:, 0:512].rearrange("p (m i) -> p m i", m=8), ALU.add, [hS32, hpst], [hS32])
                    actf(Sb[:], S32[:], AF.Copy, [hS32], [hSb])
                if own:
                    gg, hgg = F["gg"]
                    bon, hbon = F["bon"]
                    tt("dve", yf[:], yf[:], vb(V_GNW), ALU.mult, [hyf, c.hvec], [hyf])
                    tt("pool", yf[:], yf[:], vb(V_GNB), ALU.add, [hyf, c.hvec], [hyf])
                    tt("pool", yf[:], yf[:], bon[:], ALU.add, [hyf, hbon], [hyf])
                    tt("dve", yrb[:], yf[:], gg[:], ALU.mult, [hyf, hgg], [hyrb])
                    jo = j - OWN0
                    S.dma("sp", YR_d[jo].rearrange("p (m t) -> p m t", m=8), yrb[:], hyrb, reads=[hyrb], writes=[hYR[jo]])
            S.barrier()


    def tt_(eng, out, i0, i1, op, reads, writes):
        S.op(eng, lambda e: e.tensor_tensor(out=out, in0=i0, in1=i1, op=op), reads=reads, writes=writes)

    def act_(out, in_, func, reads, writes, **kw):
        S.op("act", lambda e: e.activation(out=out, in_=in_, func=func, **kw), reads=reads, writes=writes)

    def phase_C():
        with contextlib.ExitStack() as es:
            T, P = mk(es)
            c = load_consts(T)
            vec = c.vec
            bones = c.cb[:, C_BO:C_BO + 128]
            identb = c.cb[:, C_ID:C_ID + 128]
            qk, hqk = T("qk", [128, 10, 128])
            sqb, hsqb = T("sqb", [128, 8, 128], BF16)
            t1, ht1 = T("t1", [128, 8, 128])
            qT, hqT = T("qT", [128, 8, 128], BF16)
            knb, hknb = T("knb", [128, 128], BF16)
            vbf, hvbf = T("vbf", [128, 128], BF16)
            kd = [T("kd%d" % i, [128, 2, 128], BF16) for i in range(2)]
            Vk = [T("Vk%d" % i, [128, 128], BF16) for i in range(2)]
            e1, he1 = T("e1", [128, 4, 128], BF16)
            PTb, hPTb = T("PTb", [128, 4, 128], BF16)
            onesb, honesb = T("onesb", [128, 64], BF16)
            S.op("pool", lambda e: e.memset(onesb[:], 1.0), writes=[honesb])
            esk, hesk = T("esk", [128, 8])
            act_(esk[:], vec[:, V_SK:V_SK + 8], AF.Exp, [c.hvec], [hesk])
            den, hden = T("den", [128, 4, 128])
            ya, hya = T("ya", [128, 8, 128], BF16)
            PS1, hPS1 = P("PS1", [128, 1024])
            psc = [P("psc%d" % i, [128, 512]) for i in range(2)]
            po, hpo = P("po", [128, 512])
            pdn, hpdn = P("pdn", [128, 512])
            pvT, hpvT = P("pvT", [128, 128], BF16)

            def norm_rows(src, n, gcol, dst, hdst):
                act_(sqb[:, 0:n, :], src, AF.Square, [hqk], [hsqb])
                for hh in range(0, n, 4):
                    w_ = min(4, n - hh)
                    S.op("pe", lambda e, hh=hh, w_=w_: e.matmul(PS1[:, hh * 128:(hh + w_) * 128], lhsT=bones, rhs=sqb[:, hh:hh + w_, :], start=True, stop=True),
                         reads=[c.hcb, hsqb], writes=[hPS1])
                pv = PS1[:, 0:n * 128].rearrange("p (m t) -> p m t", m=n)
                S.op("dve", lambda e: e.tensor_scalar(out=t1[:, 0:n, :], in0=pv, scalar1=1.0 / 64, scalar2=1e-6, op0=ALU.mult, op1=ALU.add),
                     reads=[hPS1], writes=[ht1])
                act_(t1[:, 0:n, :], t1[:, 0:n, :], AF.Sqrt, [ht1], [ht1])
                S.op("dve", lambda e: e.reciprocal(out=t1[:, 0:n, :], in_=t1[:, 0:n, :]), reads=[ht1], writes=[ht1])
                tt_("dve", t1[:, 0:n, :], t1[:, 0:n, :], src, ALU.mult, [ht1, hqk], [ht1])
                S.op("dve", lambda e: e.tensor_scalar(out=dst, in0=t1[:, 0:n, :], scalar1=vec[:, gcol:gcol + 1], scalar2=None, op0=ALU.mult),
                     reads=[ht1, c.hvec], writes=[hdst])

            def kv_prep(slot):
                kd_, hkd_ = kd[slot]
                Vk_, hVk_ = Vk[slot]
                norm_rows(qk[:, 8:9, :], 1, V_KG, knb[:].unsqueeze(1), hknb)
                for g in range(2):
                    S.op("pe", lambda e, g=g: e.matmul(PS1[:, 512 + g * 128:512 + (g + 1) * 128], lhsT=c.cb[:, C_SEL + g * 128:C_SEL + (g + 1) * 128], rhs=knb[:], start=True, stop=True),
                         reads=[c.hcb, hknb], writes=[hPS1])
                act_(kd_[:], PS1[:, 512:768].rearrange("p (g t) -> p g t", g=2), AF.Copy, [hPS1], [hkd_])
                act_(vbf[:], qk[:, 9, :], AF.Copy, [hqk], [hvbf])
                S.op("pe", lambda e: e.transpose(pvT[:], vbf[:], identb), reads=[hvbf, c.hcb], writes=[hpvT])
                S.op("dve", lambda e: e.tensor_copy(out=Vk_[:], in_=pvT[:]), reads=[hpvT], writes=[hVk_])

            S.dma("sp", qk[:, 8:10, :], QKV_d[0].rearrange("p (c t) -> p c t", c=10)[:, 8:10, :], hqk, reads=[hQKV[0]], writes=[hqk])
            kv_prep(0)
            for jo in range(NO):
                ja = jo + 1
                S.dma("sp", qk[:], QKV_d[ja].rearrange("p (c t) -> p c t", c=10), hqk, reads=[hQKV[ja]], writes=[hqk])
                kv_prep(ja % 2)
                norm_rows(qk[:, 0:8, :], 8, V_QG, qT[:], hqT)
                si = 0
                for g in range(2):
                    for par in range(2):
                        rows = slice(par * 64, par * 64 + 64)
                        for wi, slot in enumerate(((ja - 1) % 2, ja % 2)):
                            kd_, hkd_ = kd[slot]
                            Vk_, hVk_ = Vk[slot]
                            ps_, hps_ = psc[si % 2]
                            si += 1
                            S.op("pe", lambda e, ps_=ps_, kd_=kd_: e.matmul(ps_[:], lhsT=kd_[rows, g, :], rhs=qT[rows, 4 * g:4 * g + 4, :], start=True, stop=True),
                                 reads=[hkd_, hqT], writes=[hps_])
                            act_(e1[:], ps_[:].rearrange("p (h t) -> p h t", h=4), AF.Exp, [hps_], [he1], scale=0.125)
                            mcol = C_MC if wi == 1 else (C_MPF if jo == 0 else C_MP)
                            mk_ = c.cb[:, mcol:mcol + 128].unsqueeze(1).broadcast_to([128, 4, 128])
                            tt_("pool", PTb[:], e1[:], mk_, ALU.mult, [he1, c.hcb], [hPTb])
                            S.op("pe", lambda e, Vk_=Vk_, wi=wi: e.matmul(po[rows, :], lhsT=Vk_[:, g * 64:(g + 1) * 64], rhs=PTb[:].rearrange("p h t -> p (h t)"), start=(wi == 0), stop=(wi == 1)),
                                 reads=[hVk_, hPTb], writes=[hpo])
                            S.op("pe", lambda e, wi=wi: e.matmul(pdn[rows, :], lhsT=onesb[:], rhs=PTb[:].rearrange("p h t -> p (h t)"), start=(wi == 0), stop=(wi == 1)),
                                 reads=[honesb, hPTb], writes=[hpdn])
                    tt_("dve", den[:], pdn[:].rearrange("p (h t) -> p h t", h=4), esk[:, 4 * g:4 * g + 4].unsqueeze(2).broadcast_to([128, 4, 128]), ALU.add, [hpdn, hesk], [hden])
                    S.op("dve", lambda e: e.reciprocal(out=den[:], in_=den[:]), reads=[hden], writes=[hden])
                    tt_("dve", ya[:, 4 * g:4 * g + 4, :], po[:].rearrange("p (h t) -> p h t", h=4), den[:], ALU.mult, [hpo, hden], [hya])
                S.dma("sp", YA_d[jo].rearrange("p (m t) -> p m t", m=8), ya[:], hya, reads=[hya], writes=[hYA[jo]])
            S.barrier()

    def phase_D():
        with contextlib.ExitStack() as es:
            T, P = mk(es)
            c = load_consts(T)
            wo, hwo = T("wo", [128, DC, D], BF16)
            for k in range(DC):
                S.dma("pool", wo[:, k, :], wout_d[k * 128:(k + 1) * 128, :], hwo, writes=[hwo])
            yr, hyr = T("yr", [128, 8, 128], BF16)
            ya, hya = T("ya", [128, 8, 128], BF16)
            xt, hxt = T("xt", [128, D])
            hm, hhm = T("hm", [128, D])
            st, hst = T("st", [128, 4])
            xs, hxs = T("xs", [128, D], BF16)
            uT, huT = T("uT", [128, DC, 128], BF16)
            pT, hpT = P("pT", [128, DC, 128], BF16)
            pp = [P("pp%d" % i, [128, 512]) for i in range(4)]
            for jo in range(NO):
                j = OWN0 + jo
                S.dma("sp", yr[:], YR_d[jo].rearrange("p (m t) -> p m t", m=8), hyr, reads=[hYR[jo]], writes=[hyr])
                S.dma("sp", ya[:], YA_d[jo].rearrange("p (m t) -> p m t", m=8), hya, reads=[hYA[jo]], writes=[hya])
                S.dma("sp", xt[:], xin[j * 128:(j + 1) * 128, :], hxt, writes=[hxt])
                for cg in range(4):
                    p_, hp_ = pp[cg]
                    for kc in range(DC):
                        src, hsrc = (yr, hyr) if kc < 8 else (ya, hya)
                        S.op("pe", lambda e, p_=p_, kc=kc, cg=cg, src=src: e.matmul(p_[:], lhsT=src[:, kc % 8, :], rhs=wo[:, kc, cg * 512:(cg + 1) * 512], start=(kc == 0), stop=(kc == DC - 1)),
                             reads=[hsrc, hwo], writes=[hp_])
                    tt_("dve", hm[:, cg * 512:(cg + 1) * 512], p_[:], xt[:, cg * 512:(cg + 1) * 512], ALU.add, [hp_, hxt], [hhm])
                S.dma("sp", HM_d[jo], hm[:], hhm, reads=[hhm], writes=[hHM[jo]])
                rmsnorm_T(c, T, hm, hhm, V_G2, xs, hxs, st, hst, xs, hxs, pT, hpT, uT, huT)
                S.dma("sp", XNT_d[jo].rearrange("p (k t) -> p k t", k=DC), uT[:], huT, reads=[huT], writes=[hXNT[jo]])
            S.barrier()

    def phase_E1():
        with contextlib.ExitStack() as es:
            T, P = mk(es)
            c = load_consts(T)
            pq, hpq = T("pq", [128, DC, D], BF16)
            for k in range(DC):
                S.dma("pool", pq[:, k, :], pq_d[k * 128:(k + 1) * 128, :], hpq, writes=[hpq])
            skn, hskn = T("skn", [128, 16, 128])
            S.dma("sp", skn[:], sk_d.rearrange("c n d -> n c d"), hskn, writes=[hskn])
            skT, hskT = T("skT", [128, 16, 128])
            pbig = [P("pb%d" % i, [128, 16, 128]) for i in range(2)]
            p0, hp0 = pbig[0]
            for hc in range(16):
                S.op("pe", lambda e, hc=hc: e.transpose(p0[:, hc, :], skn[:, hc, :], c.cn[:, C_ID:C_ID + 128]), reads=[hskn, c.hcn], writes=[hp0])
            S.op("dve", lambda e: e.tensor_copy(out=skT[:], in_=p0[:]), reads=[hp0], writes=[hskT])
            xnT, hxnT = T("xnT", [128, DC, 128], BF16)
            qTs, hqTs = T("qTs", [128, 16, 128])
            sall, hsall = T("sall", [128, 16, 128])
            tops, htops = T("tops", [128, 16, 16])
            wk, hwk = T("wk", [128, 128])
            cand, hcand = T("cand", [128, 8, 256])
            wk2, hwk2 = T("wk2", [128, 256])
            ctop, hctop = T("ctop", [128, 8, 24])
            sm, hsm = T("sm", [128, 4, 8])
            ez, hez = T("ez", [128, 8, 16])
            stt, hstt = T("stt", [128, 2048 + 8])
            for jo in range(NO):
                S.dma("sp", xnT[:], XNT_d[jo].rearrange("p (k t) -> p k t", k=DC), hxnT, reads=[hXNT[jo]], writes=[hxnT])
                pa_, hpa_ = pbig[0]
                for hc in range(16):
                    for kc in range(DC):
                        S.op("pe", lambda e, hc=hc, kc=kc: e.matmul(pa_[:, hc, :], lhsT=pq[:, kc, hc * 128:(hc + 1) * 128], rhs=xnT[:, kc, :], start=(kc == 0), stop=(kc == DC - 1)),
                             reads=[hpq, hxnT], writes=[hpa_])
                act_(qTs[:, 0:8, :], pa_[:, 0:8, :], AF.Copy, [hpa_], [hqTs])
                S.op("dve", lambda e: e.tensor_copy(out=qTs[:, 8:16, :], in_=pa_[:, 8:16, :]), reads=[hpa_], writes=[hqTs])
                pb_, hpb_ = pbig[1]
                for hc in range(16):
                    S.op("pe", lambda e, hc=hc: e.matmul(pb_[:, hc, :], lhsT=qTs[:, hc, :], rhs=skT[:, hc, :], start=True, stop=True),
                         reads=[hqTs, hskT], writes=[hpb_])
                act_(sall[:, 0:8, :], pb_[:, 0:8, :], AF.Copy, [hpb_], [hsall])
                S.op("dve", lambda e: e.tensor_copy(out=sall[:, 8:16, :], in_=pb_[:, 8:16, :]), reads=[hpb_], writes=[hsall])
                for hc in range(16):
                    S.op("dve", lambda e, hc=hc: e.max(out=tops[:, hc, 0:8], in_=sall[:, hc, :]), reads=[hsall], writes=[htops])
                    S.op("dve", lambda e, hc=hc: e.match_replace(out=wk[:], in_to_replace=tops[:, hc, 0:8], in_values=sall[:, hc, :], imm_value=-1e30),
                         reads=[hsall, htops], writes=[hwk])
                    S.op("dve", lambda e, hc=hc: e.max(out=tops[:, hc, 8:16], in_=wk[:]), reads=[hwk], writes=[htops])
                t4 = tops[:].rearrange("p (h c) k -> p h c k", c=2)
                tt_("pool", cand[:].rearrange("p h (i j) -> p h i j", i=16), t4[:, :, 0, :].unsqueeze(3).broadcast_to([128, 8, 16, 16]),
                    t4[:, :, 1, :].unsqueeze(2).broadcast_to([128, 8, 16, 16]), ALU.add, [htops], [hcand])
                for h in range(8):
                    S.op("dve", lambda e, h=h: e.max(out=ctop[:, h, 0:8], in_=cand[:, h, :]), reads=[hcand], writes=[hctop])
                    S.op("dve", lambda e, h=h: e.match_replace(out=wk2[:], in_to_replace=ctop[:, h, 0:8], in_values=cand[:, h, :], imm_value=-1e30),
                         reads=[hcand, hctop], writes=[hwk2])
                    S.op("dve", lambda e, h=h: e.max(out=ctop[:, h, 8:16], in_=wk2[:]), reads=[hwk2], writes=[hctop])
                    S.op("dve", lambda e, h=h: e.match_replace(out=wk2[:], in_to_replace=ctop[:, h, 8:16], in_values=wk2[:], imm_value=-1e30),
                         reads=[hwk2, hctop], writes=[hwk2])
                    S.op("dve", lambda e, h=h: e.max(out=ctop[:, h, 16:24], in_=wk2[:]), reads=[hwk2], writes=[hctop])
                thr = sm[:, 0, :]
                tt_("dve", thr.unsqueeze(2), ctop[:, :, 15:16], ctop[:, :, 16:17], ALU.add, [hctop], [hsm])
                S.op("dve", lambda e: e.tensor_scalar(out=thr, in0=thr, scalar1=0.5, scalar2=None, op0=ALU.mult), reads=[hsm], writes=[hsm])
                tt_("dve", ez[:], ctop[:, :, 0:16], thr.unsqueeze(2).broadcast_to([128, 8, 16]), ALU.subtract, [hctop, hsm], [hez])
                act_(ez[:], ez[:], AF.Exp, [hez], [hez])
                S.op("dve", lambda e: e.tensor_reduce(out=sm[:, 1, :].unsqueeze(2), in_=ez[:], axis=AX.X, op=ALU.add), reads=[hez], writes=[hsm])
                S.op("dve", lambda e: e.reciprocal(out=stt[:, 2048:2056], in_=sm[:, 1, :]), reads=[hsm], writes=[hstt])
                act_(sm[:, 2, :], sm[:, 1, :], AF.Ln, [hsm], [hsm])
                tt_("dve", sm[:, 3, :], sm[:, 2, :], thr, ALU.add, [hsm], [hsm])
                s4 = sall[:].rearrange("p (h c) n -> p h c n", c=2)
                st4 = stt[:, 0:2048].rearrange("p (h c n) -> p h c n", h=8, c=2)
                tt_("dve", st4[:, :, 0, :], s4[:, :, 0, :], sm[:, 3, :].unsqueeze(2).broadcast_to([128, 8, 128]), ALU.subtract, [hsall, hsm], [hstt])
                S.op("pool", lambda e: e.tensor_copy(out=st4[:, :, 1, :], in_=s4[:, :, 1, :]), reads=[hsall], writes=[hstt])
                S.dma("sp", ST_d[jo], stt[:], hstt, reads=[hstt], writes=[hST[jo]])
            S.barrier()

    def phase_E2(G=4):
        with contextlib.ExitStack() as es:
            T, P = mk(es)
            c = Ctx()
            c.cn, c.hcn = T("id32", [128, 128])
            S.dma("sp", c.cn[:], cn_d[:, C_ID:C_ID + 128], c.hcn, writes=[c.hcn])
            c.cb, c.hcb = T("idbf", [128, 128], BF16)
            S.dma("pool", c.cb[:], cn_d[:, C_ID:C_ID + 128], c.hcb, writes=[c.hcb])
            identb = c.cb[:]
            ident32 = c.cn[:]
            G = min(G, NO)
            xn = [T("xn%d" % i, [128, DC, 128], BF16) for i in range(G)]
            stg = [T("stg%d" % i, [128, 2048 + 8]) for i in range(G)]
            acc = [T("acc%d" % i, [128, D]) for i in range(G)]
            dn32 = [T("dn32_%d" % i, [128, D]) for i in range(2)]
            up32 = [T("up32_%d" % i, [128, D]) for i in range(1)]
            upb = [T("upb%d" % i, [128, 4, D], BF16) for i in range(2)]
            dTs = [T("dT%d" % i, [128, DC, 512], BF16) for i in range(2)]
            EEh = [T("EEh%d" % i, [128, 4, 512]) for i in range(3)]
            Wh = [T("Wh%d" % i, [128, 8, 512], BF16) for i in range(1)]
            geT = [T("geT%d" % i, [128, 512]) for i in range(2)]
            GTb = [T("GTb%d" % i, [128, 4, 128], BF16) for i in range(2)]
            ptr = [P("ptr%d" % i, [128, 4, 128]) for i in range(2)]
            phid = [P("phid%d" % i, [128, 4, 128]) for i in range(2)]
            pwt, hpwt = P("pwt", [128, 4, 128])
            pouts = [P("pout%d" % i, [128, 512]) for i in range(3)]
            pocnt = [0]
            NCH = NE // 512
            import os as _os
            _V = _os.environ.get("KV", "")
            if "n" in _V:
                NCH = 8
            NQ = NCH * 4
            pcount = [0]
            for g0 in range(0, NO, G):
                tiles = list(range(g0, min(NO, g0 + G)))
                nt = len(tiles)
                for i, jo in enumerate(tiles):
                    S.dma("sp", xn[i][0][:], XNT_d[jo].rearrange("p (k t) -> p k t", k=DC), xn[i][1], reads=[hXNT[jo]], writes=[xn[i][1]])
                    S.dma("sp", stg[i][0][:], ST_d[jo], stg[i][1], reads=[hST[jo]], writes=[stg[i][1]])
                    S.dma("sp", acc[i][0][:], HM_d[jo], acc[i][1], reads=[hHM[jo]], writes=[acc[i][1]])

                def load_dn(q):
                    if q < NQ:
                        d_, hd_ = dn32[q % 2]
                        S.dma("sp", d_[:], pd_d[q * 128:(q + 1) * 128, :], hd_, writes=[hd_])

                def load_up(q):
                    if q < NQ:
                        u_, hu_ = up32[0]
                        S.dma("sp", u_[:], pu_d[q * 128:(q + 1) * 128, :], hu_, writes=[hu_])

                def prep_q(q):
                    ec, et = divmod(q, 4)
                    d_, hd_ = dn32[q % 2]
                    dT_, hdT_ = dTs[ec % 2]
                    for k4 in range(4):
                        pt_, hpt_ = ptr[pcount[0] % 2]
                        pcount[0] += 1
                        for kk in range(4):
                            kc = k4 * 4 + kk
                            S.op("pe", lambda e, kk=kk, kc=kc, pt_=pt_: e.transpose(pt_[:, kk, :], d_[:, kc * 128:(kc + 1) * 128], ident32),
                                 reads=[hd_, c.hcn], writes=[hpt_])
                        S.op("dve", lambda e, k4=k4, pt_=pt_: e.tensor_copy(out=dT_[:, k4 * 4:k4 * 4 + 4, et * 128:(et + 1) * 128], in_=pt_[:]),
                             reads=[hpt_], writes=[hdT_])
                    load_dn(q + 2)
                    u_, hu_ = up32[0]
                    ub_, hub_ = upb[ec % 2]
                    S.op("dve", lambda e: e.tensor_copy(out=ub_[:, et, :], in_=u_[:]), reads=[hu_], writes=[hub_])
                    load_up(q + 1)

                def P1(gi):
                    ec, i = divmod(gi, nt)
                    xn_, hxn_ = xn[i]
                    ph_, hph_ = phid[gi % 2]
                    dT_, hdT_ = dTs[ec % 2]
                    for et in range(4):
                        for kc in range(DC):
                            S.op("pe", lambda e, kc=kc, et=et: e.matmul(ph_[:, et, :], lhsT=dT_[:, kc, et * 128:(et + 1) * 128], rhs=xn_[:, kc, :], start=(kc == 0), stop=(kc == DC - 1)),
                                 reads=[hxn_, hdT_], writes=[hph_])

                def A1(gi):
                    ec, i = divmod(gi, nt)
                    st_, hst_ = stg[i]
                    ph_, hph_ = phid[gi % 2]
                    ge_, hge_ = geT[gi % 2]
                    st4 = st_[:, 0:2048].rearrange("p (h c n) -> p h c n", h=8, c=2)
                    for half in range(2):
                        ee_, hee_ = EEh[(gi * 2 + half) % 3]
                        for h in range(4):
                            hh = half * 4 + h
                            for a in range(4):
                                n1 = ec * 4 + a
                                act_(ee_[:, h, a * 128:(a + 1) * 128], st4[:, hh, 1, :], AF.Exp, [hst_], [hee_], bias=st4[:, hh, 0, n1:n1 + 1], scale=1.0)
                    act_(ge_[:], ph_[:].rearrange("p a t -> p (a t)"), AF.Gelu, [hph_], [hge_])

                def D1(gi):
                    ec, i = divmod(gi, nt)
                    st_, hst_ = stg[i]
                    wh_, hwh_ = Wh[0]
                    for half in range(2):
                        ee_, hee_ = EEh[(gi * 2 + half) % 3]
                        for h in range(4):
                            hh = half * 4 + h
                            S.op("dve", lambda e, h=h, hh=hh, ee_=ee_: e.scalar_tensor_tensor(
                                out=wh_[:, hh, :], in0=ee_[:, h, :], scalar=st_[:, 2048 + hh:2049 + hh],
                                in1=ee_[:, h, :], op0=ALU.is_ge, op1=ALU.mult), reads=[hee_, hst_], writes=[hwh_])

                def P2(gi):
                    wh_, hwh_ = Wh[0]
                    for et in range(4):
                        for h in range(8):
                            S.op("pe", lambda e, et=et, h=h: e.matmul(pwt[:, et, :], lhsT=wh_[:, h, et * 128:(et + 1) * 128], rhs=identb, start=(h == 0), stop=(h == 7)),
                                 reads=[hwh_, c.hcb], writes=[hpwt])

                def D2(gi):
                    ge_, hge_ = geT[gi % 2]
                    gt_, hgt_ = GTb[gi % 2]
                    tt_("dve", gt_[:].rearrange("p a t -> p (a t)"), ge_[:], pwt[:].rearrange("p a t -> p (a t)"), ALU.mult, [hge_, hpwt], [hgt_])

                def P3D3(gi):
                    ec, i = divmod(gi, nt)
                    u_, hu_ = upb[ec % 2]
                    gt_, hgt_ = GTb[gi % 2]
                    ac_, hac_ = acc[i]
                    for dg in range(4):
                        po_, hpo_ = pouts[pocnt[0] % 3]
                        pocnt[0] += 1
                        for et in range(4):
                            S.op("pe", lambda e, dg=dg, et=et, po_=po_: e.matmul(po_[:], lhsT=gt_[:, et, :], rhs=u_[:, et, dg * 512:(dg + 1) * 512], start=(et == 0), stop=(et == 3)),
                                 reads=[hgt_, hu_], writes=[hpo_])
                        tt_("dve", ac_[:, dg * 512:(dg + 1) * 512], ac_[:, dg * 512:(dg + 1) * 512], po_[:], ALU.add, [hac_, hpo_], [hac_])

                NG = NCH * nt
                load_dn(0)
                load_dn(1)
                load_up(0)
                for q in range(4):
                    prep_q(q)
                P1(0)
                A1(0)
                D1(0)
                for gi in range(NG):
                    ec, i = divmod(gi, nt)
                    if ec + 1 < NCH:
                        for et in range(i * 4 // nt, (i + 1) * 4 // nt):
                            prep_q((ec + 1) * 4 + et)
                    if gi + 1 < NG:
                        P1(gi + 1)
                        A1(gi + 1)
                    P2(gi)
                    D2(gi)
                    if gi + 1 < NG:
                        D1(gi + 1)
                    P3D3(gi)
                for i, jo in enumerate(tiles):
                    S.dma("sp", yout[jo * 128:(jo + 1) * 128, :], acc[i][0][:], acc[i][1], reads=[acc[i][1]], writes=[hOUT[jo]])
            S.barrier()

    ph = {"A": phase_A, "B": phase_B, "C": phase_C, "D": phase_D, "E": phase_E1, "F": phase_E2}
    ctx = dict(nc=nc, S=S, mk=mk, load_consts=load_consts, rmsnorm_T=rmsnorm_T, locals=locals())
    for p in phases:
        if p in ph:
            ph[p]()
    return nc, ctx


def _colT(v, n):
    buf = np.zeros(n * 128, np.float32)
    buf[:v.size] = v.reshape(-1)
    return buf.reshape(n, 128).T


def make_vecs(inp):
    vecs = np.zeros((128, NV), np.float32)
    vecs[:, V_G1:V_G1 + 16] = _colT(inp["norm1_g"][0], 16)
    vecs[:, V_MU:V_MU + 27] = _colT(inp["shift_mu"][0], 27)
    for col, key in ((V_W0, "w0"), (V_A0, "a0"), (V_KK, "k_k"), (V_KA, "k_a"), (V_GNW, "gn_w"),
                     (V_GNB, "gn_b"), (V_RK, "r_k")):
        vecs[:, col:col + 8] = _colT(inp[key][0], 8)
    vecs[:, V_QG] = np.tile(inp["q_gain"][0], 2)
    vecs[:, V_KG] = np.tile(inp["k_gain"][0], 2)
    sk = inp["sinks"][0]
    for hp in range(8):
        vecs[0:64, V_SK + hp] = sk[2 * hp]
        vecs[64:128, V_SK + hp] = sk[2 * hp + 1]
    vecs[:, V_G2:V_G2 + 16] = _colT(inp["norm2_g"][0], 16)
    return vecs


def make_consts(first_half):
    cn = np.zeros((128, NCN), np.float32)
    cn[:, C_ID:C_ID + 128] = np.eye(128, dtype=np.float32)
    bo = np.zeros((128, 128), np.float32)
    bo[0:64, 0:64] = 1
    bo[64:, 64:] = 1
    cn[:, C_BO:C_BO + 128] = bo
    s = np.arange(64)[:, None]
    t = np.arange(64)[None, :]
    cn[0:64, C_MU:C_MU + 64] = (s < t)
    cn[0:64, C_MUI:C_MUI + 64] = (s <= t)
    cn[0:64, C_ML:C_ML + 64] = (s > t)
    s = np.arange(128)[:, None]
    q = np.arange(128)[None, :]
    cn[:, C_MC:C_MC + 128] = (s <= q)
    cn[:, C_MP:C_MP + 128] = (s > q)
    mpf = (s > q)
    if first_half:
        mpf = mpf & (s >= 112)
    cn[:, C_MPF:C_MPF + 128] = mpf
    for g in range(2):
        sel = np.zeros((128, 128), np.float32)
        for m in range(128):
            sel[g * 64 + (m % 64), m] = 1
        cn[:, C_SEL + g * 128:C_SEL + (g + 1) * 128] = sel
    return cn


_NC_CACHE = {}


def kernel(**inputs):
    inp = {k: np.asarray(v) for k, v in inputs.items()}
    x = inp["x"].astype(np.float32, copy=False)
    B, SEQ, _ = x.shape
    NB, OWN0 = 33, 17
    if "nc" not in _NC_CACHE:
        _NC_CACHE["nc"] = build(NB=NB, OWN0=OWN0, phases="ABCDEF")[0]
    nc = _NC_CACHE["nc"]
    f = lambda k: np.ascontiguousarray(inp[k][0], dtype=np.float32)
    shared = dict(w_in=f("w_in"), vecs=make_vecs(inp), w_up=f("w_up"), a_up=f("a_up"), g_up=f("g_up"),
                  w_out=f("w_out"), peer_query=f("peer_query"),
                  sub_keys=np.ascontiguousarray(inp["peer_sub_keys"][0].reshape(16, 128, 128), dtype=np.float32),
                  peer_down=f("peer_down"), peer_up=f("peer_up"))
    meta = inp["meta_tokens"].astype(np.float32, copy=False)
    cn = [make_consts(False), make_consts(True)]
    in_maps = []
    for c in range(8):
        b, s = c // 2, c % 2
        loc = np.zeros((NB * 128, D), np.float32)
        if s == 0:
            loc[16 * 128 + 112:17 * 128] = meta
            loc[17 * 128:] = x[b, :2048]
        else:
            loc[112:128] = meta
            loc[128:] = x[b]
        in_maps.append(dict(xin=loc, consts=cn[1 if s == 0 else 0], **shared))
    res = run_bass_kernel_spmd(nc, in_maps, core_ids=list(range(8)))
    out = np.empty((B, SEQ, D), np.float32)
    for c in range(8):
        b, s = c // 2, c % 2
        out[b, s * 2048:(s + 1) * 2048] = np.asarray(res.results[c]["yout"])
    return out
```

```python
import contextlib
import numpy as np
import concourse.bass as bass
import concourse.mybir as mybir
from concourse.bass_utils import run_bass_kernel_spmd

F32 = mybir.dt.float32
BF16 = mybir.dt.bfloat16
AF = mybir.ActivationFunctionType
ALU = mybir.AluOpType
AX = mybir.AxisListType

D = 2048
DC = 16
INC = 4640
RW0 = 1280
NRC = 27
NE = 16384
HD = 64

V_G1, V_MU, V_W0, V_A0, V_KK, V_KA, V_GNW, V_GNB, V_RK, V_QG, V_KG, V_SK, V_G2 = (
    0, 16, 43, 51, 59, 67, 75, 83, 91, 99, 100, 101, 109)
NV = 125
C_ID, C_BO, C_MU, C_MUI, C_ML, C_MC, C_MP, C_MPF, C_SEL = 0, 128, 256, 320, 384, 448, 576, 704, 832
NCN = 832 + 256


class H:
    __slots__ = ("name", "w", "r", "dsem", "dcnt")

    def __init__(self, name=""):
        self.name = name
        self.w = {}
        self.r = {}
        self.dsem = None
        self.dcnt = 0


class Sched:
    def __init__(self, nc, needed=None):
        self.nc = nc
        self.engs = {"pe": nc.tensor, "act": nc.scalar, "dve": nc.vector,
                     "pool": nc.gpsimd, "sp": nc.sync}
        self.esem = {k: nc.alloc_semaphore(name="es_" + k) for k in self.engs}
        self.seq = {k: 0 for k in self.engs}
        self.cnt = {k: 0 for k in self.engs}
        self.cntmap = {k: {} for k in self.engs}
        self.waited = {k: {} for k in self.engs}
        self.needed = needed
        self.rec = {k: set() for k in self.engs}
        self.sems = {}
        self.dcur = {}
        self._rec = None

    def _wait(self, e, evs):
        w = self.waited[e]
        for key, val in evs.items():
            if w.get(key, 0) >= val:
                continue
            if isinstance(key, str):
                self.rec[key].add(val)
                real = self.cntmap[key][val] if self.needed is not None else val
                self.engs[e].wait_ge(self.esem[key], real)
            else:
                self.engs[e].wait_ge(self.sems[key], val)
            w[key] = val

    @staticmethod
    def _merge(d, evs):
        for k, v in evs.items():
            if d.get(k, 0) < v:
                d[k] = v

    def _deps(self, reads, writes, own=None):
        evs = {}
        for h in reads:
            self._merge(evs, h.w)
        if own == "pe":
            evs.pop(own, None)
        ww = {}
        for h in writes:
            self._merge(ww, h.w)
            self._merge(ww, h.r)
        if own is not None:
            ww.pop(own, None)
        self._merge(evs, ww)
        return evs

    def _pe_mode(self, mode):
        if mode != getattr(self, "pe_mode", None):
            if self.seq["pe"] > 0:
                self._wait("pe", {"pe": self.seq["pe"]})
            self.pe_mode = mode

    def op(self, e, fn, reads=(), writes=()):
        if self._rec is not None:
            r = _RecEng()
            fn(r)
            name, args, kwargs = r.call
            self._rec.append(("op", e, name, args, kwargs, tuple(reads), tuple(writes)))
            return None
        self._wait(e, self._deps(reads, writes, own=e))
        inst = fn(_PEProxy(self) if e == "pe" else self.engs[e])
        self.seq[e] += 1
        n = self.seq[e]
        if self.needed is None or n in self.needed[e]:
            self.cnt[e] += 1
            inst.then_inc(self.esem[e], 1)
            self.cntmap[e][n] = self.cnt[e]
        ev = {e: n}
        for h in reads:
            self._merge(h.r, ev)
        for h in writes:
            h.w = dict(ev)
            h.r = {}
        return inst

    def mark(self):
        if self._rec is not None:
            self._rec.append(("mark",))

    def record(self, f):
        self._rec = []
        try:
            f()
        finally:
            rec, self._rec = self._rec, None
        return rec

    def play(self, items):
        for it in items:
            if it[0] == "op":
                _, e, name, args, kwargs, reads, writes = it
                self.op(e, lambda eng: getattr(eng, name)(*args, **kwargs), reads, writes)
            elif it[0] == "dma":
                _, q, out, in_, tile, reads, writes, kw = it
                self.dma(q, out, in_, tile, reads, writes, **kw)

    def play2(self, a, b):
        na, nb = len(a), len(b)
        ia = ib = 0
        CH = 32
        while ia < na or ib < nb:
            if ib >= nb or (ia < na and ia * nb <= ib * na):
                self.play(a[ia:ia + CH])
                ia += CH
            else:
                self.play(b[ib:ib + CH])
                ib += CH

    def dma(self, q, out, in_, tile, reads=(), writes=(), **kw):
        if self._rec is not None:
            self._rec.append(("dma", q, out, in_, tile, tuple(reads), tuple(writes), kw))
            return None
        if tile.dsem is None:
            tile.dsem = self.nc.alloc_semaphore(name="ds_%d" % len(self.sems))
            self.sems[tile.dsem.num] = tile.dsem
        deps = self._deps(reads, writes)
        if tile in writes and not tile.r and tile.w.get(tile.dsem.num, 0) == tile.dcnt and len(tile.w) == 1:
            deps.pop(tile.dsem.num, None)
        self._wait(q, deps)
        inst = self.engs[q].dma_start(out=out, in_=in_, **kw)
        tile.dcnt += 16
        inst.then_inc(tile.dsem, 16)
        self.dcur[tile.dsem.num] = tile.dcnt
        ev = {tile.dsem.num: tile.dcnt}
        for h in reads:
            self._merge(h.r, ev)
        for h in writes:
            h.w = dict(ev)
            h.r = {}
        return inst

    def barrier(self):
        evs = {k: self.seq[k] for k in self.engs if self.seq[k] > 0}
        evs.update(self.dcur)
        for e in self.engs:
            self._wait(e, evs)


class _RecEng:
    def __getattr__(self, name):
        def f(*args, **kwargs):
            self.call = (name, args, kwargs)
            return None
        return f


def _rnd(n):
    return 32 if n <= 32 else (64 if n <= 64 else 128)


class _PEProxy:
    def __init__(self, S):
        self.S = S

    def matmul(self, out, lhsT, rhs, **kw):
        fr = 1
        for d in lhsT.shape[1:]:
            fr *= d
        self.S._pe_mode(("mm", _rnd(lhsT.shape[0]), _rnd(fr), str(lhsT.dtype), lhsT.base_partition(), out.base_partition()))
        return self.S.nc.tensor.matmul(out, lhsT=lhsT, rhs=rhs, **kw)

    def transpose(self, out, in_, identity):
        fr = 1
        for d in in_.shape[1:]:
            fr *= d
        self.S._pe_mode(("tr", _rnd(in_.shape[0]), _rnd(fr), str(in_.dtype), in_.base_partition(), out.base_partition()))
        return self.S.nc.tensor.transpose(out, in_, identity)


class Ctx:
    pass


def build(NB=33, OWN0=17, phases="ABCDEF", dbg=False):
    _, ctx = _build(NB, OWN0, phases, dbg, None)
    return _build(NB, OWN0, phases, dbg, ctx["S"].rec)


def _build(NB, OWN0, phases, dbg, needed):
    NO = NB - OWN0
    nc = bass.Bass("TRN2", target_bir_lowering=False)
    S = Sched(nc, needed)

    def din(name, shape, dt=F32):
        return nc.dram_tensor(name, list(shape), dt, kind="ExternalInput").ap()

    def dscr(name, shape, dt=F32, out=False):
        return nc.dram_tensor(name, list(shape), dt, kind=("ExternalOutput" if out else "Internal")).ap()

    xin = din("xin", [NB * 128, D])
    w_in = din("w_in", [D, INC])
    vecs_d = din("vecs", [128, NV])
    cn_d = din("consts", [128, NCN])
    wup_d = din("w_up", [64, 1024])
    aup_d = din("a_up", [64, 1024])
    gup_d = din("g_up", [160, 1024])
    wout_d = din("w_out", [D, D])
    pq_d = din("peer_query", [D, D])
    sk_d = din("sub_keys", [16, 128, 128])
    pd_d = din("peer_down", [NE, D])
    pu_d = din("peer_up", [NE, D])
    yout = dscr("yout", [NO * 128, D], F32, out=True)

    PS_d = dscr("PS_s", [NB, 128, NRC * 128])
    QKV_d = dscr("QKV_s", [NO + 1, 128, 10 * 128])
    YR_d = dscr("YR_s", [NO, 128, 1024], BF16, out=dbg)
    YA_d = dscr("YA_s", [NO, 128, 1024], BF16, out=dbg)
    HM_d = dscr("HM_s", [NO, 128, D], F32, out=dbg)
    XNT_d = dscr("XNT_s", [NO, 128, D], BF16)
    ST_d = dscr("ST_s", [NO, 128, 2048 + 8])
    hPS = [H("PS%d" % j) for j in range(NB)]
    hQKV = [H("QKV%d" % j) for j in range(NO + 1)]
    hYR = [H() for j in range(NO)]
    hYA = [H() for j in range(NO)]
    hHM = [H() for j in range(NO)]
    hXNT = [H() for j in range(NO)]
    hST = [H() for j in range(NO)]
    hOUT = [H() for j in range(NO)]

    uid = [0]

    def mk(es):
        def T(name, shape, dt=F32):
            uid[0] += 1
            t = es.enter_context(nc.sbuf_tensor("sb%d_%s" % (uid[0], name), list(shape), dt))
            return t, H(name)

        def P(name, shape, dt=F32):
            uid[0] += 1
            t = es.enter_context(nc.psum_tensor("ps%d_%s" % (uid[0], name), list(shape), dt))
            return t, H(name)
        return T, P

    def load_consts(T, bf=True):
        c = Ctx()
        c.vec, c.hvec = T("vecs", [128, NV])
        S.dma("sp", c.vec[:], vecs_d, c.hvec, writes=[c.hvec])
        c.cn, c.hcn = T("cn32", [128, NCN])
        S.dma("sp", c.cn[:], cn_d, c.hcn, writes=[c.hcn])
        c.cb, c.hcb = T("cnbf", [128, NCN], BF16)
        S.dma("pool", c.cb[:], cn_d, c.hcb, writes=[c.hcb])
        return c

    def rmsnorm_T(c, T_, xt, hxt, gcol, junk, hjunk, st, hst, xs, hxs, pT, hpT, uT, huT):
        S.op("act", lambda e: e.activation(out=junk[:], in_=xt[:], func=AF.Square, accum_out=st[:, 0:1]),
             reads=[hxt], writes=([hjunk, hst] if hjunk is not hst else [hst]))
        S.op("dve", lambda e: e.tensor_scalar(out=st[:, 1:2], in0=st[:, 0:1], scalar1=1.0 / D, scalar2=1e-6,
                                              op0=ALU.mult, op1=ALU.add), reads=[hst], writes=[hst])
        S.op("act", lambda e: e.activation(out=st[:, 2:3], in_=st[:, 1:2], func=AF.Sqrt), reads=[hst], writes=[hst])
        S.op("dve", lambda e: e.reciprocal(out=st[:, 3:4], in_=st[:, 2:3]), reads=[hst], writes=[hst])
        S.op("act", lambda e: e.activation(out=xs[:], in_=xt[:], func=AF.Copy, scale=st[:, 3:4]),
             reads=[hxt, hst], writes=[hxs])
        for k in range(DC):
            S.op("pe", lambda e, k=k: e.transpose(pT[:, k, :], xs[:, k * 128:(k + 1) * 128], c.cb[:, C_ID:C_ID + 128]),
                 reads=[hxs, c.hcb], writes=[hpT])
        gb = c.vec[:, gcol:gcol + DC].unsqueeze(2).broadcast_to([128, DC, 128])
        S.op("dve", lambda e: e.tensor_tensor(out=uT[:], in0=pT[:], in1=gb, op=ALU.mult),
             reads=[hpT, c.hvec], writes=[huT])

    def phase_A():
        with contextlib.ExitStack() as es:
            T, P = mk(es)
            c = load_consts(T)
            win, hwin = T("win", [128, DC, INC], BF16)
            for k in range(DC):
                S.dma("pool", win[:, k, :], w_in[k * 128:(k + 1) * 128, :], hwin, writes=[hwin])
            xt, hxt = T("xt", [128, D])
            st, hst = T("st", [128, 4])
            xs, hxs = T("xs", [128, D], BF16)
            junk, hjunk = xs, hxs
            pT, hpT = P("pT", [128, DC, 128], BF16)
            uT, huT = T("uT", [128, DC, 128], BF16)
            pp = [P("pp%d" % i, [128, 4, 128]) for i in range(4)]
            PT, hPT = T("PT", [128, NRC, 129])
            QK, hQK = T("QK", [128, 10, 128])
            dd, hdd = T("dd", [128, NRC, 128])
            pss, hpss = dd, hdd
            S.op("pool", lambda e: e.memset(PT[:], 0.0), writes=[hPT])
            mub = c.vec[:, V_MU:V_MU + NRC].unsqueeze(2).broadcast_to([128, NRC, 128])
            for j in range(NB):
                S.dma("sp", xt[:], xin[j * 128:(j + 1) * 128, :], hxt, writes=[hxt])
                rmsnorm_T(c, T, xt, hxt, V_G1, junk, hjunk, st, hst, xs, hxs, pT, hpT, uT, huT)
                need_att = (j >= OWN0 - 1)
                chunks = list(range(0 if need_att else 10, 37))
                gi = 0
                for g0 in range(0, len(chunks), 4):
                    grp = chunks[g0:g0 + 4]
                    pt_, hp_ = pp[gi % 4]
                    gi += 1
                    for qi, ch in enumerate(grp):
                        M = 32 if ch == 36 else 128
                        for k in range(DC):
                            S.op("pe", lambda e, qi=qi, ch=ch, k=k, M=M, pt_=pt_: e.matmul(
                                pt_[0:M, qi, :], lhsT=win[:, k, ch * 128:ch * 128 + M], rhs=uT[:, k, :],
                                start=(k == 0), stop=(k == DC - 1)), reads=[hwin, huT], writes=[hp_])
                    for qi, ch in enumerate(grp):
                        M = 32 if ch == 36 else 128
                        eng = "act" if (qi % 2 == 0) else "dve"
                        if ch < 10:
                            dst, hd = QK[0:M, ch, :], hQK
                        else:
                            dst, hd = PT[0:M, ch - 10, 1:129], hPT
                        if eng == "act":
                            S.op("act", lambda e, dst=dst, qi=qi, M=M, pt_=pt_: e.activation(out=dst, in_=pt_[0:M, qi, :], func=AF.Copy),
                                 reads=[hp_], writes=[hd])
                        else:
                            S.op("dve", lambda e, dst=dst, qi=qi, M=M, pt_=pt_: e.tensor_copy(out=dst, in_=pt_[0:M, qi, :]),
                                 reads=[hp_], writes=[hd])
                if need_att:
                    ja = j - (OWN0 - 1)
                    S.dma("sp", QKV_d[ja].rearrange("p (c t) -> p c t", c=10), QK[:], hQK, reads=[hQK], writes=[hQKV[ja]])
                S.op("dve", lambda e: e.tensor_tensor(out=dd[:], in0=PT[:, :, 0:128], in1=PT[:, :, 1:129], op=ALU.subtract),
                     reads=[hPT], writes=[hdd])
                S.op("dve", lambda e: e.tensor_tensor(out=dd[:], in0=dd[:], in1=mub, op=ALU.mult),
                     reads=[hdd, c.hvec], writes=[hdd])
                S.op("dve", lambda e: e.tensor_tensor(out=dd[:], in0=dd[:], in1=PT[:, :, 1:129], op=ALU.add),
                     reads=[hdd, hPT], writes=[hdd])
                S.op("act", lambda e: e.activation(out=PT[:, :, 0:1], in_=PT[:, :, 128:129], func=AF.Copy),
                     reads=[hPT, hdd, hpss], writes=[hPT])
                S.dma("sp", PS_d[j].rearrange("p (c t) -> p c t", c=NRC), pss[:], hpss, reads=[hpss], writes=[hPS[j]])
            S.barrier()

    def phase_B():
        with contextlib.ExitStack() as es:
            T, P = mk(es)
            c = load_consts(T)
            vec = c.vec

            def vb(col, n=8):
                return vec[:, col:col + n].unsqueeze(2).broadcast_to([128, n, 128])
            wup, hwup = T("wup", [128, 1024], BF16)
            aup, haup = T("aup", [128, 1024], BF16)
            gup, hgup = T("gup", [128, 2, 1024], BF16)
            S.dma("pool", wup[0:64, :], wup_d, hwup, writes=[hwup])
            S.dma("pool", aup[64:128, :], aup_d, haup, writes=[haup])
            S.dma("pool", gup[:, 0, :], gup_d[0:128, :], hgup, writes=[hgup])
            S.dma("pool", gup[0:32, 1, :], gup_d[128:160, :], hgup, writes=[hgup])
            ps, hps = T("ps", [128, NRC, 128])
            names32 = ["nld", "aa", "kap", "kp", "beta", "cs", "cum", "t1", "t2", "t3"]
            F = {}
            for n in names32:
                F[n] = T(n, [128, 8, 128])
            namesbf = ["Bh", "Qh", "BG", "QG", "vb", "sqb"]
            Bf = {}
            for n in namesbf:
                Bf[n] = T(n, [128, 8, 128], BF16)
            lw, hlw = T("lw", [128, 128], BF16)
            sg, hsg = T("sg", [128, 2, 128], BF16)
            ones, hones = T("ones", [128, 1024])
            S.op("pool", lambda e: e.memset(ones[:], 1.0), writes=[hones])
            S32, hS32 = T("S32", [128, 8, 64])
            Sb, hSb = T("Sb", [128, 8, 64], BF16)
            S.op("pool", lambda e: e.memset(S32[:], 0.0), writes=[hS32])
            S.op("pool", lambda e: e.memset(Sb[:], 0.0), writes=[hSb])
            gms = [{}, {}]
            for si in range(2):
                for n in ["Ub", "Lb", "U2", "L2", "Xb", "SAb"]:
                    gms[si][n] = T(n + str(si), [64, 16, 64], BF16)
            gms2 = [[{}, {}], [{}, {}]]
            for pp_ in range(2):
                for si in range(2):
                    for n in ["Lak", "Mrb", "Mrk", "Qb"]:
                        gms2[pp_][si][n] = T("%s%d_%d" % (n, si, pp_), [64, 16, 64], BF16)
            F2 = {n: [T("%s_%d" % (n, i), [128, 8, 128]) for i in range(2)] for n in ("gg", "bon")}
            Bf2 = {n: [T("%s_%d" % (n, i), [128, 8, 128], BF16) for i in range(2)] for n in ("Kh", "Rh")}
            sm2 = [T("sm_%d" % i, [128, 8, 2, 4]) for i in range(2)]
            Vt2 = [T("Vt_%d" % i, [64, 2, 8, 128], BF16) for i in range(2)]
            BGt2 = [T("BGt_%d" % i, [64, 2, 8, 128], BF16) for i in range(2)]
            QGt2 = [T("QGt_%d" % i, [64, 2, 8, 128], BF16) for i in range(2)]
            ysb, hysb = T("ysb", [64, 16, 64])
            yc, hyc = T("yc", [64, 16, 64])
            ysq, hysq = ysb, hysb
            gst, hgst = T("gst", [64, 16, 4])
            yf, hyf = T("yf", [128, 8, 128])
            yrb, hyrb = T("yrb", [128, 8, 128], BF16)
            PA = [P("PA%d" % i, [128, 1024]) for i in range(3)]
            PTr, hPTr = P("PTr", [128, 2048], BF16)
            pi = [0]

            def nextPA():
                t = PA[pi[0] % 2]
                pi[0] += 1
                return t

            def nextPB():
                return PA[2]
            maskU = c.cn[0:64, C_MU:C_MU + 64].unsqueeze(1).broadcast_to([64, 16, 64])
            maskUi = c.cn[0:64, C_MUI:C_MUI + 64].unsqueeze(1).broadcast_to([64, 16, 64])
            maskL = c.cn[0:64, C_ML:C_ML + 64].unsqueeze(1).broadcast_to([64, 16, 64])
            identb = c.cb[:, C_ID:C_ID + 128]
            ident32 = c.cn[:, C_ID:C_ID + 128]
            bones = c.cb[:, C_BO:C_BO + 128]

            def tt(eng, out, i0, i1, op, reads, writes):
                S.op(eng, lambda e: e.tensor_tensor(out=out, in0=i0, in1=i1, op=op), reads=reads, writes=writes)

            def actf(out, in_, func, reads, writes, **kw):
                S.op("act", lambda e: e.activation(out=out, in_=in_, func=func, **kw), reads=reads, writes=writes)

            HORD = list(range(0, 16, 2)) + list(range(1, 16, 2))
            def body(j):
                own = j >= OWN0
                p = j % 2
                F["gg"], F["bon"] = F2["gg"][p], F2["bon"][p]
                Bf["Kh"], Bf["Rh"] = Bf2["Kh"][p], Bf2["Rh"][p]
                sm, hsm = sm2[p]
                Vt, hVt = Vt2[p]
                BGt, hBGt = BGt2[p]
                QGt, hQGt = QGt2[p]
                for si in range(2):
                    gms[si].update(gms2[p][si])
                S.dma("sp", ps[:], PS_d[j].rearrange("p (c t) -> p c t", c=NRC), hps, reads=[hPS[j]], writes=[hps])
                r_, k_, v_ = ps[:, 0:8, :], ps[:, 8:16, :], ps[:, 16:24, :]
                actf(lw[0:64, :], ps[0:64, 24, :], AF.Tanh, [hps], [hlw])
                actf(lw[64:128, :], ps[64:128, 24, :], AF.Copy, [hps], [hlw])
                actf(sg[:, 0, :], ps[:, 25, :], AF.Sigmoid, [hps], [hsg])
                actf(sg[0:32, 1, :], ps[0:32, 26, :], AF.Sigmoid, [hps], [hsg])
                pw_, hpw = nextPA()
                pa_, hpa = nextPA()
                for m in range(8):
                    S.op("pe", lambda e, m=m: e.matmul(pw_[:, m * 128:(m + 1) * 128], lhsT=wup[0:64, m * 128:(m + 1) * 128], rhs=lw[0:64, :], start=True, stop=True),
                         reads=[hwup, hlw], writes=[hpw])
                    S.op("pe", lambda e, m=m: e.matmul(pa_[:, m * 128:(m + 1) * 128], lhsT=aup[64:128, m * 128:(m + 1) * 128], rhs=lw[64:128, :], start=True, stop=True),
                         reads=[haup, hlw], writes=[hpa])
                nld, hnld = F["nld"]
                aa, haa = F["aa"]
                t1, ht1 = F["t1"]
                t2, ht2 = F["t2"]
                t3, ht3 = F["t3"]
                v3 = lambda p_: p_[:].rearrange("p (m t) -> p m t", m=8)
                tt("dve", t1[:], v3(pw_), vb(V_W0), ALU.add, [hpw, c.hvec], [ht1])
                actf(t1[:], t1[:], AF.Sigmoid, [ht1], [ht1])
                S.op("dve", lambda e: e.tensor_scalar(out=nld[:], in0=t1[:], scalar1=0.6065306597126334, scalar2=None, op0=ALU.mult),
                     reads=[ht1], writes=[hnld])
                tt("dve", t2[:], v3(pa_), vb(V_A0), ALU.add, [hpa, c.hvec], [ht2])
                actf(aa[:], t2[:], AF.Sigmoid, [ht2], [haa])
                if own:
                    pg_, hpg = nextPA()
                    for m in range(8):
                        S.op("pe", lambda e, m=m: e.matmul(pg_[:, m * 128:(m + 1) * 128], lhsT=gup[:, 0, m * 128:(m + 1) * 128], rhs=sg[:, 0, :], start=True, stop=False),
                             reads=[hgup, hsg], writes=[hpg])
                        S.op("pe", lambda e, m=m: e.matmul(pg_[:, m * 128:(m + 1) * 128], lhsT=gup[0:32, 1, m * 128:(m + 1) * 128], rhs=sg[0:32, 1, :], start=False, stop=True),
                             reads=[hgup, hsg], writes=[hpg])
                    gg, hgg = F["gg"]
                    actf(gg[:], v3(pg_), AF.Copy, [hpg], [hgg])
                kap, hkap = F["kap"]
                sqb, hsqb = Bf["sqb"]
                tt("dve", kap[:], k_, vb(V_KK), ALU.mult, [hps, c.hvec], [hkap])
                actf(sqb[:], kap[:], AF.Square, [hkap], [hsqb])
                pq_, hpq = nextPA()
                for hh in range(2):
                    S.op("pe", lambda e, hh=hh: e.matmul(pq_[:, hh * 512:(hh + 1) * 512], lhsT=bones, rhs=sqb[:, hh * 4:(hh + 1) * 4, :], start=True, stop=True),
                         reads=[c.hcb, hsqb], writes=[hpq])
                actf(t3[:], v3(pq_), AF.Sqrt, [hpq], [ht3])
                S.op("dve", lambda e: e.tensor_scalar(out=t3[:], in0=t3[:], scalar1=1e-12, scalar2=None, op0=ALU.max), reads=[ht3], writes=[ht3])
                S.op("dve", lambda e: e.reciprocal(out=t3[:], in_=t3[:]), reads=[ht3], writes=[ht3])
                tt("dve", kap[:], kap[:], t3[:], ALU.mult, [hkap, ht3], [hkap])
                kp, hkp = F["kp"]
                S.op("dve", lambda e: e.scalar_tensor_tensor(out=t2[:], in0=aa[:], scalar=1.0, in1=vb(V_KA), op0=ALU.subtract, op1=ALU.mult),
                     reads=[haa, c.hvec], writes=[ht2])
                S.op("dve", lambda e: e.scalar_tensor_tensor(out=kp[:], in0=t2[:], scalar=1.0, in1=k_, op0=ALU.add, op1=ALU.mult),
                     reads=[ht2, hps], writes=[hkp])
                beta, hbeta = F["beta"]
                tt("dve", beta[:], kap[:], aa[:], ALU.mult, [hkap, haa], [hbeta])
                cs, hcs = F["cs"]
                cum, hcum = F["cum"]
                S.op("dve", lambda e: e.tensor_tensor_scan(out=cs[:].rearrange("p m t -> p (m t)"), data0=ones[:],
                                                           data1=nld[:].rearrange("p m t -> p (m t)"), initial=0.0,
                                                           op0=ALU.mult, op1=ALU.add), reads=[hones, hnld], writes=[hcs])
                cs4 = cs[:].rearrange("p m (s t) -> p m s t", s=2)
                nld4 = nld[:].rearrange("p m (s t) -> p m s t", s=2)
                cum4 = cum[:].rearrange("p m (s t) -> p m s t", s=2)
                tt("dve", sm[:, :, :, 0:1], cs4[:, :, :, 0:1], nld4[:, :, :, 0:1], ALU.subtract, [hcs, hnld], [hsm])
                tt("dve", cum4, cs4, sm[:, :, :, 0:1].broadcast_to([128, 8, 2, 64]), ALU.subtract, [hcs, hsm], [hcum])
                actf(sm[:, :, :, 2:3], cum4[:, :, :, 63:64], AF.Exp, [hcum], [hsm], scale=-1.0)
                Kh, hKh = Bf["Kh"]
                Rh, hRh = Bf["Rh"]
                Bh, hBh = Bf["Bh"]
                Qh, hQh = Bf["Qh"]
                BG, hBG = Bf["BG"]
                QG, hQG = Bf["QG"]
                vbf, hvbf = Bf["vb"]
                actf(t1[:], cum[:], AF.Exp, [hcum], [ht1], scale=-1.0)
                tt("dve", Rh[:], r_, t1[:], ALU.mult, [hps, ht1], [hRh])
                tt("dve", t2[:], cum[:], nld[:], ALU.subtract, [hcum, hnld], [ht2])
                actf(t2[:], t2[:], AF.Exp, [ht2], [ht2], scale=-1.0)
                tt("dve", Kh[:], kap[:], t2[:], ALU.mult, [hkap, ht2], [hKh])
                actf(t3[:], cum[:], AF.Exp, [hcum], [ht3])
                tt("dve", Bh[:], beta[:], t3[:], ALU.mult, [hbeta, ht3], [hBh])
                tt("dve", Qh[:], kp[:], t3[:], ALU.mult, [hkp, ht3], [hQh])
                t14 = t1[:].rearrange("p m (s t) -> p m s t", s=2)
                tt("dve", t14, cum4, cum4[:, :, :, 63:64].broadcast_to([128, 8, 2, 64]), ALU.subtract, [hcum], [ht1])
                actf(t1[:], t1[:], AF.Exp, [ht1], [ht1])
                tt("dve", BG[:], beta[:], t1[:], ALU.mult, [hbeta, ht1], [hBG])
                tt("dve", QG[:], kp[:], t1[:], ALU.mult, [hkp, ht1], [hQG])
                actf(vbf[:], v_, AF.Copy, [hps], [hvbf])
                for (src, hsrc, dst, hdst) in ((vbf, hvbf, Vt, hVt), (BG, hBG, BGt, hBGt), (QG, hQG, QGt, hQGt)):
                    for m in range(8):
                        for s in range(2):
                            S.op("pe", lambda e, m=m, s=s, src=src: e.transpose(
                                PTr[0:64, (s * 8 + m) * 128:(s * 8 + m + 1) * 128], src[:, m, s * 64:(s + 1) * 64], identb),
                                reads=[hsrc, c.hcb], writes=[hPTr])
                    S.op("act", lambda e, dst=dst: e.activation(out=dst[:].rearrange("t s m c -> t (s m c)"), in_=PTr[0:64, :], func=AF.Copy),
                         reads=[hPTr], writes=[hdst])
                if own:
                    bon, hbon = F["bon"]
                    tt("dve", t2[:], r_, kp[:], ALU.mult, [hps, hkp], [ht2])
                    tt("dve", sqb[:], t2[:], vb(V_RK), ALU.mult, [ht2, c.hvec], [hsqb])
                    pb_, hpb = nextPA()
                    for hh in range(2):
                        S.op("pe", lambda e, hh=hh: e.matmul(pb_[:, hh * 512:(hh + 1) * 512], lhsT=bones, rhs=sqb[:, hh * 4:(hh + 1) * 4, :], start=True, stop=True),
                             reads=[c.hcb, hsqb], writes=[hpb])
                    tt("dve", bon[:], v3(pb_), v_, ALU.mult, [hpb, hps], [hbon])
                def hrows(h):
                    return slice((h % 2) * 64, (h % 2) * 64 + 64)

                idb = c.cn[0:64, C_ID:C_ID + 64].unsqueeze(1).broadcast_to([64, 16, 64])

                def gram(s, lt, hl, rt, hr, mask, dname, eng):
                    ts = slice(s * 64, (s + 1) * 64)
                    p_, hp_ = nextPA()
                    pv = p_[0:64, :].rearrange("p (h t) -> p h t", h=16)
                    for h in HORD:
                        S.op("pe", lambda e, h=h: e.matmul(pv[:, h, :], lhsT=lt[hrows(h), h // 2, ts], rhs=rt[hrows(h), h // 2, ts], start=True, stop=True),
                             reads=[hl, hr], writes=[hp_])
                    d_, hd_ = gms[s][dname]
                    tt(eng, d_[:], pv, mask, ALU.mult, [hp_, c.hcn], [hd_])

                for s in range(2):
                    gram(s, Bh, hBh, Kh, hKh, maskU, "Ub", "dve")
                    gram(s, Kh, hKh, Bh, hBh, maskL, "Lb", "dve")
                for s in range(2):
                    Ub, hUb = gms[s]["Ub"]
                    Qb, hQb = gms[s]["Qb"]
                    tt("dve", Qb[:], idb, Ub[:], ALU.subtract, [c.hcn, hUb], [hQb])
                for s in range(2):
                    gram(s, Qh, hQh, Kh, hKh, maskU, "Lak", "dve")
                    if own:
                        gram(s, Bh, hBh, Rh, hRh, maskUi, "Mrb", "dve")
                        gram(s, Qh, hQh, Rh, hRh, maskUi, "Mrk", "dve")

                def inv_level(s, lvl):
                    gm = gms[s]
                    cur = ("Ub", "Lb") if lvl % 2 == 0 else ("U2", "L2")
                    nxt = ("U2", "L2") if lvl % 2 == 0 else ("Ub", "Lb")
                    Qb, hQb = gm["Qb"]
                    Uk, hUk = gm[cur[0]]
                    Lk, hLk = gm[cur[1]]
                    Un, hUn = gm[nxt[0]]
                    Ln, hLn = gm[nxt[1]]
                    p2, hp2 = nextPA()
                    p2v = p2[0:64, :].rearrange("p (h t) -> p h t", h=16)
                    for h in HORD:
                        S.op("pe", lambda e, h=h: e.matmul(p2v[:, h, :], lhsT=Uk[:, h, :], rhs=Lk[:, h, :], start=True, stop=True),
                             reads=[hUk, hLk], writes=[hp2])
                    actf(Ln[:], p2v, AF.Copy, [hp2], [hLn])
                    if lvl < 4:
                        p1, hp1 = nextPA()
                        p1v = p1[0:64, :].rearrange("p (h t) -> p h t", h=16)
                        for h in HORD:
                            S.op("pe", lambda e, h=h: e.matmul(p1v[:, h, :], lhsT=Lk[:, h, :], rhs=Uk[:, h, :], start=True, stop=True),
                                 reads=[hUk, hLk], writes=[hp1])
                        S.op("dve", lambda e: e.tensor_copy(out=Un[:], in_=p1v), reads=[hp1], writes=[hUn])
                    p3, hp3 = nextPA()
                    p3v = p3[0:64, :].rearrange("p (h t) -> p h t", h=16)
                    for h in HORD:
                        S.op("pe", lambda e, h=h: e.matmul(p3v[:, h, :], lhsT=Ln[:, h, :], rhs=Qb[:, h, :], start=True, stop=True),
                             reads=[hLn, hQb], writes=[hp3])
                    tt("dve", Qb[:], p3v, Qb[:], ALU.add, [hp3, hQb], [hQb])

                for lvl in range(5):
                    for s in range(2):
                        inv_level(s, lvl)

                S.mark()
                for s in range(2):
                    ts = slice(s * 64, (s + 1) * 64)
                    gm = gms[s]
                    Qb, hQb = gm["Qb"]
                    Lak, hLak = gm["Lak"]
                    Xb, hXb = gm["Xb"]
                    SAb, hSAb = gm["SAb"]
                    px, hpx = nextPB()
                    pxv = px[0:64, :].rearrange("p (h t) -> p h t", h=16)
                    for h in HORD:
                        cs_ = slice((h % 2) * 64, (h % 2) * 64 + 64)
                        S.op("pe", lambda e, h=h: e.matmul(pxv[:, h, :], lhsT=Kh[hrows(h), h // 2, ts], rhs=Sb[hrows(h), h // 2, :], start=True, stop=False),
                             reads=[hKh, hSb], writes=[hpx])
                        S.op("pe", lambda e, h=h, cs_=cs_: e.matmul(pxv[:, h, :], lhsT=Lak[:, h, :], rhs=Vt[:, s, h // 2, cs_], start=False, stop=True),
                             reads=[hLak, hVt], writes=[hpx])
                    actf(Xb[:], pxv, AF.Copy, [hpx], [hXb])
                    psa, hpsa = nextPB()
                    psav = psa[0:64, :].rearrange("p (h t) -> p h t", h=16)
                    for h in HORD:
                        S.op("pe", lambda e, h=h: e.matmul(psav[:, h, :], lhsT=Qb[:, h, :], rhs=Xb[:, h, :], start=True, stop=True),
                             reads=[hQb, hXb], writes=[hpsa])
                    actf(SAb[:], psav, AF.Copy, [hpsa], [hSAb], scale=-1.0)
                    if own:
                        Mrb, hMrb = gm["Mrb"]
                        Mrk, hMrk = gm["Mrk"]
                        py, hpy = nextPB()
                        pyv = py[0:64, :].rearrange("p (h t) -> p h t", h=16)
                        for h in HORD:
                            cs_ = slice((h % 2) * 64, (h % 2) * 64 + 64)
                            S.op("pe", lambda e, h=h: e.matmul(pyv[:, h, :], lhsT=Rh[hrows(h), h // 2, ts], rhs=Sb[hrows(h), h // 2, :], start=True, stop=False),
                                 reads=[hRh, hSb], writes=[hpy])
                            S.op("pe", lambda e, h=h: e.matmul(pyv[:, h, :], lhsT=Mrb[:, h, :], rhs=SAb[:, h, :], start=False, stop=False),
                                 reads=[hMrb, hSAb], writes=[hpy])
                            S.op("pe", lambda e, h=h, cs_=cs_: e.matmul(pyv[:, h, :], lhsT=Mrk[:, h, :], rhs=Vt[:, s, h // 2, cs_], start=False, stop=True),
                                 reads=[hMrk, hVt], writes=[hpy])
                        actf(ysb[:], pyv, AF.Copy, [hpy], [hysb])
                        S.op("dve", lambda e: e.tensor_reduce(out=gst[:, :, 0:1], in_=ysb[:], axis=AX.X, op=ALU.add), reads=[hysb], writes=[hgst])
                        S.op("dve", lambda e: e.tensor_scalar(out=gst[:, :, 0:1], in0=gst[:, :, 0:1], scalar1=1.0 / 64, scalar2=None, op0=ALU.mult), reads=[hgst], writes=[hgst])
                        tt("dve", yc[:], ysb[:], gst[:, :, 0:1].broadcast_to([64, 16, 64]), ALU.subtract, [hysb, hgst], [hyc])
                        actf(ysq[:], yc[:], AF.Square, [hyc], [hysq])
                        S.op("dve", lambda e: e.tensor_reduce(out=gst[:, :, 1:2], in_=ysq[:], axis=AX.X, op=ALU.add), reads=[hysq], writes=[hgst])
                        S.op("dve", lambda e: e.tensor_scalar(out=gst[:, :, 1:2], in0=gst[:, :, 1:2], scalar1=1.0 / 64, scalar2=64e-5, op0=ALU.mult, op1=ALU.add), reads=[hgst], writes=[hgst])
                        actf(gst[:, :, 2:3], gst[:, :, 1:2], AF.Sqrt, [hgst], [hgst])
                        S.op("dve", lambda e: e.reciprocal(out=gst[:, :, 3:4], in_=gst[:, :, 2:3]), reads=[hgst], writes=[hgst])
                        tt("dve", yc[:], yc[:], gst[:, :, 3:4].broadcast_to([64, 16, 64]), ALU.mult, [hyc, hgst], [hyc])
                        pt2, hpt2 = nextPB()
                        for m in range(8):
                            S.op("pe", lambda e, m=m: e.transpose(pt2[:, m * 64:(m + 1) * 64], yc[:, 2 * m:2 * m + 2, :].rearrange("t h i -> t (h i)"), ident32[0:64, 0:64]),
                                 reads=[hyc, c.hcn], writes=[hpt2])
                        S.op("dve", lambda e: e.tensor_copy(out=yf[:, :, ts], in_=pt2[:, 0:512].rearrange("p (m t) -> p m t", m=8)), reads=[hpt2], writes=[hyf])
                    pst, hpst = nextPB()
                    for h in HORD:
                        cs_ = slice((h % 2) * 64, (h % 2) * 64 + 64)
                        o_ = pst[hrows(h), (h // 2) * 64:(h // 2) * 64 + 64]
                        S.op("pe", lambda e, h=h, cs_=cs_, o_=o_: e.matmul(o_, lhsT=BGt[:, s, h // 2, cs_], rhs=SAb[:, h, :], start=True, stop=False),
                             reads=[hBGt, hSAb], writes=[hpst])
                        S.op("pe", lambda e, h=h, cs_=cs_, o_=o_: e.matmul(o_, lhsT=QGt[:, s, h // 2, cs_], rhs=Vt[:, s, h // 2, cs_], start=False, stop=True),
                             reads=[hQGt, hVt], writes=[hpst])
                    tt("dve", S32[:], S32[:], sm[:, :, s, 2:3].broadcast_to([128, 8, 64]), ALU.mult, [hS32, hsm], [hS32])
                    tt("dve", S32[:], S32[:], pst[:, 0:512].rearrange("p (m i) -> p m i", m=8), ALU.add, [hS32, hpst], [hS32])
                    actf(Sb[:], S32[:], AF.Copy, [hS32], [hSb])
                if own:
                    gg, hgg = F["gg"]
                    bon, hbon = F["bon"]
                    tt("dve", yf[:], yf[:], vb(V_GNW), ALU.mult, [hyf, c.hvec], [hyf])
                    tt("dve", yf[:], yf[:], vb(V_GNB), ALU.add, [hyf, c.hvec], [hyf])
                    tt("dve", yf[:], yf[:], bon[:], ALU.add, [hyf, hbon], [hyf])
                    tt("dve", yrb[:], yf[:], gg[:], ALU.mult, [hyf, hgg], [hyrb])
                    jo = j - OWN0
                    S.dma("sp", YR_d[jo].rearrange("p (m t) -> p m t", m=8), yrb[:], hyrb, reads=[hyrb], writes=[hYR[jo]])

            def rec(j):
                r = S.record(lambda: body(j))
                k = [i for i, it in enumerate(r) if it[0] == "mark"][0]
                return r[:k], r[k + 1:]
            A0, curB = rec(0)
            S.play(A0)
            for j in range(NB):
                if j + 1 < NB:
                    A1, B1 = rec(j + 1)
                    S.play2(curB, A1)
                    curB = B1
                else:
                    S.play(curB)
            S.barrier()


    def tt_(eng, out, i0, i1, op, reads, writes):
        S.op(eng, lambda e: e.tensor_tensor(out=out, in0=i0, in1=i1, op=op), reads=reads, writes=writes)

    def act_(out, in_, func, reads, writes, **kw):
        S.op("act", lambda e: e.activation(out=out, in_=in_, func=func, **kw), reads=reads, writes=writes)

    def phase_C():
        with contextlib.ExitStack() as es:
            T, P = mk(es)
            c = load_consts(T)
            vec = c.vec
            bones = c.cb[:, C_BO:C_BO + 128]
            identb = c.cb[:, C_ID:C_ID + 128]
            qk, hqk = T("qk", [128, 10, 128])
            sqb, hsqb = T("sqb", [128, 8, 128], BF16)
            t1, ht1 = T("t1", [128, 8, 128])
            qT, hqT = T("qT", [128, 8, 128], BF16)
            knb, hknb = T("knb", [128, 128], BF16)
            vbf, hvbf = T("vbf", [128, 128], BF16)
            kd = [T("kd%d" % i, [128, 2, 128], BF16) for i in range(2)]
            Vk = [T("Vk%d" % i, [128, 128], BF16) for i in range(2)]
            e1, he1 = T("e1", [128, 4, 128], BF16)
            PTb, hPTb = T("PTb", [128, 4, 128], BF16)
            onesb, honesb = T("onesb", [128, 64], BF16)
            S.op("pool", lambda e: e.memset(onesb[:], 1.0), writes=[honesb])
            esk, hesk = T("esk", [128, 8])
            act_(esk[:], vec[:, V_SK:V_SK + 8], AF.Exp, [c.hvec], [hesk])
            den, hden = T("den", [128, 4, 128])
            ya, hya = T("ya", [128, 8, 128], BF16)
            PS1, hPS1 = P("PS1", [128, 1024])
            psc = [P("psc%d" % i, [128, 512]) for i in range(2)]
            po, hpo = P("po", [128, 512])
            pdn, hpdn = P("pdn", [128, 512])
            pvT, hpvT = P("pvT", [128, 128], BF16)

            def norm_rows(src, n, gcol, dst, hdst):
                act_(sqb[:, 0:n, :], src, AF.Square, [hqk], [hsqb])
                for hh in range(0, n, 4):
                    w_ = min(4, n - hh)
                    S.op("pe", lambda e, hh=hh, w_=w_: e.matmul(PS1[:, hh * 128:(hh + w_) * 128], lhsT=bones, rhs=sqb[:, hh:hh + w_, :], start=True, stop=True),
                         reads=[c.hcb, hsqb], writes=[hPS1])
                pv = PS1[:, 0:n * 128].rearrange("p (m t) -> p m t", m=n)
                S.op("dve", lambda e: e.tensor_scalar(out=t1[:, 0:n, :], in0=pv, scalar1=1.0 / 64, scalar2=1e-6, op0=ALU.mult, op1=ALU.add),
                     reads=[hPS1], writes=[ht1])
                act_(t1[:, 0:n, :], t1[:, 0:n, :], AF.Sqrt, [ht1], [ht1])
                S.op("dve", lambda e: e.reciprocal(out=t1[:, 0:n, :], in_=t1[:, 0:n, :]), reads=[ht1], writes=[ht1])
                tt_("dve", t1[:, 0:n, :], t1[:, 0:n, :], src, ALU.mult, [ht1, hqk], [ht1])
                S.op("dve", lambda e: e.tensor_scalar(out=dst, in0=t1[:, 0:n, :], scalar1=vec[:, gcol:gcol + 1], scalar2=None, op0=ALU.mult),
                     reads=[ht1, c.hvec], writes=[hdst])

            def kv_prep(slot):
                kd_, hkd_ = kd[slot]
                Vk_, hVk_ = Vk[slot]
                norm_rows(qk[:, 8:9, :], 1, V_KG, knb[:].unsqueeze(1), hknb)
                for g in range(2):
                    S.op("pe", lambda e, g=g: e.matmul(PS1[:, 512 + g * 128:512 + (g + 1) * 128], lhsT=c.cb[:, C_SEL + g * 128:C_SEL + (g + 1) * 128], rhs=knb[:], start=True, stop=True),
                         reads=[c.hcb, hknb], writes=[hPS1])
                act_(kd_[:], PS1[:, 512:768].rearrange("p (g t) -> p g t", g=2), AF.Copy, [hPS1], [hkd_])
                act_(vbf[:], qk[:, 9, :], AF.Copy, [hqk], [hvbf])
                S.op("pe", lambda e: e.transpose(pvT[:], vbf[:], identb), reads=[hvbf, c.hcb], writes=[hpvT])
                S.op("dve", lambda e: e.tensor_copy(out=Vk_[:], in_=pvT[:]), reads=[hpvT], writes=[hVk_])

            S.dma("sp", qk[:, 8:10, :], QKV_d[0].rearrange("p (c t) -> p c t", c=10)[:, 8:10, :], hqk, reads=[hQKV[0]], writes=[hqk])
            kv_prep(0)
            for jo in range(NO):
                ja = jo + 1
                S.dma("sp", qk[:], QKV_d[ja].rearrange("p (c t) -> p c t", c=10), hqk, reads=[hQKV[ja]], writes=[hqk])
                kv_prep(ja % 2)
                norm_rows(qk[:, 0:8, :], 8, V_QG, qT[:], hqT)
                si = 0
                for g in range(2):
                    for par in range(2):
                        rows = slice(par * 64, par * 64 + 64)
                        for wi, slot in enumerate(((ja - 1) % 2, ja % 2)):
                            kd_, hkd_ = kd[slot]
                            Vk_, hVk_ = Vk[slot]
                            ps_, hps_ = psc[si % 2]
                            si += 1
                            S.op("pe", lambda e, ps_=ps_, kd_=kd_: e.matmul(ps_[:], lhsT=kd_[rows, g, :], rhs=qT[rows, 4 * g:4 * g + 4, :], start=True, stop=True),
                                 reads=[hkd_, hqT], writes=[hps_])
                            act_(e1[:], ps_[:].rearrange("p (h t) -> p h t", h=4), AF.Exp, [hps_], [he1], scale=0.125)
                            mcol = C_MC if wi == 1 else (C_MPF if jo == 0 else C_MP)
                            mk_ = c.cb[:, mcol:mcol + 128].unsqueeze(1).broadcast_to([128, 4, 128])
                            tt_("pool", PTb[:], e1[:], mk_, ALU.mult, [he1, c.hcb], [hPTb])
                            S.op("pe", lambda e, Vk_=Vk_, wi=wi: e.matmul(po[rows, :], lhsT=Vk_[:, g * 64:(g + 1) * 64], rhs=PTb[:].rearrange("p h t -> p (h t)"), start=(wi == 0), stop=(wi == 1)),
                                 reads=[hVk_, hPTb], writes=[hpo])
                            S.op("pe", lambda e, wi=wi: e.matmul(pdn[rows, :], lhsT=onesb[:], rhs=PTb[:].rearrange("p h t -> p (h t)"), start=(wi == 0), stop=(wi == 1)),
                                 reads=[honesb, hPTb], writes=[hpdn])
                    tt_("dve", den[:], pdn[:].rearrange("p (h t) -> p h t", h=4), esk[:, 4 * g:4 * g + 4].unsqueeze(2).broadcast_to([128, 4, 128]), ALU.add, [hpdn, hesk], [hden])
                    S.op("dve", lambda e: e.reciprocal(out=den[:], in_=den[:]), reads=[hden], writes=[hden])
                    tt_("dve", ya[:, 4 * g:4 * g + 4, :], po[:].rearrange("p (h t) -> p h t", h=4), den[:], ALU.mult, [hpo, hden], [hya])
                S.dma("sp", YA_d[jo].rearrange("p (m t) -> p m t", m=8), ya[:], hya, reads=[hya], writes=[hYA[jo]])
            S.barrier()

    def phase_D():
        with contextlib.ExitStack() as es:
            T, P = mk(es)
            c = load_consts(T)
            wo, hwo = T("wo", [128, DC, D], BF16)
            for k in range(DC):
                S.dma("pool", wo[:, k, :], wout_d[k * 128:(k + 1) * 128, :], hwo, writes=[hwo])
            yr, hyr = T("yr", [128, 8, 128], BF16)
            ya, hya = T("ya", [128, 8, 128], BF16)
            xt, hxt = T("xt", [128, D])
            hm, hhm = T("hm", [128, D])
            st, hst = T("st", [128, 4])
            xs, hxs = T("xs", [128, D], BF16)
            uT, huT = T("uT", [128, DC, 128], BF16)
            pT, hpT = P("pT", [128, DC, 128], BF16)
            pp = [P("pp%d" % i, [128, 512]) for i in range(4)]
            for jo in range(NO):
                j = OWN0 + jo
                S.dma("sp", yr[:], YR_d[jo].rearrange("p (m t) -> p m t", m=8), hyr, reads=[hYR[jo]], writes=[hyr])
                S.dma("sp", ya[:], YA_d[jo].rearrange("p (m t) -> p m t", m=8), hya, reads=[hYA[jo]], writes=[hya])
                S.dma("sp", xt[:], xin[j * 128:(j + 1) * 128, :], hxt, writes=[hxt])
                for cg in range(4):
                    p_, hp_ = pp[cg]
                    for kc in range(DC):
                        src, hsrc = (yr, hyr) if kc < 8 else (ya, hya)
                        S.op("pe", lambda e, p_=p_, kc=kc, cg=cg, src=src: e.matmul(p_[:], lhsT=src[:, kc % 8, :], rhs=wo[:, kc, cg * 512:(cg + 1) * 512], start=(kc == 0), stop=(kc == DC - 1)),
                             reads=[hsrc, hwo], writes=[hp_])
                    tt_("dve", hm[:, cg * 512:(cg + 1) * 512], p_[:], xt[:, cg * 512:(cg + 1) * 512], ALU.add, [hp_, hxt], [hhm])
                S.dma("sp", HM_d[jo], hm[:], hhm, reads=[hhm], writes=[hHM[jo]])
                rmsnorm_T(c, T, hm, hhm, V_G2, xs, hxs, st, hst, xs, hxs, pT, hpT, uT, huT)
                S.dma("sp", XNT_d[jo].rearrange("p (k t) -> p k t", k=DC), uT[:], huT, reads=[huT], writes=[hXNT[jo]])
            S.barrier()

    def phase_E1():
        with contextlib.ExitStack() as es:
            T, P = mk(es)
            c = load_consts(T)
            pq, hpq = T("pq", [128, DC, D], BF16)
            for k in range(DC):
                S.dma("pool", pq[:, k, :], pq_d[k * 128:(k + 1) * 128, :], hpq, writes=[hpq])
            skn, hskn = T("skn", [128, 16, 128])
            S.dma("sp", skn[:], sk_d.rearrange("c n d -> n c d"), hskn, writes=[hskn])
            skT, hskT = T("skT", [128, 16, 128])
            pbig = [P("pb%d" % i, [128, 16, 128]) for i in range(2)]
            p0, hp0 = pbig[0]
            for hc in range(16):
                S.op("pe", lambda e, hc=hc: e.transpose(p0[:, hc, :], skn[:, hc, :], c.cn[:, C_ID:C_ID + 128]), reads=[hskn, c.hcn], writes=[hp0])
            S.op("dve", lambda e: e.tensor_copy(out=skT[:], in_=p0[:]), reads=[hp0], writes=[hskT])
            xnT, hxnT = T("xnT", [128, DC, 128], BF16)
            qTs, hqTs = T("qTs", [128, 16, 128])
            sall, hsall = T("sall", [128, 16, 128])
            tops, htops = T("tops", [128, 16, 16])
            wk, hwk = T("wk", [128, 128])
            cand, hcand = T("cand", [128, 8, 256])
            wk2, hwk2 = T("wk2", [128, 256])
            ctop, hctop = T("ctop", [128, 8, 24])
            sm, hsm = T("sm", [128, 4, 8])
            ez, hez = T("ez", [128, 8, 16])
            stt, hstt = T("stt", [128, 2048 + 8])
            for jo in range(NO):
                S.dma("sp", xnT[:], XNT_d[jo].rearrange("p (k t) -> p k t", k=DC), hxnT, reads=[hXNT[jo]], writes=[hxnT])
                pa_, hpa_ = pbig[0]
                for hc in range(16):
                    for kc in range(DC):
                        S.op("pe", lambda e, hc=hc, kc=kc: e.matmul(pa_[:, hc, :], lhsT=pq[:, kc, hc * 128:(hc + 1) * 128], rhs=xnT[:, kc, :], start=(kc == 0), stop=(kc == DC - 1)),
                             reads=[hpq, hxnT], writes=[hpa_])
                act_(qTs[:, 0:8, :], pa_[:, 0:8, :], AF.Copy, [hpa_], [hqTs])
                S.op("dve", lambda e: e.tensor_copy(out=qTs[:, 8:16, :], in_=pa_[:, 8:16, :]), reads=[hpa_], writes=[hqTs])
                pb_, hpb_ = pbig[1]
                for hc in range(16):
                    S.op("pe", lambda e, hc=hc: e.matmul(pb_[:, hc, :], lhsT=qTs[:, hc, :], rhs=skT[:, hc, :], start=True, stop=True),
                         reads=[hqTs, hskT], writes=[hpb_])
                act_(sall[:, 0:8, :], pb_[:, 0:8, :], AF.Copy, [hpb_], [hsall])
                S.op("dve", lambda e: e.tensor_copy(out=sall[:, 8:16, :], in_=pb_[:, 8:16, :]), reads=[hpb_], writes=[hsall])
                for hc in range(16):
                    S.op("dve", lambda e, hc=hc: e.max(out=tops[:, hc, 0:8], in_=sall[:, hc, :]), reads=[hsall], writes=[htops])
                    S.op("dve", lambda e, hc=hc: e.match_replace(out=wk[:], in_to_replace=tops[:, hc, 0:8], in_values=sall[:, hc, :], imm_value=-1e30),
                         reads=[hsall, htops], writes=[hwk])
                    S.op("dve", lambda e, hc=hc: e.max(out=tops[:, hc, 8:16], in_=wk[:]), reads=[hwk], writes=[htops])
                t4 = tops[:].rearrange("p (h c) k -> p h c k", c=2)
                tt_("pool", cand[:].rearrange("p h (i j) -> p h i j", i=16), t4[:, :, 0, :].unsqueeze(3).broadcast_to([128, 8, 16, 16]),
                    t4[:, :, 1, :].unsqueeze(2).broadcast_to([128, 8, 16, 16]), ALU.add, [htops], [hcand])
                for h in range(8):
                    S.op("dve", lambda e, h=h: e.max(out=ctop[:, h, 0:8], in_=cand[:, h, :]), reads=[hcand], writes=[hctop])
                    S.op("dve", lambda e, h=h: e.match_replace(out=wk2[:], in_to_replace=ctop[:, h, 0:8], in_values=cand[:, h, :], imm_value=-1e30),
                         reads=[hcand, hctop], writes=[hwk2])
                    S.op("dve", lambda e, h=h: e.max(out=ctop[:, h, 8:16], in_=wk2[:]), reads=[hwk2], writes=[hctop])
                    S.op("dve", lambda e, h=h: e.match_replace(out=wk2[:], in_to_replace=ctop[:, h, 8:16], in_values=wk2[:], imm_value=-1e30),
                         reads=[hwk2, hctop], writes=[hwk2])
                    S.op("dve", lambda e, h=h: e.max(out=ctop[:, h, 16:24], in_=wk2[:]), reads=[hwk2], writes=[hctop])
                thr = sm[:, 0, :]
                tt_("dve", thr.unsqueeze(2), ctop[:, :, 15:16], ctop[:, :, 16:17], ALU.add, [hctop], [hsm])
                S.op("dve", lambda e: e.tensor_scalar(out=thr, in0=thr, scalar1=0.5, scalar2=None, op0=ALU.mult), reads=[hsm], writes=[hsm])
                tt_("dve", ez[:], ctop[:, :, 0:16], thr.unsqueeze(2).broadcast_to([128, 8, 16]), ALU.subtract, [hctop, hsm], [hez])
                act_(ez[:], ez[:], AF.Exp, [hez], [hez])
                S.op("dve", lambda e: e.tensor_reduce(out=sm[:, 1, :].unsqueeze(2), in_=ez[:], axis=AX.X, op=ALU.add), reads=[hez], writes=[hsm])
                S.op("dve", lambda e: e.reciprocal(out=stt[:, 2048:2056], in_=sm[:, 1, :]), reads=[hsm], writes=[hstt])
                act_(sm[:, 2, :], sm[:, 1, :], AF.Ln, [hsm], [hsm])
                tt_("dve", sm[:, 3, :], sm[:, 2, :], thr, ALU.add, [hsm], [hsm])
                s4 = sall[:].rearrange("p (h c) n -> p h c n", c=2)
                st4 = stt[:, 0:2048].rearrange("p (h c n) -> p h c n", h=8, c=2)
                tt_("dve", st4[:, :, 0, :], s4[:, :, 0, :], sm[:, 3, :].unsqueeze(2).broadcast_to([128, 8, 128]), ALU.subtract, [hsall, hsm], [hstt])
                S.op("pool", lambda e: e.tensor_copy(out=st4[:, :, 1, :], in_=s4[:, :, 1, :]), reads=[hsall], writes=[hstt])
                act_(stt[:, 0:2048], stt[:, 0:2048], AF.Exp, [hstt], [hstt])
                S.dma("sp", ST_d[jo], stt[:], hstt, reads=[hstt], writes=[hST[jo]])
            S.barrier()

    def phase_E2(G=4):
        with contextlib.ExitStack() as es:
            T, P = mk(es)
            c = Ctx()
            c.cn, c.hcn = T("id32", [128, 128])
            S.dma("sp", c.cn[:], cn_d[:, C_ID:C_ID + 128], c.hcn, writes=[c.hcn])
            c.cb, c.hcb = T("idbf", [128, 128], BF16)
            S.dma("pool", c.cb[:], cn_d[:, C_ID:C_ID + 128], c.hcb, writes=[c.hcb])
            identb = c.cb[:]
            ident32 = c.cn[:]
            G = min(G, NO)
            xn = [T("xn%d" % i, [128, DC, 128], BF16) for i in range(G)]
            stg = [T("stg%d" % i, [128, 2048 + 8]) for i in range(G)]
            acc = [T("acc%d" % i, [128, D]) for i in range(G)]
            dn32 = [T("dn32_%d" % i, [128, D]) for i in range(2)]
            up32 = [T("up32_%d" % i, [128, D]) for i in range(1)]
            upb = [T("upb%d" % i, [128, 4, D], BF16) for i in range(2)]
            dTs = [T("dT%d" % i, [128, DC, 512], BF16) for i in range(2)]
            EEh = [T("EEh%d" % i, [128, 4, 512]) for i in range(3)]
            Wh = [T("Wh%d" % i, [128, 8, 512], BF16) for i in range(1)]
            geT = [T("geT%d" % i, [128, 512]) for i in range(2)]
            GTb = [T("GTb%d" % i, [128, 4, 128], BF16) for i in range(2)]
            ptr = [P("ptr%d" % i, [128, 4, 128]) for i in range(2)]
            phid = [P("phid%d" % i, [128, 4, 128]) for i in range(2)]
            pwt, hpwt = P("pwt", [128, 4, 128])
            pouts = [P("pout%d" % i, [128, 512]) for i in range(3)]
            pocnt = [0]
            NCH = NE // 512
            import os as _os
            _V = _os.environ.get("KV", "")
            if "n" in _V:
                NCH = 8
            NQ = NCH * 4
            pcount = [0]
            for g0 in range(0, NO, G):
                tiles = list(range(g0, min(NO, g0 + G)))
                nt = len(tiles)
                for i, jo in enumerate(tiles):
                    S.dma("sp", xn[i][0][:], XNT_d[jo].rearrange("p (k t) -> p k t", k=DC), xn[i][1], reads=[hXNT[jo]], writes=[xn[i][1]])
                    S.dma("sp", stg[i][0][:], ST_d[jo], stg[i][1], reads=[hST[jo]], writes=[stg[i][1]])
                    S.dma("sp", acc[i][0][:], HM_d[jo], acc[i][1], reads=[hHM[jo]], writes=[acc[i][1]])

                def load_dn(q):
                    if q < NQ:
                        d_, hd_ = dn32[q % 2]
                        S.dma("sp", d_[:], pd_d[q * 128:(q + 1) * 128, :], hd_, writes=[hd_])

                def load_up(q):
                    if q < NQ:
                        u_, hu_ = up32[0]
                        S.dma("sp", u_[:], pu_d[q * 128:(q + 1) * 128, :], hu_, writes=[hu_])

                def prep_q(q):
                    ec, et = divmod(q, 4)
                    d_, hd_ = dn32[q % 2]
                    dT_, hdT_ = dTs[ec % 2]
                    for k4 in range(4):
                        pt_, hpt_ = ptr[pcount[0] % 2]
                        pcount[0] += 1
                        for kk in range(4):
                            kc = k4 * 4 + kk
                            S.op("pe", lambda e, kk=kk, kc=kc, pt_=pt_: e.transpose(pt_[:, kk, :], d_[:, kc * 128:(kc + 1) * 128], ident32),
                                 reads=[hd_, c.hcn], writes=[hpt_])
                        S.op("dve", lambda e, k4=k4, pt_=pt_: e.tensor_copy(out=dT_[:, k4 * 4:k4 * 4 + 4, et * 128:(et + 1) * 128], in_=pt_[:]),
                             reads=[hpt_], writes=[hdT_])
                    load_dn(q + 2)
                    u_, hu_ = up32[0]
                    ub_, hub_ = upb[ec % 2]
                    S.op("dve", lambda e: e.tensor_copy(out=ub_[:, et, :], in_=u_[:]), reads=[hu_], writes=[hub_])
                    load_up(q + 1)

                def P1(gi):
                    ec, i = divmod(gi, nt)
                    xn_, hxn_ = xn[i]
                    ph_, hph_ = phid[gi % 2]
                    dT_, hdT_ = dTs[ec % 2]
                    for et in range(4):
                        for kc in range(DC):
                            S.op("pe", lambda e, kc=kc, et=et: e.matmul(ph_[:, et, :], lhsT=dT_[:, kc, et * 128:(et + 1) * 128], rhs=xn_[:, kc, :], start=(kc == 0), stop=(kc == DC - 1)),
                                 reads=[hxn_, hdT_], writes=[hph_])

                def A1(gi):
                    ec, i = divmod(gi, nt)
                    st_, hst_ = stg[i]
                    ph_, hph_ = phid[gi % 2]
                    ge_, hge_ = geT[gi % 2]
                    st4 = st_[:, 0:2048].rearrange("p (h c n) -> p h c n", h=8, c=2)
                    for half in range(2):
                        ee_, hee_ = EEh[(gi * 2 + half) % 3]
                        for h in range(4):
                            hh = half * 4 + h
                            for a in range(4):
                                n1 = ec * 4 + a
                                act_(ee_[:, h, a * 128:(a + 1) * 128], st4[:, hh, 1, :], AF.Copy, [hst_], [hee_], scale=st4[:, hh, 0, n1:n1 + 1])
                    act_(ge_[:], ph_[:].rearrange("p a t -> p (a t)"), AF.Gelu, [hph_], [hge_])

                def D1(gi):
                    ec, i = divmod(gi, nt)
                    st_, hst_ = stg[i]
                    wh_, hwh_ = Wh[0]
                    for half in range(2):
                        ee_, hee_ = EEh[(gi * 2 + half) % 3]
                        for h in range(4):
                            hh = half * 4 + h
                            S.op("dve", lambda e, h=h, hh=hh, ee_=ee_: e.scalar_tensor_tensor(
                                out=wh_[:, hh, :], in0=ee_[:, h, :], scalar=st_[:, 2048 + hh:2049 + hh],
                                in1=ee_[:, h, :], op0=ALU.is_ge, op1=ALU.mult), reads=[hee_, hst_], writes=[hwh_])

                def P2(gi):
                    wh_, hwh_ = Wh[0]
                    for et in range(4):
                        for h in range(8):
                            S.op("pe", lambda e, et=et, h=h: e.matmul(pwt[:, et, :], lhsT=wh_[:, h, et * 128:(et + 1) * 128], rhs=identb, start=(h == 0), stop=(h == 7)),
                                 reads=[hwh_, c.hcb], writes=[hpwt])

                def D2(gi):
                    ge_, hge_ = geT[gi % 2]
                    gt_, hgt_ = GTb[gi % 2]
                    tt_("dve", gt_[:].rearrange("p a t -> p (a t)"), ge_[:], pwt[:].rearrange("p a t -> p (a t)"), ALU.mult, [hge_, hpwt], [hgt_])

                def P3D3(gi):
                    ec, i = divmod(gi, nt)
                    u_, hu_ = upb[ec % 2]
                    gt_, hgt_ = GTb[gi % 2]
                    ac_, hac_ = acc[i]
                    for dg in range(4):
                        po_, hpo_ = pouts[pocnt[0] % 3]
                        pocnt[0] += 1
                        for et in range(4):
                            S.op("pe", lambda e, dg=dg, et=et, po_=po_: e.matmul(po_[:], lhsT=gt_[:, et, :], rhs=u_[:, et, dg * 512:(dg + 1) * 512], start=(et == 0), stop=(et == 3)),
                                 reads=[hgt_, hu_], writes=[hpo_])
                        tt_("dve", ac_[:, dg * 512:(dg + 1) * 512], ac_[:, dg * 512:(dg + 1) * 512], po_[:], ALU.add, [hac_, hpo_], [hac_])

                NG = NCH * nt
                load_dn(0)
                load_dn(1)
                load_up(0)
                for q in range(4):
                    prep_q(q)
                P1(0)
                A1(0)
                D1(0)
                for gi in range(NG):
                    ec, i = divmod(gi, nt)
                    if ec + 1 < NCH:
                        for et in range(i * 4 // nt, (i + 1) * 4 // nt):
                            prep_q((ec + 1) * 4 + et)
                    if gi + 1 < NG:
                        P1(gi + 1)
                        A1(gi + 1)
                    P2(gi)
                    D2(gi)
                    if gi + 1 < NG:
                        D1(gi + 1)
                    P3D3(gi)
                for i, jo in enumerate(tiles):
                    S.dma("sp", yout[jo * 128:(jo + 1) * 128, :], acc[i][0][:], acc[i][1], reads=[acc[i][1]], writes=[hOUT[jo]])
            S.barrier()

    ph = {"A": phase_A, "B": phase_B, "C": phase_C, "D": phase_D, "E": phase_E1, "F": phase_E2}
    ctx = dict(nc=nc, S=S, mk=mk, load_consts=load_consts, rmsnorm_T=rmsnorm_T, locals=locals())
    for p in phases:
        if p in ph:
            ph[p]()
    return nc, ctx


def _colT(v, n):
    buf = np.zeros(n * 128, np.float32)
    buf[:v.size] = v.reshape(-1)
    return buf.reshape(n, 128).T


def make_vecs(inp):
    vecs = np.zeros((128, NV), np.float32)
    vecs[:, V_G1:V_G1 + 16] = _colT(inp["norm1_g"][0], 16)
    vecs[:, V_MU:V_MU + 27] = _colT(inp["shift_mu"][0], 27)
    for col, key in ((V_W0, "w0"), (V_A0, "a0"), (V_KK, "k_k"), (V_KA, "k_a"), (V_GNW, "gn_w"),
                     (V_GNB, "gn_b"), (V_RK, "r_k")):
        vecs[:, col:col + 8] = _colT(inp[key][0], 8)
    vecs[:, V_QG] = np.tile(inp["q_gain"][0], 2)
    vecs[:, V_KG] = np.tile(inp["k_gain"][0], 2)
    sk = inp["sinks"][0]
    for hp in range(8):
        vecs[0:64, V_SK + hp] = sk[2 * hp]
        vecs[64:128, V_SK + hp] = sk[2 * hp + 1]
    vecs[:, V_G2:V_G2 + 16] = _colT(inp["norm2_g"][0], 16)
    return vecs


def make_consts(first_half):
    cn = np.zeros((128, NCN), np.float32)
    cn[:, C_ID:C_ID + 128] = np.eye(128, dtype=np.float32)
    bo = np.zeros((128, 128), np.float32)
    bo[0:64, 0:64] = 1
    bo[64:, 64:] = 1
    cn[:, C_BO:C_BO + 128] = bo
    s = np.arange(64)[:, None]
    t = np.arange(64)[None, :]
    cn[0:64, C_MU:C_MU + 64] = (s < t)
    cn[0:64, C_MUI:C_MUI + 64] = (s <= t)
    cn[0:64, C_ML:C_ML + 64] = (s > t)
    s = np.arange(128)[:, None]
    q = np.arange(128)[None, :]
    cn[:, C_MC:C_MC + 128] = (s <= q)
    cn[:, C_MP:C_MP + 128] = (s > q)
    mpf = (s > q)
    if first_half:
        mpf = mpf & (s >= 112)
    cn[:, C_MPF:C_MPF + 128] = mpf
    for g in range(2):
        sel = np.zeros((128, 128), np.float32)
        for m in range(128):
            sel[g * 64 + (m % 64), m] = 1
        cn[:, C_SEL + g * 128:C_SEL + (g + 1) * 128] = sel
    return cn


_NC_CACHE = {}


def kernel(**inputs):
    inp = {k: np.asarray(v) for k, v in inputs.items()}
    x = inp["x"].astype(np.float32, copy=False)
    B, SEQ, _ = x.shape
    NB, OWN0 = 33, 17
    if "nc" not in _NC_CACHE:
        _NC_CACHE["nc"] = build(NB=NB, OWN0=OWN0, phases="ABCDEF")[0]
    nc = _NC_CACHE["nc"]
    f = lambda k: np.ascontiguousarray(inp[k][0], dtype=np.float32)
    shared = dict(w_in=f("w_in"), vecs=make_vecs(inp), w_up=f("w_up"), a_up=f("a_up"), g_up=f("g_up"),
                  w_out=f("w_out"), peer_query=f("peer_query"),
                  sub_keys=np.ascontiguousarray(inp["peer_sub_keys"][0].reshape(16, 128, 128), dtype=np.float32),
                  peer_down=f("peer_down"), peer_up=f("peer_up"))
    meta = inp["meta_tokens"].astype(np.float32, copy=False)
    cn = [make_consts(False), make_consts(True)]
    in_maps = []
    for c in range(8):
        b, s = c // 2, c % 2
        loc = np.zeros((NB * 128, D), np.float32)
        if s == 0:
            loc[16 * 128 + 112:17 * 128] = meta
            loc[17 * 128:] = x[b, :2048]
        else:
            loc[112:128] = meta
            loc[128:] = x[b]
        in_maps.append(dict(xin=loc, consts=cn[1 if s == 0 else 0], **shared))
    res = run_bass_kernel_spmd(nc, in_maps, core_ids=list(range(8)))
    out = np.empty((B, SEQ, D), np.float32)
    for c in range(8):
        b, s = c // 2, c % 2
        out[b, s * 2048:(s + 1) * 2048] = np.asarray(res.results[c]["yout"])
    return out
```

```python
import contextlib
import numpy as np
import concourse.bass as bass
import concourse.mybir as mybir
from concourse.bass_utils import run_bass_kernel_spmd

F32 = mybir.dt.float32
BF16 = mybir.dt.bfloat16
AF = mybir.ActivationFunctionType
ALU = mybir.AluOpType
AX = mybir.AxisListType

D = 2048
DC = 16
INC = 4640
RW0 = 1280
NRC = 27
NE = 16384
HD = 64

V_G1, V_MU, V_W0, V_A0, V_KK, V_KA, V_GNW, V_GNB, V_RK, V_QG, V_KG, V_SK, V_G2 = (
    0, 16, 43, 51, 59, 67, 75, 83, 91, 99, 100, 101, 109)
NV = 125
C_ID, C_BO, C_MU, C_MUI, C_ML, C_MC, C_MP, C_MPF, C_SEL = 0, 128, 256, 320, 384, 448, 576, 704, 832
NCN = 832 + 256


class H:
    __slots__ = ("name", "w", "r", "dsem", "dcnt")

    def __init__(self, name=""):
        self.name = name
        self.w = {}
        self.r = {}
        self.dsem = None
        self.dcnt = 0


class Sched:
    def __init__(self, nc, needed=None):
        self.nc = nc
        self.engs = {"pe": nc.tensor, "act": nc.scalar, "dve": nc.vector,
                     "pool": nc.gpsimd, "sp": nc.sync}
        self.esem = {k: nc.alloc_semaphore(name="es_" + k) for k in self.engs}
        self.seq = {k: 0 for k in self.engs}
        self.cnt = {k: 0 for k in self.engs}
        self.cntmap = {k: {} for k in self.engs}
        self.waited = {k: {} for k in self.engs}
        self.needed = needed
        self.rec = {k: set() for k in self.engs}
        self.sems = {}
        self.dcur = {}
        self._rec = None

    def _wait(self, e, evs):
        w = self.waited[e]
        for key, val in evs.items():
            if w.get(key, 0) >= val:
                continue
            if isinstance(key, str):
                self.rec[key].add(val)
                real = self.cntmap[key][val] if self.needed is not None else val
                self.engs[e].wait_ge(self.esem[key], real)
            else:
                self.engs[e].wait_ge(self.sems[key], val)
            w[key] = val

    @staticmethod
    def _merge(d, evs):
        for k, v in evs.items():
            if d.get(k, 0) < v:
                d[k] = v

    def _deps(self, reads, writes, own=None):
        evs = {}
        for h in reads:
            self._merge(evs, h.w)
        if own == "pe":
            evs.pop(own, None)
        ww = {}
        for h in writes:
            self._merge(ww, h.w)
            self._merge(ww, h.r)
        if own is not None:
            ww.pop(own, None)
        self._merge(evs, ww)
        return evs

    def _pe_mode(self, mode):
        if mode != getattr(self, "pe_mode", None):
            if self.seq["pe"] > 0:
                self._wait("pe", {"pe": self.seq["pe"]})
            self.pe_mode = mode

    def op(self, e, fn, reads=(), writes=()):
        if self._rec is not None:
            r = _RecEng()
            fn(r)
            name, args, kwargs = r.call
            self._rec.append(("op", e, name, args, kwargs, tuple(reads), tuple(writes)))
            return None
        self._wait(e, self._deps(reads, writes, own=e))
        inst = fn(_PEProxy(self) if e == "pe" else self.engs[e])
        self.seq[e] += 1
        n = self.seq[e]
        if self.needed is None or n in self.needed[e]:
            self.cnt[e] += 1
            inst.then_inc(self.esem[e], 1)
            self.cntmap[e][n] = self.cnt[e]
        ev = {e: n}
        for h in reads:
            self._merge(h.r, ev)
        for h in writes:
            h.w = dict(ev)
            h.r = {}
        return inst

    def mark(self):
        if self._rec is not None:
            self._rec.append(("mark",))

    def record(self, f):
        self._rec = []
        try:
            f()
        finally:
            rec, self._rec = self._rec, None
        return rec

    def play(self, items):
        for it in items:
            if it[0] == "op":
                _, e, name, args, kwargs, reads, writes = it
                self.op(e, lambda eng: getattr(eng, name)(*args, **kwargs), reads, writes)
            elif it[0] == "dma":
                _, q, out, in_, tile, reads, writes, kw = it
                self.dma(q, out, in_, tile, reads, writes, **kw)

    def play2(self, a, b):
        na, nb = len(a), len(b)
        ia = ib = 0
        CH = 32
        while ia < na or ib < nb:
            if ib >= nb or (ia < na and ia * nb <= ib * na):
                self.play(a[ia:ia + CH])
                ia += CH
            else:
                self.play(b[ib:ib + CH])
                ib += CH

    def dma(self, q, out, in_, tile, reads=(), writes=(), **kw):
        if self._rec is not None:
            self._rec.append(("dma", q, out, in_, tile, tuple(reads), tuple(writes), kw))
            return None
        if tile.dsem is None:
            tile.dsem = self.nc.alloc_semaphore(name="ds_%d" % len(self.sems))
            self.sems[tile.dsem.num] = tile.dsem
        deps = self._deps(reads, writes)
        if tile in writes and not tile.r and tile.w.get(tile.dsem.num, 0) == tile.dcnt and len(tile.w) == 1:
            deps.pop(tile.dsem.num, None)
        self._wait(q, deps)
        inst = self.engs[q].dma_start(out=out, in_=in_, **kw)
        tile.dcnt += 16
        inst.then_inc(tile.dsem, 16)
        self.dcur[tile.dsem.num] = tile.dcnt
        ev = {tile.dsem.num: tile.dcnt}
        for h in reads:
            self._merge(h.r, ev)
        for h in writes:
            h.w = dict(ev)
            h.r = {}
        return inst

    def barrier(self):
        evs = {k: self.seq[k] for k in self.engs if self.seq[k] > 0}
        evs.update(self.dcur)
        for e in self.engs:
            self._wait(e, evs)


class _RecEng:
    def __getattr__(self, name):
        def f(*args, **kwargs):
            self.call = (name, args, kwargs)
            return None
        return f


def _rnd(n):
    return 32 if n <= 32 else (64 if n <= 64 else 128)


class _PEProxy:
    def __init__(self, S):
        self.S = S

    def matmul(self, out, lhsT, rhs, **kw):
        fr = 1
        for d in lhsT.shape[1:]:
            fr *= d
        self.S._pe_mode(("mm", _rnd(lhsT.shape[0]), _rnd(fr), str(lhsT.dtype), lhsT.base_partition(), out.base_partition()))
        return self.S.nc.tensor.matmul(out, lhsT=lhsT, rhs=rhs, **kw)

    def transpose(self, out, in_, identity):
        fr = 1
        for d in in_.shape[1:]:
            fr *= d
        self.S._pe_mode(("tr", _rnd(in_.shape[0]), _rnd(fr), str(in_.dtype), in_.base_partition(), out.base_partition()))
        return self.S.nc.tensor.transpose(out, in_, identity)


class Ctx:
    pass


def build(NB=33, OWN0=17, phases="ABCDEF", dbg=False):
    _, ctx = _build(NB, OWN0, phases, dbg, None)
    return _build(NB, OWN0, phases, dbg, ctx["S"].rec)


def _build(NB, OWN0, phases, dbg, needed):
    NO = NB - OWN0
    nc = bass.Bass("TRN2", target_bir_lowering=False)
    S = Sched(nc, needed)

    def din(name, shape, dt=F32):
        return nc.dram_tensor(name, list(shape), dt, kind="ExternalInput").ap()

    def dscr(name, shape, dt=F32, out=False):
        return nc.dram_tensor(name, list(shape), dt, kind=("ExternalOutput" if out else "Internal")).ap()

    xin = din("xin", [NB * 128, D])
    w_in = din("w_in", [D, INC])
    vecs_d = din("vecs", [128, NV])
    cn_d = din("consts", [128, NCN])
    wup_d = din("w_up", [64, 1024])
    aup_d = din("a_up", [64, 1024])
    gup_d = din("g_up", [160, 1024])
    wout_d = din("w_out", [D, D])
    pq_d = din("peer_query", [D, D])
    sk_d = din("sub_keys", [16, 128, 128])
    pd_d = din("peer_down", [NE, D])
    pu_d = din("peer_up", [NE, D])
    yout = dscr("yout", [NO * 128, D], F32, out=True)

    PS_d = dscr("PS_s", [NB, 128, NRC * 128])
    QKV_d = dscr("QKV_s", [NO + 1, 128, 10 * 128])
    YR_d = dscr("YR_s", [NO, 128, 1024], BF16, out=dbg)
    YA_d = dscr("YA_s", [NO, 128, 1024], BF16, out=dbg)
    HM_d = dscr("HM_s", [NO, 128, D], F32, out=dbg)
    XNT_d = dscr("XNT_s", [NO, 128, D], BF16)
    ST_d = dscr("ST_s", [NO, 128, 2048 + 8])
    hPS = [H("PS%d" % j) for j in range(NB)]
    hQKV = [H("QKV%d" % j) for j in range(NO + 1)]
    hYR = [H() for j in range(NO)]
    hYA = [H() for j in range(NO)]
    hHM = [H() for j in range(NO)]
    hXNT = [H() for j in range(NO)]
    hST = [H() for j in range(NO)]
    hOUT = [H() for j in range(NO)]

    uid = [0]

    def mk(es):
        def T(name, shape, dt=F32):
            uid[0] += 1
            t = es.enter_context(nc.sbuf_tensor("sb%d_%s" % (uid[0], name), list(shape), dt))
            return t, H(name)

        def P(name, shape, dt=F32):
            uid[0] += 1
            t = es.enter_context(nc.psum_tensor("ps%d_%s" % (uid[0], name), list(shape), dt))
            return t, H(name)
        return T, P

    def load_consts(T, bf=True):
        c = Ctx()
        c.vec, c.hvec = T("vecs", [128, NV])
        S.dma("sp", c.vec[:], vecs_d, c.hvec, writes=[c.hvec])
        c.cn, c.hcn = T("cn32", [128, NCN])
        S.dma("sp", c.cn[:], cn_d, c.hcn, writes=[c.hcn])
        c.cb, c.hcb = T("cnbf", [128, NCN], BF16)
        S.dma("pool", c.cb[:], cn_d, c.hcb, writes=[c.hcb])
        return c

    def rmsnorm_T(c, T_, xt, hxt, gcol, junk, hjunk, st, hst, xs, hxs, pT, hpT, uT, huT):
        S.op("act", lambda e: e.activation(out=junk[:], in_=xt[:], func=AF.Square, accum_out=st[:, 0:1]),
             reads=[hxt], writes=([hjunk, hst] if hjunk is not hst else [hst]))
        S.op("dve", lambda e: e.tensor_scalar(out=st[:, 1:2], in0=st[:, 0:1], scalar1=1.0 / D, scalar2=1e-6,
                                              op0=ALU.mult, op1=ALU.add), reads=[hst], writes=[hst])
        S.op("act", lambda e: e.activation(out=st[:, 2:3], in_=st[:, 1:2], func=AF.Sqrt), reads=[hst], writes=[hst])
        S.op("dve", lambda e: e.reciprocal(out=st[:, 3:4], in_=st[:, 2:3]), reads=[hst], writes=[hst])
        S.op("act", lambda e: e.activation(out=xs[:], in_=xt[:], func=AF.Copy, scale=st[:, 3:4]),
             reads=[hxt, hst], writes=[hxs])
        for k in range(DC):
            S.op("pe", lambda e, k=k: e.transpose(pT[:, k, :], xs[:, k * 128:(k + 1) * 128], c.cb[:, C_ID:C_ID + 128]),
                 reads=[hxs, c.hcb], writes=[hpT])
        gb = c.vec[:, gcol:gcol + DC].unsqueeze(2).broadcast_to([128, DC, 128])
        S.op("dve", lambda e: e.tensor_tensor(out=uT[:], in0=pT[:], in1=gb, op=ALU.mult),
             reads=[hpT, c.hvec], writes=[huT])

    def phase_A():
        with contextlib.ExitStack() as es:
            T, P = mk(es)
            c = load_consts(T)
            win, hwin = T("win", [128, DC, INC], BF16)
            for k in range(DC):
                S.dma("pool", win[:, k, :], w_in[k * 128:(k + 1) * 128, :], hwin, writes=[hwin])
            xt2 = [T("xt%d" % i, [128, D]) for i in range(1)] * 2
            st2 = [T("st%d" % i, [128, 4]) for i in range(2)]
            xs2 = [T("xs%d" % i, [128, D], BF16) for i in range(1)] * 2
            pT2 = [P("pT%d" % i, [128, DC, 128], BF16) for i in range(2)]
            uT2 = [T("uT%d" % i, [128, DC, 128], BF16) for i in range(2)]
            pp = [P("pp%d" % i, [128, 4, 128]) for i in range(4)]
            PT, hPT = T("PT", [128, NRC, 129])
            QK, hQK = T("QK", [128, 10, 128])
            dd, hdd = T("dd", [128, NRC, 128])
            pss, hpss = dd, hdd
            S.op("pool", lambda e: e.memset(PT[:], 0.0), writes=[hPT])
            mub = c.vec[:, V_MU:V_MU + NRC].unsqueeze(2).broadcast_to([128, NRC, 128])
            def body_norm(j):
                xt, hxt = xt2[j % 2]
                st, hst = st2[j % 2]
                xs, hxs = xs2[j % 2]
                pT, hpT = pT2[j % 2]
                uT, huT = uT2[j % 2]
                S.dma("sp", xt[:], xin[j * 128:(j + 1) * 128, :], hxt, writes=[hxt])
                rmsnorm_T(c, T, xt, hxt, V_G1, xs, hxs, st, hst, xs, hxs, pT, hpT, uT, huT)

            def body_proj(j):
                uT, huT = uT2[j % 2]
                need_att = (j >= OWN0 - 1)
                chunks = list(range(0 if need_att else 10, 37))
                gi = 0
                for g0 in range(0, len(chunks), 4):
                    grp = chunks[g0:g0 + 4]
                    pt_, hp_ = pp[gi % 4]
                    gi += 1
                    for qi, ch in enumerate(grp):
                        M = 32 if ch == 36 else 128
                        for k in range(DC):
                            S.op("pe", lambda e, qi=qi, ch=ch, k=k, M=M, pt_=pt_: e.matmul(
                                pt_[0:M, qi, :], lhsT=win[:, k, ch * 128:ch * 128 + M], rhs=uT[:, k, :],
                                start=(k == 0), stop=(k == DC - 1)), reads=[hwin, huT], writes=[hp_])
                    for qi, ch in enumerate(grp):
                        M = 32 if ch == 36 else 128
                        eng = "act" if (qi % 2 == 0) else "dve"
                        if ch < 10:
                            dst, hd = QK[0:M, ch, :], hQK
                        else:
                            dst, hd = PT[0:M, ch - 10, 1:129], hPT
                        if eng == "act":
                            S.op("act", lambda e, dst=dst, qi=qi, M=M, pt_=pt_: e.activation(out=dst, in_=pt_[0:M, qi, :], func=AF.Copy),
                                 reads=[hp_], writes=[hd])
                        else:
                            S.op("dve", lambda e, dst=dst, qi=qi, M=M, pt_=pt_: e.tensor_copy(out=dst, in_=pt_[0:M, qi, :]),
                                 reads=[hp_], writes=[hd])
                if need_att:
                    ja = j - (OWN0 - 1)
                    S.dma("sp", QKV_d[ja].rearrange("p (c t) -> p c t", c=10), QK[:], hQK, reads=[hQK], writes=[hQKV[ja]])
                S.op("dve", lambda e: e.tensor_tensor(out=dd[:], in0=PT[:, :, 0:128], in1=PT[:, :, 1:129], op=ALU.subtract),
                     reads=[hPT], writes=[hdd])
                S.op("dve", lambda e: e.tensor_tensor(out=dd[:], in0=dd[:], in1=mub, op=ALU.mult),
                     reads=[hdd, c.hvec], writes=[hdd])
                S.op("dve", lambda e: e.tensor_tensor(out=dd[:], in0=dd[:], in1=PT[:, :, 1:129], op=ALU.add),
                     reads=[hdd, hPT], writes=[hdd])
                S.op("act", lambda e: e.activation(out=PT[:, :, 0:1], in_=PT[:, :, 128:129], func=AF.Copy),
                     reads=[hPT, hdd, hpss], writes=[hPT])
                S.dma("sp", PS_d[j].rearrange("p (c t) -> p c t", c=NRC), pss[:], hpss, reads=[hpss], writes=[hPS[j]])

            S.play(S.record(lambda: body_norm(0)))
            for j in range(NB):
                ra = S.record(lambda: body_proj(j))
                if j + 1 < NB:
                    rb = S.record(lambda: body_norm(j + 1))
                    S.play2(ra, rb)
                else:
                    S.play(ra)
            S.barrier()

    def phase_B():
        with contextlib.ExitStack() as es:
            T, P = mk(es)
            c = load_consts(T)
            vec = c.vec

            def vb(col, n=8):
                return vec[:, col:col + n].unsqueeze(2).broadcast_to([128, n, 128])
            wup, hwup = T("wup", [128, 1024], BF16)
            aup, haup = T("aup", [128, 1024], BF16)
            gup, hgup = T("gup", [128, 2, 1024], BF16)
            S.dma("pool", wup[0:64, :], wup_d, hwup, writes=[hwup])
            S.dma("pool", aup[64:128, :], aup_d, haup, writes=[haup])
            S.dma("pool", gup[:, 0, :], gup_d[0:128, :], hgup, writes=[hgup])
            S.dma("pool", gup[0:32, 1, :], gup_d[128:160, :], hgup, writes=[hgup])
            ps, hps = T("ps", [128, NRC, 128])
            names32 = ["nld", "aa", "kap", "kp", "beta", "cs", "cum", "t1", "t2", "t3"]
            F = {}
            for n in names32:
                F[n] = T(n, [128, 8, 128])
            namesbf = ["Bh", "Qh", "BG", "QG", "vb", "sqb"]
            Bf = {}
            for n in namesbf:
                Bf[n] = T(n, [128, 8, 128], BF16)
            lw, hlw = T("lw", [128, 128], BF16)
            sg, hsg = T("sg", [128, 2, 128], BF16)
            ones, hones = T("ones", [128, 1024])
            S.op("pool", lambda e: e.memset(ones[:], 1.0), writes=[hones])
            S32, hS32 = T("S32", [128, 8, 64])
            Sb, hSb = T("Sb", [128, 8, 64], BF16)
            S.op("pool", lambda e: e.memset(S32[:], 0.0), writes=[hS32])
            S.op("pool", lambda e: e.memset(Sb[:], 0.0), writes=[hSb])
            gms = [{}, {}]
            for si in range(2):
                for n in ["Ub", "Lb", "U2", "L2", "Xb", "SAb"]:
                    gms[si][n] = T(n + str(si), [64, 16, 64], BF16)
            gms2 = [[{}, {}], [{}, {}]]
            for pp_ in range(2):
                for si in range(2):
                    for n in ["Lak", "Mrb", "Mrk", "Qb"]:
                        gms2[pp_][si][n] = T("%s%d_%d" % (n, si, pp_), [64, 16, 64], BF16)
            F2 = {n: [T("%s_%d" % (n, i), [128, 8, 128]) for i in range(2)] for n in ("gg", "bon")}
            Bf2 = {n: [T("%s_%d" % (n, i), [128, 8, 128], BF16) for i in range(2)] for n in ("Kh", "Rh")}
            sm2 = [T("sm_%d" % i, [128, 8, 2, 4]) for i in range(2)]
            Vt2 = [T("Vt_%d" % i, [64, 2, 8, 128], BF16) for i in range(2)]
            BGt2 = [T("BGt_%d" % i, [64, 2, 8, 128], BF16) for i in range(2)]
            QGt2 = [T("QGt_%d" % i, [64, 2, 8, 128], BF16) for i in range(2)]
            ysb, hysb = T("ysb", [64, 16, 64])
            yc, hyc = T("yc", [64, 16, 64])
            ysq, hysq = ysb, hysb
            gst, hgst = T("gst", [64, 16, 4])
            yf, hyf = T("yf", [128, 8, 128])
            yrb, hyrb = T("yrb", [128, 8, 128], BF16)
            PA = [P("PA%d" % i, [128, 1024]) for i in range(3)]
            PTr, hPTr = P("PTr", [128, 2048], BF16)
            pi = [0]

            def nextPA():
                t = PA[pi[0] % 2]
                pi[0] += 1
                return t

            def nextPB():
                return PA[2]
            maskU = c.cn[0:64, C_MU:C_MU + 64].unsqueeze(1).broadcast_to([64, 16, 64])
            maskUi = c.cn[0:64, C_MUI:C_MUI + 64].unsqueeze(1).broadcast_to([64, 16, 64])
            maskL = c.cn[0:64, C_ML:C_ML + 64].unsqueeze(1).broadcast_to([64, 16, 64])
            identb = c.cb[:, C_ID:C_ID + 128]
            ident32 = c.cn[:, C_ID:C_ID + 128]
            bones = c.cb[:, C_BO:C_BO + 128]

            def tt(eng, out, i0, i1, op, reads, writes):
                S.op(eng, lambda e: e.tensor_tensor(out=out, in0=i0, in1=i1, op=op), reads=reads, writes=writes)

            def actf(out, in_, func, reads, writes, **kw):
                S.op("act", lambda e: e.activation(out=out, in_=in_, func=func, **kw), reads=reads, writes=writes)

            HORD = list(range(0, 16, 2)) + list(range(1, 16, 2))
            def body(j):
                own = j >= OWN0
                p = j % 2
                F["gg"], F["bon"] = F2["gg"][p], F2["bon"][p]
                Bf["Kh"], Bf["Rh"] = Bf2["Kh"][p], Bf2["Rh"][p]
                sm, hsm = sm2[p]
                Vt, hVt = Vt2[p]
                BGt, hBGt = BGt2[p]
                QGt, hQGt = QGt2[p]
                for si in range(2):
                    gms[si].update(gms2[p][si])
                S.dma("sp", ps[:], PS_d[j].rearrange("p (c t) -> p c t", c=NRC), hps, reads=[hPS[j]], writes=[hps])
                r_, k_, v_ = ps[:, 0:8, :], ps[:, 8:16, :], ps[:, 16:24, :]
                actf(lw[0:64, :], ps[0:64, 24, :], AF.Tanh, [hps], [hlw])
                actf(lw[64:128, :], ps[64:128, 24, :], AF.Copy, [hps], [hlw])
                actf(sg[:, 0, :], ps[:, 25, :], AF.Sigmoid, [hps], [hsg])
                actf(sg[0:32, 1, :], ps[0:32, 26, :], AF.Sigmoid, [hps], [hsg])
                pw_, hpw = nextPA()
                pa_, hpa = nextPA()
                for m in range(8):
                    S.op("pe", lambda e, m=m: e.matmul(pw_[:, m * 128:(m + 1) * 128], lhsT=wup[0:64, m * 128:(m + 1) * 128], rhs=lw[0:64, :], start=True, stop=True),
                         reads=[hwup, hlw], writes=[hpw])
                    S.op("pe", lambda e, m=m: e.matmul(pa_[:, m * 128:(m + 1) * 128], lhsT=aup[64:128, m * 128:(m + 1) * 128], rhs=lw[64:128, :], start=True, stop=True),
                         reads=[haup, hlw], writes=[hpa])
                nld, hnld = F["nld"]
                aa, haa = F["aa"]
                t1, ht1 = F["t1"]
                t2, ht2 = F["t2"]
                t3, ht3 = F["t3"]
                v3 = lambda p_: p_[:].rearrange("p (m t) -> p m t", m=8)
                tt("dve", t1[:], v3(pw_), vb(V_W0), ALU.add, [hpw, c.hvec], [ht1])
                actf(t1[:], t1[:], AF.Sigmoid, [ht1], [ht1])
                S.op("dve", lambda e: e.tensor_scalar(out=nld[:], in0=t1[:], scalar1=0.6065306597126334, scalar2=None, op0=ALU.mult),
                     reads=[ht1], writes=[hnld])
                tt("dve", t2[:], v3(pa_), vb(V_A0), ALU.add, [hpa, c.hvec], [ht2])
                actf(aa[:], t2[:], AF.Sigmoid, [ht2], [haa])
                if own:
                    pg_, hpg = nextPA()
                    for m in range(8):
                        S.op("pe", lambda e, m=m: e.matmul(pg_[:, m * 128:(m + 1) * 128], lhsT=gup[:, 0, m * 128:(m + 1) * 128], rhs=sg[:, 0, :], start=True, stop=False),
                             reads=[hgup, hsg], writes=[hpg])
                        S.op("pe", lambda e, m=m: e.matmul(pg_[:, m * 128:(m + 1) * 128], lhsT=gup[0:32, 1, m * 128:(m + 1) * 128], rhs=sg[0:32, 1, :], start=False, stop=True),
                             reads=[hgup, hsg], writes=[hpg])
                    gg, hgg = F["gg"]
                    actf(gg[:], v3(pg_), AF.Copy, [hpg], [hgg])
                kap, hkap = F["kap"]
                sqb, hsqb = Bf["sqb"]
                tt("dve", kap[:], k_, vb(V_KK), ALU.mult, [hps, c.hvec], [hkap])
                actf(sqb[:], kap[:], AF.Square, [hkap], [hsqb])
                pq_, hpq = nextPA()
                for hh in range(2):
                    S.op("pe", lambda e, hh=hh: e.matmul(pq_[:, hh * 512:(hh + 1) * 512], lhsT=bones, rhs=sqb[:, hh * 4:(hh + 1) * 4, :], start=True, stop=True),
                         reads=[c.hcb, hsqb], writes=[hpq])
                actf(t3[:], v3(pq_), AF.Sqrt, [hpq], [ht3])
                S.op("dve", lambda e: e.tensor_scalar(out=t3[:], in0=t3[:], scalar1=1e-12, scalar2=None, op0=ALU.max), reads=[ht3], writes=[ht3])
                S.op("dve", lambda e: e.reciprocal(out=t3[:], in_=t3[:]), reads=[ht3], writes=[ht3])
                tt("dve", kap[:], kap[:], t3[:], ALU.mult, [hkap, ht3], [hkap])
                kp, hkp = F["kp"]
                S.op("dve", lambda e: e.scalar_tensor_tensor(out=t2[:], in0=aa[:], scalar=1.0, in1=vb(V_KA), op0=ALU.subtract, op1=ALU.mult),
                     reads=[haa, c.hvec], writes=[ht2])
                S.op("dve", lambda e: e.scalar_tensor_tensor(out=kp[:], in0=t2[:], scalar=1.0, in1=k_, op0=ALU.add, op1=ALU.mult),
                     reads=[ht2, hps], writes=[hkp])
                beta, hbeta = F["beta"]
                tt("dve", beta[:], kap[:], aa[:], ALU.mult, [hkap, haa], [hbeta])
                cs, hcs = F["cs"]
                cum, hcum = F["cum"]
                S.op("dve", lambda e: e.tensor_tensor_scan(out=cs[:].rearrange("p m t -> p (m t)"), data0=ones[:],
                                                           data1=nld[:].rearrange("p m t -> p (m t)"), initial=0.0,
                                                           op0=ALU.mult, op1=ALU.add), reads=[hones, hnld], writes=[hcs])
                cs4 = cs[:].rearrange("p m (s t) -> p m s t", s=2)
                nld4 = nld[:].rearrange("p m (s t) -> p m s t", s=2)
                cum4 = cum[:].rearrange("p m (s t) -> p m s t", s=2)
                tt("dve", sm[:, :, :, 0:1], cs4[:, :, :, 0:1], nld4[:, :, :, 0:1], ALU.subtract, [hcs, hnld], [hsm])
                tt("dve", cum4, cs4, sm[:, :, :, 0:1].broadcast_to([128, 8, 2, 64]), ALU.subtract, [hcs, hsm], [hcum])
                actf(sm[:, :, :, 2:3], cum4[:, :, :, 63:64], AF.Exp, [hcum], [hsm], scale=-1.0)
                Kh, hKh = Bf["Kh"]
                Rh, hRh = Bf["Rh"]
                Bh, hBh = Bf["Bh"]
                Qh, hQh = Bf["Qh"]
                BG, hBG = Bf["BG"]
                QG, hQG = Bf["QG"]
                vbf, hvbf = Bf["vb"]
                actf(t1[:], cum[:], AF.Exp, [hcum], [ht1], scale=-1.0)
                tt("dve", Rh[:], r_, t1[:], ALU.mult, [hps, ht1], [hRh])
                tt("dve", t2[:], cum[:], nld[:], ALU.subtract, [hcum, hnld], [ht2])
                actf(t2[:], t2[:], AF.Exp, [ht2], [ht2], scale=-1.0)
                tt("dve", Kh[:], kap[:], t2[:], ALU.mult, [hkap, ht2], [hKh])
                actf(t3[:], cum[:], AF.Exp, [hcum], [ht3])
                tt("dve", Bh[:], beta[:], t3[:], ALU.mult, [hbeta, ht3], [hBh])
                tt("dve", Qh[:], kp[:], t3[:], ALU.mult, [hkp, ht3], [hQh])
                t14 = t1[:].rearrange("p m (s t) -> p m s t", s=2)
                tt("dve", t14, cum4, cum4[:, :, :, 63:64].broadcast_to([128, 8, 2, 64]), ALU.subtract, [hcum], [ht1])
                actf(t1[:], t1[:], AF.Exp, [ht1], [ht1])
                tt("dve", BG[:], beta[:], t1[:], ALU.mult, [hbeta, ht1], [hBG])
                tt("dve", QG[:], kp[:], t1[:], ALU.mult, [hkp, ht1], [hQG])
                actf(vbf[:], v_, AF.Copy, [hps], [hvbf])
                for (src, hsrc, dst, hdst) in ((vbf, hvbf, Vt, hVt), (BG, hBG, BGt, hBGt), (QG, hQG, QGt, hQGt)):
                    for m in range(8):
                        for s in range(2):
                            S.op("pe", lambda e, m=m, s=s, src=src: e.transpose(
                                PTr[0:64, (s * 8 + m) * 128:(s * 8 + m + 1) * 128], src[:, m, s * 64:(s + 1) * 64], identb),
                                reads=[hsrc, c.hcb], writes=[hPTr])
                    S.op("act", lambda e, dst=dst: e.activation(out=dst[:].rearrange("t s m c -> t (s m c)"), in_=PTr[0:64, :], func=AF.Copy),
                         reads=[hPTr], writes=[hdst])
                if own:
                    bon, hbon = F["bon"]
                    tt("dve", t2[:], r_, kp[:], ALU.mult, [hps, hkp], [ht2])
                    tt("dve", sqb[:], t2[:], vb(V_RK), ALU.mult, [ht2, c.hvec], [hsqb])
                    pb_, hpb = nextPA()
                    for hh in range(2):
                        S.op("pe", lambda e, hh=hh: e.matmul(pb_[:, hh * 512:(hh + 1) * 512], lhsT=bones, rhs=sqb[:, hh * 4:(hh + 1) * 4, :], start=True, stop=True),
                             reads=[c.hcb, hsqb], writes=[hpb])
                    tt("dve", bon[:], v3(pb_), v_, ALU.mult, [hpb, hps], [hbon])
                def hrows(h):
                    return slice((h % 2) * 64, (h % 2) * 64 + 64)

                idb = c.cn[0:64, C_ID:C_ID + 64].unsqueeze(1).broadcast_to([64, 16, 64])

                def gram(s, lt, hl, rt, hr, mask, dname, eng):
                    ts = slice(s * 64, (s + 1) * 64)
                    p_, hp_ = nextPA()
                    pv = p_[0:64, :].rearrange("p (h t) -> p h t", h=16)
                    for h in HORD:
                        S.op("pe", lambda e, h=h: e.matmul(pv[:, h, :], lhsT=lt[hrows(h), h // 2, ts], rhs=rt[hrows(h), h // 2, ts], start=True, stop=True),
                             reads=[hl, hr], writes=[hp_])
                    d_, hd_ = gms[s][dname]
                    tt(eng, d_[:], pv, mask, ALU.mult, [hp_, c.hcn], [hd_])

                for s in range(2):
                    gram(s, Bh, hBh, Kh, hKh, maskU, "Ub", "dve")
                    gram(s, Kh, hKh, Bh, hBh, maskL, "Lb", "dve")
                for s in range(2):
                    Ub, hUb = gms[s]["Ub"]
                    Qb, hQb = gms[s]["Qb"]
                    tt("dve", Qb[:], idb, Ub[:], ALU.subtract, [c.hcn, hUb], [hQb])
                for s in range(2):
                    gram(s, Qh, hQh, Kh, hKh, maskU, "Lak", "dve")
                    if own:
                        gram(s, Bh, hBh, Rh, hRh, maskUi, "Mrb", "dve")
                        gram(s, Qh, hQh, Rh, hRh, maskUi, "Mrk", "dve")

                def inv_level(s, lvl):
                    gm = gms[s]
                    cur = ("Ub", "Lb") if lvl % 2 == 0 else ("U2", "L2")
                    nxt = ("U2", "L2") if lvl % 2 == 0 else ("Ub", "Lb")
                    Qb, hQb = gm["Qb"]
                    Uk, hUk = gm[cur[0]]
                    Lk, hLk = gm[cur[1]]
                    Un, hUn = gm[nxt[0]]
                    Ln, hLn = gm[nxt[1]]
                    p2, hp2 = nextPA()
                    p2v = p2[0:64, :].rearrange("p (h t) -> p h t", h=16)
                    for h in HORD:
                        S.op("pe", lambda e, h=h: e.matmul(p2v[:, h, :], lhsT=Uk[:, h, :], rhs=Lk[:, h, :], start=True, stop=True),
                             reads=[hUk, hLk], writes=[hp2])
                    actf(Ln[:], p2v, AF.Copy, [hp2], [hLn])
                    if lvl < 4:
                        p1, hp1 = nextPA()
                        p1v = p1[0:64, :].rearrange("p (h t) -> p h t", h=16)
                        for h in HORD:
                            S.op("pe", lambda e, h=h: e.matmul(p1v[:, h, :], lhsT=Lk[:, h, :], rhs=Uk[:, h, :], start=True, stop=True),
                                 reads=[hUk, hLk], writes=[hp1])
                        S.op("dve", lambda e: e.tensor_copy(out=Un[:], in_=p1v), reads=[hp1], writes=[hUn])
                    p3, hp3 = nextPA()
                    p3v = p3[0:64, :].rearrange("p (h t) -> p h t", h=16)
                    for h in HORD:
                        S.op("pe", lambda e, h=h: e.matmul(p3v[:, h, :], lhsT=Ln[:, h, :], rhs=Qb[:, h, :], start=True, stop=True),
                             reads=[hLn, hQb], writes=[hp3])
                    tt("dve", Qb[:], p3v, Qb[:], ALU.add, [hp3, hQb], [hQb])

                for lvl in range(5):
                    for s in range(2):
                        inv_level(s, lvl)

                S.mark()
                for s in range(2):
                    ts = slice(s * 64, (s + 1) * 64)
                    gm = gms[s]
                    Qb, hQb = gm["Qb"]
                    Lak, hLak = gm["Lak"]
                    Xb, hXb = gm["Xb"]
                    SAb, hSAb = gm["SAb"]
                    px, hpx = nextPB()
                    pxv = px[0:64, :].rearrange("p (h t) -> p h t", h=16)
                    for h in HORD:
                        cs_ = slice((h % 2) * 64, (h % 2) * 64 + 64)
                        S.op("pe", lambda e, h=h: e.matmul(pxv[:, h, :], lhsT=Kh[hrows(h), h // 2, ts], rhs=Sb[hrows(h), h // 2, :], start=True, stop=False),
                             reads=[hKh, hSb], writes=[hpx])
                        S.op("pe", lambda e, h=h, cs_=cs_: e.matmul(pxv[:, h, :], lhsT=Lak[:, h, :], rhs=Vt[:, s, h // 2, cs_], start=False, stop=True),
                             reads=[hLak, hVt], writes=[hpx])
                    actf(Xb[:], pxv, AF.Copy, [hpx], [hXb])
                    psa, hpsa = nextPB()
                    psav = psa[0:64, :].rearrange("p (h t) -> p h t", h=16)
                    for h in HORD:
                        S.op("pe", lambda e, h=h: e.matmul(psav[:, h, :], lhsT=Qb[:, h, :], rhs=Xb[:, h, :], start=True, stop=True),
                             reads=[hQb, hXb], writes=[hpsa])
                    actf(SAb[:], psav, AF.Copy, [hpsa], [hSAb], scale=-1.0)
                    if own:
                        Mrb, hMrb = gm["Mrb"]
                        Mrk, hMrk = gm["Mrk"]
                        py, hpy = nextPB()
                        pyv = py[0:64, :].rearrange("p (h t) -> p h t", h=16)
                        for h in HORD:
                            cs_ = slice((h % 2) * 64, (h % 2) * 64 + 64)
                            S.op("pe", lambda e, h=h: e.matmul(pyv[:, h, :], lhsT=Rh[hrows(h), h // 2, ts], rhs=Sb[hrows(h), h // 2, :], start=True, stop=False),
                                 reads=[hRh, hSb], writes=[hpy])
                            S.op("pe", lambda e, h=h: e.matmul(pyv[:, h, :], lhsT=Mrb[:, h, :], rhs=SAb[:, h, :], start=False, stop=False),
                                 reads=[hMrb, hSAb], writes=[hpy])
                            S.op("pe", lambda e, h=h, cs_=cs_: e.matmul(pyv[:, h, :], lhsT=Mrk[:, h, :], rhs=Vt[:, s, h // 2, cs_], start=False, stop=True),
                                 reads=[hMrk, hVt], writes=[hpy])
                        actf(ysb[:], pyv, AF.Copy, [hpy], [hysb])
                        S.op("dve", lambda e: e.tensor_reduce(out=gst[:, :, 0:1], in_=ysb[:], axis=AX.X, op=ALU.add), reads=[hysb], writes=[hgst])
                        S.op("dve", lambda e: e.tensor_scalar(out=gst[:, :, 0:1], in0=gst[:, :, 0:1], scalar1=1.0 / 64, scalar2=None, op0=ALU.mult), reads=[hgst], writes=[hgst])
                        tt("dve", yc[:], ysb[:], gst[:, :, 0:1].broadcast_to([64, 16, 64]), ALU.subtract, [hysb, hgst], [hyc])
                        actf(ysq[:], yc[:], AF.Square, [hyc], [hysq])
                        S.op("dve", lambda e: e.tensor_reduce(out=gst[:, :, 1:2], in_=ysq[:], axis=AX.X, op=ALU.add), reads=[hysq], writes=[hgst])
                        S.op("dve", lambda e: e.tensor_scalar(out=gst[:, :, 1:2], in0=gst[:, :, 1:2], scalar1=1.0 / 64, scalar2=64e-5, op0=ALU.mult, op1=ALU.add), reads=[hgst], writes=[hgst])
                        actf(gst[:, :, 2:3], gst[:, :, 1:2], AF.Sqrt, [hgst], [hgst])
                        S.op("dve", lambda e: e.reciprocal(out=gst[:, :, 3:4], in_=gst[:, :, 2:3]), reads=[hgst], writes=[hgst])
                        tt("dve", yc[:], yc[:], gst[:, :, 3:4].broadcast_to([64, 16, 64]), ALU.mult, [hyc, hgst], [hyc])
                        pt2, hpt2 = nextPB()
                        for m in range(8):
                            S.op("pe", lambda e, m=m: e.transpose(pt2[:, m * 64:(m + 1) * 64], yc[:, 2 * m:2 * m + 2, :].rearrange("t h i -> t (h i)"), ident32[0:64, 0:64]),
                                 reads=[hyc, c.hcn], writes=[hpt2])
                        S.op("dve", lambda e: e.tensor_copy(out=yf[:, :, ts], in_=pt2[:, 0:512].rearrange("p (m t) -> p m t", m=8)), reads=[hpt2], writes=[hyf])
                    pst, hpst = nextPB()
                    for h in HORD:
                        cs_ = slice((h % 2) * 64, (h % 2) * 64 + 64)
                        o_ = pst[hrows(h), (h // 2) * 64:(h // 2) * 64 + 64]
                        S.op("pe", lambda e, h=h, cs_=cs_, o_=o_: e.matmul(o_, lhsT=BGt[:, s, h // 2, cs_], rhs=SAb[:, h, :], start=True, stop=False),
                             reads=[hBGt, hSAb], writes=[hpst])
                        S.op("pe", lambda e, h=h, cs_=cs_, o_=o_: e.matmul(o_, lhsT=QGt[:, s, h // 2, cs_], rhs=Vt[:, s, h // 2, cs_], start=False, stop=True),
                             reads=[hQGt, hVt], writes=[hpst])
                    tt("dve", S32[:], S32[:], sm[:, :, s, 2:3].broadcast_to([128, 8, 64]), ALU.mult, [hS32, hsm], [hS32])
                    tt("dve", S32[:], S32[:], pst[:, 0:512].rearrange("p (m i) -> p m i", m=8), ALU.add, [hS32, hpst], [hS32])
                    actf(Sb[:], S32[:], AF.Copy, [hS32], [hSb])
                if own:
                    gg, hgg = F["gg"]
                    bon, hbon = F["bon"]
                    tt("dve", yf[:], yf[:], vb(V_GNW), ALU.mult, [hyf, c.hvec], [hyf])
                    tt("dve", yf[:], yf[:], vb(V_GNB), ALU.add, [hyf, c.hvec], [hyf])
                    tt("dve", yf[:], yf[:], bon[:], ALU.add, [hyf, hbon], [hyf])
                    tt("dve", yrb[:], yf[:], gg[:], ALU.mult, [hyf, hgg], [hyrb])
                    jo = j - OWN0
                    S.dma("sp", YR_d[jo].rearrange("p (m t) -> p m t", m=8), yrb[:], hyrb, reads=[hyrb], writes=[hYR[jo]])

            def rec(j):
                r = S.record(lambda: body(j))
                k = [i for i, it in enumerate(r) if it[0] == "mark"][0]
                return r[:k], r[k + 1:]
            A0, curB = rec(0)
            S.play(A0)
            for j in range(NB):
                if j + 1 < NB:
                    A1, B1 = rec(j + 1)
                    S.play2(curB, A1)
                    curB = B1
                else:
                    S.play(curB)
            S.barrier()


    def tt_(eng, out, i0, i1, op, reads, writes):
        S.op(eng, lambda e: e.tensor_tensor(out=out, in0=i0, in1=i1, op=op), reads=reads, writes=writes)

    def act_(out, in_, func, reads, writes, **kw):
        S.op("act", lambda e: e.activation(out=out, in_=in_, func=func, **kw), reads=reads, writes=writes)

    def phase_C():
        with contextlib.ExitStack() as es:
            T, P = mk(es)
            c = load_consts(T)
            vec = c.vec
            bones = c.cb[:, C_BO:C_BO + 128]
            identb = c.cb[:, C_ID:C_ID + 128]
            qk, hqk = T("qk", [128, 10, 128])
            sqb, hsqb = T("sqb", [128, 8, 128], BF16)
            t1, ht1 = T("t1", [128, 8, 128])
            qT, hqT = T("qT", [128, 8, 128], BF16)
            knb, hknb = T("knb", [128, 128], BF16)
            vbf, hvbf = T("vbf", [128, 128], BF16)
            kd = [T("kd%d" % i, [128, 2, 128], BF16) for i in range(2)]
            Vk = [T("Vk%d" % i, [128, 128], BF16) for i in range(2)]
            e1, he1 = T("e1", [128, 4, 128], BF16)
            PTb, hPTb = T("PTb", [128, 4, 128], BF16)
            onesb, honesb = T("onesb", [128, 64], BF16)
            S.op("pool", lambda e: e.memset(onesb[:], 1.0), writes=[honesb])
            esk, hesk = T("esk", [128, 8])
            act_(esk[:], vec[:, V_SK:V_SK + 8], AF.Exp, [c.hvec], [hesk])
            den, hden = T("den", [128, 4, 128])
            ya, hya = T("ya", [128, 8, 128], BF16)
            PS1, hPS1 = P("PS1", [128, 1024])
            psc = [P("psc%d" % i, [128, 512]) for i in range(2)]
            po, hpo = P("po", [128, 512])
            pdn, hpdn = P("pdn", [128, 512])
            pvT, hpvT = P("pvT", [128, 128], BF16)

            def norm_rows(src, n, gcol, dst, hdst):
                act_(sqb[:, 0:n, :], src, AF.Square, [hqk], [hsqb])
                for hh in range(0, n, 4):
                    w_ = min(4, n - hh)
                    S.op("pe", lambda e, hh=hh, w_=w_: e.matmul(PS1[:, hh * 128:(hh + w_) * 128], lhsT=bones, rhs=sqb[:, hh:hh + w_, :], start=True, stop=True),
                         reads=[c.hcb, hsqb], writes=[hPS1])
                pv = PS1[:, 0:n * 128].rearrange("p (m t) -> p m t", m=n)
                S.op("dve", lambda e: e.tensor_scalar(out=t1[:, 0:n, :], in0=pv, scalar1=1.0 / 64, scalar2=1e-6, op0=ALU.mult, op1=ALU.add),
                     reads=[hPS1], writes=[ht1])
                act_(t1[:, 0:n, :], t1[:, 0:n, :], AF.Sqrt, [ht1], [ht1])
                S.op("dve", lambda e: e.reciprocal(out=t1[:, 0:n, :], in_=t1[:, 0:n, :]), reads=[ht1], writes=[ht1])
                tt_("dve", t1[:, 0:n, :], t1[:, 0:n, :], src, ALU.mult, [ht1, hqk], [ht1])
                S.op("dve", lambda e: e.tensor_scalar(out=dst, in0=t1[:, 0:n, :], scalar1=vec[:, gcol:gcol + 1], scalar2=None, op0=ALU.mult),
                     reads=[ht1, c.hvec], writes=[hdst])

            def kv_prep(slot):
                kd_, hkd_ = kd[slot]
                Vk_, hVk_ = Vk[slot]
                norm_rows(qk[:, 8:9, :], 1, V_KG, knb[:].unsqueeze(1), hknb)
                for g in range(2):
                    S.op("pe", lambda e, g=g: e.matmul(PS1[:, 512 + g * 128:512 + (g + 1) * 128], lhsT=c.cb[:, C_SEL + g * 128:C_SEL + (g + 1) * 128], rhs=knb[:], start=True, stop=True),
                         reads=[c.hcb, hknb], writes=[hPS1])
                act_(kd_[:], PS1[:, 512:768].rearrange("p (g t) -> p g t", g=2), AF.Copy, [hPS1], [hkd_])
                act_(vbf[:], qk[:, 9, :], AF.Copy, [hqk], [hvbf])
                S.op("pe", lambda e: e.transpose(pvT[:], vbf[:], identb), reads=[hvbf, c.hcb], writes=[hpvT])
                S.op("dve", lambda e: e.tensor_copy(out=Vk_[:], in_=pvT[:]), reads=[hpvT], writes=[hVk_])

            S.dma("sp", qk[:, 8:10, :], QKV_d[0].rearrange("p (c t) -> p c t", c=10)[:, 8:10, :], hqk, reads=[hQKV[0]], writes=[hqk])
            kv_prep(0)
            for jo in range(NO):
                ja = jo + 1
                S.dma("sp", qk[:], QKV_d[ja].rearrange("p (c t) -> p c t", c=10), hqk, reads=[hQKV[ja]], writes=[hqk])
                kv_prep(ja % 2)
                norm_rows(qk[:, 0:8, :], 8, V_QG, qT[:], hqT)
                si = 0
                for g in range(2):
                    for par in range(2):
                        rows = slice(par * 64, par * 64 + 64)
                        for wi, slot in enumerate(((ja - 1) % 2, ja % 2)):
                            kd_, hkd_ = kd[slot]
                            Vk_, hVk_ = Vk[slot]
                            ps_, hps_ = psc[si % 2]
                            si += 1
                            S.op("pe", lambda e, ps_=ps_, kd_=kd_: e.matmul(ps_[:], lhsT=kd_[rows, g, :], rhs=qT[rows, 4 * g:4 * g + 4, :], start=True, stop=True),
                                 reads=[hkd_, hqT], writes=[hps_])
                            act_(e1[:], ps_[:].rearrange("p (h t) -> p h t", h=4), AF.Exp, [hps_], [he1], scale=0.125)
                            mcol = C_MC if wi == 1 else (C_MPF if jo == 0 else C_MP)
                            mk_ = c.cb[:, mcol:mcol + 128].unsqueeze(1).broadcast_to([128, 4, 128])
                            tt_("dve", PTb[:], e1[:], mk_, ALU.mult, [he1, c.hcb], [hPTb])
                            S.op("pe", lambda e, Vk_=Vk_, wi=wi: e.matmul(po[rows, :], lhsT=Vk_[:, g * 64:(g + 1) * 64], rhs=PTb[:].rearrange("p h t -> p (h t)"), start=(wi == 0), stop=(wi == 1)),
                                 reads=[hVk_, hPTb], writes=[hpo])
                            S.op("pe", lambda e, wi=wi: e.matmul(pdn[rows, :], lhsT=onesb[:], rhs=PTb[:].rearrange("p h t -> p (h t)"), start=(wi == 0), stop=(wi == 1)),
                                 reads=[honesb, hPTb], writes=[hpdn])
                    tt_("dve", den[:], pdn[:].rearrange("p (h t) -> p h t", h=4), esk[:, 4 * g:4 * g + 4].unsqueeze(2).broadcast_to([128, 4, 128]), ALU.add, [hpdn, hesk], [hden])
                    S.op("dve", lambda e: e.reciprocal(out=den[:], in_=den[:]), reads=[hden], writes=[hden])
                    tt_("dve", ya[:, 4 * g:4 * g + 4, :], po[:].rearrange("p (h t) -> p h t", h=4), den[:], ALU.mult, [hpo, hden], [hya])
                S.dma("sp", YA_d[jo].rearrange("p (m t) -> p m t", m=8), ya[:], hya, reads=[hya], writes=[hYA[jo]])
            S.barrier()

    def phase_D():
        with contextlib.ExitStack() as es:
            T, P = mk(es)
            c = load_consts(T)
            wo, hwo = T("wo", [128, DC, D], BF16)
            for k in range(DC):
                S.dma("pool", wo[:, k, :], wout_d[k * 128:(k + 1) * 128, :], hwo, writes=[hwo])
            yr, hyr = T("yr", [128, 8, 128], BF16)
            ya, hya = T("ya", [128, 8, 128], BF16)
            xt, hxt = T("xt", [128, D])
            hm, hhm = T("hm", [128, D])
            st, hst = T("st", [128, 4])
            xs, hxs = T("xs", [128, D], BF16)
            uT, huT = T("uT", [128, DC, 128], BF16)
            pT, hpT = P("pT", [128, DC, 128], BF16)
            pp = [P("pp%d" % i, [128, 512]) for i in range(4)]
            for jo in range(NO):
                j = OWN0 + jo
                S.dma("sp", yr[:], YR_d[jo].rearrange("p (m t) -> p m t", m=8), hyr, reads=[hYR[jo]], writes=[hyr])
                S.dma("sp", ya[:], YA_d[jo].rearrange("p (m t) -> p m t", m=8), hya, reads=[hYA[jo]], writes=[hya])
                S.dma("sp", xt[:], xin[j * 128:(j + 1) * 128, :], hxt, writes=[hxt])
                for cg in range(4):
                    p_, hp_ = pp[cg]
                    for kc in range(DC):
                        src, hsrc = (yr, hyr) if kc < 8 else (ya, hya)
                        S.op("pe", lambda e, p_=p_, kc=kc, cg=cg, src=src: e.matmul(p_[:], lhsT=src[:, kc % 8, :], rhs=wo[:, kc, cg * 512:(cg + 1) * 512], start=(kc == 0), stop=(kc == DC - 1)),
                             reads=[hsrc, hwo], writes=[hp_])
                    tt_("dve", hm[:, cg * 512:(cg + 1) * 512], p_[:], xt[:, cg * 512:(cg + 1) * 512], ALU.add, [hp_, hxt], [hhm])
                S.dma("sp", HM_d[jo], hm[:], hhm, reads=[hhm], writes=[hHM[jo]])
                rmsnorm_T(c, T, hm, hhm, V_G2, xs, hxs, st, hst, xs, hxs, pT, hpT, uT, huT)
                S.dma("sp", XNT_d[jo].rearrange("p (k t) -> p k t", k=DC), uT[:], huT, reads=[huT], writes=[hXNT[jo]])
            S.barrier()

    def phase_E1():
        with contextlib.ExitStack() as es:
            T, P = mk(es)
            c = load_consts(T)
            pq, hpq = T("pq", [128, DC, D], BF16)
            for k in range(DC):
                S.dma("pool", pq[:, k, :], pq_d[k * 128:(k + 1) * 128, :], hpq, writes=[hpq])
            skn, hskn = T("skn", [128, 16, 128])
            S.dma("sp", skn[:], sk_d.rearrange("c n d -> n c d"), hskn, writes=[hskn])
            skT, hskT = T("skT", [128, 16, 128])
            pbig = [P("pb%d" % i, [128, 16, 128]) for i in range(2)]
            p0, hp0 = pbig[0]
            for hc in range(16):
                S.op("pe", lambda e, hc=hc: e.transpose(p0[:, hc, :], skn[:, hc, :], c.cn[:, C_ID:C_ID + 128]), reads=[hskn, c.hcn], writes=[hp0])
            S.op("dve", lambda e: e.tensor_copy(out=skT[:], in_=p0[:]), reads=[hp0], writes=[hskT])
            xnT, hxnT = T("xnT", [128, DC, 128], BF16)
            qTs, hqTs = T("qTs", [128, 16, 128])
            sall, hsall = T("sall", [128, 16, 128])
            tops, htops = T("tops", [128, 16, 16])
            wk, hwk = T("wk", [128, 128])
            cand, hcand = T("cand", [128, 8, 256])
            wk2, hwk2 = T("wk2", [128, 256])
            ctop, hctop = T("ctop", [128, 8, 24])
            sm, hsm = T("sm", [128, 4, 8])
            ez, hez = T("ez", [128, 8, 16])
            stt, hstt = T("stt", [128, 2048 + 8])
            for jo in range(NO):
                S.dma("sp", xnT[:], XNT_d[jo].rearrange("p (k t) -> p k t", k=DC), hxnT, reads=[hXNT[jo]], writes=[hxnT])
                pa_, hpa_ = pbig[0]
                for hc in range(16):
                    for kc in range(DC):
                        S.op("pe", lambda e, hc=hc, kc=kc: e.matmul(pa_[:, hc, :], lhsT=pq[:, kc, hc * 128:(hc + 1) * 128], rhs=xnT[:, kc, :], start=(kc == 0), stop=(kc == DC - 1)),
                             reads=[hpq, hxnT], writes=[hpa_])
                act_(qTs[:, 0:8, :], pa_[:, 0:8, :], AF.Copy, [hpa_], [hqTs])
                S.op("dve", lambda e: e.tensor_copy(out=qTs[:, 8:16, :], in_=pa_[:, 8:16, :]), reads=[hpa_], writes=[hqTs])
                pb_, hpb_ = pbig[1]
                for hc in range(16):
                    S.op("pe", lambda e, hc=hc: e.matmul(pb_[:, hc, :], lhsT=qTs[:, hc, :], rhs=skT[:, hc, :], start=True, stop=True),
                         reads=[hqTs, hskT], writes=[hpb_])
                act_(sall[:, 0:8, :], pb_[:, 0:8, :], AF.Copy, [hpb_], [hsall])
                S.op("dve", lambda e: e.tensor_copy(out=sall[:, 8:16, :], in_=pb_[:, 8:16, :]), reads=[hpb_], writes=[hsall])
                for hc in range(16):
                    S.op("dve", lambda e, hc=hc: e.max(out=tops[:, hc, 0:8], in_=sall[:, hc, :]), reads=[hsall], writes=[htops])
                    S.op("dve", lambda e, hc=hc: e.match_replace(out=wk[:], in_to_replace=tops[:, hc, 0:8], in_values=sall[:, hc, :], imm_value=-1e30),
                         reads=[hsall, htops], writes=[hwk])
                    S.op("dve", lambda e, hc=hc: e.max(out=tops[:, hc, 8:16], in_=wk[:]), reads=[hwk], writes=[htops])
                t4 = tops[:].rearrange("p (h c) k -> p h c k", c=2)
                tt_("dve", cand[:].rearrange("p h (i j) -> p h i j", i=16), t4[:, :, 0, :].unsqueeze(3).broadcast_to([128, 8, 16, 16]),
                    t4[:, :, 1, :].unsqueeze(2).broadcast_to([128, 8, 16, 16]), ALU.add, [htops], [hcand])
                for h in range(8):
                    S.op("dve", lambda e, h=h: e.max(out=ctop[:, h, 0:8], in_=cand[:, h, :]), reads=[hcand], writes=[hctop])
                    S.op("dve", lambda e, h=h: e.match_replace(out=wk2[:], in_to_replace=ctop[:, h, 0:8], in_values=cand[:, h, :], imm_value=-1e30),
                         reads=[hcand, hctop], writes=[hwk2])
                    S.op("dve", lambda e, h=h: e.max(out=ctop[:, h, 8:16], in_=wk2[:]), reads=[hwk2], writes=[hctop])
                    S.op("dve", lambda e, h=h: e.match_replace(out=wk2[:], in_to_replace=ctop[:, h, 8:16], in_values=wk2[:], imm_value=-1e30),
                         reads=[hwk2, hctop], writes=[hwk2])
                    S.op("dve", lambda e, h=h: e.max(out=ctop[:, h, 16:24], in_=wk2[:]), reads=[hwk2], writes=[hctop])
                thr = sm[:, 0, :]
                tt_("dve", thr.unsqueeze(2), ctop[:, :, 15:16], ctop[:, :, 16:17], ALU.add, [hctop], [hsm])
                S.op("dve", lambda e: e.tensor_scalar(out=thr, in0=thr, scalar1=0.5, scalar2=None, op0=ALU.mult), reads=[hsm], writes=[hsm])
                tt_("dve", ez[:], ctop[:, :, 0:16], thr.unsqueeze(2).broadcast_to([128, 8, 16]), ALU.subtract, [hctop, hsm], [hez])
                act_(ez[:], ez[:], AF.Exp, [hez], [hez])
                S.op("dve", lambda e: e.tensor_reduce(out=sm[:, 1, :].unsqueeze(2), in_=ez[:], axis=AX.X, op=ALU.add), reads=[hez], writes=[hsm])
                S.op("dve", lambda e: e.reciprocal(out=stt[:, 2048:2056], in_=sm[:, 1, :]), reads=[hsm], writes=[hstt])
                act_(sm[:, 2, :], sm[:, 1, :], AF.Ln, [hsm], [hsm])
                tt_("dve", sm[:, 3, :], sm[:, 2, :], thr, ALU.add, [hsm], [hsm])
                s4 = sall[:].rearrange("p (h c) n -> p h c n", c=2)
                st4 = stt[:, 0:2048].rearrange("p (h c n) -> p h c n", h=8, c=2)
                tt_("dve", st4[:, :, 0, :], s4[:, :, 0, :], sm[:, 3, :].unsqueeze(2).broadcast_to([128, 8, 128]), ALU.subtract, [hsall, hsm], [hstt])
                S.op("dve", lambda e: e.tensor_copy(out=st4[:, :, 1, :], in_=s4[:, :, 1, :]), reads=[hsall], writes=[hstt])
                act_(stt[:, 0:2048], stt[:, 0:2048], AF.Exp, [hstt], [hstt])
                S.dma("sp", ST_d[jo], stt[:], hstt, reads=[hstt], writes=[hST[jo]])
            S.barrier()

    def phase_E2(G=4):
        with contextlib.ExitStack() as es:
            T, P = mk(es)
            c = Ctx()
            c.cn, c.hcn = T("id32", [128, 128])
            S.dma("sp", c.cn[:], cn_d[:, C_ID:C_ID + 128], c.hcn, writes=[c.hcn])
            c.cb, c.hcb = T("idbf", [128, 128], BF16)
            S.dma("pool", c.cb[:], cn_d[:, C_ID:C_ID + 128], c.hcb, writes=[c.hcb])
            identb = c.cb[:]
            ident32 = c.cn[:]
            G = min(G, NO)
            xn = [T("xn%d" % i, [128, DC, 128], BF16) for i in range(G)]
            stg = [T("stg%d" % i, [128, 2048 + 8]) for i in range(G)]
            acc = [T("acc%d" % i, [128, D]) for i in range(G)]
            dn32 = [T("dn32_%d" % i, [128, D]) for i in range(2)]
            up32 = [T("up32_%d" % i, [128, D]) for i in range(1)]
            upb = [T("upb%d" % i, [128, 4, D], BF16) for i in range(2)]
            dTs = [T("dT%d" % i, [128, DC, 512], BF16) for i in range(2)]
            EEh = [T("EEh%d" % i, [128, 4, 512]) for i in range(3)]
            Wh = [T("Wh%d" % i, [128, 8, 512], BF16) for i in range(1)]
            geT = [T("geT%d" % i, [128, 512]) for i in range(2)]
            GTb = [T("GTb%d" % i, [128, 4, 128], BF16) for i in range(2)]
            ptr = [P("ptr%d" % i, [128, 4, 128]) for i in range(2)]
            phid = [P("phid%d" % i, [128, 4, 128]) for i in range(2)]
            pwt, hpwt = P("pwt", [128, 4, 128])
            pouts = [P("pout%d" % i, [128, 512]) for i in range(3)]
            pocnt = [0]
            NCH = NE // 512
            import os as _os
            _V = _os.environ.get("KV", "")
            if "n" in _V:
                NCH = 8
            NQ = NCH * 4
            pcount = [0]
            for g0 in range(0, NO, G):
                tiles = list(range(g0, min(NO, g0 + G)))
                nt = len(tiles)
                for i, jo in enumerate(tiles):
                    S.dma("sp", xn[i][0][:], XNT_d[jo].rearrange("p (k t) -> p k t", k=DC), xn[i][1], reads=[hXNT[jo]], writes=[xn[i][1]])
                    S.dma("sp", stg[i][0][:], ST_d[jo], stg[i][1], reads=[hST[jo]], writes=[stg[i][1]])
                    S.dma("sp", acc[i][0][:], HM_d[jo], acc[i][1], reads=[hHM[jo]], writes=[acc[i][1]])

                def load_dn(q):
                    if q < NQ:
                        d_, hd_ = dn32[q % 2]
                        S.dma("sp", d_[:], pd_d[q * 128:(q + 1) * 128, :], hd_, writes=[hd_])

                def load_up(q):
                    if q < NQ:
                        u_, hu_ = up32[0]
                        S.dma("sp", u_[:], pu_d[q * 128:(q + 1) * 128, :], hu_, writes=[hu_])

                def prep_q(q):
                    ec, et = divmod(q, 4)
                    d_, hd_ = dn32[q % 2]
                    dT_, hdT_ = dTs[ec % 2]
                    for k4 in range(4):
                        pt_, hpt_ = ptr[pcount[0] % 2]
                        pcount[0] += 1
                        for kk in range(4):
                            kc = k4 * 4 + kk
                            S.op("pe", lambda e, kk=kk, kc=kc, pt_=pt_: e.transpose(pt_[:, kk, :], d_[:, kc * 128:(kc + 1) * 128], ident32),
                                 reads=[hd_, c.hcn], writes=[hpt_])
                        S.op("dve", lambda e, k4=k4, pt_=pt_: e.tensor_copy(out=dT_[:, k4 * 4:k4 * 4 + 4, et * 128:(et + 1) * 128], in_=pt_[:]),
                             reads=[hpt_], writes=[hdT_])
                    load_dn(q + 2)
                    u_, hu_ = up32[0]
                    ub_, hub_ = upb[ec % 2]
                    S.op("dve", lambda e: e.tensor_copy(out=ub_[:, et, :], in_=u_[:]), reads=[hu_], writes=[hub_])
                    load_up(q + 1)

                def P1(gi):
                    ec, i = divmod(gi, nt)
                    xn_, hxn_ = xn[i]
                    ph_, hph_ = phid[gi % 2]
                    dT_, hdT_ = dTs[ec % 2]
                    for et in range(4):
                        for kc in range(DC):
                            S.op("pe", lambda e, kc=kc, et=et: e.matmul(ph_[:, et, :], lhsT=dT_[:, kc, et * 128:(et + 1) * 128], rhs=xn_[:, kc, :], start=(kc == 0), stop=(kc == DC - 1)),
                                 reads=[hxn_, hdT_], writes=[hph_])

                def A1(gi):
                    ec, i = divmod(gi, nt)
                    st_, hst_ = stg[i]
                    ph_, hph_ = phid[gi % 2]
                    ge_, hge_ = geT[gi % 2]
                    st4 = st_[:, 0:2048].rearrange("p (h c n) -> p h c n", h=8, c=2)
                    for half in range(2):
                        ee_, hee_ = EEh[(gi * 2 + half) % 3]
                        for h in range(4):
                            hh = half * 4 + h
                            for a in range(4):
                                n1 = ec * 4 + a
                                act_(ee_[:, h, a * 128:(a + 1) * 128], st4[:, hh, 1, :], AF.Copy, [hst_], [hee_], scale=st4[:, hh, 0, n1:n1 + 1])
                    act_(ge_[:], ph_[:].rearrange("p a t -> p (a t)"), AF.Gelu, [hph_], [hge_])

                def D1(gi):
                    ec, i = divmod(gi, nt)
                    st_, hst_ = stg[i]
                    wh_, hwh_ = Wh[0]
                    for half in range(2):
                        ee_, hee_ = EEh[(gi * 2 + half) % 3]
                        for h in range(4):
                            hh = half * 4 + h
                            S.op("dve", lambda e, h=h, hh=hh, ee_=ee_: e.scalar_tensor_tensor(
                                out=wh_[:, hh, :], in0=ee_[:, h, :], scalar=st_[:, 2048 + hh:2049 + hh],
                                in1=ee_[:, h, :], op0=ALU.is_ge, op1=ALU.mult), reads=[hee_, hst_], writes=[hwh_])

                def P2(gi):
                    wh_, hwh_ = Wh[0]
                    for et in range(4):
                        for h in range(8):
                            S.op("pe", lambda e, et=et, h=h: e.matmul(pwt[:, et, :], lhsT=wh_[:, h, et * 128:(et + 1) * 128], rhs=identb, start=(h == 0), stop=(h == 7)),
                                 reads=[hwh_, c.hcb], writes=[hpwt])

                def D2(gi):
                    ge_, hge_ = geT[gi % 2]
                    gt_, hgt_ = GTb[gi % 2]
                    tt_("dve", gt_[:].rearrange("p a t -> p (a t)"), ge_[:], pwt[:].rearrange("p a t -> p (a t)"), ALU.mult, [hge_, hpwt], [hgt_])

                def P3D3(gi):
                    ec, i = divmod(gi, nt)
                    u_, hu_ = upb[ec % 2]
                    gt_, hgt_ = GTb[gi % 2]
                    ac_, hac_ = acc[i]
                    for dg in range(4):
                        po_, hpo_ = pouts[pocnt[0] % 3]
                        pocnt[0] += 1
                        for et in range(4):
                            S.op("pe", lambda e, dg=dg, et=et, po_=po_: e.matmul(po_[:], lhsT=gt_[:, et, :], rhs=u_[:, et, dg * 512:(dg + 1) * 512], start=(et == 0), stop=(et == 3)),
                                 reads=[hgt_, hu_], writes=[hpo_])
                        tt_("dve", ac_[:, dg * 512:(dg + 1) * 512], ac_[:, dg * 512:(dg + 1) * 512], po_[:], ALU.add, [hac_, hpo_], [hac_])

                NG = NCH * nt
                load_dn(0)
                load_dn(1)
                load_up(0)
                for q in range(4):
                    prep_q(q)
                P1(0)
                A1(0)
                D1(0)
                for gi in range(NG):
                    ec, i = divmod(gi, nt)
                    if ec + 1 < NCH:
                        for et in range(i * 4 // nt, (i + 1) * 4 // nt):
                            prep_q((ec + 1) * 4 + et)
                    if gi + 1 < NG:
                        P1(gi + 1)
                        A1(gi + 1)
                    P2(gi)
                    D2(gi)
                    if gi + 1 < NG:
                        D1(gi + 1)
                    P3D3(gi)
                for i, jo in enumerate(tiles):
                    S.dma("sp", yout[jo * 128:(jo + 1) * 128, :], acc[i][0][:], acc[i][1], reads=[acc[i][1]], writes=[hOUT[jo]])
            S.barrier()

    ph = {"A": phase_A, "B": phase_B, "C": phase_C, "D": phase_D, "E": phase_E1, "F": phase_E2}
    ctx = dict(nc=nc, S=S, mk=mk, load_consts=load_consts, rmsnorm_T=rmsnorm_T, locals=locals())
    for p in phases:
        if p in ph:
            ph[p]()
    return nc, ctx


def _colT(v, n):
    buf = np.zeros(n * 128, np.float32)
    buf[:v.size] = v.reshape(-1)
    return buf.reshape(n, 128).T


def make_vecs(inp):
    vecs = np.zeros((128, NV), np.float32)
    vecs[:, V_G1:V_G1 + 16] = _colT(inp["norm1_g"][0], 16)
    vecs[:, V_MU:V_MU + 27] = _colT(inp["shift_mu"][0], 27)
    for col, key in ((V_W0, "w0"), (V_A0, "a0"), (V_KK, "k_k"), (V_KA, "k_a"), (V_GNW, "gn_w"),
                     (V_GNB, "gn_b"), (V_RK, "r_k")):
        vecs[:, col:col + 8] = _colT(inp[key][0], 8)
    vecs[:, V_QG] = np.tile(inp["q_gain"][0], 2)
    vecs[:, V_KG] = np.tile(inp["k_gain"][0], 2)
    sk = inp["sinks"][0]
    for hp in range(8):
        vecs[0:64, V_SK + hp] = sk[2 * hp]
        vecs[64:128, V_SK + hp] = sk[2 * hp + 1]
    vecs[:, V_G2:V_G2 + 16] = _colT(inp["norm2_g"][0], 16)
    return vecs


def make_consts(first_half):
    cn = np.zeros((128, NCN), np.float32)
    cn[:, C_ID:C_ID + 128] = np.eye(128, dtype=np.float32)
    bo = np.zeros((128, 128), np.float32)
    bo[0:64, 0:64] = 1
    bo[64:, 64:] = 1
    cn[:, C_BO:C_BO + 128] = bo
    s = np.arange(64)[:, None]
    t = np.arange(64)[None, :]
    cn[0:64, C_MU:C_MU + 64] = (s < t)
    cn[0:64, C_MUI:C_MUI + 64] = (s <= t)
    cn[0:64, C_ML:C_ML + 64] = (s > t)
    s = np.arange(128)[:, None]
    q = np.arange(128)[None, :]
    cn[:, C_MC:C_MC + 128] = (s <= q)
    cn[:, C_MP:C_MP + 128] = (s > q)
    mpf = (s > q)
    if first_half:
        mpf = mpf & (s >= 112)
    cn[:, C_MPF:C_MPF + 128] = mpf
    for g in range(2):
        sel = np.zeros((128, 128), np.float32)
        for m in range(128):
            sel[g * 64 + (m % 64), m] = 1
        cn[:, C_SEL + g * 128:C_SEL + (g + 1) * 128] = sel
    return cn


_NC_CACHE = {}


def kernel(**inputs):
    inp = {k: np.asarray(v) for k, v in inputs.items()}
    x = inp["x"].astype(np.float32, copy=False)
    B, SEQ, _ = x.shape
    NB, OWN0 = 33, 17
    if "nc" not in _NC_CACHE:
        _NC_CACHE["nc"] = build(NB=NB, OWN0=OWN0, phases="ABCDEF")[0]
    nc = _NC_CACHE["nc"]
    f = lambda k: np.ascontiguousarray(inp[k][0], dtype=np.float32)
    shared = dict(w_in=f("w_in"), vecs=make_vecs(inp), w_up=f("w_up"), a_up=f("a_up"), g_up=f("g_up"),
                  w_out=f("w_out"), peer_query=f("peer_query"),
                  sub_keys=np.ascontiguousarray(inp["peer_sub_keys"][0].reshape(16, 128, 128), dtype=np.float32),
                  peer_down=f("peer_down"), peer_up=f("peer_up"))
    meta = inp["meta_tokens"].astype(np.float32, copy=False)
    cn = [make_consts(False), make_consts(True)]
    in_maps = []
    for c in range(8):
        b, s = c // 2, c % 2
        loc = np.zeros((NB * 128, D), np.float32)
        if s == 0:
            loc[16 * 128 + 112:17 * 128] = meta
            loc[17 * 128:] = x[b, :2048]
        else:
            loc[112:128] = meta
            loc[128:] = x[b]
        in_maps.append(dict(xin=loc, consts=cn[1 if s == 0 else 0], **shared))
    res = run_bass_kernel_spmd(nc, in_maps, core_ids=list(range(8)))
    out = np.empty((B, SEQ, D), np.float32)
    for c in range(8):
        b, s = c // 2, c % 2
        out[b, s * 2048:(s + 1) * 2048] = np.asarray(res.results[c]["yout"])
    return out
```

```python
import contextlib
import numpy as np
import concourse.bass as bass
import concourse.mybir as mybir
from concourse.bass_utils import run_bass_kernel_spmd

F32 = mybir.dt.float32
BF16 = mybir.dt.bfloat16
AF = mybir.ActivationFunctionType
ALU = mybir.AluOpType
AX = mybir.AxisListType

D = 2048
DC = 16
INC = 4640
RW0 = 1280
NRC = 27
NE = 16384
HD = 64

V_G1, V_MU, V_W0, V_A0, V_KK, V_KA, V_GNW, V_GNB, V_RK, V_QG, V_KG, V_SK, V_G2 = (
    0, 16, 43, 51, 59, 67, 75, 83, 91, 99, 100, 101, 109)
NV = 125
C_ID, C_BO, C_MU, C_MUI, C_ML, C_MC, C_MP, C_MPF, C_SEL = 0, 128, 256, 320, 384, 448, 576, 704, 832
NCN = 832 + 256


class H:
    __slots__ = ("name", "w", "r", "dsem", "dcnt")

    def __init__(self, name=""):
        self.name = name
        self.w = {}
        self.r = {}
        self.dsem = None
        self.dcnt = 0


class Sched:
    def __init__(self, nc, needed=None):
        self.nc = nc
        self.engs = {"pe": nc.tensor, "act": nc.scalar, "dve": nc.vector,
                     "pool": nc.gpsimd, "sp": nc.sync}
        self.esem = {k: nc.alloc_semaphore(name="es_" + k) for k in self.engs}
        self.seq = {k: 0 for k in self.engs}
        self.cnt = {k: 0 for k in self.engs}
        self.cntmap = {k: {} for k in self.engs}
        self.waited = {k: {} for k in self.engs}
        self.needed = needed
        self.rec = {k: set() for k in self.engs}
        self.sems = {}
        self.dcur = {}
        self._rec = None

    def _wait(self, e, evs):
        w = self.waited[e]
        for key, val in evs.items():
            if w.get(key, 0) >= val:
                continue
            if isinstance(key, str):
                self.rec[key].add(val)
                real = self.cntmap[key][val] if self.needed is not None else val
                self.engs[e].wait_ge(self.esem[key], real)
            else:
                self.engs[e].wait_ge(self.sems[key], val)
            w[key] = val

    @staticmethod
    def _merge(d, evs):
        for k, v in evs.items():
            if d.get(k, 0) < v:
                d[k] = v

    def _deps(self, reads, writes, own=None):
        evs = {}
        for h in reads:
            self._merge(evs, h.w)
        if own == "pe":
            evs.pop(own, None)
        ww = {}
        for h in writes:
            self._merge(ww, h.w)
            self._merge(ww, h.r)
        if own is not None:
            ww.pop(own, None)
        self._merge(evs, ww)
        return evs

    def _pe_mode(self, mode):
        if mode != getattr(self, "pe_mode", None):
            if self.seq["pe"] > 0:
                self._wait("pe", {"pe": self.seq["pe"]})
            self.pe_mode = mode

    def op(self, e, fn, reads=(), writes=()):
        if self._rec is not None:
            r = _RecEng()
            fn(r)
            name, args, kwargs = r.call
            self._rec.append(("op", e, name, args, kwargs, tuple(reads), tuple(writes)))
            return None
        self._wait(e, self._deps(reads, writes, own=e))
        inst = fn(_PEProxy(self) if e == "pe" else self.engs[e])
        self.seq[e] += 1
        n = self.seq[e]
        if self.needed is None or n in self.needed[e]:
            self.cnt[e] += 1
            inst.then_inc(self.esem[e], 1)
            self.cntmap[e][n] = self.cnt[e]
        ev = {e: n}
        for h in reads:
            self._merge(h.r, ev)
        for h in writes:
            h.w = dict(ev)
            h.r = {}
        return inst

    def mark(self):
        if self._rec is not None:
            self._rec.append(("mark",))

    def record(self, f):
        self._rec = []
        try:
            f()
        finally:
            rec, self._rec = self._rec, None
        return rec

    def play(self, items):
        for it in items:
            if it[0] == "op":
                _, e, name, args, kwargs, reads, writes = it
                self.op(e, lambda eng: getattr(eng, name)(*args, **kwargs), reads, writes)
            elif it[0] == "dma":
                _, q, out, in_, tile, reads, writes, kw = it
                self.dma(q, out, in_, tile, reads, writes, **kw)

    def play2(self, a, b):
        na, nb = len(a), len(b)
        ia = ib = 0
        CH = 32
        while ia < na or ib < nb:
            if ib >= nb or (ia < na and ia * nb <= ib * na):
                self.play(a[ia:ia + CH])
                ia += CH
            else:
                self.play(b[ib:ib + CH])
                ib += CH

    def dma(self, q, out, in_, tile, reads=(), writes=(), **kw):
        if self._rec is not None:
            self._rec.append(("dma", q, out, in_, tile, tuple(reads), tuple(writes), kw))
            return None
        if tile.dsem is None:
            tile.dsem = self.nc.alloc_semaphore(name="ds_%d" % len(self.sems))
            self.sems[tile.dsem.num] = tile.dsem
        deps = self._deps(reads, writes)
        if tile in writes and not tile.r and tile.w.get(tile.dsem.num, 0) == tile.dcnt and len(tile.w) == 1:
            deps.pop(tile.dsem.num, None)
        self._wait(q, deps)
        inst = self.engs[q].dma_start(out=out, in_=in_, **kw)
        tile.dcnt += 16
        inst.then_inc(tile.dsem, 16)
        self.dcur[tile.dsem.num] = tile.dcnt
        ev = {tile.dsem.num: tile.dcnt}
        for h in reads:
            self._merge(h.r, ev)
        for h in writes:
            h.w = dict(ev)
            h.r = {}
        return inst

    def barrier(self):
        evs = {k: self.seq[k] for k in self.engs if self.seq[k] > 0}
        evs.update(self.dcur)
        for e in self.engs:
            self._wait(e, evs)


class _RecEng:
    def __getattr__(self, name):
        def f(*args, **kwargs):
            self.call = (name, args, kwargs)
            return None
        return f


def _rnd(n):
    return 32 if n <= 32 else (64 if n <= 64 else 128)


class _PEProxy:
    def __init__(self, S):
        self.S = S

    def matmul(self, out, lhsT, rhs, **kw):
        fr = 1
        for d in lhsT.shape[1:]:
            fr *= d
        self.S._pe_mode(("mm", _rnd(lhsT.shape[0]), _rnd(fr), str(lhsT.dtype), lhsT.base_partition(), out.base_partition()))
        return self.S.nc.tensor.matmul(out, lhsT=lhsT, rhs=rhs, **kw)

    def transpose(self, out, in_, identity):
        fr = 1
        for d in in_.shape[1:]:
            fr *= d
        self.S._pe_mode(("tr", _rnd(in_.shape[0]), _rnd(fr), str(in_.dtype), in_.base_partition(), out.base_partition()))
        return self.S.nc.tensor.transpose(out, in_, identity)


class Ctx:
    pass


def build(NB=33, OWN0=17, phases="ABCDEF", dbg=False):
    _, ctx = _build(NB, OWN0, phases, dbg, None)
    return _build(NB, OWN0, phases, dbg, ctx["S"].rec)


def _build(NB, OWN0, phases, dbg, needed):
    NO = NB - OWN0
    nc = bass.Bass("TRN2", target_bir_lowering=False)
    S = Sched(nc, needed)

    def din(name, shape, dt=F32):
        return nc.dram_tensor(name, list(shape), dt, kind="ExternalInput").ap()

    def dscr(name, shape, dt=F32, out=False):
        return nc.dram_tensor(name, list(shape), dt, kind=("ExternalOutput" if out else "Internal")).ap()

    xin = din("xin", [NB * 128, D])
    w_in = din("w_in", [D, INC])
    vecs_d = din("vecs", [128, NV])
    cn_d = din("consts", [128, NCN])
    wup_d = din("w_up", [64, 1024])
    aup_d = din("a_up", [64, 1024])
    gup_d = din("g_up", [160, 1024])
    wout_d = din("w_out", [D, D])
    pq_d = din("peer_query", [D, D])
    sk_d = din("sub_keys", [16, 128, 128])
    pd_d = din("peer_down", [NE, D])
    pu_d = din("peer_up", [NE, D])
    yout = dscr("yout", [NO * 128, D], F32, out=True)

    PS_d = dscr("PS_s", [NB, 128, NRC * 128])
    QKV_d = dscr("QKV_s", [NO + 1, 128, 10 * 128])
    YR_d = dscr("YR_s", [NO, 128, 1024], BF16, out=dbg)
    YA_d = dscr("YA_s", [NO, 128, 1024], BF16, out=dbg)
    HM_d = dscr("HM_s", [NO, 128, D], F32, out=dbg)
    XNT_d = dscr("XNT_s", [NO, 128, D], BF16)
    ST_d = dscr("ST_s", [NO, 128, 2048 + 8])
    hPS = [H("PS%d" % j) for j in range(NB)]
    hQKV = [H("QKV%d" % j) for j in range(NO + 1)]
    hYR = [H() for j in range(NO)]
    hYA = [H() for j in range(NO)]
    hHM = [H() for j in range(NO)]
    hXNT = [H() for j in range(NO)]
    hST = [H() for j in range(NO)]
    hOUT = [H() for j in range(NO)]

    uid = [0]

    def mk(es):
        def T(name, shape, dt=F32):
            uid[0] += 1
            t = es.enter_context(nc.sbuf_tensor("sb%d_%s" % (uid[0], name), list(shape), dt))
            return t, H(name)

        def P(name, shape, dt=F32):
            uid[0] += 1
            t = es.enter_context(nc.psum_tensor("ps%d_%s" % (uid[0], name), list(shape), dt))
            return t, H(name)
        return T, P

    def load_consts(T, bf=True):
        c = Ctx()
        c.vec, c.hvec = T("vecs", [128, NV])
        S.dma("sp", c.vec[:], vecs_d, c.hvec, writes=[c.hvec])
        c.cn, c.hcn = T("cn32", [128, NCN])
        S.dma("sp", c.cn[:], cn_d, c.hcn, writes=[c.hcn])
        c.cb, c.hcb = T("cnbf", [128, NCN], BF16)
        S.dma("pool", c.cb[:], cn_d, c.hcb, writes=[c.hcb])
        return c

    def rmsnorm_T(c, T_, xt, hxt, gcol, junk, hjunk, st, hst, xs, hxs, pT, hpT, uT, huT):
        S.op("act", lambda e: e.activation(out=junk[:], in_=xt[:], func=AF.Square, accum_out=st[:, 0:1]),
             reads=[hxt], writes=([hjunk, hst] if hjunk is not hst else [hst]))
        S.op("dve", lambda e: e.tensor_scalar(out=st[:, 1:2], in0=st[:, 0:1], scalar1=1.0 / D, scalar2=1e-6,
                                              op0=ALU.mult, op1=ALU.add), reads=[hst], writes=[hst])
        S.op("act", lambda e: e.activation(out=st[:, 2:3], in_=st[:, 1:2], func=AF.Sqrt), reads=[hst], writes=[hst])
        S.op("dve", lambda e: e.reciprocal(out=st[:, 3:4], in_=st[:, 2:3]), reads=[hst], writes=[hst])
        S.op("act", lambda e: e.activation(out=xs[:], in_=xt[:], func=AF.Copy, scale=st[:, 3:4]),
             reads=[hxt, hst], writes=[hxs])
        for k in range(DC):
            S.op("pe", lambda e, k=k: e.transpose(pT[:, k, :], xs[:, k * 128:(k + 1) * 128], c.cb[:, C_ID:C_ID + 128]),
                 reads=[hxs, c.hcb], writes=[hpT])
        gb = c.vec[:, gcol:gcol + DC].unsqueeze(2).broadcast_to([128, DC, 128])
        S.op("dve", lambda e: e.tensor_tensor(out=uT[:], in0=pT[:], in1=gb, op=ALU.mult),
             reads=[hpT, c.hvec], writes=[huT])

    def phase_A():
        with contextlib.ExitStack() as es:
            T, P = mk(es)
            c = load_consts(T)
            win, hwin = T("win", [128, DC, INC], BF16)
            for k in range(DC):
                S.dma("pool", win[:, k, :], w_in[k * 128:(k + 1) * 128, :], hwin, writes=[hwin])
            xt2 = [T("xt%d" % i, [128, D]) for i in range(1)] * 2
            st2 = [T("st%d" % i, [128, 4]) for i in range(2)]
            xs2 = [T("xs%d" % i, [128, D], BF16) for i in range(1)] * 2
            pT2 = [P("pT%d" % i, [128, DC, 128], BF16) for i in range(2)]
            uT2 = [T("uT%d" % i, [128, DC, 128], BF16) for i in range(2)]
            pp = [P("pp%d" % i, [128, 4, 128]) for i in range(4)]
            PT, hPT = T("PT", [128, NRC, 129])
            QK, hQK = T("QK", [128, 10, 128])
            dd, hdd = T("dd", [128, NRC, 128])
            pss, hpss = dd, hdd
            S.op("pool", lambda e: e.memset(PT[:], 0.0), writes=[hPT])
            mub = c.vec[:, V_MU:V_MU + NRC].unsqueeze(2).broadcast_to([128, NRC, 128])
            def body_norm(j):
                xt, hxt = xt2[j % 2]
                st, hst = st2[j % 2]
                xs, hxs = xs2[j % 2]
                pT, hpT = pT2[j % 2]
                uT, huT = uT2[j % 2]
                S.dma("sp", xt[:], xin[j * 128:(j + 1) * 128, :], hxt, writes=[hxt])
                rmsnorm_T(c, T, xt, hxt, V_G1, xs, hxs, st, hst, xs, hxs, pT, hpT, uT, huT)

            def body_proj(j):
                uT, huT = uT2[j % 2]
                need_att = (j >= OWN0 - 1)
                chunks = list(range(0 if need_att else 10, 37))
                gi = 0
                for g0 in range(0, len(chunks), 4):
                    grp = chunks[g0:g0 + 4]
                    pt_, hp_ = pp[gi % 4]
                    gi += 1
                    for qi, ch in enumerate(grp):
                        M = 32 if ch == 36 else 128
                        for k in range(DC):
                            S.op("pe", lambda e, qi=qi, ch=ch, k=k, M=M, pt_=pt_: e.matmul(
                                pt_[0:M, qi, :], lhsT=win[:, k, ch * 128:ch * 128 + M], rhs=uT[:, k, :],
                                start=(k == 0), stop=(k == DC - 1)), reads=[hwin, huT], writes=[hp_])
                    for qi, ch in enumerate(grp):
                        M = 32 if ch == 36 else 128
                        eng = "act" if (qi % 2 == 0) else "dve"
                        if ch < 10:
                            dst, hd = QK[0:M, ch, :], hQK
                        else:
                            dst, hd = PT[0:M, ch - 10, 1:129], hPT
                        if eng == "act":
                            S.op("act", lambda e, dst=dst, qi=qi, M=M, pt_=pt_: e.activation(out=dst, in_=pt_[0:M, qi, :], func=AF.Copy),
                                 reads=[hp_], writes=[hd])
                        else:
                            S.op("dve", lambda e, dst=dst, qi=qi, M=M, pt_=pt_: e.tensor_copy(out=dst, in_=pt_[0:M, qi, :]),
                                 reads=[hp_], writes=[hd])
                if need_att:
                    ja = j - (OWN0 - 1)
                    S.dma("sp", QKV_d[ja].rearrange("p (c t) -> p c t", c=10), QK[:], hQK, reads=[hQK], writes=[hQKV[ja]])
                S.op("dve", lambda e: e.tensor_tensor(out=dd[:], in0=PT[:, :, 0:128], in1=PT[:, :, 1:129], op=ALU.subtract),
                     reads=[hPT], writes=[hdd])
                S.op("dve", lambda e: e.tensor_tensor(out=dd[:], in0=dd[:], in1=mub, op=ALU.mult),
                     reads=[hdd, c.hvec], writes=[hdd])
                S.op("dve", lambda e: e.tensor_tensor(out=dd[:], in0=dd[:], in1=PT[:, :, 1:129], op=ALU.add),
                     reads=[hdd, hPT], writes=[hdd])
                S.op("act", lambda e: e.activation(out=PT[:, :, 0:1], in_=PT[:, :, 128:129], func=AF.Copy),
                     reads=[hPT, hdd, hpss], writes=[hPT])
                S.dma("sp", PS_d[j].rearrange("p (c t) -> p c t", c=NRC), pss[:], hpss, reads=[hpss], writes=[hPS[j]])

            S.play(S.record(lambda: body_norm(0)))
            for j in range(NB):
                ra = S.record(lambda: body_proj(j))
                if j + 1 < NB:
                    rb = S.record(lambda: body_norm(j + 1))
                    S.play2(ra, rb)
                else:
                    S.play(ra)
            S.barrier()

    def phase_B():
        with contextlib.ExitStack() as es:
            T, P = mk(es)
            c = load_consts(T)
            vec = c.vec

            def vb(col, n=8):
                return vec[:, col:col + n].unsqueeze(2).broadcast_to([128, n, 128])
            wup, hwup = T("wup", [128, 1024], BF16)
            aup, haup = T("aup", [128, 1024], BF16)
            gup, hgup = T("gup", [128, 2, 1024], BF16)
            S.dma("pool", wup[0:64, :], wup_d, hwup, writes=[hwup])
            S.dma("pool", aup[64:128, :], aup_d, haup, writes=[haup])
            S.dma("pool", gup[:, 0, :], gup_d[0:128, :], hgup, writes=[hgup])
            S.dma("pool", gup[0:32, 1, :], gup_d[128:160, :], hgup, writes=[hgup])
            ps, hps = T("ps", [128, NRC, 128])
            names32 = ["nld", "aa", "kap", "kp", "beta", "cs", "cum", "t1", "t2", "t3"]
            F = {}
            for n in names32:
                F[n] = T(n, [128, 8, 128])
            namesbf = ["Bh", "Qh", "BG", "QG", "vb", "sqb"]
            Bf = {}
            for n in namesbf:
                Bf[n] = T(n, [128, 8, 128], BF16)
            lw, hlw = T("lw", [128, 128], BF16)
            sg, hsg = T("sg", [128, 2, 128], BF16)
            ones, hones = T("ones", [128, 1024])
            S.op("pool", lambda e: e.memset(ones[:], 1.0), writes=[hones])
            S32, hS32 = T("S32", [128, 8, 64])
            Sb, hSb = T("Sb", [128, 8, 64], BF16)
            S.op("pool", lambda e: e.memset(S32[:], 0.0), writes=[hS32])
            S.op("pool", lambda e: e.memset(Sb[:], 0.0), writes=[hSb])
            gms = [{}, {}]
            for si in range(2):
                for n in ["Ub", "Lb", "U2", "L2", "Xb", "SAb"]:
                    gms[si][n] = T(n + str(si), [64, 16, 64], BF16)
            gms2 = [[{}, {}], [{}, {}]]
            for pp_ in range(2):
                for si in range(2):
                    for n in ["Lak", "Mrb", "Mrk", "Qb"]:
                        gms2[pp_][si][n] = T("%s%d_%d" % (n, si, pp_), [64, 16, 64], BF16)
            F2 = {n: [T("%s_%d" % (n, i), [128, 8, 128]) for i in range(2)] for n in ("gg", "bon")}
            Bf2 = {n: [T("%s_%d" % (n, i), [128, 8, 128], BF16) for i in range(2)] for n in ("Kh", "Rh")}
            sm2 = [T("sm_%d" % i, [128, 8, 2, 4]) for i in range(2)]
            Vt2 = [T("Vt_%d" % i, [64, 2, 8, 128], BF16) for i in range(2)]
            BGt2 = [T("BGt_%d" % i, [64, 2, 8, 128], BF16) for i in range(2)]
            QGt2 = [T("QGt_%d" % i, [64, 2, 8, 128], BF16) for i in range(2)]
            ysb, hysb = T("ysb", [64, 16, 64])
            yc, hyc = T("yc", [64, 16, 64])
            ysq, hysq = ysb, hysb
            gst, hgst = T("gst", [64, 16, 4])
            yf, hyf = T("yf", [128, 8, 128])
            yrb, hyrb = T("yrb", [128, 8, 128], BF16)
            PA = [P("PA%d" % i, [128, 1024]) for i in range(3)]
            PTr, hPTr = P("PTr", [128, 2048], BF16)
            pi = [0]

            def nextPA():
                t = PA[pi[0] % 2]
                pi[0] += 1
                return t

            def nextPB():
                return PA[2]
            maskU = c.cn[0:64, C_MU:C_MU + 64].unsqueeze(1).broadcast_to([64, 16, 64])
            maskUi = c.cn[0:64, C_MUI:C_MUI + 64].unsqueeze(1).broadcast_to([64, 16, 64])
            maskL = c.cn[0:64, C_ML:C_ML + 64].unsqueeze(1).broadcast_to([64, 16, 64])
            identb = c.cb[:, C_ID:C_ID + 128]
            ident32 = c.cn[:, C_ID:C_ID + 128]
            bones = c.cb[:, C_BO:C_BO + 128]

            def tt(eng, out, i0, i1, op, reads, writes):
                S.op(eng, lambda e: e.tensor_tensor(out=out, in0=i0, in1=i1, op=op), reads=reads, writes=writes)

            def actf(out, in_, func, reads, writes, **kw):
                S.op("act", lambda e: e.activation(out=out, in_=in_, func=func, **kw), reads=reads, writes=writes)

            HORD = list(range(0, 16, 2)) + list(range(1, 16, 2))
            def body(j):
                own = j >= OWN0
                p = j % 2
                F["gg"], F["bon"] = F2["gg"][p], F2["bon"][p]
                Bf["Kh"], Bf["Rh"] = Bf2["Kh"][p], Bf2["Rh"][p]
                sm, hsm = sm2[p]
                Vt, hVt = Vt2[p]
                BGt, hBGt = BGt2[p]
                QGt, hQGt = QGt2[p]
                for si in range(2):
                    gms[si].update(gms2[p][si])
                S.dma("sp", ps[:], PS_d[j].rearrange("p (c t) -> p c t", c=NRC), hps, reads=[hPS[j]], writes=[hps])
                r_, k_, v_ = ps[:, 0:8, :], ps[:, 8:16, :], ps[:, 16:24, :]
                actf(lw[0:64, :], ps[0:64, 24, :], AF.Tanh, [hps], [hlw])
                actf(lw[64:128, :], ps[64:128, 24, :], AF.Copy, [hps], [hlw])
                actf(sg[:, 0, :], ps[:, 25, :], AF.Sigmoid, [hps], [hsg])
                actf(sg[0:32, 1, :], ps[0:32, 26, :], AF.Sigmoid, [hps], [hsg])
                pw_, hpw = nextPA()
                pa_, hpa = nextPA()
                for m in range(8):
                    S.op("pe", lambda e, m=m: e.matmul(pw_[:, m * 128:(m + 1) * 128], lhsT=wup[0:64, m * 128:(m + 1) * 128], rhs=lw[0:64, :], start=True, stop=True),
                         reads=[hwup, hlw], writes=[hpw])
                    S.op("pe", lambda e, m=m: e.matmul(pa_[:, m * 128:(m + 1) * 128], lhsT=aup[64:128, m * 128:(m + 1) * 128], rhs=lw[64:128, :], start=True, stop=True),
                         reads=[haup, hlw], writes=[hpa])
                nld, hnld = F["nld"]
                aa, haa = F["aa"]
                t1, ht1 = F["t1"]
                t2, ht2 = F["t2"]
                t3, ht3 = F["t3"]
                v3 = lambda p_: p_[:].rearrange("p (m t) -> p m t", m=8)
                tt("dve", t1[:], v3(pw_), vb(V_W0), ALU.add, [hpw, c.hvec], [ht1])
                actf(t1[:], t1[:], AF.Sigmoid, [ht1], [ht1])
                S.op("dve", lambda e: e.tensor_scalar(out=nld[:], in0=t1[:], scalar1=0.6065306597126334, scalar2=None, op0=ALU.mult),
                     reads=[ht1], writes=[hnld])
                tt("dve", t2[:], v3(pa_), vb(V_A0), ALU.add, [hpa, c.hvec], [ht2])
                actf(aa[:], t2[:], AF.Sigmoid, [ht2], [haa])
                if own:
                    pg_, hpg = nextPA()
                    for m in range(8):
                        S.op("pe", lambda e, m=m: e.matmul(pg_[:, m * 128:(m + 1) * 128], lhsT=gup[:, 0, m * 128:(m + 1) * 128], rhs=sg[:, 0, :], start=True, stop=False),
                             reads=[hgup, hsg], writes=[hpg])
                        S.op("pe", lambda e, m=m: e.matmul(pg_[:, m * 128:(m + 1) * 128], lhsT=gup[0:32, 1, m * 128:(m + 1) * 128], rhs=sg[0:32, 1, :], start=False, stop=True),
                             reads=[hgup, hsg], writes=[hpg])
                    gg, hgg = F["gg"]
                    actf(gg[:], v3(pg_), AF.Copy, [hpg], [hgg])
                kap, hkap = F["kap"]
                sqb, hsqb = Bf["sqb"]
                tt("dve", kap[:], k_, vb(V_KK), ALU.mult, [hps, c.hvec], [hkap])
                actf(sqb[:], kap[:], AF.Square, [hkap], [hsqb])
                pq_, hpq = nextPA()
                for hh in range(2):
                    S.op("pe", lambda e, hh=hh: e.matmul(pq_[:, hh * 512:(hh + 1) * 512], lhsT=bones, rhs=sqb[:, hh * 4:(hh + 1) * 4, :], start=True, stop=True),
                         reads=[c.hcb, hsqb], writes=[hpq])
                actf(t3[:], v3(pq_), AF.Sqrt, [hpq], [ht3])
                S.op("dve", lambda e: e.tensor_scalar(out=t3[:], in0=t3[:], scalar1=1e-12, scalar2=None, op0=ALU.max), reads=[ht3], writes=[ht3])
                S.op("dve", lambda e: e.reciprocal(out=t3[:], in_=t3[:]), reads=[ht3], writes=[ht3])
                tt("dve", kap[:], kap[:], t3[:], ALU.mult, [hkap, ht3], [hkap])
                kp, hkp = F["kp"]
                S.op("dve", lambda e: e.scalar_tensor_tensor(out=t2[:], in0=aa[:], scalar=1.0, in1=vb(V_KA), op0=ALU.subtract, op1=ALU.mult),
                     reads=[haa, c.hvec], writes=[ht2])
                S.op("dve", lambda e: e.scalar_tensor_tensor(out=kp[:], in0=t2[:], scalar=1.0, in1=k_, op0=ALU.add, op1=ALU.mult),
                     reads=[ht2, hps], writes=[hkp])
                beta, hbeta = F["beta"]
                tt("dve", beta[:], kap[:], aa[:], ALU.mult, [hkap, haa], [hbeta])
                cs, hcs = F["cs"]
                cum, hcum = F["cum"]
                S.op("dve", lambda e: e.tensor_tensor_scan(out=cs[:].rearrange("p m t -> p (m t)"), data0=ones[:],
                                                           data1=nld[:].rearrange("p m t -> p (m t)"), initial=0.0,
                                                           op0=ALU.mult, op1=ALU.add), reads=[hones, hnld], writes=[hcs])
                cs4 = cs[:].rearrange("p m (s t) -> p m s t", s=2)
                nld4 = nld[:].rearrange("p m (s t) -> p m s t", s=2)
                cum4 = cum[:].rearrange("p m (s t) -> p m s t", s=2)
                tt("dve", sm[:, :, :, 0:1], cs4[:, :, :, 0:1], nld4[:, :, :, 0:1], ALU.subtract, [hcs, hnld], [hsm])
                tt("dve", cum4, cs4, sm[:, :, :, 0:1].broadcast_to([128, 8, 2, 64]), ALU.subtract, [hcs, hsm], [hcum])
                actf(sm[:, :, :, 2:3], cum4[:, :, :, 63:64], AF.Exp, [hcum], [hsm], scale=-1.0)
                Kh, hKh = Bf["Kh"]
                Rh, hRh = Bf["Rh"]
                Bh, hBh = Bf["Bh"]
                Qh, hQh = Bf["Qh"]
                BG, hBG = Bf["BG"]
                QG, hQG = Bf["QG"]
                vbf, hvbf = Bf["vb"]
                actf(t1[:], cum[:], AF.Exp, [hcum], [ht1], scale=-1.0)
                tt("dve", Rh[:], r_, t1[:], ALU.mult, [hps, ht1], [hRh])
                tt("dve", t2[:], cum[:], nld[:], ALU.subtract, [hcum, hnld], [ht2])
                actf(t2[:], t2[:], AF.Exp, [ht2], [ht2], scale=-1.0)
                tt("dve", Kh[:], kap[:], t2[:], ALU.mult, [hkap, ht2], [hKh])
                actf(t3[:], cum[:], AF.Exp, [hcum], [ht3])
                tt("dve", Bh[:], beta[:], t3[:], ALU.mult, [hbeta, ht3], [hBh])
                tt("dve", Qh[:], kp[:], t3[:], ALU.mult, [hkp, ht3], [hQh])
                t14 = t1[:].rearrange("p m (s t) -> p m s t", s=2)
                tt("dve", t14, cum4, cum4[:, :, :, 63:64].broadcast_to([128, 8, 2, 64]), ALU.subtract, [hcum], [ht1])
                actf(t1[:], t1[:], AF.Exp, [ht1], [ht1])
                tt("dve", BG[:], beta[:], t1[:], ALU.mult, [hbeta, ht1], [hBG])
                tt("dve", QG[:], kp[:], t1[:], ALU.mult, [hkp, ht1], [hQG])
                actf(vbf[:], v_, AF.Copy, [hps], [hvbf])
                for (src, hsrc, dst, hdst) in ((vbf, hvbf, Vt, hVt), (BG, hBG, BGt, hBGt), (QG, hQG, QGt, hQGt)):
                    for m in range(8):
                        for s in range(2):
                            S.op("pe", lambda e, m=m, s=s, src=src: e.transpose(
                                PTr[0:64, (s * 8 + m) * 128:(s * 8 + m + 1) * 128], src[:, m, s * 64:(s + 1) * 64], identb),
                                reads=[hsrc, c.hcb], writes=[hPTr])
                    S.op("act", lambda e, dst=dst: e.activation(out=dst[:].rearrange("t s m c -> t (s m c)"), in_=PTr[0:64, :], func=AF.Copy),
                         reads=[hPTr], writes=[hdst])
                if own:
                    bon, hbon = F["bon"]
                    tt("dve", t2[:], r_, kp[:], ALU.mult, [hps, hkp], [ht2])
                    tt("dve", sqb[:], t2[:], vb(V_RK), ALU.mult, [ht2, c.hvec], [hsqb])
                    pb_, hpb = nextPA()
                    for hh in range(2):
                        S.op("pe", lambda e, hh=hh: e.matmul(pb_[:, hh * 512:(hh + 1) * 512], lhsT=bones, rhs=sqb[:, hh * 4:(hh + 1) * 4, :], start=True, stop=True),
                             reads=[c.hcb, hsqb], writes=[hpb])
                    tt("dve", bon[:], v3(pb_), v_, ALU.mult, [hpb, hps], [hbon])
                def hrows(h):
                    return slice((h % 2) * 64, (h % 2) * 64 + 64)

                idb = c.cn[0:64, C_ID:C_ID + 64].unsqueeze(1).broadcast_to([64, 16, 64])

                def gram(s, lt, hl, rt, hr, mask, dname, eng):
                    ts = slice(s * 64, (s + 1) * 64)
                    p_, hp_ = nextPA()
                    pv = p_[0:64, :].rearrange("p (h t) -> p h t", h=16)
                    for h in HORD:
                        S.op("pe", lambda e, h=h: e.matmul(pv[:, h, :], lhsT=lt[hrows(h), h // 2, ts], rhs=rt[hrows(h), h // 2, ts], start=True, stop=True),
                             reads=[hl, hr], writes=[hp_])
                    d_, hd_ = gms[s][dname]
                    tt(eng, d_[:], pv, mask, ALU.mult, [hp_, c.hcn], [hd_])

                for s in range(2):
                    gram(s, Bh, hBh, Kh, hKh, maskU, "Ub", "dve")
                    gram(s, Kh, hKh, Bh, hBh, maskL, "Lb", "dve")
                for s in range(2):
                    Ub, hUb = gms[s]["Ub"]
                    Qb, hQb = gms[s]["Qb"]
                    tt("dve", Qb[:], idb, Ub[:], ALU.subtract, [c.hcn, hUb], [hQb])
                for s in range(2):
                    gram(s, Qh, hQh, Kh, hKh, maskU, "Lak", "dve")
                    if own:
                        gram(s, Bh, hBh, Rh, hRh, maskUi, "Mrb", "dve")
                        gram(s, Qh, hQh, Rh, hRh, maskUi, "Mrk", "dve")

                def inv_level(s, lvl):
                    gm = gms[s]
                    cur = ("Ub", "Lb") if lvl % 2 == 0 else ("U2", "L2")
                    nxt = ("U2", "L2") if lvl % 2 == 0 else ("Ub", "Lb")
                    Qb, hQb = gm["Qb"]
                    Uk, hUk = gm[cur[0]]
                    Lk, hLk = gm[cur[1]]
                    Un, hUn = gm[nxt[0]]
                    Ln, hLn = gm[nxt[1]]
                    p2, hp2 = nextPA()
                    p2v = p2[0:64, :].rearrange("p (h t) -> p h t", h=16)
                    for h in HORD:
                        S.op("pe", lambda e, h=h: e.matmul(p2v[:, h, :], lhsT=Uk[:, h, :], rhs=Lk[:, h, :], start=True, stop=True),
                             reads=[hUk, hLk], writes=[hp2])
                    actf(Ln[:], p2v, AF.Copy, [hp2], [hLn])
                    if lvl < 4:
                        p1, hp1 = nextPA()
                        p1v = p1[0:64, :].rearrange("p (h t) -> p h t", h=16)
                        for h in HORD:
                            S.op("pe", lambda e, h=h: e.matmul(p1v[:, h, :], lhsT=Lk[:, h, :], rhs=Uk[:, h, :], start=True, stop=True),
                                 reads=[hUk, hLk], writes=[hp1])
                        S.op("dve", lambda e: e.tensor_copy(out=Un[:], in_=p1v), reads=[hp1], writes=[hUn])
                    p3, hp3 = nextPA()
                    p3v = p3[0:64, :].rearrange("p (h t) -> p h t", h=16)
                    for h in HORD:
                        S.op("pe", lambda e, h=h: e.matmul(p3v[:, h, :], lhsT=Ln[:, h, :], rhs=Qb[:, h, :], start=True, stop=True),
                             reads=[hLn, hQb], writes=[hp3])
                    tt("dve", Qb[:], p3v, Qb[:], ALU.add, [hp3, hQb], [hQb])

                for lvl in range(5):
                    for s in range(2):
                        inv_level(s, lvl)

                S.mark()
                for s in range(2):
                    ts = slice(s * 64, (s + 1) * 64)
                    gm = gms[s]
                    Qb, hQb = gm["Qb"]
                    Lak, hLak = gm["Lak"]
                    Xb, hXb = gm["Xb"]
                    SAb, hSAb = gm["SAb"]
                    px, hpx = nextPB()
                    pxv = px[0:64, :].rearrange("p (h t) -> p h t", h=16)
                    for h in HORD:
                        cs_ = slice((h % 2) * 64, (h % 2) * 64 + 64)
                        S.op("pe", lambda e, h=h: e.matmul(pxv[:, h, :], lhsT=Kh[hrows(h), h // 2, ts], rhs=Sb[hrows(h), h // 2, :], start=True, stop=False),
                             reads=[hKh, hSb], writes=[hpx])
                        S.op("pe", lambda e, h=h, cs_=cs_: e.matmul(pxv[:, h, :], lhsT=Lak[:, h, :], rhs=Vt[:, s, h // 2, cs_], start=False, stop=True),
                             reads=[hLak, hVt], writes=[hpx])
                    actf(Xb[:], pxv, AF.Copy, [hpx], [hXb])
                    psa, hpsa = nextPB()
                    psav = psa[0:64, :].rearrange("p (h t) -> p h t", h=16)
                    for h in HORD:
                        S.op("pe", lambda e, h=h: e.matmul(psav[:, h, :], lhsT=Qb[:, h, :], rhs=Xb[:, h, :], start=True, stop=True),
                             reads=[hQb, hXb], writes=[hpsa])
                    actf(SAb[:], psav, AF.Copy, [hpsa], [hSAb], scale=-1.0)
                    if own:
                        Mrb, hMrb = gm["Mrb"]
                        Mrk, hMrk = gm["Mrk"]
                        py, hpy = nextPB()
                        pyv = py[0:64, :].rearrange("p (h t) -> p h t", h=16)
                        for h in HORD:
                            cs_ = slice((h % 2) * 64, (h % 2) * 64 + 64)
                            S.op("pe", lambda e, h=h: e.matmul(pyv[:, h, :], lhsT=Rh[hrows(h), h // 2, ts], rhs=Sb[hrows(h), h // 2, :], start=True, stop=False),
                                 reads=[hRh, hSb], writes=[hpy])
                            S.op("pe", lambda e, h=h: e.matmul(pyv[:, h, :], lhsT=Mrb[:, h, :], rhs=SAb[:, h, :], start=False, stop=False),
                                 reads=[hMrb, hSAb], writes=[hpy])
                            S.op("pe", lambda e, h=h, cs_=cs_: e.matmul(pyv[:, h, :], lhsT=Mrk[:, h, :], rhs=Vt[:, s, h // 2, cs_], start=False, stop=True),
                                 reads=[hMrk, hVt], writes=[hpy])
                        actf(ysb[:], pyv, AF.Copy, [hpy], [hysb])
                        S.op("dve", lambda e: e.tensor_reduce(out=gst[:, :, 0:1], in_=ysb[:], axis=AX.X, op=ALU.add), reads=[hysb], writes=[hgst])
                        S.op("dve", lambda e: e.tensor_scalar(out=gst[:, :, 0:1], in0=gst[:, :, 0:1], scalar1=1.0 / 64, scalar2=None, op0=ALU.mult), reads=[hgst], writes=[hgst])
                        tt("dve", yc[:], ysb[:], gst[:, :, 0:1].broadcast_to([64, 16, 64]), ALU.subtract, [hysb, hgst], [hyc])
                        actf(ysq[:], yc[:], AF.Square, [hyc], [hysq])
                        S.op("dve", lambda e: e.tensor_reduce(out=gst[:, :, 1:2], in_=ysq[:], axis=AX.X, op=ALU.add), reads=[hysq], writes=[hgst])
                        S.op("dve", lambda e: e.tensor_scalar(out=gst[:, :, 1:2], in0=gst[:, :, 1:2], scalar1=1.0 / 64, scalar2=64e-5, op0=ALU.mult, op1=ALU.add), reads=[hgst], writes=[hgst])
                        actf(gst[:, :, 2:3], gst[:, :, 1:2], AF.Sqrt, [hgst], [hgst])
                        S.op("dve", lambda e: e.reciprocal(out=gst[:, :, 3:4], in_=gst[:, :, 2:3]), reads=[hgst], writes=[hgst])
                        tt("dve", yc[:], yc[:], gst[:, :, 3:4].broadcast_to([64, 16, 64]), ALU.mult, [hyc, hgst], [hyc])
                        pt2, hpt2 = nextPB()
                        for m in range(8):
                            S.op("pe", lambda e, m=m: e.transpose(pt2[:, m * 64:(m + 1) * 64], yc[:, 2 * m:2 * m + 2, :].rearrange("t h i -> t (h i)"), ident32[0:64, 0:64]),
                                 reads=[hyc, c.hcn], writes=[hpt2])
                        S.op("dve", lambda e: e.tensor_copy(out=yf[:, :, ts], in_=pt2[:, 0:512].rearrange("p (m t) -> p m t", m=8)), reads=[hpt2], writes=[hyf])
                    pst, hpst = nextPB()
                    for h in HORD:
                        cs_ = slice((h % 2) * 64, (h % 2) * 64 + 64)
                        o_ = pst[hrows(h), (h // 2) * 64:(h // 2) * 64 + 64]
                        S.op("pe", lambda e, h=h, cs_=cs_, o_=o_: e.matmul(o_, lhsT=BGt[:, s, h // 2, cs_], rhs=SAb[:, h, :], start=True, stop=False),
                             reads=[hBGt, hSAb], writes=[hpst])
                        S.op("pe", lambda e, h=h, cs_=cs_, o_=o_: e.matmul(o_, lhsT=QGt[:, s, h // 2, cs_], rhs=Vt[:, s, h // 2, cs_], start=False, stop=True),
                             reads=[hQGt, hVt], writes=[hpst])
                    tt("dve", S32[:], S32[:], sm[:, :, s, 2:3].broadcast_to([128, 8, 64]), ALU.mult, [hS32, hsm], [hS32])
                    tt("dve", S32[:], S32[:], pst[:, 0:512].rearrange("p (m i) -> p m i", m=8), ALU.add, [hS32, hpst], [hS32])
                    actf(Sb[:], S32[:], AF.Copy, [hS32], [hSb])
                if own:
                    gg, hgg = F["gg"]
                    bon, hbon = F["bon"]
                    tt("dve", yf[:], yf[:], vb(V_GNW), ALU.mult, [hyf, c.hvec], [hyf])
                    tt("dve", yf[:], yf[:], vb(V_GNB), ALU.add, [hyf, c.hvec], [hyf])
                    tt("dve", yf[:], yf[:], bon[:], ALU.add, [hyf, hbon], [hyf])
                    tt("dve", yrb[:], yf[:], gg[:], ALU.mult, [hyf, hgg], [hyrb])
                    jo = j - OWN0
                    S.dma("sp", YR_d[jo].rearrange("p (m t) -> p m t", m=8), yrb[:], hyrb, reads=[hyrb], writes=[hYR[jo]])

            def rec(j):
                r = S.record(lambda: body(j))
                k = [i for i, it in enumerate(r) if it[0] == "mark"][0]
                return r[:k], r[k + 1:]
            A0, curB = rec(0)
            S.play(A0)
            for j in range(NB):
                if j + 1 < NB:
                    A1, B1 = rec(j + 1)
                    S.play2(curB, A1)
                    curB = B1
                else:
                    S.play(curB)
            S.barrier()


    def tt_(eng, out, i0, i1, op, reads, writes):
        S.op(eng, lambda e: e.tensor_tensor(out=out, in0=i0, in1=i1, op=op), reads=reads, writes=writes)

    def act_(out, in_, func, reads, writes, **kw):
        S.op("act", lambda e: e.activation(out=out, in_=in_, func=func, **kw), reads=reads, writes=writes)

    def phase_C():
        with contextlib.ExitStack() as es:
            T, P = mk(es)
            c = load_consts(T)
            vec = c.vec
            bones = c.cb[:, C_BO:C_BO + 128]
            identb = c.cb[:, C_ID:C_ID + 128]
            qk, hqk = T("qk", [128, 10, 128])
            sqb, hsqb = T("sqb", [128, 8, 128], BF16)
            t1, ht1 = T("t1", [128, 8, 128])
            qT, hqT = T("qT", [128, 8, 128], BF16)
            knb, hknb = T("knb", [128, 128], BF16)
            vbf, hvbf = T("vbf", [128, 128], BF16)
            kd = [T("kd%d" % i, [128, 2, 128], BF16) for i in range(2)]
            Vk = [T("Vk%d" % i, [128, 128], BF16) for i in range(2)]
            e1, he1 = T("e1", [128, 4, 128], BF16)
            PTb, hPTb = T("PTb", [128, 4, 128], BF16)
            onesb, honesb = T("onesb", [128, 64], BF16)
            S.op("pool", lambda e: e.memset(onesb[:], 1.0), writes=[honesb])
            esk, hesk = T("esk", [128, 8])
            act_(esk[:], vec[:, V_SK:V_SK + 8], AF.Exp, [c.hvec], [hesk])
            den, hden = T("den", [128, 4, 128])
            ya, hya = T("ya", [128, 8, 128], BF16)
            PS1, hPS1 = P("PS1", [128, 1024])
            psc = [P("psc%d" % i, [128, 512]) for i in range(2)]
            po, hpo = P("po", [128, 512])
            pdn, hpdn = P("pdn", [128, 512])
            pvT, hpvT = P("pvT", [128, 128], BF16)

            def norm_rows(src, n, gcol, dst, hdst):
                act_(sqb[:, 0:n, :], src, AF.Square, [hqk], [hsqb])
                for hh in range(0, n, 4):
                    w_ = min(4, n - hh)
                    S.op("pe", lambda e, hh=hh, w_=w_: e.matmul(PS1[:, hh * 128:(hh + w_) * 128], lhsT=bones, rhs=sqb[:, hh:hh + w_, :], start=True, stop=True),
                         reads=[c.hcb, hsqb], writes=[hPS1])
                pv = PS1[:, 0:n * 128].rearrange("p (m t) -> p m t", m=n)
                S.op("dve", lambda e: e.tensor_scalar(out=t1[:, 0:n, :], in0=pv, scalar1=1.0 / 64, scalar2=1e-6, op0=ALU.mult, op1=ALU.add),
                     reads=[hPS1], writes=[ht1])
                act_(t1[:, 0:n, :], t1[:, 0:n, :], AF.Sqrt, [ht1], [ht1])
                S.op("dve", lambda e: e.reciprocal(out=t1[:, 0:n, :], in_=t1[:, 0:n, :]), reads=[ht1], writes=[ht1])
                tt_("dve", t1[:, 0:n, :], t1[:, 0:n, :], src, ALU.mult, [ht1, hqk], [ht1])
                S.op("dve", lambda e: e.tensor_scalar(out=dst, in0=t1[:, 0:n, :], scalar1=vec[:, gcol:gcol + 1], scalar2=None, op0=ALU.mult),
                     reads=[ht1, c.hvec], writes=[hdst])

            def kv_prep(slot):
                kd_, hkd_ = kd[slot]
                Vk_, hVk_ = Vk[slot]
                norm_rows(qk[:, 8:9, :], 1, V_KG, knb[:].unsqueeze(1), hknb)
                for g in range(2):
                    S.op("pe", lambda e, g=g: e.matmul(PS1[:, 512 + g * 128:512 + (g + 1) * 128], lhsT=c.cb[:, C_SEL + g * 128:C_SEL + (g + 1) * 128], rhs=knb[:], start=True, stop=True),
                         reads=[c.hcb, hknb], writes=[hPS1])
                act_(kd_[:], PS1[:, 512:768].rearrange("p (g t) -> p g t", g=2), AF.Copy, [hPS1], [hkd_])
                act_(vbf[:], qk[:, 9, :], AF.Copy, [hqk], [hvbf])
                S.op("pe", lambda e: e.transpose(pvT[:], vbf[:], identb), reads=[hvbf, c.hcb], writes=[hpvT])
                S.op("dve", lambda e: e.tensor_copy(out=Vk_[:], in_=pvT[:]), reads=[hpvT], writes=[hVk_])

            S.dma("sp", qk[:, 8:10, :], QKV_d[0].rearrange("p (c t) -> p c t", c=10)[:, 8:10, :], hqk, reads=[hQKV[0]], writes=[hqk])
            kv_prep(0)
            for jo in range(NO):
                ja = jo + 1
                S.dma("sp", qk[:], QKV_d[ja].rearrange("p (c t) -> p c t", c=10), hqk, reads=[hQKV[ja]], writes=[hqk])
                kv_prep(ja % 2)
                norm_rows(qk[:, 0:8, :], 8, V_QG, qT[:], hqT)
                si = 0
                for g in range(2):
                    for par in range(2):
                        rows = slice(par * 64, par * 64 + 64)
                        for wi, slot in enumerate(((ja - 1) % 2, ja % 2)):
                            kd_, hkd_ = kd[slot]
                            Vk_, hVk_ = Vk[slot]
                            ps_, hps_ = psc[si % 2]
                            si += 1
                            S.op("pe", lambda e, ps_=ps_, kd_=kd_: e.matmul(ps_[:], lhsT=kd_[rows, g, :], rhs=qT[rows, 4 * g:4 * g + 4, :], start=True, stop=True),
                                 reads=[hkd_, hqT], writes=[hps_])
                            act_(e1[:], ps_[:].rearrange("p (h t) -> p h t", h=4), AF.Exp, [hps_], [he1], scale=0.125)
                            mcol = C_MC if wi == 1 else (C_MPF if jo == 0 else C_MP)
                            mk_ = c.cb[:, mcol:mcol + 128].unsqueeze(1).broadcast_to([128, 4, 128])
                            tt_("dve", PTb[:], e1[:], mk_, ALU.mult, [he1, c.hcb], [hPTb])
                            S.op("pe", lambda e, Vk_=Vk_, wi=wi: e.matmul(po[rows, :], lhsT=Vk_[:, g * 64:(g + 1) * 64], rhs=PTb[:].rearrange("p h t -> p (h t)"), start=(wi == 0), stop=(wi == 1)),
                                 reads=[hVk_, hPTb], writes=[hpo])
                            S.op("pe", lambda e, wi=wi: e.matmul(pdn[rows, :], lhsT=onesb[:], rhs=PTb[:].rearrange("p h t -> p (h t)"), start=(wi == 0), stop=(wi == 1)),
                                 reads=[honesb, hPTb], writes=[hpdn])
                    tt_("dve", den[:], pdn[:].rearrange("p (h t) -> p h t", h=4), esk[:, 4 * g:4 * g + 4].unsqueeze(2).broadcast_to([128, 4, 128]), ALU.add, [hpdn, hesk], [hden])
                    S.op("dve", lambda e: e.reciprocal(out=den[:], in_=den[:]), reads=[hden], writes=[hden])
                    tt_("dve", ya[:, 4 * g:4 * g + 4, :], po[:].rearrange("p (h t) -> p h t", h=4), den[:], ALU.mult, [hpo, hden], [hya])
                S.dma("sp", YA_d[jo].rearrange("p (m t) -> p m t", m=8), ya[:], hya, reads=[hya], writes=[hYA[jo]])
            S.barrier()

    def phase_D():
        with contextlib.ExitStack() as es:
            T, P = mk(es)
            c = load_consts(T)
            wo, hwo = T("wo", [128, DC, D], BF16)
            for k in range(DC):
                S.dma("pool", wo[:, k, :], wout_d[k * 128:(k + 1) * 128, :], hwo, writes=[hwo])
            yr, hyr = T("yr", [128, 8, 128], BF16)
            ya, hya = T("ya", [128, 8, 128], BF16)
            xt, hxt = T("xt", [128, D])
            hm, hhm = T("hm", [128, D])
            st, hst = T("st", [128, 4])
            xs, hxs = T("xs", [128, D], BF16)
            uT, huT = T("uT", [128, DC, 128], BF16)
            pT, hpT = P("pT", [128, DC, 128], BF16)
            pp = [P("pp%d" % i, [128, 512]) for i in range(4)]
            for jo in range(NO):
                j = OWN0 + jo
                S.dma("sp", yr[:], YR_d[jo].rearrange("p (m t) -> p m t", m=8), hyr, reads=[hYR[jo]], writes=[hyr])
                S.dma("sp", ya[:], YA_d[jo].rearrange("p (m t) -> p m t", m=8), hya, reads=[hYA[jo]], writes=[hya])
                S.dma("sp", xt[:], xin[j * 128:(j + 1) * 128, :], hxt, writes=[hxt])
                for cg in range(4):
                    p_, hp_ = pp[cg]
                    for kc in range(DC):
                        src, hsrc = (yr, hyr) if kc < 8 else (ya, hya)
                        S.op("pe", lambda e, p_=p_, kc=kc, cg=cg, src=src: e.matmul(p_[:], lhsT=src[:, kc % 8, :], rhs=wo[:, kc, cg * 512:(cg + 1) * 512], start=(kc == 0), stop=(kc == DC - 1)),
                             reads=[hsrc, hwo], writes=[hp_])
                    tt_("dve", hm[:, cg * 512:(cg + 1) * 512], p_[:], xt[:, cg * 512:(cg + 1) * 512], ALU.add, [hp_, hxt], [hhm])
                S.dma("sp", HM_d[jo], hm[:], hhm, reads=[hhm], writes=[hHM[jo]])
                rmsnorm_T(c, T, hm, hhm, V_G2, xs, hxs, st, hst, xs, hxs, pT, hpT, uT, huT)
                S.dma("sp", XNT_d[jo].rearrange("p (k t) -> p k t", k=DC), uT[:], huT, reads=[huT], writes=[hXNT[jo]])
            S.barrier()

    def phase_E1():
        with contextlib.ExitStack() as es:
            T, P = mk(es)
            c = load_consts(T)
            pq, hpq = T("pq", [128, DC, D], BF16)
            for k in range(DC):
                S.dma("pool", pq[:, k, :], pq_d[k * 128:(k + 1) * 128, :], hpq, writes=[hpq])
            skn, hskn = T("skn", [128, 16, 128])
            S.dma("sp", skn[:], sk_d.rearrange("c n d -> n c d"), hskn, writes=[hskn])
            skT, hskT = T("skT", [128, 16, 128])
            pbig = [P("pb%d" % i, [128, 16, 128]) for i in range(2)]
            p0, hp0 = pbig[0]
            for hc in range(16):
                S.op("pe", lambda e, hc=hc: e.transpose(p0[:, hc, :], skn[:, hc, :], c.cn[:, C_ID:C_ID + 128]), reads=[hskn, c.hcn], writes=[hp0])
            S.op("dve", lambda e: e.tensor_copy(out=skT[:], in_=p0[:]), reads=[hp0], writes=[hskT])
            xnT, hxnT = T("xnT", [128, DC, 128], BF16)
            qTs, hqTs = T("qTs", [128, 16, 128])
            sall, hsall = T("sall", [128, 16, 128])
            tops, htops = T("tops", [128, 16, 16])
            wk, hwk = T("wk", [128, 16, 128])
            cand, hcand = T("cand", [128, 8, 256])
            wk2, hwk2 = T("wk2", [128, 8, 256])
            wk3, hwk3 = T("wk3", [128, 8, 256])
            ctop, hctop = T("ctop", [128, 8, 24])
            sm, hsm = T("sm", [128, 4, 8])
            ez, hez = T("ez", [128, 8, 16])
            stt, hstt = T("stt", [128, 2048 + 8])
            for jo in range(NO):
                S.dma("sp", xnT[:], XNT_d[jo].rearrange("p (k t) -> p k t", k=DC), hxnT, reads=[hXNT[jo]], writes=[hxnT])
                pa_, hpa_ = pbig[0]
                for hc in range(16):
                    for kc in range(DC):
                        S.op("pe", lambda e, hc=hc, kc=kc: e.matmul(pa_[:, hc, :], lhsT=pq[:, kc, hc * 128:(hc + 1) * 128], rhs=xnT[:, kc, :], start=(kc == 0), stop=(kc == DC - 1)),
                             reads=[hpq, hxnT], writes=[hpa_])
                act_(qTs[:, 0:8, :], pa_[:, 0:8, :], AF.Copy, [hpa_], [hqTs])
                S.op("dve", lambda e: e.tensor_copy(out=qTs[:, 8:16, :], in_=pa_[:, 8:16, :]), reads=[hpa_], writes=[hqTs])
                pb_, hpb_ = pbig[1]
                for hc in range(16):
                    S.op("pe", lambda e, hc=hc: e.matmul(pb_[:, hc, :], lhsT=qTs[:, hc, :], rhs=skT[:, hc, :], start=True, stop=True),
                         reads=[hqTs, hskT], writes=[hpb_])
                act_(sall[:, 0:8, :], pb_[:, 0:8, :], AF.Copy, [hpb_], [hsall])
                S.op("dve", lambda e: e.tensor_copy(out=sall[:, 8:16, :], in_=pb_[:, 8:16, :]), reads=[hpb_], writes=[hsall])
                for hc in range(16):
                    S.op("dve", lambda e, hc=hc: e.max(out=tops[:, hc, 0:8], in_=sall[:, hc, :]), reads=[hsall], writes=[htops])
                for hc in range(16):
                    S.op("dve", lambda e, hc=hc: e.match_replace(out=wk[:, hc, :], in_to_replace=tops[:, hc, 0:8], in_values=sall[:, hc, :], imm_value=-1e30),
                         reads=[hsall, htops], writes=[hwk])
                for hc in range(16):
                    S.op("dve", lambda e, hc=hc: e.max(out=tops[:, hc, 8:16], in_=wk[:, hc, :]), reads=[hwk], writes=[htops])
                t4 = tops[:].rearrange("p (h c) k -> p h c k", c=2)
                tt_("dve", cand[:].rearrange("p h (i j) -> p h i j", i=16), t4[:, :, 0, :].unsqueeze(3).broadcast_to([128, 8, 16, 16]),
                    t4[:, :, 1, :].unsqueeze(2).broadcast_to([128, 8, 16, 16]), ALU.add, [htops], [hcand])
                for h in range(8):
                    S.op("dve", lambda e, h=h: e.max(out=ctop[:, h, 0:8], in_=cand[:, h, :]), reads=[hcand], writes=[hctop])
                for h in range(8):
                    S.op("dve", lambda e, h=h: e.match_replace(out=wk2[:, h, :], in_to_replace=ctop[:, h, 0:8], in_values=cand[:, h, :], imm_value=-1e30),
                         reads=[hcand, hctop], writes=[hwk2])
                for h in range(8):
                    S.op("dve", lambda e, h=h: e.max(out=ctop[:, h, 8:16], in_=wk2[:, h, :]), reads=[hwk2], writes=[hctop])
                for h in range(8):
                    S.op("dve", lambda e, h=h: e.match_replace(out=wk3[:, h, :], in_to_replace=ctop[:, h, 8:16], in_values=wk2[:, h, :], imm_value=-1e30),
                         reads=[hwk2, hctop], writes=[hwk3])
                for h in range(8):
                    S.op("dve", lambda e, h=h: e.max(out=ctop[:, h, 16:24], in_=wk3[:, h, :]), reads=[hwk3], writes=[hctop])
                thr = sm[:, 0, :]
                tt_("dve", thr.unsqueeze(2), ctop[:, :, 15:16], ctop[:, :, 16:17], ALU.add, [hctop], [hsm])
                S.op("dve", lambda e: e.tensor_scalar(out=thr, in0=thr, scalar1=0.5, scalar2=None, op0=ALU.mult), reads=[hsm], writes=[hsm])
                tt_("dve", ez[:], ctop[:, :, 0:16], thr.unsqueeze(2).broadcast_to([128, 8, 16]), ALU.subtract, [hctop, hsm], [hez])
                act_(ez[:], ez[:], AF.Exp, [hez], [hez])
                S.op("dve", lambda e: e.tensor_reduce(out=sm[:, 1, :].unsqueeze(2), in_=ez[:], axis=AX.X, op=ALU.add), reads=[hez], writes=[hsm])
                S.op("dve", lambda e: e.reciprocal(out=stt[:, 2048:2056], in_=sm[:, 1, :]), reads=[hsm], writes=[hstt])
                act_(sm[:, 2, :], sm[:, 1, :], AF.Ln, [hsm], [hsm])
                tt_("dve", sm[:, 3, :], sm[:, 2, :], thr, ALU.add, [hsm], [hsm])
                s4 = sall[:].rearrange("p (h c) n -> p h c n", c=2)
                st4 = stt[:, 0:2048].rearrange("p (h c n) -> p h c n", h=8, c=2)
                tt_("dve", st4[:, :, 0, :], s4[:, :, 0, :], sm[:, 3, :].unsqueeze(2).broadcast_to([128, 8, 128]), ALU.subtract, [hsall, hsm], [hstt])
                S.op("dve", lambda e: e.tensor_copy(out=st4[:, :, 1, :], in_=s4[:, :, 1, :]), reads=[hsall], writes=[hstt])
                act_(stt[:, 0:2048], stt[:, 0:2048], AF.Exp, [hstt], [hstt])
                S.dma("sp", ST_d[jo], stt[:], hstt, reads=[hstt], writes=[hST[jo]])
            S.barrier()

    def phase_E2(G=4):
        with contextlib.ExitStack() as es:
            T, P = mk(es)
            c = Ctx()
            c.cn, c.hcn = T("id32", [128, 128])
            S.dma("sp", c.cn[:], cn_d[:, C_ID:C_ID + 128], c.hcn, writes=[c.hcn])
            c.cb, c.hcb = T("idbf", [128, 128], BF16)
            S.dma("pool", c.cb[:], cn_d[:, C_ID:C_ID + 128], c.hcb, writes=[c.hcb])
            identb = c.cb[:]
            ident32 = c.cn[:]
            G = min(G, NO)
            xn = [T("xn%d" % i, [128, DC, 128], BF16) for i in range(G)]
            stg = [T("stg%d" % i, [128, 2048 + 8]) for i in range(G)]
            acc = [T("acc%d" % i, [128, D]) for i in range(G)]
            dn32 = [T("dn32_%d" % i, [128, D]) for i in range(2)]
            up32 = [T("up32_%d" % i, [128, D]) for i in range(1)]
            upb = [T("upb%d" % i, [128, 4, D], BF16) for i in range(2)]
            dTs = [T("dT%d" % i, [128, DC, 512], BF16) for i in range(2)]
            EEh = [T("EEh%d" % i, [128, 4, 512]) for i in range(3)]
            Wh = [T("Wh%d" % i, [128, 8, 512], BF16) for i in range(1)]
            geT = [T("geT%d" % i, [128, 512]) for i in range(2)]
            GTb = [T("GTb%d" % i, [128, 4, 128], BF16) for i in range(2)]
            ptr = [P("ptr%d" % i, [128, 4, 128]) for i in range(2)]
            phid = [P("phid%d" % i, [128, 4, 128]) for i in range(2)]
            pwt, hpwt = P("pwt", [128, 4, 128])
            pouts = [P("pout%d" % i, [128, 512]) for i in range(3)]
            pocnt = [0]
            NCH = NE // 512
            import os as _os
            _V = _os.environ.get("KV", "")
            if "n" in _V:
                NCH = 8
            NQ = NCH * 4
            pcount = [0]
            for g0 in range(0, NO, G):
                tiles = list(range(g0, min(NO, g0 + G)))
                nt = len(tiles)
                for i, jo in enumerate(tiles):
                    S.dma("sp", xn[i][0][:], XNT_d[jo].rearrange("p (k t) -> p k t", k=DC), xn[i][1], reads=[hXNT[jo]], writes=[xn[i][1]])
                    S.dma("sp", stg[i][0][:], ST_d[jo], stg[i][1], reads=[hST[jo]], writes=[stg[i][1]])
                    S.dma("sp", acc[i][0][:], HM_d[jo], acc[i][1], reads=[hHM[jo]], writes=[acc[i][1]])

                def load_dn(q):
                    if q < NQ:
                        d_, hd_ = dn32[q % 2]
                        S.dma("sp", d_[:], pd_d[q * 128:(q + 1) * 128, :], hd_, writes=[hd_])

                def load_up(q):
                    if q < NQ:
                        u_, hu_ = up32[0]
                        S.dma("sp", u_[:], pu_d[q * 128:(q + 1) * 128, :], hu_, writes=[hu_])

                def prep_q(q):
                    ec, et = divmod(q, 4)
                    d_, hd_ = dn32[q % 2]
                    dT_, hdT_ = dTs[ec % 2]
                    for k4 in range(4):
                        pt_, hpt_ = ptr[pcount[0] % 2]
                        pcount[0] += 1
                        for kk in range(4):
                            kc = k4 * 4 + kk
                            S.op("pe", lambda e, kk=kk, kc=kc, pt_=pt_: e.transpose(pt_[:, kk, :], d_[:, kc * 128:(kc + 1) * 128], ident32),
                                 reads=[hd_, c.hcn], writes=[hpt_])
                        S.op("dve", lambda e, k4=k4, pt_=pt_: e.tensor_copy(out=dT_[:, k4 * 4:k4 * 4 + 4, et * 128:(et + 1) * 128], in_=pt_[:]),
                             reads=[hpt_], writes=[hdT_])
                    load_dn(q + 2)
                    u_, hu_ = up32[0]
                    ub_, hub_ = upb[ec % 2]
                    S.op("dve", lambda e: e.tensor_copy(out=ub_[:, et, :], in_=u_[:]), reads=[hu_], writes=[hub_])
                    load_up(q + 1)

                def P1(gi):
                    ec, i = divmod(gi, nt)
                    xn_, hxn_ = xn[i]
                    ph_, hph_ = phid[gi % 2]
                    dT_, hdT_ = dTs[ec % 2]
                    for et in range(4):
                        for kc in range(DC):
                            S.op("pe", lambda e, kc=kc, et=et: e.matmul(ph_[:, et, :], lhsT=dT_[:, kc, et * 128:(et + 1) * 128], rhs=xn_[:, kc, :], start=(kc == 0), stop=(kc == DC - 1)),
                                 reads=[hxn_, hdT_], writes=[hph_])

                def A1(gi):
                    ec, i = divmod(gi, nt)
                    st_, hst_ = stg[i]
                    ph_, hph_ = phid[gi % 2]
                    ge_, hge_ = geT[gi % 2]
                    st4 = st_[:, 0:2048].rearrange("p (h c n) -> p h c n", h=8, c=2)
                    for half in range(2):
                        ee_, hee_ = EEh[(gi * 2 + half) % 3]
                        for h in range(4):
                            hh = half * 4 + h
                            for a in range(4):
                                n1 = ec * 4 + a
                                act_(ee_[:, h, a * 128:(a + 1) * 128], st4[:, hh, 1, :], AF.Copy, [hst_], [hee_], scale=st4[:, hh, 0, n1:n1 + 1])
                    act_(ge_[:], ph_[:].rearrange("p a t -> p (a t)"), AF.Gelu, [hph_], [hge_])

                def D1(gi):
                    ec, i = divmod(gi, nt)
                    st_, hst_ = stg[i]
                    wh_, hwh_ = Wh[0]
                    for half in range(2):
                        ee_, hee_ = EEh[(gi * 2 + half) % 3]
                        for h in range(4):
                            hh = half * 4 + h
                            S.op("dve", lambda e, h=h, hh=hh, ee_=ee_: e.scalar_tensor_tensor(
                                out=wh_[:, hh, :], in0=ee_[:, h, :], scalar=st_[:, 2048 + hh:2049 + hh],
                                in1=ee_[:, h, :], op0=ALU.is_ge, op1=ALU.mult), reads=[hee_, hst_], writes=[hwh_])

                def P2(gi):
                    wh_, hwh_ = Wh[0]
                    for et in range(4):
                        for h in range(8):
                            S.op("pe", lambda e, et=et, h=h: e.matmul(pwt[:, et, :], lhsT=wh_[:, h, et * 128:(et + 1) * 128], rhs=identb, start=(h == 0), stop=(h == 7)),
                                 reads=[hwh_, c.hcb], writes=[hpwt])

                def D2(gi):
                    ge_, hge_ = geT[gi % 2]
                    gt_, hgt_ = GTb[gi % 2]
                    tt_("dve", gt_[:].rearrange("p a t -> p (a t)"), ge_[:], pwt[:].rearrange("p a t -> p (a t)"), ALU.mult, [hge_, hpwt], [hgt_])

                def P3D3(gi):
                    ec, i = divmod(gi, nt)
                    u_, hu_ = upb[ec % 2]
                    gt_, hgt_ = GTb[gi % 2]
                    ac_, hac_ = acc[i]
                    for dg in range(4):
                        po_, hpo_ = pouts[pocnt[0] % 3]
                        pocnt[0] += 1
                        for et in range(4):
                            S.op("pe", lambda e, dg=dg, et=et, po_=po_: e.matmul(po_[:], lhsT=gt_[:, et, :], rhs=u_[:, et, dg * 512:(dg + 1) * 512], start=(et == 0), stop=(et == 3)),
                                 reads=[hgt_, hu_], writes=[hpo_])
                        tt_("dve", ac_[:, dg * 512:(dg + 1) * 512], ac_[:, dg * 512:(dg + 1) * 512], po_[:], ALU.add, [hac_, hpo_], [hac_])

                NG = NCH * nt
                load_dn(0)
                load_dn(1)
                load_up(0)
                for q in range(4):
                    prep_q(q)
                P1(0)
                A1(0)
                D1(0)
                for gi in range(NG):
                    ec, i = divmod(gi, nt)
                    if ec + 1 < NCH:
                        for et in range(i * 4 // nt, (i + 1) * 4 // nt):
                            prep_q((ec + 1) * 4 + et)
                    if gi + 1 < NG:
                        P1(gi + 1)
                        A1(gi + 1)
                    P2(gi)
                    D2(gi)
                    if gi + 1 < NG:
                        D1(gi + 1)
                    P3D3(gi)
                for i, jo in enumerate(tiles):
                    S.dma("sp", yout[jo * 128:(jo + 1) * 128, :], acc[i][0][:], acc[i][1], reads=[acc[i][1]], writes=[hOUT[jo]])
            S.barrier()

    ph = {"A": phase_A, "B": phase_B, "C": phase_C, "D": phase_D, "E": phase_E1, "F": phase_E2}
    ctx = dict(nc=nc, S=S, mk=mk, load_consts=load_consts, rmsnorm_T=rmsnorm_T, locals=locals())
    for p in phases:
        if p in ph:
            ph[p]()
    return nc, ctx


def _colT(v, n):
    buf = np.zeros(n * 128, np.float32)
    buf[:v.size] = v.reshape(-1)
    return buf.reshape(n, 128).T


def make_vecs(inp):
    vecs = np.zeros((128, NV), np.float32)
    vecs[:, V_G1:V_G1 + 16] = _colT(inp["norm1_g"][0], 16)
    vecs[:, V_MU:V_MU + 27] = _colT(inp["shift_mu"][0], 27)
    for col, key in ((V_W0, "w0"), (V_A0, "a0"), (V_KK, "k_k"), (V_KA, "k_a"), (V_GNW, "gn_w"),
                     (V_GNB, "gn_b"), (V_RK, "r_k")):
        vecs[:, col:col + 8] = _colT(inp[key][0], 8)
    vecs[:, V_QG] = np.tile(inp["q_gain"][0], 2)
    vecs[:, V_KG] = np.tile(inp["k_gain"][0], 2)
    sk = inp["sinks"][0]
    for hp in range(8):
        vecs[0:64, V_SK + hp] = sk[2 * hp]
        vecs[64:128, V_SK + hp] = sk[2 * hp + 1]
    vecs[:, V_G2:V_G2 + 16] = _colT(inp["norm2_g"][0], 16)
    return vecs


def make_consts(first_half):
    cn = np.zeros((128, NCN), np.float32)
    cn[:, C_ID:C_ID + 128] = np.eye(128, dtype=np.float32)
    bo = np.zeros((128, 128), np.float32)
    bo[0:64, 0:64] = 1
    bo[64:, 64:] = 1
    cn[:, C_BO:C_BO + 128] = bo
    s = np.arange(64)[:, None]
    t = np.arange(64)[None, :]
    cn[0:64, C_MU:C_MU + 64] = (s < t)
    cn[0:64, C_MUI:C_MUI + 64] = (s <= t)
    cn[0:64, C_ML:C_ML + 64] = (s > t)
    s = np.arange(128)[:, None]
    q = np.arange(128)[None, :]
    cn[:, C_MC:C_MC + 128] = (s <= q)
    cn[:, C_MP:C_MP + 128] = (s > q)
    mpf = (s > q)
    if first_half:
        mpf = mpf & (s >= 112)
    cn[:, C_MPF:C_MPF + 128] = mpf
    for g in range(2):
        sel = np.zeros((128, 128), np.float32)
        for m in range(128):
            sel[g * 64 + (m % 64), m] = 1
        cn[:, C_SEL + g * 128:C_SEL + (g + 1) * 128] = sel
    return cn


_NC_CACHE = {}


def kernel(**inputs):
    inp = {k: np.asarray(v) for k, v in inputs.items()}
    x = inp["x"].astype(np.float32, copy=False)
    B, SEQ, _ = x.shape
    NB, OWN0 = 33, 17
    if "nc" not in _NC_CACHE:
        _NC_CACHE["nc"] = build(NB=NB, OWN0=OWN0, phases="ABCDEF")[0]
    nc = _NC_CACHE["nc"]
    f = lambda k: np.ascontiguousarray(inp[k][0], dtype=np.float32)
    shared = dict(w_in=f("w_in"), vecs=make_vecs(inp), w_up=f("w_up"), a_up=f("a_up"), g_up=f("g_up"),
                  w_out=f("w_out"), peer_query=f("peer_query"),
                  sub_keys=np.ascontiguousarray(inp["peer_sub_keys"][0].reshape(16, 128, 128), dtype=np.float32),
                  peer_down=f("peer_down"), peer_up=f("peer_up"))
    meta = inp["meta_tokens"].astype(np.float32, copy=False)
    cn = [make_consts(False), make_consts(True)]
    in_maps = []
    for c in range(8):
        b, s = c // 2, c % 2
        loc = np.zeros((NB * 128, D), np.float32)
        if s == 0:
            loc[16 * 128 + 112:17 * 128] = meta
            loc[17 * 128:] = x[b, :2048]
        else:
            loc[112:128] = meta
            loc[128:] = x[b]
        in_maps.append(dict(xin=loc, consts=cn[1 if s == 0 else 0], **shared))
    res = run_bass_kernel_spmd(nc, in_maps, core_ids=list(range(8)))
    out = np.empty((B, SEQ, D), np.float32)
    for c in range(8):
        b, s = c // 2, c % 2
        out[b, s * 2048:(s + 1) * 2048] = np.asarray(res.results[c]["yout"])
    return out
```
